# Optimizing a Trainium2 kernel written in Bass

```python
import math
import jax, jax.numpy as jnp
from jax import lax
import numpy as np

D_MODEL = 1024
BATCH = 8
SEQ = 2048
DEPTH = 2

HEAD_DIM = 64
A_GROUPS = 4
A_WIDTH = A_GROUPS * HEAD_DIM
A_CHUNK = 128
B_HEADS = 4
B_WIDTH = B_HEADS * HEAD_DIM
RET_CHUNK = 128
ROPE_BASE = 10000.0
C_HEADS = 8
C_KV_HEADS = 2
C_WIDTH = C_HEADS * HEAD_DIM
C_KV_WIDTH = C_KV_HEADS * HEAD_DIM
CMP_BLOCK = 32
CMP_STRIDE = 16
SEL_BLOCK = 64
N_SEL = 8
WINDOW = 512
Q_BLOCK = 128
D_MIX = A_WIDTH + B_WIDTH + C_WIDTH
IN_COLS = 2 * A_WIDTH + 4 * B_WIDTH + C_WIDTH + 6 * C_KV_WIDTH + 3 * C_HEADS
D_FF = 2816
CONV_WIDTH = 3
DN_ALPHA = (2 * DEPTH) ** 0.25
DN_BETA = (8 * DEPTH) ** -0.25
LN_EPS = 1e-5
NEG = -1e30
BIG = 1e30

kernel_name = 'hymba_style_gmlp_retnet_nsa_deepnorm_block'


def layer_norm(x, g, b):
    xf = x.astype(jnp.float32)
    mu = jnp.mean(xf, axis=-1, keepdims=True)
    var = jnp.mean(jnp.square(xf - mu), axis=-1, keepdims=True)
    y = (xf - mu) * lax.rsqrt(var + LN_EPS)
    return (y * g.astype(jnp.float32) + b.astype(jnp.float32)).astype(x.dtype)


def masked_softmax(scores, mask):
    s = jnp.where(mask, scores.astype(jnp.float32), NEG)
    return jax.nn.softmax(s, axis=-1) * mask


def rotary(x, pos):
    half = HEAD_DIM // 2
    inv = jnp.power(ROPE_BASE, -jnp.arange(half, dtype=jnp.float32) / half)
    ang = pos.astype(jnp.float32)[:, None] * inv[None, :]
    cos = jnp.cos(ang)[None, :, None, :].astype(x.dtype)
    sin = jnp.sin(ang)[None, :, None, :].astype(x.dtype)
    x1, x2 = x[..., :half], x[..., half:]
    return jnp.concatenate([x1 * cos - x2 * sin, x1 * sin + x2 * cos], axis=-1)


def spatial_gating_mixer(z, ln_g, ln_b, w_s, b_s):
    Bn, S, _ = z.shape
    z = jax.nn.gelu(z)
    u, v = jnp.split(z, 2, axis=-1)
    v = layer_norm(v.reshape(Bn, S, A_GROUPS, HEAD_DIM), ln_g, ln_b)
    nch = S // A_CHUNK
    v = v.reshape(Bn, nch, A_CHUNK, A_GROUPS, HEAD_DIM)
    causal = jnp.tril(jnp.ones((A_CHUNK, A_CHUNK), dtype=bool))
    w = jnp.where(causal, w_s, 0).astype(v.dtype)
    vs = jnp.einsum('gts,bcsgd->bctgd', w, v) + b_s.T[None, None, :, :, None]
    return u * vs.reshape(Bn, S, A_WIDTH)


def retention_mixer(q, k, v, g, gn_g, gn_b):
    Bn, S, _ = q.shape
    H, d, L = B_HEADS, HEAD_DIM, RET_CHUNK
    nch = S // L
    dt = q.dtype
    pos = jnp.arange(S)
    q = rotary(q.reshape(Bn, S, H, d), pos)
    k = rotary(k.reshape(Bn, S, H, d), pos) * (d ** -0.5)
    v = v.reshape(Bn, S, H, d)
    log_gamma = jnp.log1p(-jnp.exp2(-5.0 - jnp.arange(H, dtype=jnp.float32)))
    idx = jnp.arange(L, dtype=jnp.float32)
    diff = idx[:, None] - idx[None, :]
    decay_in = jnp.where(diff >= 0, jnp.exp(log_gamma[:, None, None] * jnp.maximum(diff, 0.0)), 0.0)
    xi = jnp.exp(log_gamma[:, None] * (idx + 1.0))
    zeta = jnp.exp(log_gamma[:, None] * (L - 1.0 - idx))
    chunk_decay = jnp.exp(log_gamma * L).astype(dt)[None, :, None, None]
    qc = q.reshape(Bn, nch, L, H, d)
    kc = k.reshape(Bn, nch, L, H, d)
    vc = v.reshape(Bn, nch, L, H, d)
    scores = jnp.einsum('bclhd,bcmhd->bchlm', qc, kc) * decay_in.astype(dt)
    o_inner = jnp.einsum('bchlm,bcmhe->bclhe', scores, vc)
    kv = jnp.einsum('bcmhd,bcmhe->bchde', kc * zeta.T[:, :, None].astype(dt), vc)

    def step(state, kv_c):
        return state * chunk_decay + kv_c, state

    state0 = jnp.zeros((Bn, H, d, d), dt)
    _, states = lax.scan(step, state0, jnp.moveaxis(kv, 1, 0))
    states = jnp.moveaxis(states, 0, 1)
    o_cross = jnp.einsum('bclhd,bchde->bclhe', qc, states) * xi.T[:, :, None].astype(dt)
    o = (o_inner + o_cross).reshape(Bn, S, H, d)
    o = layer_norm(o, gn_g, gn_b)
    return jax.nn.silu(g) * o.reshape(Bn, S, B_WIDTH)


def compress(kv, pos_emb, w1, w2):
    Bn, S, G, d = kv.shape
    nc = (S - CMP_BLOCK) // CMP_STRIDE + 1
    idx = (jnp.arange(nc) * CMP_STRIDE)[:, None] + jnp.arange(CMP_BLOCK)[None, :]
    blocks = kv[:, idx] + pos_emb[None, None, :, None, :]
    flat = jnp.moveaxis(blocks, 3, 2).reshape(Bn, nc, G, CMP_BLOCK * d)
    return jax.nn.gelu(flat @ w1) @ w2


def cmp_to_sel_overlap(nc, ns):
    c0 = np.arange(nc)[:, None] * CMP_STRIDE
    s0 = np.arange(ns)[None, :] * SEL_BLOCK
    ov = np.clip(np.minimum(c0 + CMP_BLOCK, s0 + SEL_BLOCK) - np.maximum(c0, s0), 0, None)
    return (ov / CMP_BLOCK).astype(np.float32)


def nsa_mixer(q, k_c, v_c, k_s, v_s, k_w, v_w, gates, pos_k, w1_k, w2_k, pos_v, w1_v, w2_v):
    Bn, S, _ = q.shape
    G, d = C_KV_HEADS, HEAD_DIM
    R = C_HEADS // C_KV_HEADS
    dt = q.dtype
    q = q.reshape(Bn, S, G, R, d) * (d ** -0.5)
    k_cmp = compress(k_c.reshape(Bn, S, G, d), pos_k, w1_k, w2_k)
    v_cmp = compress(v_c.reshape(Bn, S, G, d), pos_v, w1_v, w2_v)
    nc = k_cmp.shape[1]
    ns = S // SEL_BLOCK
    n_sel = min(N_SEL, ns)
    overlap = jnp.asarray(cmp_to_sel_overlap(nc, ns))
    cmp_end = jnp.arange(nc) * CMP_STRIDE + CMP_BLOCK - 1
    ks_blocks = jnp.moveaxis(k_s.reshape(Bn, ns, SEL_BLOCK, G, d), 3, 1)
    vs_blocks = jnp.moveaxis(v_s.reshape(Bn, ns, SEL_BLOCK, G, d), 3, 1)
    pad = jnp.zeros((Bn, WINDOW, G, d), k_w.dtype)
    kw_pad = jnp.concatenate([pad, k_w.reshape(Bn, S, G, d)], axis=1)
    vw_pad = jnp.concatenate([pad, v_w.reshape(Bn, S, G, d)], axis=1)
    gates = jax.nn.sigmoid(gates.reshape(Bn, S, G, R, 3))
    b_ix = jnp.arange(Bn)[:, None, None, None]
    g_ix = jnp.arange(G)[None, :, None, None]
    blk = jnp.arange(ns)
    span = n_sel * SEL_BLOCK

    def query_block(i):
        s0 = i * Q_BLOCK
        t = s0 + jnp.arange(Q_BLOCK)
        qb = lax.dynamic_slice_in_dim(q, s0, Q_BLOCK, axis=1)
        sc = jnp.einsum('bqgrd,bkgd->bgrqk', qb, k_cmp)
        p_cmp = masked_softmax(sc, cmp_end[None, :] <= t[:, None])
        o_cmp = jnp.einsum('bgrqk,bkgd->bqgrd', p_cmp.astype(dt), v_cmp)
        imp = jnp.einsum('bgrqk,kj->bgqj', p_cmp, overlap)
        cur = t // SEL_BLOCK
        future = blk[None, :] > cur[:, None]
        forced = (blk[None, :] == 0) | (blk[None, :] == cur[:, None]) | (blk[None, :] == cur[:, None] - 1)
        imp = jnp.where(forced, BIG, jnp.where(future, NEG, imp))
        _, sel = lax.top_k(imp, n_sel)
        k_sel = ks_blocks[b_ix, g_ix, sel]
        v_sel = vs_blocks[b_ix, g_ix, sel]
        key_pos = sel[..., None] * SEL_BLOCK + jnp.arange(SEL_BLOCK)
        m_sel = (key_pos <= t[None, None, :, None, None]).reshape(Bn, G, 1, Q_BLOCK, span)
        ss = jnp.einsum('bqgrd,bgqnld->bgrqnl', qb, k_sel).reshape(Bn, G, R, Q_BLOCK, span)
        p_sel = masked_softmax(ss, m_sel).reshape(Bn, G, R, Q_BLOCK, n_sel, SEL_BLOCK)
        o_sel = jnp.einsum('bgrqnl,bgqnld->bqgrd', p_sel.astype(dt), v_sel)
        kwb = lax.dynamic_slice_in_dim(kw_pad, s0, WINDOW + Q_BLOCK, axis=1)
        vwb = lax.dynamic_slice_in_dim(vw_pad, s0, WINDOW + Q_BLOCK, axis=1)
        kpos = s0 - WINDOW + jnp.arange(WINDOW + Q_BLOCK)
        rel = t[:, None] - kpos[None, :]
        m_win = (kpos[None, :] >= 0) & (rel >= 0) & (rel < WINDOW)
        sw = jnp.einsum('bqgrd,bkgd->bgrqk', qb, kwb)
        p_win = masked_softmax(sw, m_win)
        o_win = jnp.einsum('bgrqk,bkgd->bqgrd', p_win.astype(dt), vwb)
        gb = lax.dynamic_slice_in_dim(gates, s0, Q_BLOCK, axis=1)
        o = gb[..., 0:1] * o_cmp + gb[..., 1:2] * o_sel + gb[..., 2:3] * o_win
        return o.reshape(Bn, Q_BLOCK, C_WIDTH)

    out = lax.map(query_block, jnp.arange(S // Q_BLOCK))
    return jnp.moveaxis(out, 0, 1).reshape(Bn, S, C_WIDTH)


def hybrid_mixer(h, w_in, a_ln_g, a_ln_b, a_ws, a_bs, b_gn_g, b_gn_b,
                 c_pos_k, c_w1_k, c_w2_k, c_pos_v, c_w1_v, c_w2_v, w_out):
    p = h @ w_in
    widths = [2 * A_WIDTH, B_WIDTH, B_WIDTH, B_WIDTH, B_WIDTH, C_WIDTH] + [C_KV_WIDTH] * 6
    splits = [int(s) for s in np.cumsum(widths)]
    za, qb, kb, vb, gb, qc, kcm, vcm, ksl, vsl, kwn, vwn, gc = jnp.split(p, splits, axis=-1)
    y_a = spatial_gating_mixer(za, a_ln_g, a_ln_b, a_ws, a_bs)
    y_b = retention_mixer(qb, kb, vb, gb, b_gn_g, b_gn_b)
    y_c = nsa_mixer(qc, kcm, vcm, ksl, vsl, kwn, vwn, gc,
                    c_pos_k, c_w1_k, c_w2_k, c_pos_v, c_w1_v, c_w2_v)
    return jnp.concatenate([y_a, y_b, y_c], axis=-1) @ w_out


def conv_ffn(h, w_up, conv_w, conv_b, w_down):
    a = h @ w_up
    ch = a.shape[-1]
    a = lax.conv_general_dilated(a, conv_w[:, None, :].astype(a.dtype), window_strides=(1,),
                                 padding=[(CONV_WIDTH - 1, 0)],
                                 dimension_numbers=('NWC', 'WIO', 'NWC'),
                                 feature_group_count=ch) + conv_b
    gate, up = jnp.split(a, 2, axis=-1)
    return (jax.nn.silu(gate) * up) @ w_down


def setup_inputs(seed: int = 0) -> dict:
    key = jax.random.key(seed)
    ks = jax.random.split(key, 26)
    n = lambda k, shape: jax.random.normal(k, shape, jnp.float32)
    L = DEPTH
    return {
        'x': n(ks[0], (BATCH, SEQ, D_MODEL)),
        'c': n(ks[1], (BATCH, D_MODEL)),
        'w_ada': n(ks[2], (L, D_MODEL, 6 * D_MODEL)) * D_MODEL ** -0.5,
        'b_ada': n(ks[3], (L, 6 * D_MODEL)) * 0.01,
        'w_in': n(ks[4], (L, D_MODEL, IN_COLS)) * D_MODEL ** -0.5,
        'a_ln_g': 1.0 + 0.02 * n(ks[5], (L, A_GROUPS, HEAD_DIM)),
        'a_ln_b': 0.02 * n(ks[6], (L, A_GROUPS, HEAD_DIM)),
        'a_ws': n(ks[7], (L, A_GROUPS, A_CHUNK, A_CHUNK)) * A_CHUNK ** -0.5,
        'a_bs': 1.0 + 0.02 * n(ks[8], (L, A_GROUPS, A_CHUNK)),
        'b_gn_g': 1.0 + 0.02 * n(ks[9], (L, B_HEADS, HEAD_DIM)),
        'b_gn_b': 0.02 * n(ks[10], (L, B_HEADS, HEAD_DIM)),
        'c_pos_k': 0.02 * n(ks[11], (L, CMP_BLOCK, HEAD_DIM)),
        'c_w1_k': n(ks[12], (L, CMP_BLOCK * HEAD_DIM, HEAD_DIM)) * (CMP_BLOCK * HEAD_DIM) ** -0.5,
        'c_w2_k': n(ks[13], (L, HEAD_DIM, HEAD_DIM)) * HEAD_DIM ** -0.5,
        'c_pos_v': 0.02 * n(ks[14], (L, CMP_BLOCK, HEAD_DIM)),
        'c_w1_v': n(ks[15], (L, CMP_BLOCK * HEAD_DIM, HEAD_DIM)) * (CMP_BLOCK * HEAD_DIM) ** -0.5,
        'c_w2_v': n(ks[16], (L, HEAD_DIM, HEAD_DIM)) * HEAD_DIM ** -0.5,
        'w_out': n(ks[17], (L, D_MIX, D_MODEL)) * (D_MIX ** -0.5) * DN_BETA,
        'ln1_g': 1.0 + 0.02 * n(ks[18], (L, D_MODEL)),
        'ln1_b': 0.02 * n(ks[19], (L, D_MODEL)),
        'w_up': n(ks[20], (L, D_MODEL, 2 * D_FF)) * D_MODEL ** -0.5,
        'conv_w': n(ks[21], (L, CONV_WIDTH, 2 * D_FF)) * CONV_WIDTH ** -0.5,
        'conv_b': 0.02 * n(ks[22], (L, 2 * D_FF)),
        'w_down': n(ks[23], (L, D_FF, D_MODEL)) * (D_FF ** -0.5) * DN_BETA,
        'ln2_g': 1.0 + 0.02 * n(ks[24], (L, D_MODEL)),
        'ln2_b': 0.02 * n(ks[25], (L, D_MODEL)),
    }


def reference(x, c, w_ada, b_ada, w_in, a_ln_g, a_ln_b, a_ws, a_bs, b_gn_g, b_gn_b,
              c_pos_k, c_w1_k, c_w2_k, c_pos_v, c_w1_v, c_w2_v, w_out, ln1_g, ln1_b,
              w_up, conv_w, conv_b, w_down, ln2_g, ln2_b):
    cond = jax.nn.silu(c)
    for l in range(DEPTH):
        mod = cond @ w_ada[l] + b_ada[l]
        sh1, sc1, g1, sh2, sc2, g2 = [m[:, None, :] for m in jnp.split(mod, 6, axis=-1)]
        h = x * (1 + sc1) + sh1
        y = hybrid_mixer(h, w_in[l], a_ln_g[l], a_ln_b[l], a_ws[l], a_bs[l], b_gn_g[l], b_gn_b[l],
                         c_pos_k[l], c_w1_k[l], c_w2_k[l], c_pos_v[l], c_w1_v[l], c_w2_v[l], w_out[l])
        x = layer_norm(DN_ALPHA * x + g1 * y, ln1_g[l], ln1_b[l])
        h = x * (1 + sc2) + sh2
        y = conv_ffn(h, w_up[l], conv_w[l], conv_b[l], w_down[l])
        x = layer_norm(DN_ALPHA * x + g2 * y, ln2_g[l], ln2_b[l])
    return x
```

```python
import math
from contextlib import ExitStack
import numpy as np
import concourse.bass as bass
import concourse.mybir as mybir
from concourse.bass_utils import run_bass_kernel_spmd

F32 = mybir.dt.float32
BF16 = mybir.dt.bfloat16
AF = mybir.ActivationFunctionType
ALU = mybir.AluOpType
AX = mybir.AxisListType

ENGS = ['pe', 'dve', 'act', 'pool', 'sp']
NRING = 8
DEPTH = 2
SEQ = 2048
DM = 1024
ALPHA = (2 * DEPTH) ** 0.25
LN_EPS = 1e-5
NEGB = -30000.0
FF_SPLIT = [6, 6, 5, 5]


class Sched:
    def __init__(self, nc, stack):
        self.nc = nc
        self.prog = {e: [] for e in ENGS}
        self.cnt = {e: 0 for e in ENGS}
        self.seen = {e: {} for e in ENGS}
        self.lastw = {}
        self.readers = {}
        self.sems = {}
        self.semval = {}
        self.relay_fn = None
        for e in ENGS:
            self.sems[e] = stack.enter_context(nc.semaphore("s_" + e))
        self.dma_n = {}
        for q in ['sp', 'act', 'pool']:
            self.dma_n[q] = 0
            for j in range(NRING):
                nm = "d_%s%d" % (q, j)
                self.sems[nm] = stack.enter_context(nc.semaphore(nm))

    def _deps(self, eng, reads, writes, is_dma=False):
        deps = []
        for k in reads:
            t = self.lastw.get(k)
            if t is not None:
                deps.append((t, 'raw'))
        for k in writes:
            t = self.lastw.get(k)
            if t is not None:
                deps.append((t, 'waw'))
            for s, v in self.readers.get(k, {}).items():
                deps.append(((s, v), 'war'))
        need = {}
        for (s, v), kind in deps:
            if s == eng and not is_dma:
                if kind != 'raw' or eng == 'pe':
                    continue
            if self.seen[eng].get(s, 0) >= v:
                continue
            if need.get(s, 0) < v:
                need[s] = v
        return need

    def _emit_waits(self, eng, need):
        if eng in ('sp', 'pool') and 'pe' in need and self.relay_fn is not None:
            need = dict(need)
            v = need.pop('pe')
            self.seen[eng]['pe'] = v
            R = 'dve'
            if self.seen[R].get('pe', 0) < v:
                self.prog[R].append(('wait', 'pe', v))
                self.seen[R]['pe'] = v
            lr = getattr(self, 'last_relay', 0)
            if lr and self.seen[R].get(R, 0) < lr:
                self.prog[R].append(('wait', R, lr))
                self.seen[R][R] = lr
            self.cnt[R] += 1
            self.last_relay = self.cnt[R]
            self.semval[R] = self.cnt[R]
            self.prog[R].append(('op', self.relay_fn, R, 1))
            if self.seen[eng].get(R, 0) < self.cnt[R]:
                need[R] = max(need.get(R, 0), self.cnt[R])
        for s, v in need.items():
            self.prog[eng].append(('wait', s, v))
            self.seen[eng][s] = v

    def _commit(self, tok, reads, writes):
        for k in writes:
            self.lastw[k] = tok
            self.readers[k] = {}
        for k in reads:
            d = self.readers.setdefault(k, {})
            if d.get(tok[0], 0) < tok[1]:
                d[tok[0]] = tok[1]

    def op(self, eng, fn, reads=(), writes=()):
        need = self._deps(eng, reads, writes)
        self._emit_waits(eng, need)
        self.cnt[eng] += 1
        tok = (eng, self.cnt[eng])
        self.semval[eng] = self.cnt[eng]
        self.prog[eng].append(('op', fn, eng, 1))
        self._commit(tok, reads, writes)
        return tok

    def dma_multi(self, q, pairs, reads=(), writes=()):
        i = self.dma_n[q]
        self.dma_n[q] += 1
        slot = "d_%s%d" % (q, i % NRING)
        prev = self.semval.get(slot, 0)
        val = prev + 16 * len(pairs)
        need = self._deps(q, reads, writes, is_dma=True)
        if prev > 0 and self.seen[q].get(slot, 0) < prev:
            need[slot] = max(need.get(slot, 0), prev)
        self._emit_waits(q, need)
        for out, in_ in pairs:
            self.prog[q].append(('op', (lambda o_, i_: (lambda e: e.dma_start(out=o_, in_=i_)))(out, in_), slot, 16))
        self.semval[slot] = val
        tok = (slot, val)
        self._commit(tok, reads, writes)
        return tok

    def dma(self, q, out, in_, reads=(), writes=()):
        return self.dma_multi(q, [(out, in_)], reads, writes)

    def _wait_all(self, e):
        need = {}
        for s_, v in self.semval.items():
            if s_ == e:
                continue
            if self.seen[e].get(s_, 0) < v:
                need[s_] = v
        self._emit_waits(e, need)

    def barrier(self, engs=('pe', 'dve', 'act', 'sp'), relay=None):
        if relay is None or 'sp' not in engs:
            for e in engs:
                self._wait_all(e)
            return
        snap = dict(self.semval)
        self._wait_all('sp')
        tok = self.dma('sp', relay[0], relay[1], (), ['__bar'])
        for e in engs:
            if e == 'sp':
                continue
            self._emit_waits(e, {tok[0]: tok[1]} if self.seen[e].get(tok[0], 0) < tok[1] else {})
            for s_, v in snap.items():
                if s_ != e and self.seen[e].get(s_, 0) < v:
                    self.seen[e][s_] = v

    def finish(self):
        self.barrier(engs=('sp',))

    def replay(self):
        nc = self.nc
        sems = self.sems
        prog = self.prog

        def run(e, name):
            for it in prog[name]:
                if it[0] == 'wait':
                    e.wait_ge(sems[it[1]], it[2])
                else:
                    it[1](e).then_inc(sems[it[2]], it[3])

        with nc.Block() as block:
            @block.tensor
            def _(e):
                run(e, 'pe')

            @block.vector
            def _(e):
                run(e, 'dve')

            @block.scalar
            def _(e):
                run(e, 'act')

            @block.gpsimd
            def _(e):
                run(e, 'pool')

            @block.sync
            def _(e):
                run(e, 'sp')


def _pad64(a):
    a = np.asarray(a, dtype=np.float32)
    out = np.zeros(a.shape[:-1] + (64,), np.float32)
    out[..., :a.shape[-1]] = a
    return out


def _tables():
    t = {}
    half = 32
    inv = np.power(np.float32(10000.0), -np.arange(half, dtype=np.float32) / np.float32(half)).astype(np.float32)
    pos = np.arange(SEQ, dtype=np.float32)
    ang = pos[:, None] * inv[None, :]
    cos = np.cos(ang).astype(np.float32).T
    sin = np.sin(ang).astype(np.float32).T
    cosT = np.concatenate([cos, cos, cos, cos], 0)
    sinT = np.concatenate([-sin, sin, -sin, sin], 0)
    t['cosT'] = np.ascontiguousarray(cosT)
    t['sinT'] = np.ascontiguousarray(sinT)
    H = 4
    L = 128
    lg = np.log1p(-np.exp2(-5.0 - np.arange(H, dtype=np.float32))).astype(np.float32)
    idx = np.arange(L, dtype=np.float32)
    diff = idx[:, None] - idx[None, :]
    dec = np.where(diff >= 0, np.exp(lg[:, None, None] * np.maximum(diff, 0.0)), 0.0).astype(np.float32)
    t['decayT'] = np.ascontiguousarray(np.transpose(dec, (2, 0, 1)) * np.float32(0.125)).reshape(128, 512)
    xi = np.exp(lg[:, None] * (idx + 1.0)).astype(np.float32)
    zeta = np.exp(lg[:, None] * (L - 1.0 - idx)).astype(np.float32)
    xiT = np.zeros((128, 2, 128), np.float32)
    for p in range(2):
        for hh in range(2):
            xiT[hh * 64:(hh + 1) * 64, p, :] = xi[2 * p + hh][None, :]
    t['xiT'] = xiT.reshape(128, 256)
    t['zeta'] = _pad64(zeta.T * np.float32(0.125))
    cd = np.exp(lg * L).astype(np.float32)
    cdv = np.zeros((128, 2), np.float32)
    for p in range(2):
        for hh in range(2):
            cdv[hh * 64:(hh + 1) * 64, p] = cd[2 * p + hh]
    t['cdv'] = _pad64(cdv)
    key = np.arange(SEQ)
    ex = np.zeros((128, SEQ), np.float32)
    ex[key // 64, key] = -NEGB
    t['expand'] = ex
    kk = np.arange(128)[:, None]
    tt = np.arange(128)[None, :]
    t['causalb'] = np.where(kk > tt, NEGB, 0.0).astype(np.float32)
    t['winb'] = np.where(kk <= tt, NEGB, 0.0).astype(np.float32)
    t['identf'] = np.eye(128, dtype=np.float32)
    t['causal01'] = np.where(tt >= kk, 1.0, 0.0).astype(np.float32)
    k127 = np.arange(128)[:, None]
    tpos = np.arange(SEQ)[None, :]
    t['cmpb'] = np.where(16 * k127 + 31 > tpos, NEGB, 0.0).astype(np.float32)
    fb = np.zeros((128, 16, 32), np.float32)
    for qt in range(16):
        tq = qt * 128 + np.arange(128)
        cur = tq // 64
        blk = np.arange(32)
        future = blk[None, :] > cur[:, None]
        forced = (blk[None, :] == 0) | (blk[None, :] == cur[:, None]) | (blk[None, :] == cur[:, None] - 1)
        fb[:, qt, :] = np.where(forced, 1e30, np.where(future, -1e30, 0.0))
    t['fbias'] = fb.reshape(128, 512)
    ov = np.zeros((128, 32), np.float32)
    c0 = np.arange(127)[:, None] * 16
    s0 = np.arange(32)[None, :] * 64
    ov[:127] = np.clip(np.minimum(c0 + 32, s0 + 64) - np.maximum(c0, s0), 0, None) / 32.0
    t['ovl'] = _pad64(ov)
    return t


TABLE_SHAPES = {'cosT': [128, 2048], 'sinT': [128, 2048], 'decayT': [128, 512], 'xiT': [128, 256],
                'zeta': [128, 64], 'cdv': [128, 64], 'expand': [128, 2048], 'causalb': [128, 128],
                'winb': [128, 128], 'causal01': [128, 128], 'identf': [128, 128], 'cmpb': [128, 2048], 'fbias': [128, 512], 'ovl': [128, 64]}


def _prep_weights(inp):
    w = {}
    f = lambda a: np.ascontiguousarray(a, dtype=np.float32)
    w_in = inp['w_in']
    L = DEPTH
    w['w_ada'] = f(inp['w_ada'])
    w['b_adaT'] = _pad64(inp['b_ada'].reshape(L, 48, 128).transpose(0, 2, 1))
    w['w_inA'] = f(w_in[:, :, 0:512])
    cols = []
    for p in range(2):
        for base in (512, 768):
            hd = np.arange(128)
            h = 2 * p + hd // 64
            d = hd % 64
            cols.append(base + h * 64 + d)
            cols.append(base + h * 64 + (d + 32) % 64)
    cols = np.concatenate(cols)
    w['w_inBf'] = f(w_in[:, :, cols])
    cols = []
    for p in range(2):
        cols.append(1024 + p * 128 + np.arange(128))
        cols.append(1280 + p * 128 + np.arange(128))
    w['w_inBt'] = f(w_in[:, :, np.concatenate(cols)])
    cols = []
    for g in range(2):
        cols.append(1536 + g * 256 + np.arange(256))
        ks = 2304 + g * 64 + np.arange(64)
        kw = 2560 + g * 64 + np.arange(64)
        cols += [ks, ks, kw, kw]
        cols.append(2048 + g * 64 + np.arange(64))
        cols.append(2176 + g * 64 + np.arange(64))
    w['w_inCf'] = f(w_in[:, :, np.concatenate(cols)])
    cols = []
    for g in range(2):
        cols.append(2432 + g * 64 + np.arange(64))
        cols.append(2688 + g * 64 + np.arange(64))
        cols.append(2816 + g * 12 + np.arange(12))
    w['w_inCt'] = f(w_in[:, :, np.concatenate(cols)])
    w['WsT'] = f(inp['a_ws'].transpose(0, 3, 1, 2).reshape(L, 128, 512))
    w['bsT'] = _pad64(inp['a_bs'].transpose(0, 2, 1))
    w['a_lng'] = f(np.broadcast_to(inp['a_ln_g'].reshape(L, 1, 256), (L, 128, 256)))
    w['a_lnb'] = f(np.broadcast_to(inp['a_ln_b'].reshape(L, 1, 256), (L, 128, 256)))
    w['b_gng'] = f(np.broadcast_to(inp['b_gn_g'].reshape(L, 1, 256), (L, 128, 256)))
    w['b_gnb'] = f(np.broadcast_to(inp['b_gn_b'].reshape(L, 1, 256), (L, 128, 256)))
    posT = np.concatenate([inp['c_pos_k'].transpose(0, 2, 1), inp['c_pos_v'].transpose(0, 2, 1)], 1)
    w['posT'] = _pad64(posT)
    w1k = inp['c_w1_k'].reshape(L, 32, 64, 64).transpose(0, 2, 1, 3)
    w1v = inp['c_w1_v'].reshape(L, 32, 64, 64).transpose(0, 2, 1, 3)
    w['w1s'] = f(np.concatenate([w1k, w1v], 1).reshape(L, 128, 2048))
    w['w2k'] = f(np.concatenate([inp['c_w2_k'], inp['c_w2_k']], 2))
    w['w2v'] = f(inp['c_w2_v'])
    w['w_out'] = f(inp['w_out'])
    w['lnpk'] = _pad64(np.concatenate([inp[k].reshape(L, 8, 128).transpose(0, 2, 1)
                                       for k in ('ln1_g', 'ln1_b', 'ln2_g', 'ln2_b')], 2))
    w['w_up'] = f(inp['w_up'])
    w['cwT'] = f(inp['conv_w'].reshape(L, 3, 44, 128).transpose(0, 3, 2, 1).reshape(L, 128, 132))
    w['cbT'] = _pad64(inp['conv_b'].reshape(L, 44, 128).transpose(0, 2, 1))
    w['w_down'] = f(inp['w_down'])
    return w


W_SHAPES = {'w_ada': [2, 1024, 6144], 'b_adaT': [2, 128, 64], 'w_inA': [2, 1024, 512], 'w_inBf': [2, 1024, 1024],
            'w_inBt': [2, 1024, 512], 'w_inCf': [2, 1024, 1280], 'w_inCt': [2, 1024, 280], 'WsT': [2, 128, 512],
            'bsT': [2, 128, 64], 'a_lng': [2, 128, 256], 'a_lnb': [2, 128, 256], 'b_gng': [2, 128, 256], 'b_gnb': [2, 128, 256],
            'posT': [2, 128, 64], 'w1s': [2, 128, 2048], 'w2k': [2, 64, 128], 'w2v': [2, 64, 64],
            'w_out': [2, 1024, 1024], 'lnpk': [2, 128, 64],
            'w_up': [2, 1024, 5632], 'cwT': [2, 128, 132], 'cbT': [2, 128, 64], 'w_down': [2, 2816, 1024]}


def build(n_layers=DEPTH, mixers=('A', 'B', 'C'), do_ffn=True, dbg=None):
    nc = bass.Bass("TRN2", target_bir_lowering=False)
    Dr = {}
    Dr['x'] = nc.dram_tensor("x", [SEQ, DM], F32, kind="ExternalInput").ap()
    Dr['cT'] = nc.dram_tensor("cT", [128, 64], F32, kind="ExternalInput").ap()
    for k, shp in W_SHAPES.items():
        Dr[k] = nc.dram_tensor(k, shp, F32, kind="ExternalInput").ap()
    for k, shp in TABLE_SHAPES.items():
        Dr[k] = nc.dram_tensor(k, shp, F32, kind="ExternalInput").ap()
    out_d = nc.dram_tensor("out", [SEQ, DM], F32, kind="ExternalOutput").ap()
    dbg_d = None
    if dbg is not None:
        dbg_d = nc.dram_tensor("dbg", [SEQ, DM], F32, kind="ExternalOutput").ap()

    st = ExitStack()
    with st:
        S = Sched(nc, st)
        T = lambda name, shape, dt=F32: st.enter_context(nc.sbuf_tensor("s_" + name, shape, dt))
        xT = T("xT", [128, 8, SEQ], F32)
        hT = T("hT", [128, 8, SEQ], BF16)
        WR = T("WR", [128, 4, 4096], BF16)
        AW = 51 * 256
        ARENA = T("ARENA", [128, AW], F32)
        PSA = st.enter_context(nc.psum_tensor("PSA", [128, 2048], F32))
        PSB = st.enter_context(nc.psum_tensor("PSB", [128, 2048], F32))

        def bank(i):
            t = PSA if i < 4 else PSB
            return t[:, (i % 4) * 512:(i % 4 + 1) * 512]

        BK = ['B%d' % i for i in range(8)]

        class Arena:
            def __init__(self):
                self.off = 0

            def reset(self, off=0):
                self.off = off

            def get(self, shape, dt=F32):
                n = int(np.prod(shape))
                nb = n * (2 if dt == BF16 else 4)
                w0 = self.off // 4
                w1 = w0 + (nb + 3) // 4
                assert w1 <= AW, ("arena overflow", w1 * 4, AW * 4)
                self.off = w1 * 4
                ap = ARENA[:, w0:w1]
                if dt == BF16:
                    ap = ap.bitcast(BF16)
                ap = ap[:, 0:n]
                if len(shape) == 2:
                    ap = ap.rearrange("p (a b) -> p a b", a=shape[0])
                elif len(shape) == 3:
                    ap = ap.rearrange("p (a b c) -> p a b c", a=shape[0], b=shape[1])
                return ap

        AR = Arena()

        def MM(out, lhsT, rhs, start, stop, rd, wr):
            S.op('pe', lambda e: e.matmul(out, lhsT=lhsT, rhs=rhs, start=start, stop=stop, skip_group_check=True), rd, wr)

        def TR(out, in_, ident, rd, wr):
            S.op('pe', lambda e: e.transpose(out, in_, ident), rd, wr)

        def ACT(out, in_, func, rd, wr, scale=1.0, bias=None):
            if bias is None:
                S.op('act', lambda e: e.activation(out, in_, func, scale=scale), rd, wr)
            else:
                S.op('act', lambda e: e.activation(out, in_, func, bias=bias, scale=scale), rd, wr)

        def TT(out, in0, in1, op, rd, wr, eng='dve'):
            S.op(eng, lambda e: e.tensor_tensor(out, in0, in1, op), rd, wr)

        def TS(out, in0, s1, s2, op0, op1, rd, wr, eng='dve'):
            if s2 is None:
                S.op(eng, lambda e: e.tensor_scalar(out, in0, s1, None, op0=op0), rd, wr)
            else:
                S.op(eng, lambda e: e.tensor_scalar(out, in0, s1, s2, op0=op0, op1=op1), rd, wr)

        def STT(out, in0, scalar, in1, op0, op1, rd, wr):
            S.op('dve', lambda e: e.scalar_tensor_tensor(out, in0, scalar, in1, op0=op0, op1=op1), rd, wr)

        def CP(out, in_, rd, wr, eng='dve'):
            if eng == 'act':
                S.op('act', lambda e: e.activation(out, in_, AF.Identity), rd, wr)
            else:
                S.op(eng, lambda e: e.tensor_copy(out, in_), rd, wr)

        def RED(out, in_, op, rd, wr):
            S.op('dve', lambda e: e.tensor_reduce(out, in_, axis=AX.X, op=op), rd, wr)

        def RCP(out, in_, rd, wr):
            S.op('dve', lambda e: e.reciprocal(out, in_), rd, wr)

        evt = [0]

        def EV():
            evt[0] += 1
            return 'act' if evt[0] % 2 else 'dve'

        def DMA(q, out, in_, rd, wr):
            pieces = []

            def split(o, i):
                shp = tuple(o.shape)
                assert tuple(i.shape) == shp, (shp, i.shape)
                if len(shp) == 3:
                    for a in range(shp[1]):
                        split(o[:, a, :], i[:, a, :])
                elif len(shp) == 2 and shp[1] > 512:
                    for c0 in range(0, shp[1], 512):
                        c1 = min(shp[1], c0 + 512)
                        pieces.append((o[:, c0:c1], i[:, c0:c1]))
                else:
                    pieces.append((o, i))
            split(out, in_)
            S.dma_multi(q, pieces, rd, wr)

        ident_f = T("ident_f", [128, 128], F32)
        ident_b = T("ident_b", [128, 128], BF16)
        onesm = T("onesm", [128, 128], BF16)
        epsA = T("epsA", [128, 2], F32)
        condT = T("condT", [128, 64], F32)
        modT = [T("modT%d" % l, [128, 48], F32) for l in range(DEPTH)]
        drv = [T("drv%d" % l, [128, 64], F32) for l in range(DEPTH)]
        lnp = [T("lnp%d" % l, [128, 64], F32) for l in range(DEPTH)]
        cwT = T("cwT", [128, 132], F32)
        cbT = T("cbT", [128, 64], F32)
        decayT = T("decayT", [128, 512], F32)
        xiT = T("xiT", [128, 256], F32)
        zeta = T("zeta", [128, 64], F32)
        cdv = T("cdv", [128, 64], F32)
        expand = T("expand", [128, 2048], BF16)
        causalb = T("causalb", [128, 128], BF16)
        winb = T("winb", [128, 128], BF16)
        cmpb = T("cmpb", [128, 2048], BF16)
        fbias = T("fbias", [128, 512], F32)
        causal01 = T("causal01", [128, 128], F32)
        ovl = T("ovl", [128, 64], F32)
        WsT = T("WsT", [128, 512], BF16)
        bsT = T("bsT", [128, 64], F32)
        lnbc = T("lnbc", [128, 4, 256], F32)
        posT = T("posT", [128, 64], F32)
        w1s = T("w1s", [128, 2048], BF16)
        w2k = T("w2k", [64, 128], BF16)
        w2v = T("w2v", [64, 64], BF16)

        DMA('sp', ident_f[:], Dr['identf'], (), ['ident_f'])
        S.op('dve', lambda e: e.tensor_copy(ident_b[:], ident_f[:]), ['ident_f'], ['ident_b'])
        S.op('dve', lambda e: e.memset(onesm[:], 1.0 / 1024.0), (), ['onesm'])
        S.op('dve', lambda e: e.memset(epsA[:, 0:1], LN_EPS), (), ['epsA'])
        S.op('dve', lambda e: e.memset(epsA[:, 1:2], LN_EPS / (ALPHA * ALPHA)), (), ['epsA'])
        barscr = T("barscr", [128, 64], F32)
        RELAY = (barscr[:], Dr['zeta'])
        rlscr = T("rlscr", [128, 2], F32)
        S.relay_fn = lambda e: e.memset(rlscr[:, 0:1], 0.0)
        DMA('sp', condT[:], Dr['cT'], (), ['condT'])
        for nm, tl in (('decayT', decayT), ('xiT', xiT), ('zeta', zeta), ('cdv', cdv), ('fbias', fbias), ('causal01', causal01), ('ovl', ovl)):
            DMA('sp', tl[:], Dr[nm], (), [nm])
        for nm, tl in (('causalb', causalb), ('winb', winb)):
            DMA('pool', tl[:], Dr[nm], (), [nm])
        for nm, tl in (('expand', expand), ('cmpb', cmpb)):
            DMA('pool', tl[:].rearrange("p (a b) -> p a b", a=4), Dr[nm].rearrange("p (a b) -> p a b", a=4), (), [nm])
        ACT(condT[:], condT[:], AF.Silu, ['condT'], ['condT'])

        wr_n = [0]

        def wload(src_ap, shape):
            i = wr_n[0] % 4
            wr_n[0] += 1
            n = int(np.prod(shape))
            v = WR[:, i, 0:n]
            if len(shape) == 2:
                v = v.rearrange("p (a b) -> p a b", a=shape[0])
            key = 'WR%d' % i
            DMA('pool', v, src_ap, (), [key])
            return v, key

        def kchunks(ap2d):
            return ap2d.rearrange("(k p) n -> p k n", p=128)

        AR.reset()
        ada_buf = [AR.get([8, 512], BF16) for _ in range(2)]
        condTb = AR.get([8, 8], BF16)
        CP(condTb, condT[:, 0:8].unsqueeze(2).to_broadcast([128, 8, 8]), ['condT'], ['condTb'])
        for l in range(n_layers):
            for blk in range(12):
                buf = ada_buf[blk % 2]
                bk = 'ada%d' % (blk % 2)
                DMA('pool', buf, kchunks(Dr['w_ada'][l][:, blk * 512:(blk + 1) * 512]), (), [bk])
                for jj in range(4):
                    j = blk * 4 + jj
                    for k in range(8):
                        MM(bank(0)[:, j * 8:j * 8 + 8], buf[:, k, jj * 128:(jj + 1) * 128], condTb[:, k, :],
                           k == 0, k == 7, [bk, 'condTb'], ['B0'])
            btmp = AR.get([64], F32) if l == 0 else btmp
            DMA('sp', btmp, Dr['b_adaT'][l], (), ['btmp'])
            TT(modT[l][:], bank(0)[:, 0:384].rearrange('p (j r) -> p j r', r=8)[:, :, 0], btmp[:, 0:48], ALU.add, ['B0', 'btmp'], ['modT%d' % l])
            DMA('sp', lnp[l][:], Dr['lnpk'][l], (), ['lnp%d' % l])
        for l in range(n_layers):
            m = modT[l]
            d = drv[l]
            mk, dk = 'modT%d' % l, 'drv%d' % l
            TS(d[:, 0:8], m[:, 8:16], 1.0, None, ALU.add, None, [mk], [dk])
            TS(d[:, 8:16], m[:, 16:24], 1.0 / ALPHA, None, ALU.mult, None, [mk], [dk])
            TS(d[:, 16:24], m[:, 32:40], 1.0, None, ALU.add, None, [mk], [dk])
            TS(d[:, 24:32], m[:, 40:48], 1.0 / ALPHA, None, ALU.mult, None, [mk], [dk])
            TT(d[:, 32:40], lnp[l][:, 0:8], d[:, 16:24], ALU.mult, ['lnp%d' % l, dk], [dk])
            TT(d[:, 40:48], lnp[l][:, 8:16], d[:, 16:24], ALU.mult, ['lnp%d' % l, dk], [dk])
            TT(d[:, 40:48], d[:, 40:48], m[:, 24:32], ALU.add, [dk, mk], [dk])
        for l in range(n_layers - 1):
            d, dn = drv[l], drv[l + 1]
            dk, dnk = 'drv%d' % l, 'drv%d' % (l + 1)
            TT(d[:, 48:56], lnp[l][:, 16:24], dn[:, 0:8], ALU.mult, ['lnp%d' % l, dnk, dk], [dk])
            TT(d[:, 56:64], lnp[l][:, 24:32], dn[:, 0:8], ALU.mult, ['lnp%d' % l, dnk, dk], [dk])
            TT(d[:, 56:64], d[:, 56:64], modT[l + 1][:, 0:8], ALU.add, [dk, 'modT%d' % (l + 1)], [dk])

        S.barrier(relay=RELAY)
        AR.reset()
        xst = [AR.get([1024], F32) for _ in range(8)]
        bi = 0
        for tb in (range(4) if dbg != 'skip_xload' else []):
            for q in range(4):
                tt = tb * 4 + q
                DMA('sp', xst[tt % 8], Dr['x'][tt * 128:(tt + 1) * 128, :], (), ['xst%d' % (tt % 8)])
            for c in range(8):
                b = bi % 8
                bi += 1
                for q in range(4):
                    tt = tb * 4 + q
                    TR(bank(b)[:, q * 128:(q + 1) * 128], xst[tt % 8][:, c * 128:(c + 1) * 128], ident_f[:],
                       ['xst%d' % (tt % 8), 'ident_f'], [BK[b]])
                ACT(xT[:, c, tb * 512:(tb + 1) * 512], bank(b), AF.Identity, [BK[b]], ['xT%d_%d' % (c, tb)])
                if dbg != 'no_ts':
                    TS(hT[:, c, tb * 512:(tb + 1) * 512], xT[:, c, tb * 512:(tb + 1) * 512], drv[0][:, c:c + 1], modT[0][:, c:c + 1],
                       ALU.mult, ALU.add, ['xT%d_%d' % (c, tb), 'drv0', 'modT0'], ['hT%d_%d' % (c, tb)])

        def out_proj(l, mixtm, mixkeys, nch, row0, mixT, alias=()):
            nonlocal_bi = [0]
            for c in range(nch):
                for tb in range(4):
                    b = 4 + (nonlocal_bi[0] % 4)
                    nonlocal_bi[0] += 1
                    pb = bank(b).bitcast(BF16)
                    for q in range(4):
                        tt = tb * 4 + q
                        TR(pb[:, q * 128:(q + 1) * 128], mixtm[:, tt, c * 128:(c + 1) * 128], ident_b[:],
                           [mixkeys[tt], 'ident_b'], [BK[b]])
                    CP(mixT[:, c, tb * 512:(tb + 1) * 512], pb[:, 0:512], [BK[b]], ['mixT%d_%d' % (c, tb)] + list(alias), eng=EV())
            wv, wk = wload(kchunks(Dr['w_out'][l][row0:row0 + nch * 128, :]), [nch, 1024])
            for fb in range(8):
                for tb in range(4):
                    b = nonlocal_bi[0] % 4
                    nonlocal_bi[0] += 1
                    for c in range(nch):
                        MM(bank(b), wv[:, c, fb * 128:(fb + 1) * 128], mixT[:, c, tb * 512:(tb + 1) * 512],
                           c == 0, c == nch - 1, [wk, 'mixT%d_%d' % (c, tb)], [BK[b]])
                    xs = xT[:, fb, tb * 512:(tb + 1) * 512]
                    STT(xs, bank(b), drv[l][:, 8 + fb:9 + fb], xs, ALU.mult, ALU.add,
                        [BK[b], 'drv%d' % l, 'xT%d_%d' % (fb, tb)], ['xT%d_%d' % (fb, tb)])

        def layer_norm(l, which, last):
            goff = 0 if which == 1 else 16
            aoff = 32 if which == 1 else 48
            AR.reset()
            xb = [AR.get([512], BF16) for _ in range(3)]
            sq = [AR.get([512], BF16) for _ in range(3)]
            rstd = [AR.get([512], F32) for _ in range(2)]
            nmr = [AR.get([512], F32) for _ in range(2)]
            tmp = [AR.get([512], F32) for _ in range(3)]
            n = 0
            for tb in range(4):
                bm, be = 0 + 2 * (tb % 2), 1 + 2 * (tb % 2)
                for c in range(8):
                    i = n % 3
                    n += 1
                    xs = xT[:, c, tb * 512:(tb + 1) * 512]
                    xk = 'xT%d_%d' % (c, tb)
                    ACT(sq[i], xs, AF.Square, [xk], ['lsq%d' % i])
                    CP(xb[i], xs, [xk], ['lxb%d' % i], eng='dve')
                    MM(bank(bm), onesm[:], xb[i], c == 0, c == 7, ['onesm', 'lxb%d' % i], [BK[bm]])
                    MM(bank(be), onesm[:], sq[i], c == 0, c == 7, ['onesm', 'lsq%d' % i], [BK[be]])
                r = tb % 2
                rk, nk = 'lrstd%d' % r, 'lnmr%d' % r
                ACT(nmr[r], bank(bm), AF.Square, [BK[bm]], [nk])
                TT(rstd[r], bank(be), nmr[r], ALU.subtract, [BK[be], nk], [rk])
                TS(rstd[r], rstd[r], 0.0, None, ALU.max, None, [rk], [rk])
                ACT(rstd[r], rstd[r], AF.Sqrt, [rk, 'epsA'], [rk], bias=epsA[:, 1:2])
                RCP(rstd[r], rstd[r], [rk], [rk])
                STT(nmr[r], bank(bm), -1.0, rstd[r], ALU.mult, ALU.mult, [BK[bm], rk], [nk])
                for c in range(8):
                    i = n % 3
                    n += 1
                    xs = xT[:, c, tb * 512:(tb + 1) * 512]
                    xk = 'xT%d_%d' % (c, tb)
                    tk = 'ltmp%d' % i
                    TT(tmp[i], xs, rstd[r], ALU.mult, [xk, rk], [tk])
                    TT(tmp[i], tmp[i], nmr[r], ALU.add, [tk, nk], [tk])
                    ACT(xs, tmp[i], AF.Identity, [tk, 'lnp%d' % l], [xk],
                        scale=lnp[l][:, goff + c:goff + c + 1], bias=lnp[l][:, goff + 8 + c:goff + 9 + c])
                    if not last:
                        TS(hT[:, c, tb * 512:(tb + 1) * 512], tmp[i], drv[l][:, aoff + c:aoff + c + 1],
                           drv[l][:, aoff + 8 + c:aoff + 9 + c], ALU.mult, ALU.add,
                           [tk, 'drv%d' % l], ['hT%d_%d' % (c, tb)])

        def dump_tm(src, c0, ncol, keys, dcol):
            dst = AR.get([ncol], F32)
            for tt in range(16):
                CP(dst, src[:, tt, c0:c0 + ncol], [keys[tt]], ['dst'])
                DMA('sp', dbg_d[tt * 128:(tt + 1) * 128, dcol:dcol + ncol], dst, ['dst'], ['dbg'])

        def group_ln_core(src, sqv, nb, rk, wk_sq, eps_ap):
            s1 = AR.get([nb], F32)
            s2 = AR.get([nb], F32)
            s3 = AR.get([nb], F32)
            TT(sqv, src, src, ALU.mult, rk, [wk_sq])
            RED(s1, src, ALU.add, rk, ['gs1'])
            RED(s2, sqv, ALU.add, [wk_sq], ['gs2'])
            TS(s1, s1, 1.0 / 64, None, ALU.mult, None, ['gs1'], ['gs1'])
            TT(s3, s1, s1, ALU.mult, ['gs1'], ['gs3'])
            STT(s2, s2, 1.0 / 64, s3, ALU.mult, ALU.subtract, ['gs2', 'gs3'], ['gs2'])
            TS(s2, s2, 0.0, None, ALU.max, None, ['gs2'], ['gs2'])
            ACT(s2, s2, AF.Sqrt, ['gs2', 'epsA'], ['gs2'], bias=eps_ap)
            RCP(s2, s2, ['gs2'], ['gs2'])
            TT(sqv, src, s1.unsqueeze(2).to_broadcast([128, nb, 64]), ALU.subtract, rk + ['gs1'], [wk_sq])
            TT(sqv, sqv, s2.unsqueeze(2).to_broadcast([128, nb, 64]), ALU.mult, [wk_sq, 'gs2'], [wk_sq])

        def mixer_B(l):
            hk = lambda k, tb: 'hT%d_%d' % (k, tb)
            S.barrier(relay=RELAY)
            AR.reset()
            qrT = AR.get([2, 2048], BF16)
            krT = AR.get([2, 2048], BF16)
            off_x = AR.off
            cosT = AR.get([2048], F32)
            sinT = AR.get([2048], F32)
            rt1 = [AR.get([512], F32) for _ in range(2)]
            rt2 = [AR.get([512], F32) for _ in range(2)]
            DMA('sp', cosT, Dr['cosT'], (), ['cosT'])
            DMA('sp', sinT, Dr['sinT'], (), ['sinT'])
            DMA('sp', lnbc[:, 2, :], Dr['b_gng'][l], (), ['lnbc'])
            DMA('sp', lnbc[:, 3, :], Dr['b_gnb'][l], (), ['lnbc'])
            n = 0
            for p in range(2):
                wv, wk = wload(kchunks(Dr['w_inBf'][l][:, p * 512:(p + 1) * 512]), [8, 512])
                for kind in range(2):
                    dst = qrT if kind == 0 else krT
                    for tb in range(4):
                        i = n % 2
                        b1, b2 = 2 * (n % 4), 2 * (n % 4) + 1
                        n += 1
                        for k in range(8):
                            MM(bank(b1), wv[:, k, (2 * kind) * 128:(2 * kind + 1) * 128], hT[:, k, tb * 512:(tb + 1) * 512],
                               k == 0, k == 7, [wk, hk(k, tb)], [BK[b1]])
                        for k in range(8):
                            MM(bank(b2), wv[:, k, (2 * kind + 1) * 128:(2 * kind + 2) * 128], hT[:, k, tb * 512:(tb + 1) * 512],
                               k == 0, k == 7, [wk, hk(k, tb)], [BK[b2]])
                        TT(rt1[i], bank(b1), cosT[:, tb * 512:(tb + 1) * 512], ALU.mult, [BK[b1], 'cosT'], ['rt1_%d' % i])
                        TT(rt2[i], bank(b2), sinT[:, tb * 512:(tb + 1) * 512], ALU.mult, [BK[b2], 'sinT'], ['rt2_%d' % i])
                        TT(dst[:, p, tb * 512:(tb + 1) * 512], rt1[i], rt2[i], ALU.add, ['rt1_%d' % i, 'rt2_%d' % i],
                           ['qk%d_%d' % (kind, p)])
            wvt, wkt = wload(kchunks(Dr['w_inBt'][l]), [8, 512])
            for p in range(2):
                S.barrier(relay=RELAY)
                AR.reset(off_x)
                vg = AR.get([16, 256], BF16)
                kvs = AR.get([16, 128], F32)
                s16 = AR.get([16, 128], BF16)
                oall = AR.get([16, 128], F32)
                kz = [AR.get([128], BF16) for _ in range(2)]
                qx = [AR.get([128], BF16) for _ in range(2)]
                sT = [AR.get([256], BF16) for _ in range(2)]
                mixT = AR.get([1, 2048], BF16)
                qkk = ['qk0_%d' % p, 'qk1_%d' % p]
                for tt in range(16):
                    b = tt % 4
                    for k in range(8):
                        MM(bank(b)[:, 0:256], hT[:, k, tt * 128:(tt + 1) * 128], wvt[:, k, p * 256:(p + 1) * 256],
                           k == 0, k == 7, [hk(k, tt // 4), wkt], [BK[b]])
                    CP(vg[:, tt, 0:128], bank(b)[:, 0:128], [BK[b]], ['vg%d' % tt], eng='dve')
                    ACT(vg[:, tt, 128:256], bank(b)[:, 128:256], AF.Silu, [BK[b]], ['vg%d' % tt])
                S.op('dve', lambda e: e.memset(kvs[:, 0, :], 0.0), (), ['kvs'])
                for c in range(15):
                    i = c % 2
                    bt = 4 + (c % 2)
                    bm = 6 + (c % 2)
                    pb = bank(bt).bitcast(BF16)
                    TR(pb[:, 0:128], krT[:, p, c * 128:(c + 1) * 128], ident_b[:], [qkk[1], 'ident_b'], [BK[bt]])
                    TT(kz[i].rearrange("p (h d) -> p h d", h=2), pb[:, 0:128].rearrange("p (h d) -> p h d", h=2),
                       zeta[:, 2 * p:2 * p + 2].unsqueeze(2).to_broadcast([128, 2, 64]), ALU.mult,
                       [BK[bt], 'zeta'], ['kz%d' % i])
                    MM(bank(bm)[:, 0:128], kz[i], vg[:, c, 0:128], True, True, ['kz%d' % i, 'vg%d' % c], [BK[bm]])
                    STT(kvs[:, c + 1, :], kvs[:, c, :], cdv[:, p:p + 1], bank(bm)[:, 0:128], ALU.mult, ALU.add,
                        ['kvs', 'cdv', BK[bm]], ['kvs'])
                CP(s16, kvs, ['kvs'], ['s16'], eng='dve')
                for c in range(16):
                    i = c % 2
                    bs0 = 2 * (c % 2)
                    bo0 = 4 + 2 * (c % 2)
                    cs = slice(c * 128, (c + 1) * 128)
                    if c > 0:
                        TT(qx[i], qrT[:, p, cs], xiT[:, p * 128:(p + 1) * 128], ALU.mult, [qkk[0], 'xiT'], ['qx%d' % i])
                    for hh in range(2):
                        ps_ = slice(hh * 64, (hh + 1) * 64)
                        MM(bank(bs0 + hh)[:, 0:128], krT[ps_, p, cs], qrT[ps_, p, cs], True, True,
                           qkk, [BK[bs0 + hh]])
                    TT(sT[i].rearrange("p (h l) -> p h l", h=2),
                       PSA[:, bs0 * 512:(bs0 + 2) * 512].rearrange("p (h x) -> p h x", h=2)[:, :, 0:128],
                       decayT[:, 2 * p * 128:(2 * p + 2) * 128].rearrange("p (h l) -> p h l", h=2), ALU.mult,
                       [BK[bs0], BK[bs0 + 1], 'decayT'], ['sT%d' % i])
                    for hh in range(2):
                        ps_ = slice(hh * 64, (hh + 1) * 64)
                        MM(bank(bo0 + hh)[:, 0:64], sT[i][:, hh * 128:(hh + 1) * 128], vg[:, c, hh * 64:(hh + 1) * 64],
                           True, c == 0, ['sT%d' % i, 'vg%d' % c], [BK[bo0 + hh]])
                        if c > 0:
                            MM(bank(bo0 + hh)[:, 0:64], qx[i][ps_, :], s16[ps_, c, hh * 64:(hh + 1) * 64],
                               False, True, ['qx%d' % i, 's16'], [BK[bo0 + hh]])
                    CP(oall[:, c, :].rearrange("p (h e) -> p h e", h=2),
                       PSB[:, (bo0 - 4) * 512:(bo0 - 2) * 512].rearrange("p (h x) -> p h x", h=2)[:, :, 0:64],
                       [BK[bo0], BK[bo0 + 1]], ['oall'], eng='act')
                o3 = oall.rearrange("p c (h e) -> p (c h) e", h=2)
                sq3 = kvs.rearrange("p c (h e) -> p (c h) e", h=2)
                gam_b = lnbc[:, 2, p * 128:(p + 1) * 128].rearrange("p (h e) -> p h e", h=2).unsqueeze(1).to_broadcast([128, 16, 2, 64])
                bet_b = lnbc[:, 3, p * 128:(p + 1) * 128].rearrange("p (h e) -> p h e", h=2).unsqueeze(1).to_broadcast([128, 16, 2, 64])
                sq4 = kvs.rearrange("p c (h e) -> p c h e", h=2)
                group_ln_core(o3, sq3, 32, ['oall'], 'kvs', epsA[:, 0:1])
                TT(sq4, sq4, gam_b, ALU.mult, ['kvs', 'lnbc'], ['kvs'])
                TT(sq4, sq4, bet_b, ALU.add, ['kvs', 'lnbc'], ['kvs'])
                vkeys = ['vg%d' % tt for tt in range(16)]
                TT(vg[:, :, 0:128], kvs, vg[:, :, 128:256], ALU.mult, ['kvs'] + vkeys, vkeys)
                if dbg == 'mixB%d_%d' % (l, p):
                    dump_tm(vg, 0, 128, vkeys, p * 128)
                out_proj(l, vg, vkeys, 1, 256 + p * 128, mixT)

        def mixer_C(l):
            hk = lambda k, tb: 'hT%d_%d' % (k, tb)
            DMA('pool', w1s[:].rearrange("p (a b) -> p a b", a=4), Dr['w1s'][l].rearrange("p (a b) -> p a b", a=4), (), ['w1s'])
            DMA('pool', w2k[:], Dr['w2k'][l], (), ['w2k'])
            DMA('pool', w2v[:], Dr['w2v'][l], (), ['w2v'])
            DMA('sp', posT[:], Dr['posT'][l], (), ['posT'])
            for g in range(2):
                S.barrier(relay=RELAY)
                AR.reset()
                qz = AR.get([4, 2048], BF16)
                KsT = AR.get([2048], BF16)
                KwT = AR.get([2048], BF16)
                Vaug = AR.get([16, 2, 65], BF16)
                gates = AR.get([16, 12], F32)
                mixC = AR.get([16, 256], BF16)
                kcmpT = AR.get([127], BF16)
                vcaug = AR.get([97], BF16)
                hidT = AR.get([2, 127], BF16)
                off_r = AR.off
                kvcT = AR.get([2048], BF16)
                kcp = AR.get([32, 127], BF16)
                wv, wk = wload(kchunks(Dr['w_inCf'][l][:, g * 640:g * 640 + 512]), [8, 512])
                wv2, wk2 = wload(kchunks(Dr['w_inCf'][l][:, g * 640 + 512:g * 640 + 640]), [8, 128])
                S.op('dve', lambda e: e.memset(qz, 0.0), (), ['qT'])
                dsts = [(None, 'qT', 0.125), (None, 'qT', 0.125), (KsT, 'KsT', 1.0), (KwT, 'KwT', 1.0),
                        (kvcT, 'kvcT', 1.0)]
                n = 0
                for bi_, (dst, dk, scl) in enumerate(dsts):
                    for tb in range(4):
                        b = n % 4
                        n += 1
                        for k in range(8):
                            lw = wv[:, k, bi_ * 128:(bi_ + 1) * 128] if bi_ < 4 else wv2[:, k, :]
                            MM(bank(b), lw, hT[:, k, tb * 512:(tb + 1) * 512], k == 0, k == 7,
                               [wk if bi_ < 4 else wk2, hk(k, tb)], [BK[b]])
                        if dst is None:
                            ACT(qz[0:64, 2 * bi_, tb * 512:(tb + 1) * 512], bank(b)[0:64, :], AF.Identity, [BK[b]], [dk], scale=scl)
                            TS(qz[64:128, 2 * bi_ + 1, tb * 512:(tb + 1) * 512], bank(b)[64:128, :], scl, None, ALU.mult, None, [BK[b]], [dk])
                        elif n % 2:
                            ACT(dst[:, tb * 512:(tb + 1) * 512], bank(b), AF.Identity, [BK[b]], [dk], scale=scl)
                        else:
                            TS(dst[:, tb * 512:(tb + 1) * 512], bank(b), scl, None, ALU.mult, None, [BK[b]], [dk])
                wv3, wk3 = wload(kchunks(Dr['w_inCt'][l][:, g * 140:(g + 1) * 140]), [8, 140])
                S.op('dve', lambda e: e.memset(Vaug[:, :, :, 64:65], 1.0), (), ['Vaug'])
                for tt in range(16):
                    b = 4 + tt % 4
                    for k in range(8):
                        MM(bank(b)[:, 0:140], hT[:, k, tt * 128:(tt + 1) * 128], wv3[:, k, :], k == 0, k == 7,
                           [hk(k, tt // 4), wk3], [BK[b]])
                    CP(Vaug[:, tt, :, 0:64], bank(b)[:, 0:128].rearrange("p (s d) -> p s d", s=2), [BK[b]], ['Vaug'], eng='dve')
                    ACT(gates[:, tt, :], bank(b)[:, 128:140], AF.Sigmoid, [BK[b]], ['gates'])
                kv_t = kvcT.tensor
                win = bass.AP(kv_t, kvcT.offset, [list(kvcT.ap[0]), [1, 32], [16, 127]])
                TT(kcp, win, posT[:, 0:32].unsqueeze(2).to_broadcast([128, 32, 127]), ALU.add, ['kvcT', 'posT'], ['kcp'])
                for kv in range(2):
                    ps_ = slice(kv * 64, (kv + 1) * 64)
                    for ll in range(32):
                        MM(bank(kv)[0:64, 0:127], w1s[ps_, ll * 64:(ll + 1) * 64], kcp[ps_, ll, :],
                           ll == 0, ll == 31, ['w1s', 'kcp'], [BK[kv]])
                    ACT(hidT[0:64, kv, :], bank(kv)[0:64, 0:127], AF.Gelu_apprx_tanh, [BK[kv]], ['hidT'])
                MM(bank(2)[:, 0:127], w2k[:], hidT[0:64, 0, :], True, True, ['w2k', 'hidT'], ['B2'])
                CP(kcmpT, bank(2)[:, 0:127], ['B2'], ['kcmpT'], eng='dve')
                MM(bank(3)[0:127, 0:64], hidT[0:64, 1, :], w2v[:], True, True, ['w2v', 'hidT'], ['B3'])
                S.op('dve', lambda e: e.memset(vcaug[:, 64:65], 1.0), (), ['vcaug'])
                CP(vcaug[0:127, 0:64], bank(3)[0:127, 0:64], ['B3'], ['vcaug'], eng='dve')
                CP(vcaug[:, 65:97], ovl[:, 0:32], ['ovl'], ['vcaug'], eng='dve')
                S.barrier(relay=RELAY)
                AR.reset(off_r)
                PT = [AR.get([512], BF16) for _ in range(3)]
                MbT = [AR.get([128], BF16) for _ in range(2)]
                for j_ in range(2):
                    S.op('dve', (lambda t_: (lambda e: e.memset(t_, 0.0)))(MbT[j_]), (), ['MbT%d' % j_])
                Mb = [AR.get([32], BF16) for _ in range(2)]
                ocg = [AR.get([4, 64], F32) for _ in range(2)]
                t1 = [AR.get([4, 64], F32) for _ in range(2)]
                t2 = [AR.get([4, 64], F32) for _ in range(2)]
                sm = [AR.get([96], F32) for _ in range(2)]
                impt = [AR.get([4, 32], F32) for _ in range(2)]
                pn = [0]

                def qslice(r, qt):
                    return qz[:, r, qt * 128:(qt + 1) * 128]

                def attend(qt, kts, KT, vsel, bS, bO, extra_fn):
                    for ki, kt in enumerate(kts):
                        bs_ = bS[ki % len(bS)]
                        extras = extra_fn(kt)
                        for r in range(4):
                            MM(bank(bs_)[:, r * 128:(r + 1) * 128], KT[:, kt * 128:(kt + 1) * 128], qslice(r, qt),
                               r == 0, (r == 3 and not extras), ['KsT', 'KwT', 'qT'], [BK[bs_]])
                        v4 = bank(bs_).rearrange("p (r t) -> p r t", r=4)
                        for ei, (lh, rh, ks) in enumerate(extras):
                            MM(v4, lh, rh, False, ei == len(extras) - 1, ks, [BK[bs_]])
                        i = pn[0] % 3
                        pn[0] += 1
                        ACT(PT[i], bank(bs_), AF.Exp, [BK[bs_]], ['PT%d' % i])
                        for r in range(4):
                            MM(bank(bO)[:, r * 65:(r + 1) * 65], PT[i][:, r * 128:(r + 1) * 128], Vaug[:, kt, vsel, :],
                               ki == 0 and r == 0, ki == len(kts) - 1 and r == 3, ['PT%d' % i, 'Vaug'], [BK[bO]])

                for qt in range(16):
                    j = qt % 2
                    smj = sm[j]
                    sk = 'sm%d' % j
                    qs = slice(qt * 128, (qt + 1) * 128)
                    for r in range(4):
                        MM(bank(0)[0:127, r * 128:(r + 1) * 128], kcmpT[:, :], qslice(r, qt), r == 0, False,
                           ['kcmpT', 'qT'], ['B0'])
                    MM(bank(0)[0:127, :].rearrange("p (r t) -> p r t", r=4), ident_b[0:127, 0:127],
                       cmpb[0:127, qs].unsqueeze(1).to_broadcast([127, 4, 128]), False, True, ['ident_b', 'cmpb'], ['B0'])
                    i = pn[0] % 3
                    pn[0] += 1
                    ACT(PT[i][0:127, :], bank(0)[0:127, :], AF.Exp, ['B0'], ['PT%d' % i])
                    for r in range(4):
                        MM(bank(1)[:, r * 97:(r + 1) * 97], PT[i][0:127, r * 128:(r + 1) * 128], vcaug[0:127, :],
                           r == 0, r == 3, ['PT%d' % i, 'vcaug'], ['B1'])
                    O = bank(1)[:, 0:388].rearrange("p (r c) -> p r c", r=4)
                    cb4 = causalb[:].unsqueeze(1).to_broadcast([128, 4, 128])
                    wb4 = winb[:].unsqueeze(1).to_broadcast([128, 4, 128])

                    def wmask(kt, qt=qt, cb4=cb4, wb4=wb4):
                        if kt == qt:
                            return [(ident_b[:], cb4, ['ident_b', 'causalb'])]
                        if kt == qt - 4:
                            return [(ident_b[:], wb4, ['ident_b', 'winb'])]
                        return []
                    attend(qt, list(range(max(0, qt - 4), qt + 1)), KwT, 1, [2, 3], 5, wmask)
                    TS(smj[:, 0:4], O[:, :, 64], 1e-30, None, ALU.max, None, ['B1'], [sk])
                    RCP(smj[:, 4:8], smj[:, 0:4], [sk], [sk])
                    TT(impt[j], O[:, :, 65:97], smj[:, 4:8].unsqueeze(2).to_broadcast([128, 4, 32]), ALU.mult, ['B1', sk], ['impt%d' % j])
                    RED(smj[:, 32:64], impt[j].rearrange("p r c -> p c r"), ALU.add, ['impt%d' % j], [sk])
                    TT(smj[:, 32:64], smj[:, 32:64], fbias[:, qt * 32:(qt + 1) * 32], ALU.add, [sk, 'fbias'], [sk])
                    S.op('dve', (lambda o_, i_: (lambda e: e.max(o_, i_)))(smj[:, 64:72], smj[:, 32:64]), [sk], [sk])
                    TS(Mb[j], smj[:, 32:64], smj[:, 71:72], -1.0, ALU.is_ge, ALU.add, [sk], ['Mb%d' % j])
                    pbt = bank(0).bitcast(BF16)
                    TR(pbt[0:32, 0:128], Mb[j], ident_b[:], ['Mb%d' % j, 'ident_b'], ['B0'])
                    CP(MbT[j][0:32, :], pbt[0:32, 0:128], ['B0'], ['MbT%d' % j], eng='dve')
                    gv = gates[:, qt, :].rearrange("p (r c) -> p r c", r=4)
                    TT(smj[:, 8:12], smj[:, 4:8], gv[:, :, 0], ALU.mult, [sk, 'gates'], [sk])
                    TT(ocg[j], O[:, :, 0:64], smj[:, 8:12].unsqueeze(2).to_broadcast([128, 4, 64]), ALU.mult, ['B1', sk], ['ocg%d' % j])

                    mb4 = MbT[j][:, :].unsqueeze(1).to_broadcast([128, 4, 128])

                    def smask(kt, qt=qt, j=j, cb4=cb4, mb4=mb4):
                        ex = [(expand[:, kt * 128:(kt + 1) * 128], mb4, ['expand', 'MbT%d' % j])]
                        if kt == qt:
                            ex.append((ident_b[:], cb4, ['ident_b', 'causalb']))
                        return ex
                    attend(qt, list(range(0, qt + 1)), KsT, 0, [6, 7], 4, smask)
                    Os = bank(4)[:, 0:260].rearrange("p (r c) -> p r c", r=4)
                    Ow = bank(5)[:, 0:260].rearrange("p (r c) -> p r c", r=4)
                    RCP(smj[:, 12:16], Os[:, :, 64], ['B4'], [sk])
                    TT(smj[:, 12:16], smj[:, 12:16], gv[:, :, 1], ALU.mult, [sk, 'gates'], [sk])
                    RCP(smj[:, 16:20], Ow[:, :, 64], ['B5'], [sk])
                    TT(smj[:, 16:20], smj[:, 16:20], gv[:, :, 2], ALU.mult, [sk, 'gates'], [sk])
                    TT(t1[j], Os[:, :, 0:64], smj[:, 12:16].unsqueeze(2).to_broadcast([128, 4, 64]), ALU.mult, ['B4', sk], ['t1_%d' % j])
                    TT(t2[j], Ow[:, :, 0:64], smj[:, 16:20].unsqueeze(2).to_broadcast([128, 4, 64]), ALU.mult, ['B5', sk], ['t2_%d' % j])
                    TT(t1[j], t1[j], ocg[j], ALU.add, ['t1_%d' % j, 'ocg%d' % j], ['t1_%d' % j])
                    TT(mixC[:, qt, :].rearrange("p (r d) -> p r d", r=4), t1[j], t2[j], ALU.add, ['t1_%d' % j, 't2_%d' % j], ['mixC%d' % qt])
                mkeys = ['mixC%d' % tt for tt in range(16)]
                if dbg == 'mixC%d_%d' % (l, g):
                    dump_tm(mixC, 0, 256, mkeys, g * 256)
                S.barrier(relay=RELAY)
                AR.reset(off_r)
                mixT = AR.get([2, 2048], BF16)
                out_proj(l, mixC, mkeys, 2, 512 + g * 256, mixT)

        for l in range(n_layers):
            hk = lambda k, tb: 'hT%d_%d' % (k, tb)
            if 'A' in mixers:
                S.barrier(relay=RELAY)
                AR.reset()
                zag = AR.get([16, 512], BF16)
                off_sqv = AR.off
                sqv = AR.get([16, 256], F32)
                st1 = AR.get([64], F32)
                st2 = AR.get([64], F32)
                st3 = AR.get([64], F32)
                mixA = AR.get([16, 256], BF16)
                atmp = [AR.get([256], F32) for _ in range(2)]
                wstage = AR.get([4, 128], F32)
                DMA('sp', wstage, Dr['WsT'][l].rearrange("p (g t) -> p g t", g=4), (), ['wstage'])
                TT(WsT[:].rearrange("p (g t) -> p g t", g=4), wstage,
                   causal01[:].unsqueeze(1).to_broadcast([128, 4, 128]), ALU.mult, ['wstage', 'causal01'], ['WsT'])
                DMA('sp', bsT[:], Dr['bsT'][l], (), ['bsT'])
                DMA('sp', lnbc[:, 0, :], Dr['a_lng'][l], (), ['lnbcA'])
                DMA('sp', lnbc[:, 1, :], Dr['a_lnb'][l], (), ['lnbcA'])
                wv, wk = wload(kchunks(Dr['w_inA'][l]), [8, 512])
                for tt in range(16):
                    b = tt % 4
                    for k in range(8):
                        MM(bank(b), hT[:, k, tt * 128:(tt + 1) * 128], wv[:, k, :], k == 0, k == 7,
                           [hk(k, tt // 4), wk], [BK[b]])
                    ACT(zag[:, tt, :], bank(b), AF.Gelu_apprx_tanh, [BK[b]], ['zag'])
                v4 = zag[:, :, 256:512].rearrange("p t (g d) -> p t g d", g=4)
                TT(sqv, zag[:, :, 256:512], zag[:, :, 256:512], ALU.mult, ['zag'], ['sqv'])
                RED(st1.rearrange("p (t g) -> p t g", t=16), v4, ALU.add, ['zag'], ['st1'])
                RED(st2.rearrange("p (t g) -> p t g", t=16), sqv.rearrange("p t (g d) -> p t g d", g=4), ALU.add, ['sqv'], ['st2'])
                TS(st1, st1, 1.0 / 64, None, ALU.mult, None, ['st1'], ['st1'])
                TT(st3, st1, st1, ALU.mult, ['st1'], ['st3'])
                STT(st2, st2, 1.0 / 64, st3, ALU.mult, ALU.subtract, ['st2', 'st3'], ['st2'])
                TS(st2, st2, 0.0, None, ALU.max, None, ['st2'], ['st2'])
                ACT(st2, st2, AF.Sqrt, ['st2', 'epsA'], ['st2'], bias=epsA[:, 0:1])
                RCP(st2, st2, ['st2'], ['st2'])
                mean_b = st1.rearrange("p (t g) -> p t g", t=16).unsqueeze(3).to_broadcast([128, 16, 4, 64])
                rstd_b = st2.rearrange("p (t g) -> p t g", t=16).unsqueeze(3).to_broadcast([128, 16, 4, 64])
                sq4 = sqv.rearrange("p t (g d) -> p t g d", g=4)
                TT(sq4, v4, mean_b, ALU.subtract, ['zag', 'st1'], ['sqv'])
                TT(sq4, sq4, rstd_b, ALU.mult, ['sqv', 'st2'], ['sqv'])
                gam_b = lnbc[:, 0, :].unsqueeze(1).to_broadcast([128, 16, 256])
                bet_b = lnbc[:, 1, :].unsqueeze(1).to_broadcast([128, 16, 256])
                TT(sqv, sqv, gam_b, ALU.mult, ['sqv', 'lnbcA'], ['sqv'])
                TT(zag[:, :, 256:512], sqv, bet_b, ALU.add, ['sqv', 'lnbcA'], ['zag'])
                bs_b = bsT[:, 0:4].unsqueeze(2).to_broadcast([128, 4, 64])
                for tt in range(16):
                    b = 4 + tt % 4
                    for g in range(4):
                        MM(bank(b)[:, g * 64:(g + 1) * 64], WsT[:, g * 128:(g + 1) * 128],
                           zag[:, tt, 256 + g * 64:256 + (g + 1) * 64], g == 0, g == 3, ['WsT', 'zag'], [BK[b]])
                    at = atmp[tt % 2]
                    ak = 'atmp%d' % (tt % 2)
                    TT(at.rearrange("p (g d) -> p g d", g=4), bank(b)[:, 0:256].rearrange("p (g d) -> p g d", g=4),
                       bs_b, ALU.add, [BK[b], 'bsT'], [ak])
                    TT(mixA[:, tt, :], at, zag[:, tt, 0:256], ALU.mult, [ak, 'zag'], ['mixA%d' % tt])
                if dbg == 'mixA%d' % l:
                    dst = AR.get([256], F32)
                    for tt in range(16):
                        CP(dst, mixA[:, tt, :], ['mixA%d' % tt], ['dst'])
                        DMA('sp', dbg_d[tt * 128:(tt + 1) * 128, 0:256], dst, ['dst'], ['dbg'])
                AR.reset(off_sqv)
                mixT = AR.get([2, 2048], BF16)
                out_proj(l, mixA, ['mixA%d' % tt for tt in range(16)], 2, 0, mixT, ['sqv'])

            if 'B' in mixers:
                mixer_B(l)
            if 'C' in mixers:
                mixer_C(l)

            S.barrier(relay=RELAY)
            layer_norm(l, 1, last=False)
            if dbg == 'ln1_%d' % l:
                break

            if do_ffn:
                S.barrier(relay=RELAY)
                AR.reset()
                DMA('sp', cwT[:], Dr['cwT'][l], (), ['cwT'])
                DMA('sp', cbT[:], Dr['cbT'][l], (), ['cbT'])
                gT = AR.get([max(FF_SPLIT), 2048], BF16)
                ctmp = [[AR.get([1024], F32) for _ in range(2)] for _ in range(2)]
                sgt = [AR.get([1024], BF16) for _ in range(2)]
                j0 = 0
                PS4 = [PSA, PSB]
                P4K = [BK[0:4], BK[4:8]]
                for part_n in FF_SPLIT:
                    wgrp = {}
                    for jl in range(part_n):
                        j = j0 + jl
                        if jl % 4 == 0:
                            ng = min(4, part_n - jl)
                            for part in range(2):
                                jj0 = part * 22 + j
                                wgrp[part] = wload(kchunks(Dr['w_up'][l][:, jj0 * 128:(jj0 + ng) * 128]), [8, ng * 128])
                        for part in range(2):
                            jj = part * 22 + j
                            wvf, wk = wgrp[part]
                            wv = wvf[:, :, (jl % 4) * 128:(jl % 4 + 1) * 128]
                            ps = PS4[part]
                            for tb in range(4):
                                for k in range(8):
                                    MM(ps[:, tb * 512:(tb + 1) * 512], wv[:, k, :], hT[:, k, tb * 512:(tb + 1) * 512],
                                       k == 0, k == 7, [wk, hk(k, tb)], [P4K[part][tb]])
                            for hf in range(2):
                                ct = ctmp[part][hf]
                                ck = 'ctmp%d_%d' % (part, hf)
                                o = hf * 1024
                                pk = P4K[part][2 * hf:2 * hf + 2]
                                pkp = P4K[part][max(0, 2 * hf - 1):2 * hf + 2]
                                ACT(ct, ps[:, o:o + 1024], AF.Identity, pk + ['cwT', 'cbT'], [ck],
                                    scale=cwT[:, jj * 3 + 2:jj * 3 + 3], bias=cbT[:, jj:jj + 1])
                                if hf == 0:
                                    STT(ct[:, 1:1024], ps[:, 0:1023], cwT[:, jj * 3 + 1:jj * 3 + 2], ct[:, 1:1024],
                                        ALU.mult, ALU.add, pk + ['cwT', ck], [ck])
                                    STT(ct[:, 2:1024], ps[:, 0:1022], cwT[:, jj * 3:jj * 3 + 1], ct[:, 2:1024],
                                        ALU.mult, ALU.add, pk + ['cwT', ck], [ck])
                                else:
                                    STT(ct, ps[:, o - 1:o + 1023], cwT[:, jj * 3 + 1:jj * 3 + 2], ct,
                                        ALU.mult, ALU.add, pkp + ['cwT', ck], [ck])
                                    STT(ct, ps[:, o - 2:o + 1022], cwT[:, jj * 3:jj * 3 + 1], ct,
                                        ALU.mult, ALU.add, pkp + ['cwT', ck], [ck])
                                if part == 0:
                                    ACT(sgt[hf], ct, AF.Silu, [ck], ['sgt%d' % hf])
                                else:
                                    TT(gT[:, jl, o:o + 1024], ct, sgt[hf], ALU.mult, [ck, 'sgt%d' % hf], ['gT%d' % jl])
                    nsl = (part_n + 3) // 4
                    wvs = []
                    for s in range(nsl):
                        r0 = (j0 + 4 * s) * 128
                        nr = min(4, part_n - 4 * s)
                        wvs.append(wload(kchunks(Dr['w_down'][l][r0:r0 + nr * 128, :]), [nr, 1024]))
                    bi2 = 0
                    for fb in range(8):
                        for tb in range(4):
                            b = bi2 % 8
                            bi2 += 1
                            for jl in range(part_n):
                                wv, wk = wvs[jl // 4]
                                MM(bank(b), wv[:, jl % 4, fb * 128:(fb + 1) * 128], gT[:, jl, tb * 512:(tb + 1) * 512],
                                   jl == 0, jl == part_n - 1, [wk, 'gT%d' % jl], [BK[b]])
                            xs = xT[:, fb, tb * 512:(tb + 1) * 512]
                            STT(xs, bank(b), drv[l][:, 24 + fb:25 + fb], xs, ALU.mult, ALU.add,
                                [BK[b], 'drv%d' % l, 'xT%d_%d' % (fb, tb)], ['xT%d_%d' % (fb, tb)])
                    j0 += part_n
                S.barrier(relay=RELAY)
            layer_norm(l, 2, last=(l == n_layers - 1))

        S.barrier(relay=RELAY)
        AR.reset()
        ost = [AR.get([1024], F32) for _ in range(4)]
        bi = 0
        for tt in range(16):
            o = ost[tt % 4]
            ok = 'ost%d' % (tt % 4)
            for cg in range(2):
                b = bi % 8
                bi += 1
                for q in range(4):
                    c = cg * 4 + q
                    TR(bank(b)[:, q * 128:(q + 1) * 128], xT[:, c, tt * 128:(tt + 1) * 128], ident_f[:],
                       ['xT%d_%d' % (c, tt // 4), 'ident_f'], [BK[b]])
                CP(o[:, cg * 512:(cg + 1) * 512], bank(b), [BK[b]], [ok], eng=EV())
            DMA('sp', out_d[tt * 128:(tt + 1) * 128, :], o, [ok], ['out'])
        S.finish()
        S.replay()
    return nc


_CACHE = {}


def kernel(**inputs):
    inp = {k: np.asarray(v) for k, v in inputs.items()}
    if 'nc' not in _CACHE:
        _CACHE['nc'] = build()
    nc = _CACHE['nc']
    w = _prep_weights(inp)
    tb = _tables()
    in_maps = []
    for b in range(8):
        m = dict(w)
        m.update(tb)
        m['x'] = np.ascontiguousarray(inp['x'][b], dtype=np.float32)
        m['cT'] = _pad64(inp['c'][b].reshape(8, 128).T)
        in_maps.append(m)
    res = run_bass_kernel_spmd(nc, in_maps, core_ids=list(range(8)))
    out = np.stack([np.asarray(r['out'], dtype=np.float32) for r in res.results], 0)
    return out
```

```python
import math
from contextlib import ExitStack
import numpy as np
import concourse.bass as bass
import concourse.mybir as mybir
from concourse.bass_utils import run_bass_kernel_spmd

F32 = mybir.dt.float32
BF16 = mybir.dt.bfloat16
AF = mybir.ActivationFunctionType
ALU = mybir.AluOpType
AX = mybir.AxisListType

ENGS = ['pe', 'dve', 'act', 'pool', 'sp']
NRING = 8
DEPTH = 2
SEQ = 2048
DM = 1024
ALPHA = (2 * DEPTH) ** 0.25
LN_EPS = 1e-5
NEGB = -30000.0
FF_SPLIT = [6, 6, 5, 5]


class Sched:
    def __init__(self, nc, stack):
        self.nc = nc
        self.prog = {e: [] for e in ENGS}
        self.cnt = {e: 0 for e in ENGS}
        self.seen = {e: {} for e in ENGS}
        self.lastw = {}
        self.readers = {}
        self.sems = {}
        self.semval = {}
        self.relay_fn = None
        for e in ENGS:
            self.sems[e] = stack.enter_context(nc.semaphore("s_" + e))
        self.dma_n = {}
        for q in ['sp', 'act', 'pool']:
            self.dma_n[q] = 0
            for j in range(NRING):
                nm = "d_%s%d" % (q, j)
                self.sems[nm] = stack.enter_context(nc.semaphore(nm))

    def _deps(self, eng, reads, writes, is_dma=False):
        deps = []
        for k in reads:
            t = self.lastw.get(k)
            if t is not None:
                deps.append((t, 'raw'))
        for k in writes:
            t = self.lastw.get(k)
            if t is not None:
                deps.append((t, 'waw'))
            for s, v in self.readers.get(k, {}).items():
                deps.append(((s, v), 'war'))
        need = {}
        for (s, v), kind in deps:
            if s == eng and not is_dma:
                if kind != 'raw' or eng == 'pe':
                    continue
            if self.seen[eng].get(s, 0) >= v:
                continue
            if need.get(s, 0) < v:
                need[s] = v
        return need

    def _emit_waits(self, eng, need):
        if eng in ('sp', 'pool') and 'pe' in need and self.relay_fn is not None:
            need = dict(need)
            v = need.pop('pe')
            self.seen[eng]['pe'] = v
            R = 'dve'
            if self.seen[R].get('pe', 0) < v:
                self.prog[R].append(('wait', 'pe', v))
                self.seen[R]['pe'] = v
            lr = getattr(self, 'last_relay', 0)
            if lr and self.seen[R].get(R, 0) < lr:
                self.prog[R].append(('wait', R, lr))
                self.seen[R][R] = lr
            self.cnt[R] += 1
            self.last_relay = self.cnt[R]
            self.semval[R] = self.cnt[R]
            self.prog[R].append(('op', self.relay_fn, R, 1))
            if self.seen[eng].get(R, 0) < self.cnt[R]:
                need[R] = max(need.get(R, 0), self.cnt[R])
        for s, v in need.items():
            self.prog[eng].append(('wait', s, v))
            self.seen[eng][s] = v

    def _commit(self, tok, reads, writes):
        for k in writes:
            self.lastw[k] = tok
            self.readers[k] = {}
        for k in reads:
            d = self.readers.setdefault(k, {})
            if d.get(tok[0], 0) < tok[1]:
                d[tok[0]] = tok[1]

    def relay_readers(self, keys):
        if self.relay_fn is None:
            return
        v = 0
        for k in keys:
            d = self.readers.get(k)
            if d and 'pe' in d:
                v = max(v, d['pe'])
        if v == 0:
            return
        R = 'dve'
        if self.seen[R].get('pe', 0) < v:
            self.prog[R].append(('wait', 'pe', v))
            self.seen[R]['pe'] = v
        lr = getattr(self, 'last_relay', 0)
        if lr and self.seen[R].get(R, 0) < lr:
            self.prog[R].append(('wait', R, lr))
            self.seen[R][R] = lr
        self.cnt[R] += 1
        self.semval[R] = self.cnt[R]
        self.last_relay = self.cnt[R]
        self.prog[R].append(('op', self.relay_fn, R, 1))
        for k in keys:
            d = self.readers.get(k)
            if d and 'pe' in d:
                d.pop('pe')
                d[R] = max(d.get(R, 0), self.cnt[R])

    def op(self, eng, fn, reads=(), writes=()):
        need = self._deps(eng, reads, writes)
        self._emit_waits(eng, need)
        self.cnt[eng] += 1
        tok = (eng, self.cnt[eng])
        self.semval[eng] = self.cnt[eng]
        self.prog[eng].append(('op', fn, eng, 1))
        self._commit(tok, reads, writes)
        return tok

    def dma_multi(self, q, pairs, reads=(), writes=()):
        i = self.dma_n[q]
        self.dma_n[q] += 1
        slot = "d_%s%d" % (q, i % NRING)
        prev = self.semval.get(slot, 0)
        val = prev + 16 * len(pairs)
        need = self._deps(q, reads, writes, is_dma=True)
        if prev > 0 and self.seen[q].get(slot, 0) < prev:
            need[slot] = max(need.get(slot, 0), prev)
        self._emit_waits(q, need)
        for out, in_ in pairs:
            self.prog[q].append(('op', (lambda o_, i_: (lambda e: e.dma_start(out=o_, in_=i_)))(out, in_), slot, 16))
        self.semval[slot] = val
        tok = (slot, val)
        self._commit(tok, reads, writes)
        return tok

    def dma(self, q, out, in_, reads=(), writes=()):
        return self.dma_multi(q, [(out, in_)], reads, writes)

    def _wait_all(self, e):
        need = {}
        for s_, v in self.semval.items():
            if s_ == e:
                continue
            if self.seen[e].get(s_, 0) < v:
                need[s_] = v
        self._emit_waits(e, need)

    def barrier(self, engs=('pe', 'dve', 'act', 'sp'), relay=None):
        if relay is None or 'sp' not in engs:
            for e in engs:
                self._wait_all(e)
            return
        snap = dict(self.semval)
        self._wait_all('sp')
        tok = self.dma('sp', relay[0], relay[1], (), ['__bar'])
        for e in engs:
            if e == 'sp':
                continue
            self._emit_waits(e, {tok[0]: tok[1]} if self.seen[e].get(tok[0], 0) < tok[1] else {})
            for s_, v in snap.items():
                if s_ != e and self.seen[e].get(s_, 0) < v:
                    self.seen[e][s_] = v

    def finish(self):
        self.barrier(engs=('sp',))

    def replay(self):
        nc = self.nc
        sems = self.sems
        prog = self.prog

        def run(e, name):
            for it in prog[name]:
                if it[0] == 'wait':
                    e.wait_ge(sems[it[1]], it[2])
                else:
                    it[1](e).then_inc(sems[it[2]], it[3])

        with nc.Block() as block:
            @block.tensor
            def _(e):
                run(e, 'pe')

            @block.vector
            def _(e):
                run(e, 'dve')

            @block.scalar
            def _(e):
                run(e, 'act')

            @block.gpsimd
            def _(e):
                run(e, 'pool')

            @block.sync
            def _(e):
                run(e, 'sp')


def _pad64(a):
    a = np.asarray(a, dtype=np.float32)
    out = np.zeros(a.shape[:-1] + (64,), np.float32)
    out[..., :a.shape[-1]] = a
    return out


def _tables():
    t = {}
    half = 32
    inv = np.power(np.float32(10000.0), -np.arange(half, dtype=np.float32) / np.float32(half)).astype(np.float32)
    pos = np.arange(SEQ, dtype=np.float32)
    ang = pos[:, None] * inv[None, :]
    cos = np.cos(ang).astype(np.float32).T
    sin = np.sin(ang).astype(np.float32).T
    cosT = np.concatenate([cos, cos, cos, cos], 0)
    sinT = np.concatenate([-sin, sin, -sin, sin], 0)
    t['cosT'] = np.ascontiguousarray(cosT)
    t['sinT'] = np.ascontiguousarray(sinT)
    H = 4
    L = 128
    lg = np.log1p(-np.exp2(-5.0 - np.arange(H, dtype=np.float32))).astype(np.float32)
    idx = np.arange(L, dtype=np.float32)
    diff = idx[:, None] - idx[None, :]
    dec = np.where(diff >= 0, np.exp(lg[:, None, None] * np.maximum(diff, 0.0)), 0.0).astype(np.float32)
    t['decayT'] = np.ascontiguousarray(np.transpose(dec, (2, 0, 1)) * np.float32(0.125)).reshape(128, 512)
    xi = np.exp(lg[:, None] * (idx + 1.0)).astype(np.float32)
    zeta = np.exp(lg[:, None] * (L - 1.0 - idx)).astype(np.float32)
    xiT = np.zeros((128, 2, 128), np.float32)
    for p in range(2):
        for hh in range(2):
            xiT[hh * 64:(hh + 1) * 64, p, :] = xi[2 * p + hh][None, :]
    t['xiT'] = xiT.reshape(128, 256)
    t['zeta'] = _pad64(zeta.T * np.float32(0.125))
    cd = np.exp(lg * L).astype(np.float32)
    cdv = np.zeros((128, 2), np.float32)
    for p in range(2):
        for hh in range(2):
            cdv[hh * 64:(hh + 1) * 64, p] = cd[2 * p + hh]
    t['cdv'] = _pad64(cdv)
    key = np.arange(SEQ)
    ex = np.zeros((128, SEQ), np.float32)
    ex[key // 64, key] = -NEGB
    t['expand'] = ex
    kk = np.arange(128)[:, None]
    tt = np.arange(128)[None, :]
    t['causalb'] = np.where(kk > tt, NEGB, 0.0).astype(np.float32)
    t['winb'] = np.where(kk <= tt, NEGB, 0.0).astype(np.float32)
    t['identf'] = np.eye(128, dtype=np.float32)
    t['causal01'] = np.where(tt >= kk, 1.0, 0.0).astype(np.float32)
    k127 = np.arange(128)[:, None]
    tpos = np.arange(SEQ)[None, :]
    t['cmpb'] = np.where(16 * k127 + 31 > tpos, NEGB, 0.0).astype(np.float32)
    fb = np.zeros((128, 16, 32), np.float32)
    for qt in range(16):
        tq = qt * 128 + np.arange(128)
        cur = tq // 64
        blk = np.arange(32)
        future = blk[None, :] > cur[:, None]
        forced = (blk[None, :] == 0) | (blk[None, :] == cur[:, None]) | (blk[None, :] == cur[:, None] - 1)
        fb[:, qt, :] = np.where(forced, 1e30, np.where(future, -1e30, 0.0))
    t['fbias'] = fb.reshape(128, 512)
    ov = np.zeros((128, 32), np.float32)
    c0 = np.arange(127)[:, None] * 16
    s0 = np.arange(32)[None, :] * 64
    ov[:127] = np.clip(np.minimum(c0 + 32, s0 + 64) - np.maximum(c0, s0), 0, None) / 32.0
    t['ovl'] = _pad64(ov)
    return t


TABLE_SHAPES = {'cosT': [128, 2048], 'sinT': [128, 2048], 'decayT': [128, 512], 'xiT': [128, 256],
                'zeta': [128, 64], 'cdv': [128, 64], 'expand': [128, 2048], 'causalb': [128, 128],
                'winb': [128, 128], 'causal01': [128, 128], 'identf': [128, 128], 'cmpb': [128, 2048], 'fbias': [128, 512], 'ovl': [128, 64]}


def _prep_weights(inp):
    w = {}
    f = lambda a: np.ascontiguousarray(a, dtype=np.float32)
    w_in = inp['w_in']
    L = DEPTH
    w['w_ada'] = f(inp['w_ada'])
    w['b_adaT'] = _pad64(inp['b_ada'].reshape(L, 48, 128).transpose(0, 2, 1))
    w['w_inA'] = f(w_in[:, :, 0:512])
    cols = []
    for p in range(2):
        for base in (512, 768):
            hd = np.arange(128)
            h = 2 * p + hd // 64
            d = hd % 64
            cols.append(base + h * 64 + d)
            cols.append(base + h * 64 + (d + 32) % 64)
    cols = np.concatenate(cols)
    w['w_inBf'] = f(w_in[:, :, cols])
    cols = []
    for p in range(2):
        cols.append(1024 + p * 128 + np.arange(128))
        cols.append(1280 + p * 128 + np.arange(128))
    w['w_inBt'] = f(w_in[:, :, np.concatenate(cols)])
    cols = []
    for g in range(2):
        cols.append(1536 + g * 256 + np.arange(256))
        ks = 2304 + g * 64 + np.arange(64)
        kw = 2560 + g * 64 + np.arange(64)
        cols += [ks, ks, kw, kw]
        cols.append(2048 + g * 64 + np.arange(64))
        cols.append(2176 + g * 64 + np.arange(64))
    w['w_inCf'] = f(w_in[:, :, np.concatenate(cols)])
    cols = []
    for g in range(2):
        cols.append(2432 + g * 64 + np.arange(64))
        cols.append(2688 + g * 64 + np.arange(64))
        cols.append(2816 + g * 12 + np.arange(12))
    w['w_inCt'] = f(w_in[:, :, np.concatenate(cols)])
    w['WsT'] = f(inp['a_ws'].transpose(0, 3, 1, 2).reshape(L, 128, 512))
    w['bsT'] = _pad64(inp['a_bs'].transpose(0, 2, 1))
    w['a_lng'] = f(np.broadcast_to(inp['a_ln_g'].reshape(L, 1, 256), (L, 128, 256)))
    w['a_lnb'] = f(np.broadcast_to(inp['a_ln_b'].reshape(L, 1, 256), (L, 128, 256)))
    w['b_gng'] = f(np.broadcast_to(inp['b_gn_g'].reshape(L, 1, 256), (L, 128, 256)))
    w['b_gnb'] = f(np.broadcast_to(inp['b_gn_b'].reshape(L, 1, 256), (L, 128, 256)))
    posT = np.concatenate([inp['c_pos_k'].transpose(0, 2, 1), inp['c_pos_v'].transpose(0, 2, 1)], 1)
    w['posT'] = _pad64(posT)
    w1k = inp['c_w1_k'].reshape(L, 32, 64, 64).transpose(0, 2, 1, 3)
    w1v = inp['c_w1_v'].reshape(L, 32, 64, 64).transpose(0, 2, 1, 3)
    w['w1s'] = f(np.concatenate([w1k, w1v], 1).reshape(L, 128, 2048))
    w['w2k'] = f(np.concatenate([inp['c_w2_k'], inp['c_w2_k']], 2))
    w['w2v'] = f(inp['c_w2_v'])
    w['w_out'] = f(inp['w_out'])
    w['lnpk'] = _pad64(np.concatenate([inp[k].reshape(L, 8, 128).transpose(0, 2, 1)
                                       for k in ('ln1_g', 'ln1_b', 'ln2_g', 'ln2_b')], 2))
    w['w_up'] = f(inp['w_up'])
    w['cwT'] = f(inp['conv_w'].reshape(L, 3, 44, 128).transpose(0, 3, 2, 1).reshape(L, 128, 132))
    w['cbT'] = _pad64(inp['conv_b'].reshape(L, 44, 128).transpose(0, 2, 1))
    w['w_down'] = f(inp['w_down'])
    return w


W_SHAPES = {'w_ada': [2, 1024, 6144], 'b_adaT': [2, 128, 64], 'w_inA': [2, 1024, 512], 'w_inBf': [2, 1024, 1024],
            'w_inBt': [2, 1024, 512], 'w_inCf': [2, 1024, 1280], 'w_inCt': [2, 1024, 280], 'WsT': [2, 128, 512],
            'bsT': [2, 128, 64], 'a_lng': [2, 128, 256], 'a_lnb': [2, 128, 256], 'b_gng': [2, 128, 256], 'b_gnb': [2, 128, 256],
            'posT': [2, 128, 64], 'w1s': [2, 128, 2048], 'w2k': [2, 64, 128], 'w2v': [2, 64, 64],
            'w_out': [2, 1024, 1024], 'lnpk': [2, 128, 64],
            'w_up': [2, 1024, 5632], 'cwT': [2, 128, 132], 'cbT': [2, 128, 64], 'w_down': [2, 2816, 1024]}


def build(n_layers=DEPTH, mixers=('A', 'B', 'C'), do_ffn=True, dbg=None):
    nc = bass.Bass("TRN2", target_bir_lowering=False)
    Dr = {}
    Dr['x'] = nc.dram_tensor("x", [SEQ, DM], F32, kind="ExternalInput").ap()
    Dr['cT'] = nc.dram_tensor("cT", [128, 64], F32, kind="ExternalInput").ap()
    for k, shp in W_SHAPES.items():
        Dr[k] = nc.dram_tensor(k, shp, F32, kind="ExternalInput").ap()
    for k, shp in TABLE_SHAPES.items():
        Dr[k] = nc.dram_tensor(k, shp, F32, kind="ExternalInput").ap()
    out_d = nc.dram_tensor("out", [SEQ, DM], F32, kind="ExternalOutput").ap()
    dbg_d = None
    if dbg is not None:
        dbg_d = nc.dram_tensor("dbg", [SEQ, DM], F32, kind="ExternalOutput").ap()

    st = ExitStack()
    with st:
        S = Sched(nc, st)
        T = lambda name, shape, dt=F32: st.enter_context(nc.sbuf_tensor("s_" + name, shape, dt))
        xT = T("xT", [128, 8, SEQ], F32)
        hT = T("hT", [128, 8, SEQ], BF16)
        WR = T("WR", [128, 4, 4096], BF16)
        AW = 51 * 256
        ARENA = T("ARENA", [128, AW], F32)
        PSA = st.enter_context(nc.psum_tensor("PSA", [128, 2048], F32))
        PSB = st.enter_context(nc.psum_tensor("PSB", [128, 2048], F32))

        def bank(i):
            t = PSA if i < 4 else PSB
            return t[:, (i % 4) * 512:(i % 4 + 1) * 512]

        BK = ['B%d' % i for i in range(8)]

        class Arena:
            def __init__(self):
                self.off = 0

            def reset(self, off=0):
                self.off = off

            def get(self, shape, dt=F32):
                n = int(np.prod(shape))
                nb = n * (2 if dt == BF16 else 4)
                w0 = self.off // 4
                w1 = w0 + (nb + 3) // 4
                assert w1 <= AW, ("arena overflow", w1 * 4, AW * 4)
                self.off = w1 * 4
                ap = ARENA[:, w0:w1]
                if dt == BF16:
                    ap = ap.bitcast(BF16)
                ap = ap[:, 0:n]
                if len(shape) == 2:
                    ap = ap.rearrange("p (a b) -> p a b", a=shape[0])
                elif len(shape) == 3:
                    ap = ap.rearrange("p (a b c) -> p a b c", a=shape[0], b=shape[1])
                return ap

        AR = Arena()

        def MM(out, lhsT, rhs, start, stop, rd, wr):
            S.op('pe', lambda e: e.matmul(out, lhsT=lhsT, rhs=rhs, start=start, stop=stop, skip_group_check=True), rd, wr)

        def TR(out, in_, ident, rd, wr):
            S.op('pe', lambda e: e.transpose(out, in_, ident), rd, wr)

        def ACT(out, in_, func, rd, wr, scale=1.0, bias=None):
            if bias is None:
                S.op('act', lambda e: e.activation(out, in_, func, scale=scale), rd, wr)
            else:
                S.op('act', lambda e: e.activation(out, in_, func, bias=bias, scale=scale), rd, wr)

        def TT(out, in0, in1, op, rd, wr, eng='dve'):
            S.op(eng, lambda e: e.tensor_tensor(out, in0, in1, op), rd, wr)

        def TS(out, in0, s1, s2, op0, op1, rd, wr, eng='dve'):
            if s2 is None:
                S.op(eng, lambda e: e.tensor_scalar(out, in0, s1, None, op0=op0), rd, wr)
            else:
                S.op(eng, lambda e: e.tensor_scalar(out, in0, s1, s2, op0=op0, op1=op1), rd, wr)

        def STT(out, in0, scalar, in1, op0, op1, rd, wr):
            S.op('dve', lambda e: e.scalar_tensor_tensor(out, in0, scalar, in1, op0=op0, op1=op1), rd, wr)

        def CP(out, in_, rd, wr, eng='dve'):
            if eng == 'act':
                S.op('act', lambda e: e.activation(out, in_, AF.Identity), rd, wr)
            else:
                S.op(eng, lambda e: e.tensor_copy(out, in_), rd, wr)

        def RED(out, in_, op, rd, wr):
            S.op('dve', lambda e: e.tensor_reduce(out, in_, axis=AX.X, op=op), rd, wr)

        def RCP(out, in_, rd, wr):
            S.op('dve', lambda e: e.reciprocal(out, in_), rd, wr)

        evt = [0]

        def EV():
            evt[0] += 1
            return 'act' if evt[0] % 2 else 'dve'

        def DMA(q, out, in_, rd, wr):
            pieces = []

            def split(o, i):
                shp = tuple(o.shape)
                assert tuple(i.shape) == shp, (shp, i.shape)
                if len(shp) == 3:
                    for a in range(shp[1]):
                        split(o[:, a, :], i[:, a, :])
                elif len(shp) == 2 and shp[1] > 512:
                    for c0 in range(0, shp[1], 512):
                        c1 = min(shp[1], c0 + 512)
                        pieces.append((o[:, c0:c1], i[:, c0:c1]))
                else:
                    pieces.append((o, i))
            split(out, in_)
            S.dma_multi(q, pieces, rd, wr)

        ident_f = T("ident_f", [128, 128], F32)
        ident_b = T("ident_b", [128, 128], BF16)
        onesm = T("onesm", [128, 128], BF16)
        epsA = T("epsA", [128, 2], F32)
        condT = T("condT", [128, 64], F32)
        modT = [T("modT%d" % l, [128, 48], F32) for l in range(DEPTH)]
        drv = [T("drv%d" % l, [128, 64], F32) for l in range(DEPTH)]
        lnp = [T("lnp%d" % l, [128, 64], F32) for l in range(DEPTH)]
        cwT = T("cwT", [128, 132], F32)
        cbT = T("cbT", [128, 64], F32)
        decayT = T("decayT", [128, 512], F32)
        xiT = T("xiT", [128, 256], F32)
        zeta = T("zeta", [128, 64], F32)
        cdv = T("cdv", [128, 64], F32)
        expand = T("expand", [128, 2048], BF16)
        causalb = T("causalb", [128, 128], BF16)
        winb = T("winb", [128, 128], BF16)
        cmpb = T("cmpb", [128, 2048], BF16)
        fbias = T("fbias", [128, 512], F32)
        causal01 = T("causal01", [128, 128], F32)
        ovl = T("ovl", [128, 64], F32)
        WsT = T("WsT", [128, 512], BF16)
        bsT = T("bsT", [128, 64], F32)
        lnbc = T("lnbc", [128, 4, 256], F32)
        posT = T("posT", [128, 64], F32)
        w1s = T("w1s", [128, 2048], BF16)
        w2k = T("w2k", [64, 128], BF16)
        w2v = T("w2v", [64, 64], BF16)

        DMA('sp', ident_f[:], Dr['identf'], (), ['ident_f'])
        S.op('dve', lambda e: e.tensor_copy(ident_b[:], ident_f[:]), ['ident_f'], ['ident_b'])
        S.op('dve', lambda e: e.memset(onesm[:], 1.0 / 1024.0), (), ['onesm'])
        S.op('dve', lambda e: e.memset(epsA[:, 0:1], LN_EPS), (), ['epsA'])
        S.op('dve', lambda e: e.memset(epsA[:, 1:2], LN_EPS / (ALPHA * ALPHA)), (), ['epsA'])
        barscr = T("barscr", [128, 64], F32)
        RELAY = (barscr[:], Dr['zeta'])
        rlscr = T("rlscr", [128, 2], F32)
        S.relay_fn = lambda e: e.memset(rlscr[:, 0:1], 0.0)
        DMA('sp', condT[:], Dr['cT'], (), ['condT'])
        for nm, tl in (('decayT', decayT), ('xiT', xiT), ('zeta', zeta), ('cdv', cdv), ('fbias', fbias), ('causal01', causal01), ('ovl', ovl)):
            DMA('sp', tl[:], Dr[nm], (), [nm])
        for nm, tl in (('causalb', causalb), ('winb', winb)):
            DMA('pool', tl[:], Dr[nm], (), [nm])
        for nm, tl in (('expand', expand), ('cmpb', cmpb)):
            DMA('pool', tl[:].rearrange("p (a b) -> p a b", a=4), Dr[nm].rearrange("p (a b) -> p a b", a=4), (), [nm])
        ACT(condT[:], condT[:], AF.Silu, ['condT'], ['condT'])

        wr_n = [0]

        def wload(src_ap, shape):
            S.relay_readers(['WR%d' % j_ for j_ in range(4)])
            i = wr_n[0] % 4
            wr_n[0] += 1
            n = int(np.prod(shape))
            v = WR[:, i, 0:n]
            if len(shape) == 2:
                v = v.rearrange("p (a b) -> p a b", a=shape[0])
            key = 'WR%d' % i
            DMA('pool', v, src_ap, (), [key])
            return v, key

        def kchunks(ap2d):
            return ap2d.rearrange("(k p) n -> p k n", p=128)

        AR.reset()
        ada_buf = [AR.get([8, 512], BF16) for _ in range(2)]
        condTb = AR.get([8, 8], BF16)
        CP(condTb, condT[:, 0:8].unsqueeze(2).to_broadcast([128, 8, 8]), ['condT'], ['condTb'])
        for l in range(n_layers):
            for blk in range(12):
                buf = ada_buf[blk % 2]
                bk = 'ada%d' % (blk % 2)
                DMA('pool', buf, kchunks(Dr['w_ada'][l][:, blk * 512:(blk + 1) * 512]), (), [bk])
                for jj in range(4):
                    j = blk * 4 + jj
                    for k in range(8):
                        MM(bank(0)[:, j * 8:j * 8 + 8], buf[:, k, jj * 128:(jj + 1) * 128], condTb[:, k, :],
                           k == 0, k == 7, [bk, 'condTb'], ['B0'])
            btmp = AR.get([64], F32) if l == 0 else btmp
            DMA('sp', btmp, Dr['b_adaT'][l], (), ['btmp'])
            TT(modT[l][:], bank(0)[:, 0:384].rearrange('p (j r) -> p j r', r=8)[:, :, 0], btmp[:, 0:48], ALU.add, ['B0', 'btmp'], ['modT%d' % l])
            DMA('sp', lnp[l][:], Dr['lnpk'][l], (), ['lnp%d' % l])
        for l in range(n_layers):
            m = modT[l]
            d = drv[l]
            mk, dk = 'modT%d' % l, 'drv%d' % l
            TS(d[:, 0:8], m[:, 8:16], 1.0, None, ALU.add, None, [mk], [dk])
            TS(d[:, 8:16], m[:, 16:24], 1.0 / ALPHA, None, ALU.mult, None, [mk], [dk])
            TS(d[:, 16:24], m[:, 32:40], 1.0, None, ALU.add, None, [mk], [dk])
            TS(d[:, 24:32], m[:, 40:48], 1.0 / ALPHA, None, ALU.mult, None, [mk], [dk])
            TT(d[:, 32:40], lnp[l][:, 0:8], d[:, 16:24], ALU.mult, ['lnp%d' % l, dk], [dk])
            TT(d[:, 40:48], lnp[l][:, 8:16], d[:, 16:24], ALU.mult, ['lnp%d' % l, dk], [dk])
            TT(d[:, 40:48], d[:, 40:48], m[:, 24:32], ALU.add, [dk, mk], [dk])
        for l in range(n_layers - 1):
            d, dn = drv[l], drv[l + 1]
            dk, dnk = 'drv%d' % l, 'drv%d' % (l + 1)
            TT(d[:, 48:56], lnp[l][:, 16:24], dn[:, 0:8], ALU.mult, ['lnp%d' % l, dnk, dk], [dk])
            TT(d[:, 56:64], lnp[l][:, 24:32], dn[:, 0:8], ALU.mult, ['lnp%d' % l, dnk, dk], [dk])
            TT(d[:, 56:64], d[:, 56:64], modT[l + 1][:, 0:8], ALU.add, [dk, 'modT%d' % (l + 1)], [dk])

        S.barrier(relay=RELAY)
        AR.reset()
        xst = [AR.get([1024], F32) for _ in range(8)]
        bi = 0
        for tb in (range(4) if dbg != 'skip_xload' else []):
            for q in range(4):
                tt = tb * 4 + q
                DMA('sp', xst[tt % 8], Dr['x'][tt * 128:(tt + 1) * 128, :], (), ['xst%d' % (tt % 8)])
            for c in range(8):
                b = bi % 8
                bi += 1
                for q in range(4):
                    tt = tb * 4 + q
                    TR(bank(b)[:, q * 128:(q + 1) * 128], xst[tt % 8][:, c * 128:(c + 1) * 128], ident_f[:],
                       ['xst%d' % (tt % 8), 'ident_f'], [BK[b]])
                ACT(xT[:, c, tb * 512:(tb + 1) * 512], bank(b), AF.Identity, [BK[b]], ['xT%d_%d' % (c, tb)])
                if dbg != 'no_ts':
                    TS(hT[:, c, tb * 512:(tb + 1) * 512], xT[:, c, tb * 512:(tb + 1) * 512], drv[0][:, c:c + 1], modT[0][:, c:c + 1],
                       ALU.mult, ALU.add, ['xT%d_%d' % (c, tb), 'drv0', 'modT0'], ['hT%d_%d' % (c, tb)])

        def out_proj(l, mixtm, mixkeys, nch, row0, mixT, alias=()):
            nonlocal_bi = [0]
            for c in range(nch):
                for tb in range(4):
                    b = 4 + (nonlocal_bi[0] % 4)
                    nonlocal_bi[0] += 1
                    pb = bank(b).bitcast(BF16)
                    for q in range(4):
                        tt = tb * 4 + q
                        TR(pb[:, q * 128:(q + 1) * 128], mixtm[:, tt, c * 128:(c + 1) * 128], ident_b[:],
                           [mixkeys[tt], 'ident_b'], [BK[b]])
                    CP(mixT[:, c, tb * 512:(tb + 1) * 512], pb[:, 0:512], [BK[b]], ['mixT%d_%d' % (c, tb)] + list(alias), eng=EV())
            wv, wk = wload(kchunks(Dr['w_out'][l][row0:row0 + nch * 128, :]), [nch, 1024])
            for fb in range(8):
                for tb in range(4):
                    b = nonlocal_bi[0] % 4
                    nonlocal_bi[0] += 1
                    for c in range(nch):
                        MM(bank(b), wv[:, c, fb * 128:(fb + 1) * 128], mixT[:, c, tb * 512:(tb + 1) * 512],
                           c == 0, c == nch - 1, [wk, 'mixT%d_%d' % (c, tb)], [BK[b]])
                    xs = xT[:, fb, tb * 512:(tb + 1) * 512]
                    STT(xs, bank(b), drv[l][:, 8 + fb:9 + fb], xs, ALU.mult, ALU.add,
                        [BK[b], 'drv%d' % l, 'xT%d_%d' % (fb, tb)], ['xT%d_%d' % (fb, tb)])

        def layer_norm(l, which, last):
            goff = 0 if which == 1 else 16
            aoff = 32 if which == 1 else 48
            AR.reset()
            xb = [AR.get([512], BF16) for _ in range(3)]
            sq = [AR.get([512], BF16) for _ in range(3)]
            rstd = [AR.get([512], F32) for _ in range(2)]
            nmr = [AR.get([512], F32) for _ in range(2)]
            tmp = [AR.get([512], F32) for _ in range(3)]
            n = 0
            for tb in range(4):
                bm, be = 0 + 2 * (tb % 2), 1 + 2 * (tb % 2)
                for c in range(8):
                    i = n % 3
                    n += 1
                    xs = xT[:, c, tb * 512:(tb + 1) * 512]
                    xk = 'xT%d_%d' % (c, tb)
                    ACT(sq[i], xs, AF.Square, [xk], ['lsq%d' % i])
                    CP(xb[i], xs, [xk], ['lxb%d' % i], eng='dve')
                    MM(bank(bm), onesm[:], xb[i], c == 0, c == 7, ['onesm', 'lxb%d' % i], [BK[bm]])
                    MM(bank(be), onesm[:], sq[i], c == 0, c == 7, ['onesm', 'lsq%d' % i], [BK[be]])
                r = tb % 2
                rk, nk = 'lrstd%d' % r, 'lnmr%d' % r
                ACT(nmr[r], bank(bm), AF.Square, [BK[bm]], [nk])
                TT(rstd[r], bank(be), nmr[r], ALU.subtract, [BK[be], nk], [rk])
                TS(rstd[r], rstd[r], 0.0, None, ALU.max, None, [rk], [rk])
                ACT(rstd[r], rstd[r], AF.Sqrt, [rk, 'epsA'], [rk], bias=epsA[:, 1:2])
                RCP(rstd[r], rstd[r], [rk], [rk])
                STT(nmr[r], bank(bm), -1.0, rstd[r], ALU.mult, ALU.mult, [BK[bm], rk], [nk])
                for c in range(8):
                    i = n % 3
                    n += 1
                    xs = xT[:, c, tb * 512:(tb + 1) * 512]
                    xk = 'xT%d_%d' % (c, tb)
                    tk = 'ltmp%d' % i
                    TT(tmp[i], xs, rstd[r], ALU.mult, [xk, rk], [tk])
                    TT(tmp[i], tmp[i], nmr[r], ALU.add, [tk, nk], [tk])
                    ACT(xs, tmp[i], AF.Identity, [tk, 'lnp%d' % l], [xk],
                        scale=lnp[l][:, goff + c:goff + c + 1], bias=lnp[l][:, goff + 8 + c:goff + 9 + c])
                    if not last:
                        TS(hT[:, c, tb * 512:(tb + 1) * 512], tmp[i], drv[l][:, aoff + c:aoff + c + 1],
                           drv[l][:, aoff + 8 + c:aoff + 9 + c], ALU.mult, ALU.add,
                           [tk, 'drv%d' % l], ['hT%d_%d' % (c, tb)])

        def dump_tm(src, c0, ncol, keys, dcol):
            dst = AR.get([ncol], F32)
            for tt in range(16):
                CP(dst, src[:, tt, c0:c0 + ncol], [keys[tt]], ['dst'])
                DMA('sp', dbg_d[tt * 128:(tt + 1) * 128, dcol:dcol + ncol], dst, ['dst'], ['dbg'])

        def group_ln_core(src, sqv, nb, rk, wk_sq, eps_ap):
            s1 = AR.get([nb], F32)
            s2 = AR.get([nb], F32)
            s3 = AR.get([nb], F32)
            TT(sqv, src, src, ALU.mult, rk, [wk_sq])
            RED(s1, src, ALU.add, rk, ['gs1'])
            RED(s2, sqv, ALU.add, [wk_sq], ['gs2'])
            TS(s1, s1, 1.0 / 64, None, ALU.mult, None, ['gs1'], ['gs1'])
            TT(s3, s1, s1, ALU.mult, ['gs1'], ['gs3'])
            STT(s2, s2, 1.0 / 64, s3, ALU.mult, ALU.subtract, ['gs2', 'gs3'], ['gs2'])
            TS(s2, s2, 0.0, None, ALU.max, None, ['gs2'], ['gs2'])
            ACT(s2, s2, AF.Sqrt, ['gs2', 'epsA'], ['gs2'], bias=eps_ap)
            RCP(s2, s2, ['gs2'], ['gs2'])
            TT(sqv, src, s1.unsqueeze(2).to_broadcast([128, nb, 64]), ALU.subtract, rk + ['gs1'], [wk_sq])
            TT(sqv, sqv, s2.unsqueeze(2).to_broadcast([128, nb, 64]), ALU.mult, [wk_sq, 'gs2'], [wk_sq])

        def mixer_B(l):
            hk = lambda k, tb: 'hT%d_%d' % (k, tb)
            S.barrier(relay=RELAY)
            AR.reset()
            qrT = AR.get([2, 2048], BF16)
            krT = AR.get([2, 2048], BF16)
            off_x = AR.off
            cosT = AR.get([2048], F32)
            sinT = AR.get([2048], F32)
            rt1 = [AR.get([512], F32) for _ in range(2)]
            rt2 = [AR.get([512], F32) for _ in range(2)]
            DMA('sp', cosT, Dr['cosT'], (), ['cosT'])
            DMA('sp', sinT, Dr['sinT'], (), ['sinT'])
            DMA('sp', lnbc[:, 2, :], Dr['b_gng'][l], (), ['lnbc'])
            DMA('sp', lnbc[:, 3, :], Dr['b_gnb'][l], (), ['lnbc'])
            n = 0
            for p in range(2):
                wv, wk = wload(kchunks(Dr['w_inBf'][l][:, p * 512:(p + 1) * 512]), [8, 512])
                for kind in range(2):
                    dst = qrT if kind == 0 else krT
                    for tb in range(4):
                        i = n % 2
                        b1, b2 = 2 * (n % 4), 2 * (n % 4) + 1
                        n += 1
                        for k in range(8):
                            MM(bank(b1), wv[:, k, (2 * kind) * 128:(2 * kind + 1) * 128], hT[:, k, tb * 512:(tb + 1) * 512],
                               k == 0, k == 7, [wk, hk(k, tb)], [BK[b1]])
                        for k in range(8):
                            MM(bank(b2), wv[:, k, (2 * kind + 1) * 128:(2 * kind + 2) * 128], hT[:, k, tb * 512:(tb + 1) * 512],
                               k == 0, k == 7, [wk, hk(k, tb)], [BK[b2]])
                        TT(rt1[i], bank(b1), cosT[:, tb * 512:(tb + 1) * 512], ALU.mult, [BK[b1], 'cosT'], ['rt1_%d' % i])
                        TT(rt2[i], bank(b2), sinT[:, tb * 512:(tb + 1) * 512], ALU.mult, [BK[b2], 'sinT'], ['rt2_%d' % i])
                        TT(dst[:, p, tb * 512:(tb + 1) * 512], rt1[i], rt2[i], ALU.add, ['rt1_%d' % i, 'rt2_%d' % i],
                           ['qk%d_%d' % (kind, p)])
            wvt, wkt = wload(kchunks(Dr['w_inBt'][l]), [8, 512])
            for p in range(2):
                S.barrier(relay=RELAY)
                AR.reset(off_x)
                vg = AR.get([16, 256], BF16)
                kvs = AR.get([16, 128], F32)
                s16 = AR.get([16, 128], BF16)
                oall = AR.get([16, 128], F32)
                kz = [AR.get([128], BF16) for _ in range(2)]
                qx = [AR.get([128], BF16) for _ in range(2)]
                sT = [AR.get([256], BF16) for _ in range(2)]
                mixT = AR.get([1, 2048], BF16)
                qkk = ['qk0_%d' % p, 'qk1_%d' % p]
                for tt in range(16):
                    b = tt % 4
                    for k in range(8):
                        MM(bank(b)[:, 0:256], hT[:, k, tt * 128:(tt + 1) * 128], wvt[:, k, p * 256:(p + 1) * 256],
                           k == 0, k == 7, [hk(k, tt // 4), wkt], [BK[b]])
                    CP(vg[:, tt, 0:128], bank(b)[:, 0:128], [BK[b]], ['vg%d' % tt], eng='dve')
                    ACT(vg[:, tt, 128:256], bank(b)[:, 128:256], AF.Silu, [BK[b]], ['vg%d' % tt])
                S.op('dve', lambda e: e.memset(kvs[:, 0, :], 0.0), (), ['kvs'])
                for c in range(15):
                    i = c % 2
                    bt = 4 + (c % 2)
                    bm = 6 + (c % 2)
                    pb = bank(bt).bitcast(BF16)
                    TR(pb[:, 0:128], krT[:, p, c * 128:(c + 1) * 128], ident_b[:], [qkk[1], 'ident_b'], [BK[bt]])
                    TT(kz[i].rearrange("p (h d) -> p h d", h=2), pb[:, 0:128].rearrange("p (h d) -> p h d", h=2),
                       zeta[:, 2 * p:2 * p + 2].unsqueeze(2).to_broadcast([128, 2, 64]), ALU.mult,
                       [BK[bt], 'zeta'], ['kz%d' % i])
                    MM(bank(bm)[:, 0:128], kz[i], vg[:, c, 0:128], True, True, ['kz%d' % i, 'vg%d' % c], [BK[bm]])
                    STT(kvs[:, c + 1, :], kvs[:, c, :], cdv[:, p:p + 1], bank(bm)[:, 0:128], ALU.mult, ALU.add,
                        ['kvs', 'cdv', BK[bm]], ['kvs'])
                CP(s16, kvs, ['kvs'], ['s16'], eng='dve')
                for c in range(16):
                    i = c % 2
                    bs0 = 2 * (c % 2)
                    bo0 = 4 + 2 * (c % 2)
                    cs = slice(c * 128, (c + 1) * 128)
                    if c > 0:
                        TT(qx[i], qrT[:, p, cs], xiT[:, p * 128:(p + 1) * 128], ALU.mult, [qkk[0], 'xiT'], ['qx%d' % i])
                    for hh in range(2):
                        ps_ = slice(hh * 64, (hh + 1) * 64)
                        MM(bank(bs0 + hh)[:, 0:128], krT[ps_, p, cs], qrT[ps_, p, cs], True, True,
                           qkk, [BK[bs0 + hh]])
                    TT(sT[i].rearrange("p (h l) -> p h l", h=2),
                       PSA[:, bs0 * 512:(bs0 + 2) * 512].rearrange("p (h x) -> p h x", h=2)[:, :, 0:128],
                       decayT[:, 2 * p * 128:(2 * p + 2) * 128].rearrange("p (h l) -> p h l", h=2), ALU.mult,
                       [BK[bs0], BK[bs0 + 1], 'decayT'], ['sT%d' % i])
                    for hh in range(2):
                        ps_ = slice(hh * 64, (hh + 1) * 64)
                        MM(bank(bo0 + hh)[:, 0:64], sT[i][:, hh * 128:(hh + 1) * 128], vg[:, c, hh * 64:(hh + 1) * 64],
                           True, c == 0, ['sT%d' % i, 'vg%d' % c], [BK[bo0 + hh]])
                        if c > 0:
                            MM(bank(bo0 + hh)[:, 0:64], qx[i][ps_, :], s16[ps_, c, hh * 64:(hh + 1) * 64],
                               False, True, ['qx%d' % i, 's16'], [BK[bo0 + hh]])
                    CP(oall[:, c, :].rearrange("p (h e) -> p h e", h=2),
                       PSB[:, (bo0 - 4) * 512:(bo0 - 2) * 512].rearrange("p (h x) -> p h x", h=2)[:, :, 0:64],
                       [BK[bo0], BK[bo0 + 1]], ['oall'], eng='act')
                o3 = oall.rearrange("p c (h e) -> p (c h) e", h=2)
                sq3 = kvs.rearrange("p c (h e) -> p (c h) e", h=2)
                gam_b = lnbc[:, 2, p * 128:(p + 1) * 128].rearrange("p (h e) -> p h e", h=2).unsqueeze(1).to_broadcast([128, 16, 2, 64])
                bet_b = lnbc[:, 3, p * 128:(p + 1) * 128].rearrange("p (h e) -> p h e", h=2).unsqueeze(1).to_broadcast([128, 16, 2, 64])
                sq4 = kvs.rearrange("p c (h e) -> p c h e", h=2)
                group_ln_core(o3, sq3, 32, ['oall'], 'kvs', epsA[:, 0:1])
                TT(sq4, sq4, gam_b, ALU.mult, ['kvs', 'lnbc'], ['kvs'])
                TT(sq4, sq4, bet_b, ALU.add, ['kvs', 'lnbc'], ['kvs'])
                vkeys = ['vg%d' % tt for tt in range(16)]
                TT(vg[:, :, 0:128], kvs, vg[:, :, 128:256], ALU.mult, ['kvs'] + vkeys, vkeys)
                if dbg == 'mixB%d_%d' % (l, p):
                    dump_tm(vg, 0, 128, vkeys, p * 128)
                out_proj(l, vg, vkeys, 1, 256 + p * 128, mixT)

        def mixer_C(l):
            hk = lambda k, tb: 'hT%d_%d' % (k, tb)
            DMA('pool', w1s[:].rearrange("p (a b) -> p a b", a=4), Dr['w1s'][l].rearrange("p (a b) -> p a b", a=4), (), ['w1s'])
            DMA('pool', w2k[:], Dr['w2k'][l], (), ['w2k'])
            DMA('pool', w2v[:], Dr['w2v'][l], (), ['w2v'])
            DMA('sp', posT[:], Dr['posT'][l], (), ['posT'])
            for g in range(2):
                S.barrier(relay=RELAY)
                AR.reset()
                qz = AR.get([4, 2048], BF16)
                KsT = AR.get([2048], BF16)
                KwT = AR.get([2048], BF16)
                Vaug = AR.get([16, 2, 65], BF16)
                gates = AR.get([16, 12], F32)
                mixC = AR.get([16, 256], BF16)
                kcmpT = AR.get([127], BF16)
                vcaug = AR.get([97], BF16)
                hidT = AR.get([2, 127], BF16)
                off_r = AR.off
                kvcT = AR.get([2048], BF16)
                kcp = AR.get([32, 127], BF16)
                wv, wk = wload(kchunks(Dr['w_inCf'][l][:, g * 640:g * 640 + 512]), [8, 512])
                wv2, wk2 = wload(kchunks(Dr['w_inCf'][l][:, g * 640 + 512:g * 640 + 640]), [8, 128])
                S.op('dve', lambda e: e.memset(qz, 0.0), (), ['qT'])
                dsts = [(None, 'qT', 0.125), (None, 'qT', 0.125), (KsT, 'KsT', 1.0), (KwT, 'KwT', 1.0),
                        (kvcT, 'kvcT', 1.0)]
                n = 0
                for bi_, (dst, dk, scl) in enumerate(dsts):
                    for tb in range(4):
                        b = n % 4
                        n += 1
                        for k in range(8):
                            lw = wv[:, k, bi_ * 128:(bi_ + 1) * 128] if bi_ < 4 else wv2[:, k, :]
                            MM(bank(b), lw, hT[:, k, tb * 512:(tb + 1) * 512], k == 0, k == 7,
                               [wk if bi_ < 4 else wk2, hk(k, tb)], [BK[b]])
                        if dst is None:
                            ACT(qz[0:64, 2 * bi_, tb * 512:(tb + 1) * 512], bank(b)[0:64, :], AF.Identity, [BK[b]], [dk], scale=scl)
                            TS(qz[64:128, 2 * bi_ + 1, tb * 512:(tb + 1) * 512], bank(b)[64:128, :], scl, None, ALU.mult, None, [BK[b]], [dk])
                        elif n % 2:
                            ACT(dst[:, tb * 512:(tb + 1) * 512], bank(b), AF.Identity, [BK[b]], [dk], scale=scl)
                        else:
                            TS(dst[:, tb * 512:(tb + 1) * 512], bank(b), scl, None, ALU.mult, None, [BK[b]], [dk])
                wv3, wk3 = wload(kchunks(Dr['w_inCt'][l][:, g * 140:(g + 1) * 140]), [8, 140])
                S.op('dve', lambda e: e.memset(Vaug[:, :, :, 64:65], 1.0), (), ['Vaug'])
                for tt in range(16):
                    b = 4 + tt % 4
                    for k in range(8):
                        MM(bank(b)[:, 0:140], hT[:, k, tt * 128:(tt + 1) * 128], wv3[:, k, :], k == 0, k == 7,
                           [hk(k, tt // 4), wk3], [BK[b]])
                    CP(Vaug[:, tt, :, 0:64], bank(b)[:, 0:128].rearrange("p (s d) -> p s d", s=2), [BK[b]], ['Vaug'], eng='dve')
                    ACT(gates[:, tt, :], bank(b)[:, 128:140], AF.Sigmoid, [BK[b]], ['gates'])
                kv_t = kvcT.tensor
                win = bass.AP(kv_t, kvcT.offset, [list(kvcT.ap[0]), [1, 32], [16, 127]])
                TT(kcp, win, posT[:, 0:32].unsqueeze(2).to_broadcast([128, 32, 127]), ALU.add, ['kvcT', 'posT'], ['kcp'])
                for kv in range(2):
                    ps_ = slice(kv * 64, (kv + 1) * 64)
                    for ll in range(32):
                        MM(bank(kv)[0:64, 0:127], w1s[ps_, ll * 64:(ll + 1) * 64], kcp[ps_, ll, :],
                           ll == 0, ll == 31, ['w1s', 'kcp'], [BK[kv]])
                    ACT(hidT[0:64, kv, :], bank(kv)[0:64, 0:127], AF.Gelu_apprx_tanh, [BK[kv]], ['hidT'])
                MM(bank(2)[:, 0:127], w2k[:], hidT[0:64, 0, :], True, True, ['w2k', 'hidT'], ['B2'])
                CP(kcmpT, bank(2)[:, 0:127], ['B2'], ['kcmpT'], eng='dve')
                MM(bank(3)[0:127, 0:64], hidT[0:64, 1, :], w2v[:], True, True, ['w2v', 'hidT'], ['B3'])
                S.op('dve', lambda e: e.memset(vcaug[:, 64:65], 1.0), (), ['vcaug'])
                CP(vcaug[0:127, 0:64], bank(3)[0:127, 0:64], ['B3'], ['vcaug'], eng='dve')
                CP(vcaug[:, 65:97], ovl[:, 0:32], ['ovl'], ['vcaug'], eng='dve')
                S.barrier(relay=RELAY)
                AR.reset(off_r)
                PT = [AR.get([512], BF16) for _ in range(3)]
                MbT = [AR.get([128], BF16) for _ in range(2)]
                for j_ in range(2):
                    S.op('dve', (lambda t_: (lambda e: e.memset(t_, 0.0)))(MbT[j_]), (), ['MbT%d' % j_])
                Mb = [AR.get([32], BF16) for _ in range(2)]
                ocg = [AR.get([4, 64], F32) for _ in range(2)]
                t1 = [AR.get([4, 64], F32) for _ in range(2)]
                t2 = [AR.get([4, 64], F32) for _ in range(2)]
                sm = [AR.get([96], F32) for _ in range(2)]
                impt = [AR.get([4, 32], F32) for _ in range(2)]
                pn = [0]

                def qslice(r, qt):
                    return qz[:, r, qt * 128:(qt + 1) * 128]

                def attend(qt, kts, KT, vsel, bS, bO, extra_fn):
                    for ki, kt in enumerate(kts):
                        bs_ = bS[ki % len(bS)]
                        extras = extra_fn(kt)
                        for r in range(4):
                            MM(bank(bs_)[:, r * 128:(r + 1) * 128], KT[:, kt * 128:(kt + 1) * 128], qslice(r, qt),
                               r == 0, (r == 3 and not extras), ['KsT', 'KwT', 'qT'], [BK[bs_]])
                        v4 = bank(bs_).rearrange("p (r t) -> p r t", r=4)
                        for ei, (lh, rh, ks) in enumerate(extras):
                            MM(v4, lh, rh, False, ei == len(extras) - 1, ks, [BK[bs_]])
                        i = pn[0] % 3
                        pn[0] += 1
                        ACT(PT[i], bank(bs_), AF.Exp, [BK[bs_]], ['PT%d' % i])
                        for r in range(4):
                            MM(bank(bO)[:, r * 65:(r + 1) * 65], PT[i][:, r * 128:(r + 1) * 128], Vaug[:, kt, vsel, :],
                               ki == 0 and r == 0, ki == len(kts) - 1 and r == 3, ['PT%d' % i, 'Vaug'], [BK[bO]])

                for qt in range(16):
                    j = qt % 2
                    smj = sm[j]
                    sk = 'sm%d' % j
                    qs = slice(qt * 128, (qt + 1) * 128)
                    for r in range(4):
                        MM(bank(0)[0:127, r * 128:(r + 1) * 128], kcmpT[:, :], qslice(r, qt), r == 0, False,
                           ['kcmpT', 'qT'], ['B0'])
                    MM(bank(0)[0:127, :].rearrange("p (r t) -> p r t", r=4), ident_b[0:127, 0:127],
                       cmpb[0:127, qs].unsqueeze(1).to_broadcast([127, 4, 128]), False, True, ['ident_b', 'cmpb'], ['B0'])
                    i = pn[0] % 3
                    pn[0] += 1
                    ACT(PT[i][0:127, :], bank(0)[0:127, :], AF.Exp, ['B0'], ['PT%d' % i])
                    for r in range(4):
                        MM(bank(1)[:, r * 97:(r + 1) * 97], PT[i][0:127, r * 128:(r + 1) * 128], vcaug[0:127, :],
                           r == 0, r == 3, ['PT%d' % i, 'vcaug'], ['B1'])
                    O = bank(1)[:, 0:388].rearrange("p (r c) -> p r c", r=4)
                    cb4 = causalb[:].unsqueeze(1).to_broadcast([128, 4, 128])
                    wb4 = winb[:].unsqueeze(1).to_broadcast([128, 4, 128])

                    def wmask(kt, qt=qt, cb4=cb4, wb4=wb4):
                        if kt == qt:
                            return [(ident_b[:], cb4, ['ident_b', 'causalb'])]
                        if kt == qt - 4:
                            return [(ident_b[:], wb4, ['ident_b', 'winb'])]
                        return []
                    attend(qt, list(range(max(0, qt - 4), qt + 1)), KwT, 1, [2, 3], 5, wmask)
                    TS(smj[:, 0:4], O[:, :, 64], 1e-30, None, ALU.max, None, ['B1'], [sk])
                    RCP(smj[:, 4:8], smj[:, 0:4], [sk], [sk])
                    TT(impt[j], O[:, :, 65:97], smj[:, 4:8].unsqueeze(2).to_broadcast([128, 4, 32]), ALU.mult, ['B1', sk], ['impt%d' % j])
                    RED(smj[:, 32:64], impt[j].rearrange("p r c -> p c r"), ALU.add, ['impt%d' % j], [sk])
                    TT(smj[:, 32:64], smj[:, 32:64], fbias[:, qt * 32:(qt + 1) * 32], ALU.add, [sk, 'fbias'], [sk])
                    S.op('dve', (lambda o_, i_: (lambda e: e.max(o_, i_)))(smj[:, 64:72], smj[:, 32:64]), [sk], [sk])
                    TS(Mb[j], smj[:, 32:64], smj[:, 71:72], -1.0, ALU.is_ge, ALU.add, [sk], ['Mb%d' % j])
                    pbt = bank(0).bitcast(BF16)
                    TR(pbt[0:32, 0:128], Mb[j], ident_b[:], ['Mb%d' % j, 'ident_b'], ['B0'])
                    CP(MbT[j][0:32, :], pbt[0:32, 0:128], ['B0'], ['MbT%d' % j], eng='dve')
                    gv = gates[:, qt, :].rearrange("p (r c) -> p r c", r=4)
                    TT(smj[:, 8:12], smj[:, 4:8], gv[:, :, 0], ALU.mult, [sk, 'gates'], [sk])
                    TT(ocg[j], O[:, :, 0:64], smj[:, 8:12].unsqueeze(2).to_broadcast([128, 4, 64]), ALU.mult, ['B1', sk], ['ocg%d' % j])

                    mb4 = MbT[j][:, :].unsqueeze(1).to_broadcast([128, 4, 128])

                    def smask(kt, qt=qt, j=j, cb4=cb4, mb4=mb4):
                        ex = [(expand[:, kt * 128:(kt + 1) * 128], mb4, ['expand', 'MbT%d' % j])]
                        if kt == qt:
                            ex.append((ident_b[:], cb4, ['ident_b', 'causalb']))
                        return ex
                    attend(qt, list(range(0, qt + 1)), KsT, 0, [6, 7], 4, smask)
                    Os = bank(4)[:, 0:260].rearrange("p (r c) -> p r c", r=4)
                    Ow = bank(5)[:, 0:260].rearrange("p (r c) -> p r c", r=4)
                    RCP(smj[:, 12:16], Os[:, :, 64], ['B4'], [sk])
                    TT(smj[:, 12:16], smj[:, 12:16], gv[:, :, 1], ALU.mult, [sk, 'gates'], [sk])
                    RCP(smj[:, 16:20], Ow[:, :, 64], ['B5'], [sk])
                    TT(smj[:, 16:20], smj[:, 16:20], gv[:, :, 2], ALU.mult, [sk, 'gates'], [sk])
                    TT(t1[j], Os[:, :, 0:64], smj[:, 12:16].unsqueeze(2).to_broadcast([128, 4, 64]), ALU.mult, ['B4', sk], ['t1_%d' % j])
                    TT(t2[j], Ow[:, :, 0:64], smj[:, 16:20].unsqueeze(2).to_broadcast([128, 4, 64]), ALU.mult, ['B5', sk], ['t2_%d' % j])
                    TT(t1[j], t1[j], ocg[j], ALU.add, ['t1_%d' % j, 'ocg%d' % j], ['t1_%d' % j])
                    TT(mixC[:, qt, :].rearrange("p (r d) -> p r d", r=4), t1[j], t2[j], ALU.add, ['t1_%d' % j, 't2_%d' % j], ['mixC%d' % qt])
                mkeys = ['mixC%d' % tt for tt in range(16)]
                if dbg == 'mixC%d_%d' % (l, g):
                    dump_tm(mixC, 0, 256, mkeys, g * 256)
                S.barrier(relay=RELAY)
                AR.reset(off_r)
                mixT = AR.get([2, 2048], BF16)
                out_proj(l, mixC, mkeys, 2, 512 + g * 256, mixT)

        for l in range(n_layers):
            hk = lambda k, tb: 'hT%d_%d' % (k, tb)
            if 'A' in mixers:
                S.barrier(relay=RELAY)
                AR.reset()
                zag = AR.get([16, 512], BF16)
                off_sqv = AR.off
                sqv = AR.get([16, 256], F32)
                st1 = AR.get([64], F32)
                st2 = AR.get([64], F32)
                st3 = AR.get([64], F32)
                mixA = AR.get([16, 256], BF16)
                atmp = [AR.get([256], F32) for _ in range(2)]
                wstage = AR.get([4, 128], F32)
                DMA('sp', wstage, Dr['WsT'][l].rearrange("p (g t) -> p g t", g=4), (), ['wstage'])
                TT(WsT[:].rearrange("p (g t) -> p g t", g=4), wstage,
                   causal01[:].unsqueeze(1).to_broadcast([128, 4, 128]), ALU.mult, ['wstage', 'causal01'], ['WsT'])
                DMA('sp', bsT[:], Dr['bsT'][l], (), ['bsT'])
                DMA('sp', lnbc[:, 0, :], Dr['a_lng'][l], (), ['lnbcA'])
                DMA('sp', lnbc[:, 1, :], Dr['a_lnb'][l], (), ['lnbcA'])
                wv, wk = wload(kchunks(Dr['w_inA'][l]), [8, 512])
                for tt in range(16):
                    b = tt % 4
                    for k in range(8):
                        MM(bank(b), hT[:, k, tt * 128:(tt + 1) * 128], wv[:, k, :], k == 0, k == 7,
                           [hk(k, tt // 4), wk], [BK[b]])
                    ACT(zag[:, tt, :], bank(b), AF.Gelu_apprx_tanh, [BK[b]], ['zag'])
                v4 = zag[:, :, 256:512].rearrange("p t (g d) -> p t g d", g=4)
                TT(sqv, zag[:, :, 256:512], zag[:, :, 256:512], ALU.mult, ['zag'], ['sqv'])
                RED(st1.rearrange("p (t g) -> p t g", t=16), v4, ALU.add, ['zag'], ['st1'])
                RED(st2.rearrange("p (t g) -> p t g", t=16), sqv.rearrange("p t (g d) -> p t g d", g=4), ALU.add, ['sqv'], ['st2'])
                TS(st1, st1, 1.0 / 64, None, ALU.mult, None, ['st1'], ['st1'])
                TT(st3, st1, st1, ALU.mult, ['st1'], ['st3'])
                STT(st2, st2, 1.0 / 64, st3, ALU.mult, ALU.subtract, ['st2', 'st3'], ['st2'])
                TS(st2, st2, 0.0, None, ALU.max, None, ['st2'], ['st2'])
                ACT(st2, st2, AF.Sqrt, ['st2', 'epsA'], ['st2'], bias=epsA[:, 0:1])
                RCP(st2, st2, ['st2'], ['st2'])
                mean_b = st1.rearrange("p (t g) -> p t g", t=16).unsqueeze(3).to_broadcast([128, 16, 4, 64])
                rstd_b = st2.rearrange("p (t g) -> p t g", t=16).unsqueeze(3).to_broadcast([128, 16, 4, 64])
                sq4 = sqv.rearrange("p t (g d) -> p t g d", g=4)
                TT(sq4, v4, mean_b, ALU.subtract, ['zag', 'st1'], ['sqv'])
                TT(sq4, sq4, rstd_b, ALU.mult, ['sqv', 'st2'], ['sqv'])
                gam_b = lnbc[:, 0, :].unsqueeze(1).to_broadcast([128, 16, 256])
                bet_b = lnbc[:, 1, :].unsqueeze(1).to_broadcast([128, 16, 256])
                TT(sqv, sqv, gam_b, ALU.mult, ['sqv', 'lnbcA'], ['sqv'])
                TT(zag[:, :, 256:512], sqv, bet_b, ALU.add, ['sqv', 'lnbcA'], ['zag'])
                bs_b = bsT[:, 0:4].unsqueeze(2).to_broadcast([128, 4, 64])
                for tt in range(16):
                    b = 4 + tt % 4
                    for g in range(4):
                        MM(bank(b)[:, g * 64:(g + 1) * 64], WsT[:, g * 128:(g + 1) * 128],
                           zag[:, tt, 256 + g * 64:256 + (g + 1) * 64], g == 0, g == 3, ['WsT', 'zag'], [BK[b]])
                    at = atmp[tt % 2]
                    ak = 'atmp%d' % (tt % 2)
                    TT(at.rearrange("p (g d) -> p g d", g=4), bank(b)[:, 0:256].rearrange("p (g d) -> p g d", g=4),
                       bs_b, ALU.add, [BK[b], 'bsT'], [ak])
                    TT(mixA[:, tt, :], at, zag[:, tt, 0:256], ALU.mult, [ak, 'zag'], ['mixA%d' % tt])
                if dbg == 'mixA%d' % l:
                    dst = AR.get([256], F32)
                    for tt in range(16):
                        CP(dst, mixA[:, tt, :], ['mixA%d' % tt], ['dst'])
                        DMA('sp', dbg_d[tt * 128:(tt + 1) * 128, 0:256], dst, ['dst'], ['dbg'])
                AR.reset(off_sqv)
                mixT = AR.get([2, 2048], BF16)
                out_proj(l, mixA, ['mixA%d' % tt for tt in range(16)], 2, 0, mixT, ['sqv'])

            if 'B' in mixers:
                mixer_B(l)
            if 'C' in mixers:
                mixer_C(l)

            S.barrier(relay=RELAY)
            layer_norm(l, 1, last=False)
            if dbg == 'ln1_%d' % l:
                break

            if do_ffn:
                S.barrier(relay=RELAY)
                AR.reset()
                DMA('sp', cwT[:], Dr['cwT'][l], (), ['cwT'])
                DMA('sp', cbT[:], Dr['cbT'][l], (), ['cbT'])
                gT = AR.get([max(FF_SPLIT), 2048], BF16)
                ctmp = [[AR.get([1024], F32) for _ in range(2)] for _ in range(2)]
                sgt = [AR.get([1024], BF16) for _ in range(2)]
                j0 = 0
                PS4 = [PSA, PSB]
                P4K = [BK[0:4], BK[4:8]]
                for part_n in FF_SPLIT:
                    wgrp = {}
                    for jl in range(part_n):
                        j = j0 + jl
                        if jl % 4 == 0:
                            ng = min(4, part_n - jl)
                            for part in range(2):
                                jj0 = part * 22 + j
                                wgrp[part] = wload(kchunks(Dr['w_up'][l][:, jj0 * 128:(jj0 + ng) * 128]), [8, ng * 128])
                        for part in range(2):
                            jj = part * 22 + j
                            wvf, wk = wgrp[part]
                            wv = wvf[:, :, (jl % 4) * 128:(jl % 4 + 1) * 128]
                            ps = PS4[part]
                            for tb in range(4):
                                for k in range(8):
                                    MM(ps[:, tb * 512:(tb + 1) * 512], wv[:, k, :], hT[:, k, tb * 512:(tb + 1) * 512],
                                       k == 0, k == 7, [wk, hk(k, tb)], [P4K[part][tb]])
                            for hf in range(2):
                                ct = ctmp[part][hf]
                                ck = 'ctmp%d_%d' % (part, hf)
                                o = hf * 1024
                                pk = P4K[part][2 * hf:2 * hf + 2]
                                pkp = P4K[part][max(0, 2 * hf - 1):2 * hf + 2]
                                ACT(ct, ps[:, o:o + 1024], AF.Identity, pk + ['cwT', 'cbT'], [ck],
                                    scale=cwT[:, jj * 3 + 2:jj * 3 + 3], bias=cbT[:, jj:jj + 1])
                                if hf == 0:
                                    STT(ct[:, 1:1024], ps[:, 0:1023], cwT[:, jj * 3 + 1:jj * 3 + 2], ct[:, 1:1024],
                                        ALU.mult, ALU.add, pk + ['cwT', ck], [ck])
                                    STT(ct[:, 2:1024], ps[:, 0:1022], cwT[:, jj * 3:jj * 3 + 1], ct[:, 2:1024],
                                        ALU.mult, ALU.add, pk + ['cwT', ck], [ck])
                                else:
                                    STT(ct, ps[:, o - 1:o + 1023], cwT[:, jj * 3 + 1:jj * 3 + 2], ct,
                                        ALU.mult, ALU.add, pkp + ['cwT', ck], [ck])
                                    STT(ct, ps[:, o - 2:o + 1022], cwT[:, jj * 3:jj * 3 + 1], ct,
                                        ALU.mult, ALU.add, pkp + ['cwT', ck], [ck])
                                if part == 0:
                                    ACT(sgt[hf], ct, AF.Silu, [ck], ['sgt%d' % hf])
                                else:
                                    TT(gT[:, jl, o:o + 1024], ct, sgt[hf], ALU.mult, [ck, 'sgt%d' % hf], ['gT%d' % jl])
                    nsl = (part_n + 3) // 4
                    wvs = []
                    for s in range(nsl):
                        r0 = (j0 + 4 * s) * 128
                        nr = min(4, part_n - 4 * s)
                        wvs.append(wload(kchunks(Dr['w_down'][l][r0:r0 + nr * 128, :]), [nr, 1024]))
                    bi2 = 0
                    for fb in range(8):
                        for tb in range(4):
                            b = bi2 % 8
                            bi2 += 1
                            for jl in range(part_n):
                                wv, wk = wvs[jl // 4]
                                MM(bank(b), wv[:, jl % 4, fb * 128:(fb + 1) * 128], gT[:, jl, tb * 512:(tb + 1) * 512],
                                   jl == 0, jl == part_n - 1, [wk, 'gT%d' % jl], [BK[b]])
                            xs = xT[:, fb, tb * 512:(tb + 1) * 512]
                            STT(xs, bank(b), drv[l][:, 24 + fb:25 + fb], xs, ALU.mult, ALU.add,
                                [BK[b], 'drv%d' % l, 'xT%d_%d' % (fb, tb)], ['xT%d_%d' % (fb, tb)])
                    j0 += part_n
                S.barrier(relay=RELAY)
            layer_norm(l, 2, last=(l == n_layers - 1))

        S.barrier(relay=RELAY)
        AR.reset()
        ost = [AR.get([1024], F32) for _ in range(4)]
        bi = 0
        for tt in range(16):
            o = ost[tt % 4]
            ok = 'ost%d' % (tt % 4)
            for cg in range(2):
                b = bi % 8
                bi += 1
                for q in range(4):
                    c = cg * 4 + q
                    TR(bank(b)[:, q * 128:(q + 1) * 128], xT[:, c, tt * 128:(tt + 1) * 128], ident_f[:],
                       ['xT%d_%d' % (c, tt // 4), 'ident_f'], [BK[b]])
                CP(o[:, cg * 512:(cg + 1) * 512], bank(b), [BK[b]], [ok], eng=EV())
            DMA('sp', out_d[tt * 128:(tt + 1) * 128, :], o, [ok], ['out'])
        S.finish()
        S.replay()
    return nc


_CACHE = {}


def kernel(**inputs):
    inp = {k: np.asarray(v) for k, v in inputs.items()}
    if 'nc' not in _CACHE:
        _CACHE['nc'] = build()
    nc = _CACHE['nc']
    w = _prep_weights(inp)
    tb = _tables()
    in_maps = []
    for b in range(8):
        m = dict(w)
        m.update(tb)
        m['x'] = np.ascontiguousarray(inp['x'][b], dtype=np.float32)
        m['cT'] = _pad64(inp['c'][b].reshape(8, 128).T)
        in_maps.append(m)
    res = run_bass_kernel_spmd(nc, in_maps, core_ids=list(range(8)))
    out = np.stack([np.asarray(r['out'], dtype=np.float32) for r in res.results], 0)
    return out
```

```python
import math
from contextlib import ExitStack
import numpy as np
import concourse.bass as bass
import concourse.mybir as mybir
from concourse.bass_utils import run_bass_kernel_spmd

F32 = mybir.dt.float32
BF16 = mybir.dt.bfloat16
AF = mybir.ActivationFunctionType
ALU = mybir.AluOpType
AX = mybir.AxisListType

ENGS = ['pe', 'dve', 'act', 'pool', 'sp']
NRING = 8
DEPTH = 2
SEQ = 2048
DM = 1024
ALPHA = (2 * DEPTH) ** 0.25
LN_EPS = 1e-5
NEGB = -30000.0
FF_SPLIT = [6, 6, 5, 5]


class Sched:
    def __init__(self, nc, stack):
        self.nc = nc
        self.prog = {e: [] for e in ENGS}
        self.cnt = {e: 0 for e in ENGS}
        self.seen = {e: {} for e in ENGS}
        self.lastw = {}
        self.readers = {}
        self.sems = {}
        self.semval = {}
        self.relay_fn = None
        for e in ENGS:
            self.sems[e] = stack.enter_context(nc.semaphore("s_" + e))
        self.dma_n = {}
        for q in ['sp', 'act', 'pool']:
            self.dma_n[q] = 0
            for j in range(NRING):
                nm = "d_%s%d" % (q, j)
                self.sems[nm] = stack.enter_context(nc.semaphore(nm))

    def _deps(self, eng, reads, writes, is_dma=False):
        deps = []
        for k in reads:
            t = self.lastw.get(k)
            if t is not None:
                deps.append((t, 'raw'))
        for k in writes:
            t = self.lastw.get(k)
            if t is not None:
                deps.append((t, 'waw'))
            for s, v in self.readers.get(k, {}).items():
                deps.append(((s, v), 'war'))
        need = {}
        for (s, v), kind in deps:
            if s == eng and not is_dma:
                if kind != 'raw' or eng == 'pe':
                    continue
            if self.seen[eng].get(s, 0) >= v:
                continue
            if need.get(s, 0) < v:
                need[s] = v
        return need

    def _emit_waits(self, eng, need):
        if eng in ('sp', 'pool') and 'pe' in need and self.relay_fn is not None:
            need = dict(need)
            v = need.pop('pe')
            self.seen[eng]['pe'] = v
            R = 'dve'
            if self.seen[R].get('pe', 0) < v:
                self.prog[R].append(('wait', 'pe', v))
                self.seen[R]['pe'] = v
            lr = getattr(self, 'last_relay', 0)
            if lr and self.seen[R].get(R, 0) < lr:
                self.prog[R].append(('wait', R, lr))
                self.seen[R][R] = lr
            self.cnt[R] += 1
            self.last_relay = self.cnt[R]
            self.semval[R] = self.cnt[R]
            self.prog[R].append(('op', self.relay_fn, R, 1))
            if self.seen[eng].get(R, 0) < self.cnt[R]:
                need[R] = max(need.get(R, 0), self.cnt[R])
        for s, v in need.items():
            self.prog[eng].append(('wait', s, v))
            self.seen[eng][s] = v

    def _commit(self, tok, reads, writes):
        for k in writes:
            self.lastw[k] = tok
            self.readers[k] = {}
        for k in reads:
            d = self.readers.setdefault(k, {})
            if d.get(tok[0], 0) < tok[1]:
                d[tok[0]] = tok[1]

    def relay_readers(self, keys):
        if self.relay_fn is None:
            return
        v = 0
        for k in keys:
            d = self.readers.get(k)
            if d and 'pe' in d:
                v = max(v, d['pe'])
        if v == 0:
            return
        R = 'dve'
        if self.seen[R].get('pe', 0) < v:
            self.prog[R].append(('wait', 'pe', v))
            self.seen[R]['pe'] = v
        lr = getattr(self, 'last_relay', 0)
        if lr and self.seen[R].get(R, 0) < lr:
            self.prog[R].append(('wait', R, lr))
            self.seen[R][R] = lr
        self.cnt[R] += 1
        self.semval[R] = self.cnt[R]
        self.last_relay = self.cnt[R]
        self.prog[R].append(('op', self.relay_fn, R, 1))
        for k in keys:
            d = self.readers.get(k)
            if d and 'pe' in d:
                d.pop('pe')
                d[R] = max(d.get(R, 0), self.cnt[R])

    def op(self, eng, fn, reads=(), writes=()):
        need = self._deps(eng, reads, writes)
        self._emit_waits(eng, need)
        self.cnt[eng] += 1
        tok = (eng, self.cnt[eng])
        self.semval[eng] = self.cnt[eng]
        self.prog[eng].append(('op', fn, eng, 1))
        self._commit(tok, reads, writes)
        return tok

    def dma_multi(self, q, pairs, reads=(), writes=()):
        i = self.dma_n[q]
        self.dma_n[q] += 1
        slot = "d_%s%d" % (q, i % NRING)
        prev = self.semval.get(slot, 0)
        val = prev + 16 * len(pairs)
        need = self._deps(q, reads, writes, is_dma=True)
        if prev > 0 and self.seen[q].get(slot, 0) < prev:
            need[slot] = max(need.get(slot, 0), prev)
        self._emit_waits(q, need)
        for out, in_ in pairs:
            self.prog[q].append(('op', (lambda o_, i_: (lambda e: e.dma_start(out=o_, in_=i_)))(out, in_), slot, 16))
        self.semval[slot] = val
        tok = (slot, val)
        self._commit(tok, reads, writes)
        return tok

    def dma(self, q, out, in_, reads=(), writes=()):
        return self.dma_multi(q, [(out, in_)], reads, writes)

    def _wait_all(self, e):
        need = {}
        for s_, v in self.semval.items():
            if s_ == e:
                continue
            if self.seen[e].get(s_, 0) < v:
                need[s_] = v
        self._emit_waits(e, need)

    def barrier(self, engs=('pe', 'dve', 'act', 'sp'), relay=None):
        if relay is None or 'sp' not in engs:
            for e in engs:
                self._wait_all(e)
            return
        snap = dict(self.semval)
        self._wait_all('sp')
        tok = self.dma('sp', relay[0], relay[1], (), ['__bar'])
        for e in engs:
            if e == 'sp':
                continue
            self._emit_waits(e, {tok[0]: tok[1]} if self.seen[e].get(tok[0], 0) < tok[1] else {})
            for s_, v in snap.items():
                if s_ != e and self.seen[e].get(s_, 0) < v:
                    self.seen[e][s_] = v

    def finish(self):
        self.barrier(engs=('sp',))

    def replay(self):
        nc = self.nc
        sems = self.sems
        prog = self.prog

        def run(e, name):
            for it in prog[name]:
                if it[0] == 'wait':
                    e.wait_ge(sems[it[1]], it[2])
                else:
                    it[1](e).then_inc(sems[it[2]], it[3])

        with nc.Block() as block:
            @block.tensor
            def _(e):
                run(e, 'pe')

            @block.vector
            def _(e):
                run(e, 'dve')

            @block.scalar
            def _(e):
                run(e, 'act')

            @block.gpsimd
            def _(e):
                run(e, 'pool')

            @block.sync
            def _(e):
                run(e, 'sp')


def _pad64(a):
    a = np.asarray(a, dtype=np.float32)
    out = np.zeros(a.shape[:-1] + (64,), np.float32)
    out[..., :a.shape[-1]] = a
    return out


def _tables():
    t = {}
    half = 32
    inv = np.power(np.float32(10000.0), -np.arange(half, dtype=np.float32) / np.float32(half)).astype(np.float32)
    pos = np.arange(SEQ, dtype=np.float32)
    ang = pos[:, None] * inv[None, :]
    cos = np.cos(ang).astype(np.float32).T
    sin = np.sin(ang).astype(np.float32).T
    cosT = np.concatenate([cos, cos, cos, cos], 0)
    sinT = np.concatenate([-sin, sin, -sin, sin], 0)
    t['cosT'] = np.ascontiguousarray(cosT)
    t['sinT'] = np.ascontiguousarray(sinT)
    H = 4
    L = 128
    lg = np.log1p(-np.exp2(-5.0 - np.arange(H, dtype=np.float32))).astype(np.float32)
    idx = np.arange(L, dtype=np.float32)
    diff = idx[:, None] - idx[None, :]
    dec = np.where(diff >= 0, np.exp(lg[:, None, None] * np.maximum(diff, 0.0)), 0.0).astype(np.float32)
    t['decayT'] = np.ascontiguousarray(np.transpose(dec, (2, 0, 1)) * np.float32(0.125)).reshape(128, 512)
    xi = np.exp(lg[:, None] * (idx + 1.0)).astype(np.float32)
    zeta = np.exp(lg[:, None] * (L - 1.0 - idx)).astype(np.float32)
    xiT = np.zeros((128, 2, 128), np.float32)
    for p in range(2):
        for hh in range(2):
            xiT[hh * 64:(hh + 1) * 64, p, :] = xi[2 * p + hh][None, :]
    t['xiT'] = xiT.reshape(128, 256)
    t['zeta'] = _pad64(zeta.T * np.float32(0.125))
    cd = np.exp(lg * L).astype(np.float32)
    cdv = np.zeros((128, 2), np.float32)
    for p in range(2):
        for hh in range(2):
            cdv[hh * 64:(hh + 1) * 64, p] = cd[2 * p + hh]
    t['cdv'] = _pad64(cdv)
    key = np.arange(SEQ)
    ex = np.zeros((128, SEQ), np.float32)
    ex[key // 64, key] = -NEGB
    t['expand'] = ex
    kk = np.arange(128)[:, None]
    tt = np.arange(128)[None, :]
    t['causalb'] = np.where(kk > tt, NEGB, 0.0).astype(np.float32)
    t['winb'] = np.where(kk <= tt, NEGB, 0.0).astype(np.float32)
    t['identf'] = np.eye(128, dtype=np.float32)
    t['causal01'] = np.where(tt >= kk, 1.0, 0.0).astype(np.float32)
    k127 = np.arange(128)[:, None]
    tpos = np.arange(SEQ)[None, :]
    t['cmpb'] = np.where(16 * k127 + 31 > tpos, NEGB, 0.0).astype(np.float32)
    fb = np.zeros((128, 16, 32), np.float32)
    for qt in range(16):
        tq = qt * 128 + np.arange(128)
        cur = tq // 64
        blk = np.arange(32)
        future = blk[None, :] > cur[:, None]
        forced = (blk[None, :] == 0) | (blk[None, :] == cur[:, None]) | (blk[None, :] == cur[:, None] - 1)
        fb[:, qt, :] = np.where(forced, 1e30, np.where(future, -1e30, 0.0))
    t['fbias'] = fb.reshape(128, 512)
    ov = np.zeros((128, 32), np.float32)
    c0 = np.arange(127)[:, None] * 16
    s0 = np.arange(32)[None, :] * 64
    ov[:127] = np.clip(np.minimum(c0 + 32, s0 + 64) - np.maximum(c0, s0), 0, None) / 32.0
    t['ovl'] = _pad64(ov)
    return t


TABLE_SHAPES = {'cosT': [128, 2048], 'sinT': [128, 2048], 'decayT': [128, 512], 'xiT': [128, 256],
                'zeta': [128, 64], 'cdv': [128, 64], 'expand': [128, 2048], 'causalb': [128, 128],
                'winb': [128, 128], 'causal01': [128, 128], 'identf': [128, 128], 'cmpb': [128, 2048], 'fbias': [128, 512], 'ovl': [128, 64]}


def _prep_weights(inp):
    w = {}
    f = lambda a: np.ascontiguousarray(a, dtype=np.float32)
    w_in = inp['w_in']
    L = DEPTH
    w['w_ada'] = f(inp['w_ada'])
    w['b_adaT'] = _pad64(inp['b_ada'].reshape(L, 48, 128).transpose(0, 2, 1))
    w['w_inA'] = f(w_in[:, :, 0:512])
    cols = []
    for p in range(2):
        for base in (512, 768):
            hd = np.arange(128)
            h = 2 * p + hd // 64
            d = hd % 64
            cols.append(base + h * 64 + d)
            cols.append(base + h * 64 + (d + 32) % 64)
    cols = np.concatenate(cols)
    w['w_inBf'] = f(w_in[:, :, cols])
    cols = []
    for p in range(2):
        cols.append(1024 + p * 128 + np.arange(128))
        cols.append(1280 + p * 128 + np.arange(128))
    w['w_inBt'] = f(w_in[:, :, np.concatenate(cols)])
    cols = []
    for g in range(2):
        cols.append(1536 + g * 256 + np.arange(256))
        ks = 2304 + g * 64 + np.arange(64)
        kw = 2560 + g * 64 + np.arange(64)
        cols += [ks, ks, kw, kw]
        cols.append(2048 + g * 64 + np.arange(64))
        cols.append(2176 + g * 64 + np.arange(64))
    w['w_inCf'] = f(w_in[:, :, np.concatenate(cols)])
    cols = []
    for g in range(2):
        cols.append(2432 + g * 64 + np.arange(64))
        cols.append(2688 + g * 64 + np.arange(64))
        cols.append(2816 + g * 12 + np.arange(12))
    w['w_inCt'] = f(w_in[:, :, np.concatenate(cols)])
    w['WsT'] = f(inp['a_ws'].transpose(0, 3, 1, 2).reshape(L, 128, 512))
    w['bsT'] = _pad64(inp['a_bs'].transpose(0, 2, 1))
    w['a_lng'] = f(np.broadcast_to(inp['a_ln_g'].reshape(L, 1, 256), (L, 128, 256)))
    w['a_lnb'] = f(np.broadcast_to(inp['a_ln_b'].reshape(L, 1, 256), (L, 128, 256)))
    w['b_gng'] = f(np.broadcast_to(inp['b_gn_g'].reshape(L, 1, 256), (L, 128, 256)))
    w['b_gnb'] = f(np.broadcast_to(inp['b_gn_b'].reshape(L, 1, 256), (L, 128, 256)))
    posT = np.concatenate([inp['c_pos_k'].transpose(0, 2, 1), inp['c_pos_v'].transpose(0, 2, 1)], 1)
    w['posT'] = _pad64(posT)
    w1k = inp['c_w1_k'].reshape(L, 32, 64, 64).transpose(0, 2, 1, 3)
    w1v = inp['c_w1_v'].reshape(L, 32, 64, 64).transpose(0, 2, 1, 3)
    w['w1s'] = f(np.concatenate([w1k, w1v], 1).reshape(L, 128, 2048))
    w['w2k'] = f(np.concatenate([inp['c_w2_k'], inp['c_w2_k']], 2))
    w['w2v'] = f(inp['c_w2_v'])
    w['w_out'] = f(inp['w_out'])
    w['lnpk'] = _pad64(np.concatenate([inp[k].reshape(L, 8, 128).transpose(0, 2, 1)
                                       for k in ('ln1_g', 'ln1_b', 'ln2_g', 'ln2_b')], 2))
    w['w_up'] = f(inp['w_up'])
    w['cwT'] = f(inp['conv_w'].reshape(L, 3, 44, 128).transpose(0, 3, 2, 1).reshape(L, 128, 132))
    w['cbT'] = _pad64(inp['conv_b'].reshape(L, 44, 128).transpose(0, 2, 1))
    w['w_down'] = f(inp['w_down'])
    return w


W_SHAPES = {'w_ada': [2, 1024, 6144], 'b_adaT': [2, 128, 64], 'w_inA': [2, 1024, 512], 'w_inBf': [2, 1024, 1024],
            'w_inBt': [2, 1024, 512], 'w_inCf': [2, 1024, 1280], 'w_inCt': [2, 1024, 280], 'WsT': [2, 128, 512],
            'bsT': [2, 128, 64], 'a_lng': [2, 128, 256], 'a_lnb': [2, 128, 256], 'b_gng': [2, 128, 256], 'b_gnb': [2, 128, 256],
            'posT': [2, 128, 64], 'w1s': [2, 128, 2048], 'w2k': [2, 64, 128], 'w2v': [2, 64, 64],
            'w_out': [2, 1024, 1024], 'lnpk': [2, 128, 64],
            'w_up': [2, 1024, 5632], 'cwT': [2, 128, 132], 'cbT': [2, 128, 64], 'w_down': [2, 2816, 1024]}


def build(n_layers=DEPTH, mixers=('A', 'B', 'C'), do_ffn=True, dbg=None):
    nc = bass.Bass("TRN2", target_bir_lowering=False)
    Dr = {}
    Dr['x'] = nc.dram_tensor("x", [SEQ, DM], F32, kind="ExternalInput").ap()
    Dr['cT'] = nc.dram_tensor("cT", [128, 64], F32, kind="ExternalInput").ap()
    for k, shp in W_SHAPES.items():
        Dr[k] = nc.dram_tensor(k, shp, F32, kind="ExternalInput").ap()
    for k, shp in TABLE_SHAPES.items():
        Dr[k] = nc.dram_tensor(k, shp, F32, kind="ExternalInput").ap()
    out_d = nc.dram_tensor("out", [SEQ, DM], F32, kind="ExternalOutput").ap()
    dbg_d = None
    if dbg is not None:
        dbg_d = nc.dram_tensor("dbg", [SEQ, DM], F32, kind="ExternalOutput").ap()

    st = ExitStack()
    with st:
        S = Sched(nc, st)
        T = lambda name, shape, dt=F32: st.enter_context(nc.sbuf_tensor("s_" + name, shape, dt))
        xT = T("xT", [128, 8, SEQ], F32)
        hT = T("hT", [128, 8, SEQ], BF16)
        WR = T("WR", [128, 4, 4096], BF16)
        AW = 51 * 256
        ARENA = T("ARENA", [128, AW], F32)
        PSA = st.enter_context(nc.psum_tensor("PSA", [128, 2048], F32))
        PSB = st.enter_context(nc.psum_tensor("PSB", [128, 2048], F32))

        def bank(i):
            t = PSA if i < 4 else PSB
            return t[:, (i % 4) * 512:(i % 4 + 1) * 512]

        BK = ['B%d' % i for i in range(8)]

        class Arena:
            def __init__(self):
                self.off = 0

            def reset(self, off=0):
                self.off = off

            def get(self, shape, dt=F32):
                n = int(np.prod(shape))
                nb = n * (2 if dt == BF16 else 4)
                w0 = self.off // 4
                w1 = w0 + (nb + 3) // 4
                assert w1 <= AW, ("arena overflow", w1 * 4, AW * 4)
                self.off = w1 * 4
                ap = ARENA[:, w0:w1]
                if dt == BF16:
                    ap = ap.bitcast(BF16)
                ap = ap[:, 0:n]
                if len(shape) == 2:
                    ap = ap.rearrange("p (a b) -> p a b", a=shape[0])
                elif len(shape) == 3:
                    ap = ap.rearrange("p (a b c) -> p a b c", a=shape[0], b=shape[1])
                return ap

        AR = Arena()

        def MM(out, lhsT, rhs, start, stop, rd, wr):
            S.op('pe', lambda e: e.matmul(out, lhsT=lhsT, rhs=rhs, start=start, stop=stop, skip_group_check=True), rd, wr)

        def TR(out, in_, ident, rd, wr):
            S.op('pe', lambda e: e.transpose(out, in_, ident), rd, wr)

        def ACT(out, in_, func, rd, wr, scale=1.0, bias=None):
            if bias is None:
                S.op('act', lambda e: e.activation(out, in_, func, scale=scale), rd, wr)
            else:
                S.op('act', lambda e: e.activation(out, in_, func, bias=bias, scale=scale), rd, wr)

        def TT(out, in0, in1, op, rd, wr, eng='dve'):
            S.op(eng, lambda e: e.tensor_tensor(out, in0, in1, op), rd, wr)

        def TS(out, in0, s1, s2, op0, op1, rd, wr, eng='dve'):
            if s2 is None:
                S.op(eng, lambda e: e.tensor_scalar(out, in0, s1, None, op0=op0), rd, wr)
            else:
                S.op(eng, lambda e: e.tensor_scalar(out, in0, s1, s2, op0=op0, op1=op1), rd, wr)

        def STT(out, in0, scalar, in1, op0, op1, rd, wr):
            S.op('dve', lambda e: e.scalar_tensor_tensor(out, in0, scalar, in1, op0=op0, op1=op1), rd, wr)

        def CP(out, in_, rd, wr, eng='dve'):
            if eng == 'act':
                S.op('act', lambda e: e.activation(out, in_, AF.Identity), rd, wr)
            else:
                S.op(eng, lambda e: e.tensor_copy(out, in_), rd, wr)

        def RED(out, in_, op, rd, wr):
            S.op('dve', lambda e: e.tensor_reduce(out, in_, axis=AX.X, op=op), rd, wr)

        def RCP(out, in_, rd, wr):
            S.op('dve', lambda e: e.reciprocal(out, in_), rd, wr)

        evt = [0]

        def EV():
            evt[0] += 1
            return 'act' if evt[0] % 2 else 'dve'

        def DMA(q, out, in_, rd, wr):
            pieces = []

            def split(o, i):
                shp = tuple(o.shape)
                assert tuple(i.shape) == shp, (shp, i.shape)
                if len(shp) == 3:
                    for a in range(shp[1]):
                        split(o[:, a, :], i[:, a, :])
                elif len(shp) == 2 and shp[1] > 512:
                    for c0 in range(0, shp[1], 512):
                        c1 = min(shp[1], c0 + 512)
                        pieces.append((o[:, c0:c1], i[:, c0:c1]))
                else:
                    pieces.append((o, i))
            split(out, in_)
            S.dma_multi(q, pieces, rd, wr)

        ident_f = T("ident_f", [128, 128], F32)
        ident_b = T("ident_b", [128, 128], BF16)
        onesm = T("onesm", [128, 128], BF16)
        epsA = T("epsA", [128, 2], F32)
        condT = T("condT", [128, 64], F32)
        modT = [T("modT%d" % l, [128, 48], F32) for l in range(DEPTH)]
        drv = [T("drv%d" % l, [128, 64], F32) for l in range(DEPTH)]
        lnp = [T("lnp%d" % l, [128, 64], F32) for l in range(DEPTH)]
        cwT = T("cwT", [128, 132], F32)
        cbT = T("cbT", [128, 64], F32)
        decayT = T("decayT", [128, 512], F32)
        xiT = T("xiT", [128, 256], F32)
        zeta = T("zeta", [128, 64], F32)
        cdv = T("cdv", [128, 64], F32)
        expand = T("expand", [128, 2048], BF16)
        causalb = T("causalb", [128, 128], BF16)
        winb = T("winb", [128, 128], BF16)
        cmpb = T("cmpb", [128, 2048], BF16)
        fbias = T("fbias", [128, 512], F32)
        causal01 = T("causal01", [128, 128], F32)
        ovl = T("ovl", [128, 64], F32)
        WsT = T("WsT", [128, 512], BF16)
        bsT = T("bsT", [128, 64], F32)
        lnbc = T("lnbc", [128, 4, 256], F32)
        posT = T("posT", [128, 64], F32)
        w1s = T("w1s", [128, 2048], BF16)
        w2k = T("w2k", [64, 128], BF16)
        w2v = T("w2v", [64, 64], BF16)

        DMA('sp', ident_f[:], Dr['identf'], (), ['ident_f'])
        S.op('dve', lambda e: e.tensor_copy(ident_b[:], ident_f[:]), ['ident_f'], ['ident_b'])
        S.op('dve', lambda e: e.memset(onesm[:], 1.0 / 1024.0), (), ['onesm'])
        S.op('dve', lambda e: e.memset(epsA[:, 0:1], LN_EPS), (), ['epsA'])
        S.op('dve', lambda e: e.memset(epsA[:, 1:2], LN_EPS / (ALPHA * ALPHA)), (), ['epsA'])
        barscr = T("barscr", [128, 64], F32)
        RELAY = (barscr[:], Dr['zeta'])
        rlscr = T("rlscr", [128, 2], F32)
        S.relay_fn = lambda e: e.memset(rlscr[:, 0:1], 0.0)
        DMA('sp', condT[:], Dr['cT'], (), ['condT'])
        for nm, tl in (('decayT', decayT), ('xiT', xiT), ('zeta', zeta), ('cdv', cdv), ('fbias', fbias), ('causal01', causal01), ('ovl', ovl)):
            DMA('sp', tl[:], Dr[nm], (), [nm])
        for nm, tl in (('causalb', causalb), ('winb', winb)):
            DMA('pool', tl[:], Dr[nm], (), [nm])
        for nm, tl in (('expand', expand), ('cmpb', cmpb)):
            DMA('pool', tl[:].rearrange("p (a b) -> p a b", a=4), Dr[nm].rearrange("p (a b) -> p a b", a=4), (), [nm])
        ACT(condT[:], condT[:], AF.Silu, ['condT'], ['condT'])

        wr_n = [0]

        def wload(src_ap, shape):
            S.relay_readers(['WR%d' % j_ for j_ in range(4)])
            i = wr_n[0] % 4
            wr_n[0] += 1
            n = int(np.prod(shape))
            v = WR[:, i, 0:n]
            if len(shape) == 2:
                v = v.rearrange("p (a b) -> p a b", a=shape[0])
            key = 'WR%d' % i
            DMA('pool', v, src_ap, (), [key])
            return v, key

        def kchunks(ap2d):
            return ap2d.rearrange("(k p) n -> p k n", p=128)

        AR.reset()
        ada_buf = [AR.get([8, 512], BF16) for _ in range(2)]
        condTb = AR.get([8, 8], BF16)
        CP(condTb, condT[:, 0:8].unsqueeze(2).to_broadcast([128, 8, 8]), ['condT'], ['condTb'])
        for l in range(n_layers):
            for blk in range(12):
                buf = ada_buf[blk % 2]
                bk = 'ada%d' % (blk % 2)
                DMA('pool', buf, kchunks(Dr['w_ada'][l][:, blk * 512:(blk + 1) * 512]), (), [bk])
                for jj in range(4):
                    j = blk * 4 + jj
                    for k in range(8):
                        MM(bank(0)[:, j * 8:j * 8 + 8], buf[:, k, jj * 128:(jj + 1) * 128], condTb[:, k, :],
                           k == 0, k == 7, [bk, 'condTb'], ['B0'])
            btmp = AR.get([64], F32) if l == 0 else btmp
            DMA('sp', btmp, Dr['b_adaT'][l], (), ['btmp'])
            TT(modT[l][:], bank(0)[:, 0:384].rearrange('p (j r) -> p j r', r=8)[:, :, 0], btmp[:, 0:48], ALU.add, ['B0', 'btmp'], ['modT%d' % l])
            DMA('sp', lnp[l][:], Dr['lnpk'][l], (), ['lnp%d' % l])
        for l in range(n_layers):
            m = modT[l]
            d = drv[l]
            mk, dk = 'modT%d' % l, 'drv%d' % l
            TS(d[:, 0:8], m[:, 8:16], 1.0, None, ALU.add, None, [mk], [dk])
            TS(d[:, 8:16], m[:, 16:24], 1.0 / ALPHA, None, ALU.mult, None, [mk], [dk])
            TS(d[:, 16:24], m[:, 32:40], 1.0, None, ALU.add, None, [mk], [dk])
            TS(d[:, 24:32], m[:, 40:48], 1.0 / ALPHA, None, ALU.mult, None, [mk], [dk])
            TT(d[:, 32:40], lnp[l][:, 0:8], d[:, 16:24], ALU.mult, ['lnp%d' % l, dk], [dk])
            TT(d[:, 40:48], lnp[l][:, 8:16], d[:, 16:24], ALU.mult, ['lnp%d' % l, dk], [dk])
            TT(d[:, 40:48], d[:, 40:48], m[:, 24:32], ALU.add, [dk, mk], [dk])
        for l in range(n_layers - 1):
            d, dn = drv[l], drv[l + 1]
            dk, dnk = 'drv%d' % l, 'drv%d' % (l + 1)
            TT(d[:, 48:56], lnp[l][:, 16:24], dn[:, 0:8], ALU.mult, ['lnp%d' % l, dnk, dk], [dk])
            TT(d[:, 56:64], lnp[l][:, 24:32], dn[:, 0:8], ALU.mult, ['lnp%d' % l, dnk, dk], [dk])
            TT(d[:, 56:64], d[:, 56:64], modT[l + 1][:, 0:8], ALU.add, [dk, 'modT%d' % (l + 1)], [dk])

        S.barrier(relay=RELAY)
        AR.reset()
        xst = [AR.get([1024], F32) for _ in range(8)]
        bi = 0
        for tb in (range(4) if dbg != 'skip_xload' else []):
            for q in range(4):
                tt = tb * 4 + q
                DMA('sp', xst[tt % 8], Dr['x'][tt * 128:(tt + 1) * 128, :], (), ['xst%d' % (tt % 8)])
            for c in range(8):
                b = bi % 8
                bi += 1
                for q in range(4):
                    tt = tb * 4 + q
                    TR(bank(b)[:, q * 128:(q + 1) * 128], xst[tt % 8][:, c * 128:(c + 1) * 128], ident_f[:],
                       ['xst%d' % (tt % 8), 'ident_f'], [BK[b]])
                ACT(xT[:, c, tb * 512:(tb + 1) * 512], bank(b), AF.Identity, [BK[b]], ['xT%d_%d' % (c, tb)])
                if dbg != 'no_ts':
                    TS(hT[:, c, tb * 512:(tb + 1) * 512], xT[:, c, tb * 512:(tb + 1) * 512], drv[0][:, c:c + 1], modT[0][:, c:c + 1],
                       ALU.mult, ALU.add, ['xT%d_%d' % (c, tb), 'drv0', 'modT0'], ['hT%d_%d' % (c, tb)])

        def out_proj(l, mixtm, mixkeys, nch, row0, mixT, alias=()):
            nonlocal_bi = [0]
            for c in range(nch):
                for tb in range(4):
                    b = 4 + (nonlocal_bi[0] % 4)
                    nonlocal_bi[0] += 1
                    pb = bank(b).bitcast(BF16)
                    for q in range(4):
                        tt = tb * 4 + q
                        TR(pb[:, q * 128:(q + 1) * 128], mixtm[:, tt, c * 128:(c + 1) * 128], ident_b[:],
                           [mixkeys[tt], 'ident_b'], [BK[b]])
                    CP(mixT[:, c, tb * 512:(tb + 1) * 512], pb[:, 0:512], [BK[b]], ['mixT%d_%d' % (c, tb)] + list(alias), eng=EV())
            wv, wk = wload(kchunks(Dr['w_out'][l][row0:row0 + nch * 128, :]), [nch, 1024])
            for fb in range(8):
                for tb in range(4):
                    b = nonlocal_bi[0] % 4
                    nonlocal_bi[0] += 1
                    for c in range(nch):
                        MM(bank(b), wv[:, c, fb * 128:(fb + 1) * 128], mixT[:, c, tb * 512:(tb + 1) * 512],
                           c == 0, c == nch - 1, [wk, 'mixT%d_%d' % (c, tb)], [BK[b]])
                    xs = xT[:, fb, tb * 512:(tb + 1) * 512]
                    STT(xs, bank(b), drv[l][:, 8 + fb:9 + fb], xs, ALU.mult, ALU.add,
                        [BK[b], 'drv%d' % l, 'xT%d_%d' % (fb, tb)], ['xT%d_%d' % (fb, tb)])

        def layer_norm(l, which, last):
            goff = 0 if which == 1 else 16
            aoff = 32 if which == 1 else 48
            AR.reset()
            xb = [AR.get([512], BF16) for _ in range(3)]
            sq = [AR.get([512], BF16) for _ in range(3)]
            rstd = [AR.get([512], F32) for _ in range(2)]
            nmr = [AR.get([512], F32) for _ in range(2)]
            tmp = [AR.get([512], F32) for _ in range(3)]
            n = 0
            for tb in range(4):
                bm, be = 0 + 2 * (tb % 2), 1 + 2 * (tb % 2)
                for c in range(8):
                    i = n % 3
                    n += 1
                    xs = xT[:, c, tb * 512:(tb + 1) * 512]
                    xk = 'xT%d_%d' % (c, tb)
                    ACT(sq[i], xs, AF.Square, [xk], ['lsq%d' % i])
                    CP(xb[i], xs, [xk], ['lxb%d' % i], eng='dve')
                    MM(bank(bm), onesm[:], xb[i], c == 0, c == 7, ['onesm', 'lxb%d' % i], [BK[bm]])
                    MM(bank(be), onesm[:], sq[i], c == 0, c == 7, ['onesm', 'lsq%d' % i], [BK[be]])
                r = tb % 2
                rk, nk = 'lrstd%d' % r, 'lnmr%d' % r
                ACT(nmr[r], bank(bm), AF.Square, [BK[bm]], [nk])
                TT(rstd[r], bank(be), nmr[r], ALU.subtract, [BK[be], nk], [rk])
                TS(rstd[r], rstd[r], 0.0, None, ALU.max, None, [rk], [rk])
                ACT(rstd[r], rstd[r], AF.Sqrt, [rk, 'epsA'], [rk], bias=epsA[:, 1:2])
                RCP(rstd[r], rstd[r], [rk], [rk])
                STT(nmr[r], bank(bm), -1.0, rstd[r], ALU.mult, ALU.mult, [BK[bm], rk], [nk])
                for c in range(8):
                    i = n % 3
                    n += 1
                    xs = xT[:, c, tb * 512:(tb + 1) * 512]
                    xk = 'xT%d_%d' % (c, tb)
                    tk = 'ltmp%d' % i
                    TT(tmp[i], xs, rstd[r], ALU.mult, [xk, rk], [tk])
                    TT(tmp[i], tmp[i], nmr[r], ALU.add, [tk, nk], [tk])
                    ACT(xs, tmp[i], AF.Identity, [tk, 'lnp%d' % l], [xk],
                        scale=lnp[l][:, goff + c:goff + c + 1], bias=lnp[l][:, goff + 8 + c:goff + 9 + c])
                    if not last:
                        TS(hT[:, c, tb * 512:(tb + 1) * 512], tmp[i], drv[l][:, aoff + c:aoff + c + 1],
                           drv[l][:, aoff + 8 + c:aoff + 9 + c], ALU.mult, ALU.add,
                           [tk, 'drv%d' % l], ['hT%d_%d' % (c, tb)])

        def dump_tm(src, c0, ncol, keys, dcol):
            dst = AR.get([ncol], F32)
            for tt in range(16):
                CP(dst, src[:, tt, c0:c0 + ncol], [keys[tt]], ['dst'])
                DMA('sp', dbg_d[tt * 128:(tt + 1) * 128, dcol:dcol + ncol], dst, ['dst'], ['dbg'])

        def group_ln_core(src, sqv, nb, rk, wk_sq, eps_ap):
            s1 = AR.get([nb], F32)
            s2 = AR.get([nb], F32)
            s3 = AR.get([nb], F32)
            TT(sqv, src, src, ALU.mult, rk, [wk_sq])
            RED(s1, src, ALU.add, rk, ['gs1'])
            RED(s2, sqv, ALU.add, [wk_sq], ['gs2'])
            TS(s1, s1, 1.0 / 64, None, ALU.mult, None, ['gs1'], ['gs1'])
            TT(s3, s1, s1, ALU.mult, ['gs1'], ['gs3'])
            STT(s2, s2, 1.0 / 64, s3, ALU.mult, ALU.subtract, ['gs2', 'gs3'], ['gs2'])
            TS(s2, s2, 0.0, None, ALU.max, None, ['gs2'], ['gs2'])
            ACT(s2, s2, AF.Sqrt, ['gs2', 'epsA'], ['gs2'], bias=eps_ap)
            RCP(s2, s2, ['gs2'], ['gs2'])
            TT(sqv, src, s1.unsqueeze(2).to_broadcast([128, nb, 64]), ALU.subtract, rk + ['gs1'], [wk_sq])
            TT(sqv, sqv, s2.unsqueeze(2).to_broadcast([128, nb, 64]), ALU.mult, [wk_sq, 'gs2'], [wk_sq])

        def mixer_B(l):
            hk = lambda k, tb: 'hT%d_%d' % (k, tb)
            S.barrier(relay=RELAY)
            AR.reset()
            qrT = AR.get([2, 2048], BF16)
            krT = AR.get([2, 2048], BF16)
            off_x = AR.off
            cosT = AR.get([2048], F32)
            sinT = AR.get([2048], F32)
            rt1 = [AR.get([512], F32) for _ in range(2)]
            rt2 = [AR.get([512], F32) for _ in range(2)]
            DMA('sp', cosT, Dr['cosT'], (), ['cosT'])
            DMA('sp', sinT, Dr['sinT'], (), ['sinT'])
            DMA('sp', lnbc[:, 2, :], Dr['b_gng'][l], (), ['lnbc'])
            DMA('sp', lnbc[:, 3, :], Dr['b_gnb'][l], (), ['lnbc'])
            n = 0
            for p in range(2):
                wv, wk = wload(kchunks(Dr['w_inBf'][l][:, p * 512:(p + 1) * 512]), [8, 512])
                for kind in range(2):
                    dst = qrT if kind == 0 else krT
                    for tb in range(4):
                        i = n % 2
                        b1, b2 = 2 * (n % 4), 2 * (n % 4) + 1
                        n += 1
                        for k in range(8):
                            MM(bank(b1), wv[:, k, (2 * kind) * 128:(2 * kind + 1) * 128], hT[:, k, tb * 512:(tb + 1) * 512],
                               k == 0, k == 7, [wk, hk(k, tb)], [BK[b1]])
                        for k in range(8):
                            MM(bank(b2), wv[:, k, (2 * kind + 1) * 128:(2 * kind + 2) * 128], hT[:, k, tb * 512:(tb + 1) * 512],
                               k == 0, k == 7, [wk, hk(k, tb)], [BK[b2]])
                        TT(rt1[i], bank(b1), cosT[:, tb * 512:(tb + 1) * 512], ALU.mult, [BK[b1], 'cosT'], ['rt1_%d' % i])
                        TT(rt2[i], bank(b2), sinT[:, tb * 512:(tb + 1) * 512], ALU.mult, [BK[b2], 'sinT'], ['rt2_%d' % i])
                        TT(dst[:, p, tb * 512:(tb + 1) * 512], rt1[i], rt2[i], ALU.add, ['rt1_%d' % i, 'rt2_%d' % i],
                           ['qk%d_%d' % (kind, p)])
            wvt, wkt = wload(kchunks(Dr['w_inBt'][l]), [8, 512])
            for p in range(2):
                S.barrier(relay=RELAY)
                AR.reset(off_x)
                vg = AR.get([16, 256], BF16)
                kvs = AR.get([16, 128], F32)
                s16 = AR.get([16, 128], BF16)
                oall = AR.get([16, 128], F32)
                kz = [AR.get([128], BF16) for _ in range(2)]
                qx = [AR.get([128], BF16) for _ in range(2)]
                sT = [AR.get([256], BF16) for _ in range(2)]
                mixT = AR.get([1, 2048], BF16)
                qkk = ['qk0_%d' % p, 'qk1_%d' % p]
                for tt in range(16):
                    b = tt % 4
                    for k in range(8):
                        MM(bank(b)[:, 0:256], hT[:, k, tt * 128:(tt + 1) * 128], wvt[:, k, p * 256:(p + 1) * 256],
                           k == 0, k == 7, [hk(k, tt // 4), wkt], [BK[b]])
                    CP(vg[:, tt, 0:128], bank(b)[:, 0:128], [BK[b]], ['vg%d' % tt], eng='dve')
                    ACT(vg[:, tt, 128:256], bank(b)[:, 128:256], AF.Silu, [BK[b]], ['vg%d' % tt])
                S.op('dve', lambda e: e.memset(kvs[:, 0, :], 0.0), (), ['kvs'])
                for c in range(15):
                    i = c % 2
                    bt = 4 + (c % 2)
                    bm = 6 + (c % 2)
                    pb = bank(bt).bitcast(BF16)
                    TR(pb[:, 0:128], krT[:, p, c * 128:(c + 1) * 128], ident_b[:], [qkk[1], 'ident_b'], [BK[bt]])
                    TT(kz[i].rearrange("p (h d) -> p h d", h=2), pb[:, 0:128].rearrange("p (h d) -> p h d", h=2),
                       zeta[:, 2 * p:2 * p + 2].unsqueeze(2).to_broadcast([128, 2, 64]), ALU.mult,
                       [BK[bt], 'zeta'], ['kz%d' % i])
                    MM(bank(bm)[:, 0:128], kz[i], vg[:, c, 0:128], True, True, ['kz%d' % i, 'vg%d' % c], [BK[bm]])
                    STT(kvs[:, c + 1, :], kvs[:, c, :], cdv[:, p:p + 1], bank(bm)[:, 0:128], ALU.mult, ALU.add,
                        ['kvs', 'cdv', BK[bm]], ['kvs'])
                CP(s16, kvs, ['kvs'], ['s16'], eng='dve')
                for c in range(16):
                    i = c % 2
                    bs0 = 2 * (c % 2)
                    bo0 = 4 + 2 * (c % 2)
                    cs = slice(c * 128, (c + 1) * 128)
                    if c > 0:
                        TT(qx[i], qrT[:, p, cs], xiT[:, p * 128:(p + 1) * 128], ALU.mult, [qkk[0], 'xiT'], ['qx%d' % i])
                    for hh in range(2):
                        ps_ = slice(hh * 64, (hh + 1) * 64)
                        MM(bank(bs0 + hh)[:, 0:128], krT[ps_, p, cs], qrT[ps_, p, cs], True, True,
                           qkk, [BK[bs0 + hh]])
                    TT(sT[i].rearrange("p (h l) -> p h l", h=2),
                       PSA[:, bs0 * 512:(bs0 + 2) * 512].rearrange("p (h x) -> p h x", h=2)[:, :, 0:128],
                       decayT[:, 2 * p * 128:(2 * p + 2) * 128].rearrange("p (h l) -> p h l", h=2), ALU.mult,
                       [BK[bs0], BK[bs0 + 1], 'decayT'], ['sT%d' % i])
                    for hh in range(2):
                        ps_ = slice(hh * 64, (hh + 1) * 64)
                        MM(bank(bo0 + hh)[:, 0:64], sT[i][:, hh * 128:(hh + 1) * 128], vg[:, c, hh * 64:(hh + 1) * 64],
                           True, c == 0, ['sT%d' % i, 'vg%d' % c], [BK[bo0 + hh]])
                        if c > 0:
                            MM(bank(bo0 + hh)[:, 0:64], qx[i][ps_, :], s16[ps_, c, hh * 64:(hh + 1) * 64],
                               False, True, ['qx%d' % i, 's16'], [BK[bo0 + hh]])
                    CP(oall[:, c, :].rearrange("p (h e) -> p h e", h=2),
                       PSB[:, (bo0 - 4) * 512:(bo0 - 2) * 512].rearrange("p (h x) -> p h x", h=2)[:, :, 0:64],
                       [BK[bo0], BK[bo0 + 1]], ['oall'], eng='act')
                o3 = oall.rearrange("p c (h e) -> p (c h) e", h=2)
                sq3 = kvs.rearrange("p c (h e) -> p (c h) e", h=2)
                gam_b = lnbc[:, 2, p * 128:(p + 1) * 128].rearrange("p (h e) -> p h e", h=2).unsqueeze(1).to_broadcast([128, 16, 2, 64])
                bet_b = lnbc[:, 3, p * 128:(p + 1) * 128].rearrange("p (h e) -> p h e", h=2).unsqueeze(1).to_broadcast([128, 16, 2, 64])
                sq4 = kvs.rearrange("p c (h e) -> p c h e", h=2)
                group_ln_core(o3, sq3, 32, ['oall'], 'kvs', epsA[:, 0:1])
                TT(sq4, sq4, gam_b, ALU.mult, ['kvs', 'lnbc'], ['kvs'])
                TT(sq4, sq4, bet_b, ALU.add, ['kvs', 'lnbc'], ['kvs'])
                vkeys = ['vg%d' % tt for tt in range(16)]
                TT(vg[:, :, 0:128], kvs, vg[:, :, 128:256], ALU.mult, ['kvs'] + vkeys, vkeys)
                if dbg == 'mixB%d_%d' % (l, p):
                    dump_tm(vg, 0, 128, vkeys, p * 128)
                out_proj(l, vg, vkeys, 1, 256 + p * 128, mixT)

        def mixer_C(l):
            hk = lambda k, tb: 'hT%d_%d' % (k, tb)
            DMA('pool', w1s[:].rearrange("p (a b) -> p a b", a=4), Dr['w1s'][l].rearrange("p (a b) -> p a b", a=4), (), ['w1s'])
            DMA('pool', w2k[:], Dr['w2k'][l], (), ['w2k'])
            DMA('pool', w2v[:], Dr['w2v'][l], (), ['w2v'])
            DMA('sp', posT[:], Dr['posT'][l], (), ['posT'])
            for g in range(2):
                S.barrier(relay=RELAY)
                AR.reset()
                qz = AR.get([4, 2048], BF16)
                KsT = AR.get([2048], BF16)
                KwT = AR.get([2048], BF16)
                Vaug = AR.get([16, 2, 65], BF16)
                gates = AR.get([16, 12], F32)
                mixC = AR.get([16, 256], BF16)
                kcmpT = AR.get([127], BF16)
                vcaug = AR.get([97], BF16)
                hidT = AR.get([2, 127], BF16)
                off_r = AR.off
                kvcT = AR.get([2048], BF16)
                kcp = AR.get([32, 127], BF16)
                wv, wk = wload(kchunks(Dr['w_inCf'][l][:, g * 640:g * 640 + 512]), [8, 512])
                wv2, wk2 = wload(kchunks(Dr['w_inCf'][l][:, g * 640 + 512:g * 640 + 640]), [8, 128])
                S.op('dve', lambda e: e.memset(qz, 0.0), (), ['qT'])
                dsts = [(None, 'qT', 0.125), (None, 'qT', 0.125), (KsT, 'KsT', 1.0), (KwT, 'KwT', 1.0),
                        (kvcT, 'kvcT', 1.0)]
                n = 0
                for bi_, (dst, dk, scl) in enumerate(dsts):
                    for tb in range(4):
                        b = n % 4
                        n += 1
                        for k in range(8):
                            lw = wv[:, k, bi_ * 128:(bi_ + 1) * 128] if bi_ < 4 else wv2[:, k, :]
                            MM(bank(b), lw, hT[:, k, tb * 512:(tb + 1) * 512], k == 0, k == 7,
                               [wk if bi_ < 4 else wk2, hk(k, tb)], [BK[b]])
                        if dst is None:
                            ACT(qz[0:64, 2 * bi_, tb * 512:(tb + 1) * 512], bank(b)[0:64, :], AF.Identity, [BK[b]], [dk], scale=scl)
                            TS(qz[64:128, 2 * bi_ + 1, tb * 512:(tb + 1) * 512], bank(b)[64:128, :], scl, None, ALU.mult, None, [BK[b]], [dk])
                        elif n % 2:
                            ACT(dst[:, tb * 512:(tb + 1) * 512], bank(b), AF.Identity, [BK[b]], [dk], scale=scl)
                        else:
                            TS(dst[:, tb * 512:(tb + 1) * 512], bank(b), scl, None, ALU.mult, None, [BK[b]], [dk])
                wv3, wk3 = wload(kchunks(Dr['w_inCt'][l][:, g * 140:(g + 1) * 140]), [8, 140])
                S.op('dve', lambda e: e.memset(Vaug[:, :, :, 64:65], 1.0), (), ['Vaug'])
                for tt in range(16):
                    b = 4 + tt % 4
                    for k in range(8):
                        MM(bank(b)[:, 0:140], hT[:, k, tt * 128:(tt + 1) * 128], wv3[:, k, :], k == 0, k == 7,
                           [hk(k, tt // 4), wk3], [BK[b]])
                    CP(Vaug[:, tt, :, 0:64], bank(b)[:, 0:128].rearrange("p (s d) -> p s d", s=2), [BK[b]], ['Vaug'], eng='dve')
                    ACT(gates[:, tt, :], bank(b)[:, 128:140], AF.Sigmoid, [BK[b]], ['gates'])
                kv_t = kvcT.tensor
                win = bass.AP(kv_t, kvcT.offset, [list(kvcT.ap[0]), [1, 32], [16, 127]])
                TT(kcp, win, posT[:, 0:32].unsqueeze(2).to_broadcast([128, 32, 127]), ALU.add, ['kvcT', 'posT'], ['kcp'])
                for kv in range(2):
                    ps_ = slice(kv * 64, (kv + 1) * 64)
                    for ll in range(32):
                        MM(bank(kv)[0:64, 0:127], w1s[ps_, ll * 64:(ll + 1) * 64], kcp[ps_, ll, :],
                           ll == 0, ll == 31, ['w1s', 'kcp'], [BK[kv]])
                    ACT(hidT[0:64, kv, :], bank(kv)[0:64, 0:127], AF.Gelu_apprx_tanh, [BK[kv]], ['hidT'])
                MM(bank(2)[:, 0:127], w2k[:], hidT[0:64, 0, :], True, True, ['w2k', 'hidT'], ['B2'])
                CP(kcmpT, bank(2)[:, 0:127], ['B2'], ['kcmpT'], eng='dve')
                MM(bank(3)[0:127, 0:64], hidT[0:64, 1, :], w2v[:], True, True, ['w2v', 'hidT'], ['B3'])
                S.op('dve', lambda e: e.memset(vcaug[:, 64:65], 1.0), (), ['vcaug'])
                CP(vcaug[0:127, 0:64], bank(3)[0:127, 0:64], ['B3'], ['vcaug'], eng='dve')
                CP(vcaug[:, 65:97], ovl[:, 0:32], ['ovl'], ['vcaug'], eng='dve')
                S.barrier(relay=RELAY)
                AR.reset(off_r)
                PT = [AR.get([512], BF16) for _ in range(3)]
                MbT = [AR.get([128], BF16) for _ in range(2)]
                for j_ in range(2):
                    S.op('dve', (lambda t_: (lambda e: e.memset(t_, 0.0)))(MbT[j_]), (), ['MbT%d' % j_])
                Mb = [AR.get([32], BF16) for _ in range(2)]
                ocg = [AR.get([4, 64], F32) for _ in range(2)]
                t1 = [AR.get([4, 64], F32) for _ in range(2)]
                t2 = [AR.get([4, 64], F32) for _ in range(2)]
                sm = [AR.get([96], F32) for _ in range(2)]
                impt = [AR.get([4, 32], F32) for _ in range(2)]
                pn = [0]

                def qslice(r, qt):
                    return qz[:, r, qt * 128:(qt + 1) * 128]

                def attend(qt, kts, KT, vsel, bS, bO, extra_fn):
                    def scores(ki):
                        kt = kts[ki]
                        bs_ = bS[ki % len(bS)]
                        extras = extra_fn(kt)
                        for r in range(4):
                            MM(bank(bs_)[:, r * 128:(r + 1) * 128], KT[:, kt * 128:(kt + 1) * 128], qslice(r, qt),
                               r == 0, (r == 3 and not extras), ['KsT', 'KwT', 'qT'], [BK[bs_]])
                        v4 = bank(bs_).rearrange("p (r t) -> p r t", r=4)
                        for ei, (lh, rh, ks) in enumerate(extras):
                            MM(v4, lh, rh, False, ei == len(extras) - 1, ks, [BK[bs_]])

                    def rest(ki):
                        kt = kts[ki]
                        bs_ = bS[ki % len(bS)]
                        i = pn[0] % 3
                        pn[0] += 1
                        ACT(PT[i], bank(bs_), AF.Exp, [BK[bs_]], ['PT%d' % i])
                        for r in range(4):
                            MM(bank(bO)[:, r * 65:(r + 1) * 65], PT[i][:, r * 128:(r + 1) * 128], Vaug[:, kt, vsel, :],
                               ki == 0 and r == 0, ki == len(kts) - 1 and r == 3, ['PT%d' % i, 'Vaug'], [BK[bO]])

                    scores(0)
                    for ki in range(len(kts)):
                        if ki + 1 < len(kts):
                            scores(ki + 1)
                        rest(ki)

                for qt in range(16):
                    j = qt % 2
                    smj = sm[j]
                    sk = 'sm%d' % j
                    qs = slice(qt * 128, (qt + 1) * 128)
                    for r in range(4):
                        MM(bank(0)[0:127, r * 128:(r + 1) * 128], kcmpT[:, :], qslice(r, qt), r == 0, False,
                           ['kcmpT', 'qT'], ['B0'])
                    MM(bank(0)[0:127, :].rearrange("p (r t) -> p r t", r=4), ident_b[0:127, 0:127],
                       cmpb[0:127, qs].unsqueeze(1).to_broadcast([127, 4, 128]), False, True, ['ident_b', 'cmpb'], ['B0'])
                    i = pn[0] % 3
                    pn[0] += 1
                    ACT(PT[i][0:127, :], bank(0)[0:127, :], AF.Exp, ['B0'], ['PT%d' % i])
                    for r in range(4):
                        MM(bank(1)[:, r * 97:(r + 1) * 97], PT[i][0:127, r * 128:(r + 1) * 128], vcaug[0:127, :],
                           r == 0, r == 3, ['PT%d' % i, 'vcaug'], ['B1'])
                    O = bank(1)[:, 0:388].rearrange("p (r c) -> p r c", r=4)
                    cb4 = causalb[:].unsqueeze(1).to_broadcast([128, 4, 128])
                    wb4 = winb[:].unsqueeze(1).to_broadcast([128, 4, 128])

                    def wmask(kt, qt=qt, cb4=cb4, wb4=wb4):
                        if kt == qt:
                            return [(ident_b[:], cb4, ['ident_b', 'causalb'])]
                        if kt == qt - 4:
                            return [(ident_b[:], wb4, ['ident_b', 'winb'])]
                        return []
                    attend(qt, list(range(max(0, qt - 4), qt + 1)), KwT, 1, [2, 3], 5, wmask)
                    TS(smj[:, 0:4], O[:, :, 64], 1e-30, None, ALU.max, None, ['B1'], [sk])
                    RCP(smj[:, 4:8], smj[:, 0:4], [sk], [sk])
                    TT(impt[j], O[:, :, 65:97], smj[:, 4:8].unsqueeze(2).to_broadcast([128, 4, 32]), ALU.mult, ['B1', sk], ['impt%d' % j])
                    RED(smj[:, 32:64], impt[j].rearrange("p r c -> p c r"), ALU.add, ['impt%d' % j], [sk])
                    TT(smj[:, 32:64], smj[:, 32:64], fbias[:, qt * 32:(qt + 1) * 32], ALU.add, [sk, 'fbias'], [sk])
                    S.op('dve', (lambda o_, i_: (lambda e: e.max(o_, i_)))(smj[:, 64:72], smj[:, 32:64]), [sk], [sk])
                    TS(Mb[j], smj[:, 32:64], smj[:, 71:72], -1.0, ALU.is_ge, ALU.add, [sk], ['Mb%d' % j])
                    pbt = bank(0).bitcast(BF16)
                    TR(pbt[0:32, 0:128], Mb[j], ident_b[:], ['Mb%d' % j, 'ident_b'], ['B0'])
                    CP(MbT[j][0:32, :], pbt[0:32, 0:128], ['B0'], ['MbT%d' % j], eng='dve')
                    gv = gates[:, qt, :].rearrange("p (r c) -> p r c", r=4)
                    TT(smj[:, 8:12], smj[:, 4:8], gv[:, :, 0], ALU.mult, [sk, 'gates'], [sk])
                    TT(ocg[j], O[:, :, 0:64], smj[:, 8:12].unsqueeze(2).to_broadcast([128, 4, 64]), ALU.mult, ['B1', sk], ['ocg%d' % j])

                    mb4 = MbT[j][:, :].unsqueeze(1).to_broadcast([128, 4, 128])

                    def smask(kt, qt=qt, j=j, cb4=cb4, mb4=mb4):
                        ex = [(expand[:, kt * 128:(kt + 1) * 128], mb4, ['expand', 'MbT%d' % j])]
                        if kt == qt:
                            ex.append((ident_b[:], cb4, ['ident_b', 'causalb']))
                        return ex
                    attend(qt, list(range(0, qt + 1)), KsT, 0, [6, 7], 4, smask)
                    Os = bank(4)[:, 0:260].rearrange("p (r c) -> p r c", r=4)
                    Ow = bank(5)[:, 0:260].rearrange("p (r c) -> p r c", r=4)
                    RCP(smj[:, 12:16], Os[:, :, 64], ['B4'], [sk])
                    TT(smj[:, 12:16], smj[:, 12:16], gv[:, :, 1], ALU.mult, [sk, 'gates'], [sk])
                    RCP(smj[:, 16:20], Ow[:, :, 64], ['B5'], [sk])
                    TT(smj[:, 16:20], smj[:, 16:20], gv[:, :, 2], ALU.mult, [sk, 'gates'], [sk])
                    TT(t1[j], Os[:, :, 0:64], smj[:, 12:16].unsqueeze(2).to_broadcast([128, 4, 64]), ALU.mult, ['B4', sk], ['t1_%d' % j])
                    TT(t2[j], Ow[:, :, 0:64], smj[:, 16:20].unsqueeze(2).to_broadcast([128, 4, 64]), ALU.mult, ['B5', sk], ['t2_%d' % j])
                    TT(t1[j], t1[j], ocg[j], ALU.add, ['t1_%d' % j, 'ocg%d' % j], ['t1_%d' % j])
                    TT(mixC[:, qt, :].rearrange("p (r d) -> p r d", r=4), t1[j], t2[j], ALU.add, ['t1_%d' % j, 't2_%d' % j], ['mixC%d' % qt])
                mkeys = ['mixC%d' % tt for tt in range(16)]
                if dbg == 'mixC%d_%d' % (l, g):
                    dump_tm(mixC, 0, 256, mkeys, g * 256)
                S.barrier(relay=RELAY)
                AR.reset(off_r)
                mixT = AR.get([2, 2048], BF16)
                out_proj(l, mixC, mkeys, 2, 512 + g * 256, mixT)

        for l in range(n_layers):
            hk = lambda k, tb: 'hT%d_%d' % (k, tb)
            if 'A' in mixers:
                S.barrier(relay=RELAY)
                AR.reset()
                zag = AR.get([16, 512], BF16)
                off_sqv = AR.off
                sqv = AR.get([16, 256], F32)
                st1 = AR.get([64], F32)
                st2 = AR.get([64], F32)
                st3 = AR.get([64], F32)
                mixA = AR.get([16, 256], BF16)
                atmp = [AR.get([256], F32) for _ in range(2)]
                wstage = AR.get([4, 128], F32)
                DMA('sp', wstage, Dr['WsT'][l].rearrange("p (g t) -> p g t", g=4), (), ['wstage'])
                TT(WsT[:].rearrange("p (g t) -> p g t", g=4), wstage,
                   causal01[:].unsqueeze(1).to_broadcast([128, 4, 128]), ALU.mult, ['wstage', 'causal01'], ['WsT'])
                DMA('sp', bsT[:], Dr['bsT'][l], (), ['bsT'])
                DMA('sp', lnbc[:, 0, :], Dr['a_lng'][l], (), ['lnbcA'])
                DMA('sp', lnbc[:, 1, :], Dr['a_lnb'][l], (), ['lnbcA'])
                wv, wk = wload(kchunks(Dr['w_inA'][l]), [8, 512])
                for tt in range(16):
                    b = tt % 4
                    for k in range(8):
                        MM(bank(b), hT[:, k, tt * 128:(tt + 1) * 128], wv[:, k, :], k == 0, k == 7,
                           [hk(k, tt // 4), wk], [BK[b]])
                    ACT(zag[:, tt, :], bank(b), AF.Gelu_apprx_tanh, [BK[b]], ['zag'])
                v4 = zag[:, :, 256:512].rearrange("p t (g d) -> p t g d", g=4)
                TT(sqv, zag[:, :, 256:512], zag[:, :, 256:512], ALU.mult, ['zag'], ['sqv'])
                RED(st1.rearrange("p (t g) -> p t g", t=16), v4, ALU.add, ['zag'], ['st1'])
                RED(st2.rearrange("p (t g) -> p t g", t=16), sqv.rearrange("p t (g d) -> p t g d", g=4), ALU.add, ['sqv'], ['st2'])
                TS(st1, st1, 1.0 / 64, None, ALU.mult, None, ['st1'], ['st1'])
                TT(st3, st1, st1, ALU.mult, ['st1'], ['st3'])
                STT(st2, st2, 1.0 / 64, st3, ALU.mult, ALU.subtract, ['st2', 'st3'], ['st2'])
                TS(st2, st2, 0.0, None, ALU.max, None, ['st2'], ['st2'])
                ACT(st2, st2, AF.Sqrt, ['st2', 'epsA'], ['st2'], bias=epsA[:, 0:1])
                RCP(st2, st2, ['st2'], ['st2'])
                mean_b = st1.rearrange("p (t g) -> p t g", t=16).unsqueeze(3).to_broadcast([128, 16, 4, 64])
                rstd_b = st2.rearrange("p (t g) -> p t g", t=16).unsqueeze(3).to_broadcast([128, 16, 4, 64])
                sq4 = sqv.rearrange("p t (g d) -> p t g d", g=4)
                TT(sq4, v4, mean_b, ALU.subtract, ['zag', 'st1'], ['sqv'])
                TT(sq4, sq4, rstd_b, ALU.mult, ['sqv', 'st2'], ['sqv'])
                gam_b = lnbc[:, 0, :].unsqueeze(1).to_broadcast([128, 16, 256])
                bet_b = lnbc[:, 1, :].unsqueeze(1).to_broadcast([128, 16, 256])
                TT(sqv, sqv, gam_b, ALU.mult, ['sqv', 'lnbcA'], ['sqv'])
                TT(zag[:, :, 256:512], sqv, bet_b, ALU.add, ['sqv', 'lnbcA'], ['zag'])
                bs_b = bsT[:, 0:4].unsqueeze(2).to_broadcast([128, 4, 64])
                for tt in range(16):
                    b = 4 + tt % 4
                    for g in range(4):
                        MM(bank(b)[:, g * 64:(g + 1) * 64], WsT[:, g * 128:(g + 1) * 128],
                           zag[:, tt, 256 + g * 64:256 + (g + 1) * 64], g == 0, g == 3, ['WsT', 'zag'], [BK[b]])
                    at = atmp[tt % 2]
                    ak = 'atmp%d' % (tt % 2)
                    TT(at.rearrange("p (g d) -> p g d", g=4), bank(b)[:, 0:256].rearrange("p (g d) -> p g d", g=4),
                       bs_b, ALU.add, [BK[b], 'bsT'], [ak])
                    TT(mixA[:, tt, :], at, zag[:, tt, 0:256], ALU.mult, [ak, 'zag'], ['mixA%d' % tt])
                if dbg == 'mixA%d' % l:
                    dst = AR.get([256], F32)
                    for tt in range(16):
                        CP(dst, mixA[:, tt, :], ['mixA%d' % tt], ['dst'])
                        DMA('sp', dbg_d[tt * 128:(tt + 1) * 128, 0:256], dst, ['dst'], ['dbg'])
                AR.reset(off_sqv)
                mixT = AR.get([2, 2048], BF16)
                out_proj(l, mixA, ['mixA%d' % tt for tt in range(16)], 2, 0, mixT, ['sqv'])

            if 'B' in mixers:
                mixer_B(l)
            if 'C' in mixers:
                mixer_C(l)

            S.barrier(relay=RELAY)
            layer_norm(l, 1, last=False)
            if dbg == 'ln1_%d' % l:
                break

            if do_ffn:
                S.barrier(relay=RELAY)
                AR.reset()
                DMA('sp', cwT[:], Dr['cwT'][l], (), ['cwT'])
                DMA('sp', cbT[:], Dr['cbT'][l], (), ['cbT'])
                gT = AR.get([max(FF_SPLIT), 2048], BF16)
                ctmp = [[AR.get([1024], F32) for _ in range(2)] for _ in range(2)]
                sgt = [AR.get([1024], BF16) for _ in range(2)]
                j0 = 0
                PS4 = [PSA, PSB]
                P4K = [BK[0:4], BK[4:8]]
                for part_n in FF_SPLIT:
                    wgrp = {}
                    for jl in range(part_n):
                        j = j0 + jl
                        if jl % 4 == 0:
                            ng = min(4, part_n - jl)
                            for part in range(2):
                                jj0 = part * 22 + j
                                wgrp[part] = wload(kchunks(Dr['w_up'][l][:, jj0 * 128:(jj0 + ng) * 128]), [8, ng * 128])
                        for part in range(2):
                            jj = part * 22 + j
                            wvf, wk = wgrp[part]
                            wv = wvf[:, :, (jl % 4) * 128:(jl % 4 + 1) * 128]
                            ps = PS4[part]
                            for tb in range(4):
                                for k in range(8):
                                    MM(ps[:, tb * 512:(tb + 1) * 512], wv[:, k, :], hT[:, k, tb * 512:(tb + 1) * 512],
                                       k == 0, k == 7, [wk, hk(k, tb)], [P4K[part][tb]])
                            for hf in range(2):
                                ct = ctmp[part][hf]
                                ck = 'ctmp%d_%d' % (part, hf)
                                o = hf * 1024
                                pk = P4K[part][2 * hf:2 * hf + 2]
                                pkp = P4K[part][max(0, 2 * hf - 1):2 * hf + 2]
                                ACT(ct, ps[:, o:o + 1024], AF.Identity, pk + ['cwT', 'cbT'], [ck],
                                    scale=cwT[:, jj * 3 + 2:jj * 3 + 3], bias=cbT[:, jj:jj + 1])
                                if hf == 0:
                                    STT(ct[:, 1:1024], ps[:, 0:1023], cwT[:, jj * 3 + 1:jj * 3 + 2], ct[:, 1:1024],
                                        ALU.mult, ALU.add, pk + ['cwT', ck], [ck])
                                    STT(ct[:, 2:1024], ps[:, 0:1022], cwT[:, jj * 3:jj * 3 + 1], ct[:, 2:1024],
                                        ALU.mult, ALU.add, pk + ['cwT', ck], [ck])
                                else:
                                    STT(ct, ps[:, o - 1:o + 1023], cwT[:, jj * 3 + 1:jj * 3 + 2], ct,
                                        ALU.mult, ALU.add, pkp + ['cwT', ck], [ck])
                                    STT(ct, ps[:, o - 2:o + 1022], cwT[:, jj * 3:jj * 3 + 1], ct,
                                        ALU.mult, ALU.add, pkp + ['cwT', ck], [ck])
                                if part == 0:
                                    ACT(sgt[hf], ct, AF.Silu, [ck], ['sgt%d' % hf])
                                else:
                                    TT(gT[:, jl, o:o + 1024], ct, sgt[hf], ALU.mult, [ck, 'sgt%d' % hf], ['gT%d' % jl])
                    nsl = (part_n + 3) // 4
                    wvs = []
                    for s in range(nsl):
                        r0 = (j0 + 4 * s) * 128
                        nr = min(4, part_n - 4 * s)
                        wvs.append(wload(kchunks(Dr['w_down'][l][r0:r0 + nr * 128, :]), [nr, 1024]))
                    bi2 = 0
                    for fb in range(8):
                        for tb in range(4):
                            b = bi2 % 8
                            bi2 += 1
                            for jl in range(part_n):
                                wv, wk = wvs[jl // 4]
                                MM(bank(b), wv[:, jl % 4, fb * 128:(fb + 1) * 128], gT[:, jl, tb * 512:(tb + 1) * 512],
                                   jl == 0, jl == part_n - 1, [wk, 'gT%d' % jl], [BK[b]])
                            xs = xT[:, fb, tb * 512:(tb + 1) * 512]
                            STT(xs, bank(b), drv[l][:, 24 + fb:25 + fb], xs, ALU.mult, ALU.add,
                                [BK[b], 'drv%d' % l, 'xT%d_%d' % (fb, tb)], ['xT%d_%d' % (fb, tb)])
                    j0 += part_n
                S.barrier(relay=RELAY)
            layer_norm(l, 2, last=(l == n_layers - 1))

        S.barrier(relay=RELAY)
        AR.reset()
        ost = [AR.get([1024], F32) for _ in range(4)]
        bi = 0
        for tt in range(16):
            o = ost[tt % 4]
            ok = 'ost%d' % (tt % 4)
            for cg in range(2):
                b = bi % 8
                bi += 1
                for q in range(4):
                    c = cg * 4 + q
                    TR(bank(b)[:, q * 128:(q + 1) * 128], xT[:, c, tt * 128:(tt + 1) * 128], ident_f[:],
                       ['xT%d_%d' % (c, tt // 4), 'ident_f'], [BK[b]])
                CP(o[:, cg * 512:(cg + 1) * 512], bank(b), [BK[b]], [ok], eng=EV())
            DMA('sp', out_d[tt * 128:(tt + 1) * 128, :], o, [ok], ['out'])
        S.finish()
        S.replay()
    return nc


_CACHE = {}


def kernel(**inputs):
    inp = {k: np.asarray(v) for k, v in inputs.items()}
    if 'nc' not in _CACHE:
        _CACHE['nc'] = build()
    nc = _CACHE['nc']
    w = _prep_weights(inp)
    tb = _tables()
    in_maps = []
    for b in range(8):
        m = dict(w)
        m.update(tb)
        m['x'] = np.ascontiguousarray(inp['x'][b], dtype=np.float32)
        m['cT'] = _pad64(inp['c'][b].reshape(8, 128).T)
        in_maps.append(m)
    res = run_bass_kernel_spmd(nc, in_maps, core_ids=list(range(8)))
    out = np.stack([np.asarray(r['out'], dtype=np.float32) for r in res.results], 0)
    return out
```

```python
import math
from contextlib import ExitStack
import numpy as np
import concourse.bass as bass
import concourse.mybir as mybir
from concourse.bass_utils import run_bass_kernel_spmd

F32 = mybir.dt.float32
BF16 = mybir.dt.bfloat16
AF = mybir.ActivationFunctionType
ALU = mybir.AluOpType
AX = mybir.AxisListType

ENGS = ['pe', 'dve', 'act', 'pool', 'sp']
NRING = 8
DEPTH = 2
SEQ = 2048
DM = 1024
ALPHA = (2 * DEPTH) ** 0.25
LN_EPS = 1e-5
NEGB = -30000.0
FF_SPLIT = [6, 6, 5, 5]


class Sched:
    def __init__(self, nc, stack):
        self.nc = nc
        self.prog = {e: [] for e in ENGS}
        self.cnt = {e: 0 for e in ENGS}
        self.seen = {e: {} for e in ENGS}
        self.lastw = {}
        self.readers = {}
        self.sems = {}
        self.semval = {}
        self.relay_fn = None
        for e in ENGS:
            self.sems[e] = stack.enter_context(nc.semaphore("s_" + e))
        self.dma_n = {}
        for q in ['sp', 'act', 'pool']:
            self.dma_n[q] = 0
            for j in range(NRING):
                nm = "d_%s%d" % (q, j)
                self.sems[nm] = stack.enter_context(nc.semaphore(nm))

    def _deps(self, eng, reads, writes, is_dma=False):
        deps = []
        for k in reads:
            t = self.lastw.get(k)
            if t is not None:
                deps.append((t, 'raw'))
        for k in writes:
            t = self.lastw.get(k)
            if t is not None:
                deps.append((t, 'waw'))
            for s, v in self.readers.get(k, {}).items():
                deps.append(((s, v), 'war'))
        need = {}
        for (s, v), kind in deps:
            if s == eng and not is_dma:
                if kind != 'raw' or eng == 'pe':
                    continue
            if self.seen[eng].get(s, 0) >= v:
                continue
            if need.get(s, 0) < v:
                need[s] = v
        return need

    def _emit_waits(self, eng, need):
        if eng in ('sp', 'pool') and 'pe' in need and self.relay_fn is not None:
            need = dict(need)
            v = need.pop('pe')
            self.seen[eng]['pe'] = v
            R = 'dve'
            if self.seen[R].get('pe', 0) < v:
                self.prog[R].append(('wait', 'pe', v))
                self.seen[R]['pe'] = v
            lr = getattr(self, 'last_relay', 0)
            if lr and self.seen[R].get(R, 0) < lr:
                self.prog[R].append(('wait', R, lr))
                self.seen[R][R] = lr
            self.cnt[R] += 1
            self.last_relay = self.cnt[R]
            self.semval[R] = self.cnt[R]
            self.prog[R].append(('op', self.relay_fn, R, 1))
            if self.seen[eng].get(R, 0) < self.cnt[R]:
                need[R] = max(need.get(R, 0), self.cnt[R])
        for s, v in need.items():
            self.prog[eng].append(('wait', s, v))
            self.seen[eng][s] = v

    def _commit(self, tok, reads, writes):
        for k in writes:
            self.lastw[k] = tok
            self.readers[k] = {}
        for k in reads:
            d = self.readers.setdefault(k, {})
            if d.get(tok[0], 0) < tok[1]:
                d[tok[0]] = tok[1]

    def relay_readers(self, keys):
        if self.relay_fn is None:
            return
        v = 0
        for k in keys:
            d = self.readers.get(k)
            if d and 'pe' in d:
                v = max(v, d['pe'])
        if v == 0:
            return
        R = 'dve'
        if self.seen[R].get('pe', 0) < v:
            self.prog[R].append(('wait', 'pe', v))
            self.seen[R]['pe'] = v
        lr = getattr(self, 'last_relay', 0)
        if lr and self.seen[R].get(R, 0) < lr:
            self.prog[R].append(('wait', R, lr))
            self.seen[R][R] = lr
        self.cnt[R] += 1
        self.semval[R] = self.cnt[R]
        self.last_relay = self.cnt[R]
        self.prog[R].append(('op', self.relay_fn, R, 1))
        for k in keys:
            d = self.readers.get(k)
            if d and 'pe' in d:
                d.pop('pe')
                d[R] = max(d.get(R, 0), self.cnt[R])

    def op(self, eng, fn, reads=(), writes=()):
        need = self._deps(eng, reads, writes)
        self._emit_waits(eng, need)
        self.cnt[eng] += 1
        tok = (eng, self.cnt[eng])
        self.semval[eng] = self.cnt[eng]
        self.prog[eng].append(('op', fn, eng, 1))
        self._commit(tok, reads, writes)
        return tok

    def dma_multi(self, q, pairs, reads=(), writes=()):
        i = self.dma_n[q]
        self.dma_n[q] += 1
        slot = "d_%s%d" % (q, i % NRING)
        prev = self.semval.get(slot, 0)
        val = prev + 16 * len(pairs)
        need = self._deps(q, reads, writes, is_dma=True)
        if prev > 0 and self.seen[q].get(slot, 0) < prev:
            need[slot] = max(need.get(slot, 0), prev)
        self._emit_waits(q, need)
        for out, in_ in pairs:
            self.prog[q].append(('op', (lambda o_, i_: (lambda e: e.dma_start(out=o_, in_=i_)))(out, in_), slot, 16))
        self.semval[slot] = val
        tok = (slot, val)
        self._commit(tok, reads, writes)
        return tok

    def dma(self, q, out, in_, reads=(), writes=()):
        return self.dma_multi(q, [(out, in_)], reads, writes)

    def _wait_all(self, e):
        need = {}
        for s_, v in self.semval.items():
            if s_ == e:
                continue
            if self.seen[e].get(s_, 0) < v:
                need[s_] = v
        self._emit_waits(e, need)

    def barrier(self, engs=('pe', 'dve', 'act', 'sp'), relay=None):
        if relay is None or 'sp' not in engs:
            for e in engs:
                self._wait_all(e)
            return
        snap = dict(self.semval)
        self._wait_all('sp')
        tok = self.dma('sp', relay[0], relay[1], (), ['__bar'])
        for e in engs:
            if e == 'sp':
                continue
            self._emit_waits(e, {tok[0]: tok[1]} if self.seen[e].get(tok[0], 0) < tok[1] else {})
            for s_, v in snap.items():
                if s_ != e and self.seen[e].get(s_, 0) < v:
                    self.seen[e][s_] = v

    def finish(self):
        self.barrier(engs=('sp',))

    def replay(self):
        nc = self.nc
        sems = self.sems
        prog = self.prog

        def run(e, name):
            for it in prog[name]:
                if it[0] == 'wait':
                    e.wait_ge(sems[it[1]], it[2])
                else:
                    it[1](e).then_inc(sems[it[2]], it[3])

        with nc.Block() as block:
            @block.tensor
            def _(e):
                run(e, 'pe')

            @block.vector
            def _(e):
                run(e, 'dve')

            @block.scalar
            def _(e):
                run(e, 'act')

            @block.gpsimd
            def _(e):
                run(e, 'pool')

            @block.sync
            def _(e):
                run(e, 'sp')


def _pad64(a):
    a = np.asarray(a, dtype=np.float32)
    out = np.zeros(a.shape[:-1] + (64,), np.float32)
    out[..., :a.shape[-1]] = a
    return out


def _tables():
    t = {}
    half = 32
    inv = np.power(np.float32(10000.0), -np.arange(half, dtype=np.float32) / np.float32(half)).astype(np.float32)
    pos = np.arange(SEQ, dtype=np.float32)
    ang = pos[:, None] * inv[None, :]
    cos = np.cos(ang).astype(np.float32).T
    sin = np.sin(ang).astype(np.float32).T
    cosT = np.concatenate([cos, cos, cos, cos], 0)
    sinT = np.concatenate([-sin, sin, -sin, sin], 0)
    t['cosT'] = np.ascontiguousarray(cosT)
    t['sinT'] = np.ascontiguousarray(sinT)
    H = 4
    L = 128
    lg = np.log1p(-np.exp2(-5.0 - np.arange(H, dtype=np.float32))).astype(np.float32)
    idx = np.arange(L, dtype=np.float32)
    diff = idx[:, None] - idx[None, :]
    dec = np.where(diff >= 0, np.exp(lg[:, None, None] * np.maximum(diff, 0.0)), 0.0).astype(np.float32)
    t['decayT'] = np.ascontiguousarray(np.transpose(dec, (2, 0, 1)) * np.float32(0.125)).reshape(128, 512)
    xi = np.exp(lg[:, None] * (idx + 1.0)).astype(np.float32)
    zeta = np.exp(lg[:, None] * (L - 1.0 - idx)).astype(np.float32)
    xiT = np.zeros((128, 2, 128), np.float32)
    for p in range(2):
        for hh in range(2):
            xiT[hh * 64:(hh + 1) * 64, p, :] = xi[2 * p + hh][None, :]
    t['xiT'] = xiT.reshape(128, 256)
    t['zeta'] = _pad64(zeta.T * np.float32(0.125))
    cd = np.exp(lg * L).astype(np.float32)
    cdv = np.zeros((128, 2), np.float32)
    for p in range(2):
        for hh in range(2):
            cdv[hh * 64:(hh + 1) * 64, p] = cd[2 * p + hh]
    t['cdv'] = _pad64(cdv)
    key = np.arange(SEQ)
    ex = np.zeros((128, SEQ), np.float32)
    ex[key // 64, key] = -NEGB
    t['expand'] = ex
    kk = np.arange(128)[:, None]
    tt = np.arange(128)[None, :]
    t['causalb'] = np.where(kk > tt, NEGB, 0.0).astype(np.float32)
    t['winb'] = np.where(kk <= tt, NEGB, 0.0).astype(np.float32)
    t['identf'] = np.eye(128, dtype=np.float32)
    t['causal01'] = np.where(tt >= kk, 1.0, 0.0).astype(np.float32)
    k127 = np.arange(128)[:, None]
    tpos = np.arange(SEQ)[None, :]
    t['cmpb'] = np.where(16 * k127 + 31 > tpos, NEGB, 0.0).astype(np.float32)
    fb = np.zeros((128, 16, 32), np.float32)
    for qt in range(16):
        tq = qt * 128 + np.arange(128)
        cur = tq // 64
        blk = np.arange(32)
        future = blk[None, :] > cur[:, None]
        forced = (blk[None, :] == 0) | (blk[None, :] == cur[:, None]) | (blk[None, :] == cur[:, None] - 1)
        fb[:, qt, :] = np.where(forced, 1e30, np.where(future, -1e30, 0.0))
    t['fbias'] = fb.reshape(128, 512)
    ov = np.zeros((128, 32), np.float32)
    c0 = np.arange(127)[:, None] * 16
    s0 = np.arange(32)[None, :] * 64
    ov[:127] = np.clip(np.minimum(c0 + 32, s0 + 64) - np.maximum(c0, s0), 0, None) / 32.0
    t['ovl'] = _pad64(ov)
    return t


TABLE_SHAPES = {'cosT': [128, 2048], 'sinT': [128, 2048], 'decayT': [128, 512], 'xiT': [128, 256],
                'zeta': [128, 64], 'cdv': [128, 64], 'expand': [128, 2048], 'causalb': [128, 128],
                'winb': [128, 128], 'causal01': [128, 128], 'identf': [128, 128], 'cmpb': [128, 2048], 'fbias': [128, 512], 'ovl': [128, 64]}


def _prep_weights(inp):
    w = {}
    f = lambda a: np.ascontiguousarray(a, dtype=np.float32)
    w_in = inp['w_in']
    L = DEPTH
    w['w_ada'] = f(inp['w_ada'])
    w['b_adaT'] = _pad64(inp['b_ada'].reshape(L, 48, 128).transpose(0, 2, 1))
    w['w_inA'] = f(w_in[:, :, 0:512])
    cols = []
    for p in range(2):
        for base in (512, 768):
            hd = np.arange(128)
            h = 2 * p + hd // 64
            d = hd % 64
            cols.append(base + h * 64 + d)
            cols.append(base + h * 64 + (d + 32) % 64)
    cols = np.concatenate(cols)
    w['w_inBf'] = f(w_in[:, :, cols])
    cols = []
    for p in range(2):
        cols.append(1024 + p * 128 + np.arange(128))
        cols.append(1280 + p * 128 + np.arange(128))
    w['w_inBt'] = f(w_in[:, :, np.concatenate(cols)])
    cols = []
    for g in range(2):
        cols.append(1536 + g * 256 + np.arange(256))
        ks = 2304 + g * 64 + np.arange(64)
        kw = 2560 + g * 64 + np.arange(64)
        cols += [ks, ks, kw, kw]
        cols.append(2048 + g * 64 + np.arange(64))
        cols.append(2176 + g * 64 + np.arange(64))
    w['w_inCf'] = f(w_in[:, :, np.concatenate(cols)])
    cols = []
    for g in range(2):
        cols.append(2432 + g * 64 + np.arange(64))
        cols.append(2688 + g * 64 + np.arange(64))
        cols.append(2816 + g * 12 + np.arange(12))
    w['w_inCt'] = f(w_in[:, :, np.concatenate(cols)])
    w['WsT'] = f(inp['a_ws'].transpose(0, 3, 1, 2).reshape(L, 128, 512))
    w['bsT'] = _pad64(inp['a_bs'].transpose(0, 2, 1))
    w['a_lng'] = f(np.broadcast_to(inp['a_ln_g'].reshape(L, 1, 256), (L, 128, 256)))
    w['a_lnb'] = f(np.broadcast_to(inp['a_ln_b'].reshape(L, 1, 256), (L, 128, 256)))
    w['b_gng'] = f(np.broadcast_to(inp['b_gn_g'].reshape(L, 1, 256), (L, 128, 256)))
    w['b_gnb'] = f(np.broadcast_to(inp['b_gn_b'].reshape(L, 1, 256), (L, 128, 256)))
    posT = np.concatenate([inp['c_pos_k'].transpose(0, 2, 1), inp['c_pos_v'].transpose(0, 2, 1)], 1)
    w['posT'] = _pad64(posT)
    w1k = inp['c_w1_k'].reshape(L, 32, 64, 64).transpose(0, 2, 1, 3)
    w1v = inp['c_w1_v'].reshape(L, 32, 64, 64).transpose(0, 2, 1, 3)
    w['w1s'] = f(np.concatenate([w1k, w1v], 1).reshape(L, 128, 2048))
    w['w2k'] = f(np.concatenate([inp['c_w2_k'], inp['c_w2_k']], 2))
    w['w2v'] = f(inp['c_w2_v'])
    w['w_out'] = f(inp['w_out'])
    w['lnpk'] = _pad64(np.concatenate([inp[k].reshape(L, 8, 128).transpose(0, 2, 1)
                                       for k in ('ln1_g', 'ln1_b', 'ln2_g', 'ln2_b')], 2))
    w['w_up'] = f(inp['w_up'])
    w['cwT'] = f(inp['conv_w'].reshape(L, 3, 44, 128).transpose(0, 3, 2, 1).reshape(L, 128, 132))
    w['cbT'] = _pad64(inp['conv_b'].reshape(L, 44, 128).transpose(0, 2, 1))
    w['w_down'] = f(inp['w_down'])
    return w


W_SHAPES = {'w_ada': [2, 1024, 6144], 'b_adaT': [2, 128, 64], 'w_inA': [2, 1024, 512], 'w_inBf': [2, 1024, 1024],
            'w_inBt': [2, 1024, 512], 'w_inCf': [2, 1024, 1280], 'w_inCt': [2, 1024, 280], 'WsT': [2, 128, 512],
            'bsT': [2, 128, 64], 'a_lng': [2, 128, 256], 'a_lnb': [2, 128, 256], 'b_gng': [2, 128, 256], 'b_gnb': [2, 128, 256],
            'posT': [2, 128, 64], 'w1s': [2, 128, 2048], 'w2k': [2, 64, 128], 'w2v': [2, 64, 64],
            'w_out': [2, 1024, 1024], 'lnpk': [2, 128, 64],
            'w_up': [2, 1024, 5632], 'cwT': [2, 128, 132], 'cbT': [2, 128, 64], 'w_down': [2, 2816, 1024]}


def build(n_layers=DEPTH, mixers=('A', 'B', 'C'), do_ffn=True, dbg=None):
    nc = bass.Bass("TRN2", target_bir_lowering=False)
    Dr = {}
    Dr['x'] = nc.dram_tensor("x", [SEQ, DM], F32, kind="ExternalInput").ap()
    Dr['cT'] = nc.dram_tensor("cT", [128, 64], F32, kind="ExternalInput").ap()
    for k, shp in W_SHAPES.items():
        Dr[k] = nc.dram_tensor(k, shp, F32, kind="ExternalInput").ap()
    for k, shp in TABLE_SHAPES.items():
        Dr[k] = nc.dram_tensor(k, shp, F32, kind="ExternalInput").ap()
    out_d = nc.dram_tensor("out", [SEQ, DM], F32, kind="ExternalOutput").ap()
    dbg_d = None
    if dbg is not None:
        dbg_d = nc.dram_tensor("dbg", [SEQ, DM], F32, kind="ExternalOutput").ap()

    st = ExitStack()
    with st:
        S = Sched(nc, st)
        T = lambda name, shape, dt=F32: st.enter_context(nc.sbuf_tensor("s_" + name, shape, dt))
        xT = T("xT", [128, 8, SEQ], F32)
        hT = T("hT", [128, 8, SEQ], BF16)
        WR = T("WR", [128, 4, 4096], BF16)
        AW = 51 * 256
        ARENA = T("ARENA", [128, AW], F32)
        PSA = st.enter_context(nc.psum_tensor("PSA", [128, 2048], F32))
        PSB = st.enter_context(nc.psum_tensor("PSB", [128, 2048], F32))

        def bank(i):
            t = PSA if i < 4 else PSB
            return t[:, (i % 4) * 512:(i % 4 + 1) * 512]

        BK = ['B%d' % i for i in range(8)]

        class Arena:
            def __init__(self):
                self.off = 0

            def reset(self, off=0):
                self.off = off

            def get(self, shape, dt=F32):
                n = int(np.prod(shape))
                nb = n * (2 if dt == BF16 else 4)
                w0 = self.off // 4
                w1 = w0 + (nb + 3) // 4
                assert w1 <= AW, ("arena overflow", w1 * 4, AW * 4)
                self.off = w1 * 4
                ap = ARENA[:, w0:w1]
                if dt == BF16:
                    ap = ap.bitcast(BF16)
                ap = ap[:, 0:n]
                if len(shape) == 2:
                    ap = ap.rearrange("p (a b) -> p a b", a=shape[0])
                elif len(shape) == 3:
                    ap = ap.rearrange("p (a b c) -> p a b c", a=shape[0], b=shape[1])
                return ap

        AR = Arena()

        def MM(out, lhsT, rhs, start, stop, rd, wr):
            S.op('pe', lambda e: e.matmul(out, lhsT=lhsT, rhs=rhs, start=start, stop=stop, skip_group_check=True), rd, wr)

        def TR(out, in_, ident, rd, wr):
            S.op('pe', lambda e: e.transpose(out, in_, ident), rd, wr)

        def ACT(out, in_, func, rd, wr, scale=1.0, bias=None):
            if bias is None:
                S.op('act', lambda e: e.activation(out, in_, func, scale=scale), rd, wr)
            else:
                S.op('act', lambda e: e.activation(out, in_, func, bias=bias, scale=scale), rd, wr)

        def TT(out, in0, in1, op, rd, wr, eng='dve'):
            S.op(eng, lambda e: e.tensor_tensor(out, in0, in1, op), rd, wr)

        def TS(out, in0, s1, s2, op0, op1, rd, wr, eng='dve'):
            if s2 is None:
                S.op(eng, lambda e: e.tensor_scalar(out, in0, s1, None, op0=op0), rd, wr)
            else:
                S.op(eng, lambda e: e.tensor_scalar(out, in0, s1, s2, op0=op0, op1=op1), rd, wr)

        def STT(out, in0, scalar, in1, op0, op1, rd, wr):
            S.op('dve', lambda e: e.scalar_tensor_tensor(out, in0, scalar, in1, op0=op0, op1=op1), rd, wr)

        def CP(out, in_, rd, wr, eng='dve'):
            if eng == 'act':
                S.op('act', lambda e: e.activation(out, in_, AF.Identity), rd, wr)
            else:
                S.op(eng, lambda e: e.tensor_copy(out, in_), rd, wr)

        def RED(out, in_, op, rd, wr):
            S.op('dve', lambda e: e.tensor_reduce(out, in_, axis=AX.X, op=op), rd, wr)

        def RCP(out, in_, rd, wr):
            S.op('dve', lambda e: e.reciprocal(out, in_), rd, wr)

        evt = [0]

        def EV():
            evt[0] += 1
            return 'act' if evt[0] % 2 else 'dve'

        def DMA(q, out, in_, rd, wr):
            pieces = []

            def split(o, i):
                shp = tuple(o.shape)
                assert tuple(i.shape) == shp, (shp, i.shape)
                if len(shp) == 3:
                    for a in range(shp[1]):
                        split(o[:, a, :], i[:, a, :])
                elif len(shp) == 2 and shp[1] > 512:
                    for c0 in range(0, shp[1], 512):
                        c1 = min(shp[1], c0 + 512)
                        pieces.append((o[:, c0:c1], i[:, c0:c1]))
                else:
                    pieces.append((o, i))
            split(out, in_)
            S.dma_multi(q, pieces, rd, wr)

        ident_f = T("ident_f", [128, 128], F32)
        ident_b = T("ident_b", [128, 128], BF16)
        onesm = T("onesm", [128, 128], BF16)
        epsA = T("epsA", [128, 2], F32)
        condT = T("condT", [128, 64], F32)
        modT = [T("modT%d" % l, [128, 48], F32) for l in range(DEPTH)]
        drv = [T("drv%d" % l, [128, 64], F32) for l in range(DEPTH)]
        lnp = [T("lnp%d" % l, [128, 64], F32) for l in range(DEPTH)]
        cwT = T("cwT", [128, 132], F32)
        cbT = T("cbT", [128, 64], F32)
        decayT = T("decayT", [128, 512], F32)
        xiT = T("xiT", [128, 256], F32)
        zeta = T("zeta", [128, 64], F32)
        cdv = T("cdv", [128, 64], F32)
        expand = T("expand", [128, 2048], BF16)
        causalb = T("causalb", [128, 128], BF16)
        winb = T("winb", [128, 128], BF16)
        cmpb = T("cmpb", [128, 2048], BF16)
        fbias = T("fbias", [128, 512], F32)
        causal01 = T("causal01", [128, 128], F32)
        ovl = T("ovl", [128, 64], F32)
        WsT = T("WsT", [128, 512], BF16)
        bsT = T("bsT", [128, 64], F32)
        lnbc = T("lnbc", [128, 4, 256], F32)
        posT = T("posT", [128, 64], F32)
        w1s = T("w1s", [128, 2048], BF16)
        w2k = T("w2k", [64, 128], BF16)
        w2v = T("w2v", [64, 64], BF16)

        DMA('sp', ident_f[:], Dr['identf'], (), ['ident_f'])
        S.op('dve', lambda e: e.tensor_copy(ident_b[:], ident_f[:]), ['ident_f'], ['ident_b'])
        S.op('dve', lambda e: e.memset(onesm[:], 1.0 / 1024.0), (), ['onesm'])
        S.op('dve', lambda e: e.memset(epsA[:, 0:1], LN_EPS), (), ['epsA'])
        S.op('dve', lambda e: e.memset(epsA[:, 1:2], LN_EPS / (ALPHA * ALPHA)), (), ['epsA'])
        barscr = T("barscr", [128, 64], F32)
        RELAY = (barscr[:], Dr['zeta'])
        rlscr = T("rlscr", [128, 2], F32)
        S.relay_fn = lambda e: e.memset(rlscr[:, 0:1], 0.0)
        DMA('sp', condT[:], Dr['cT'], (), ['condT'])
        for nm, tl in (('decayT', decayT), ('xiT', xiT), ('zeta', zeta), ('cdv', cdv), ('fbias', fbias), ('causal01', causal01), ('ovl', ovl)):
            DMA('sp', tl[:], Dr[nm], (), [nm])
        for nm, tl in (('causalb', causalb), ('winb', winb)):
            DMA('pool', tl[:], Dr[nm], (), [nm])
        for nm, tl in (('expand', expand), ('cmpb', cmpb)):
            DMA('pool', tl[:].rearrange("p (a b) -> p a b", a=4), Dr[nm].rearrange("p (a b) -> p a b", a=4), (), [nm])
        ACT(condT[:], condT[:], AF.Silu, ['condT'], ['condT'])

        wr_n = [0]

        def wload(src_ap, shape):
            S.relay_readers(['WR%d' % j_ for j_ in range(4)])
            i = wr_n[0] % 4
            wr_n[0] += 1
            n = int(np.prod(shape))
            v = WR[:, i, 0:n]
            if len(shape) == 2:
                v = v.rearrange("p (a b) -> p a b", a=shape[0])
            key = 'WR%d' % i
            DMA('pool', v, src_ap, (), [key])
            return v, key

        def kchunks(ap2d):
            return ap2d.rearrange("(k p) n -> p k n", p=128)

        AR.reset()
        ada_buf = [AR.get([8, 512], BF16) for _ in range(2)]
        condTb = AR.get([8, 8], BF16)
        CP(condTb, condT[:, 0:8].unsqueeze(2).to_broadcast([128, 8, 8]), ['condT'], ['condTb'])
        for l in range(n_layers):
            for blk in range(12):
                buf = ada_buf[blk % 2]
                bk = 'ada%d' % (blk % 2)
                DMA('pool', buf, kchunks(Dr['w_ada'][l][:, blk * 512:(blk + 1) * 512]), (), [bk])
                for jj in range(4):
                    j = blk * 4 + jj
                    for k in range(8):
                        MM(bank(0)[:, j * 8:j * 8 + 8], buf[:, k, jj * 128:(jj + 1) * 128], condTb[:, k, :],
                           k == 0, k == 7, [bk, 'condTb'], ['B0'])
            btmp = AR.get([64], F32) if l == 0 else btmp
            DMA('sp', btmp, Dr['b_adaT'][l], (), ['btmp'])
            TT(modT[l][:], bank(0)[:, 0:384].rearrange('p (j r) -> p j r', r=8)[:, :, 0], btmp[:, 0:48], ALU.add, ['B0', 'btmp'], ['modT%d' % l])
            DMA('sp', lnp[l][:], Dr['lnpk'][l], (), ['lnp%d' % l])
        for l in range(n_layers):
            m = modT[l]
            d = drv[l]
            mk, dk = 'modT%d' % l, 'drv%d' % l
            TS(d[:, 0:8], m[:, 8:16], 1.0, None, ALU.add, None, [mk], [dk])
            TS(d[:, 8:16], m[:, 16:24], 1.0 / ALPHA, None, ALU.mult, None, [mk], [dk])
            TS(d[:, 16:24], m[:, 32:40], 1.0, None, ALU.add, None, [mk], [dk])
            TS(d[:, 24:32], m[:, 40:48], 1.0 / ALPHA, None, ALU.mult, None, [mk], [dk])
            TT(d[:, 32:40], lnp[l][:, 0:8], d[:, 16:24], ALU.mult, ['lnp%d' % l, dk], [dk])
            TT(d[:, 40:48], lnp[l][:, 8:16], d[:, 16:24], ALU.mult, ['lnp%d' % l, dk], [dk])
            TT(d[:, 40:48], d[:, 40:48], m[:, 24:32], ALU.add, [dk, mk], [dk])
        for l in range(n_layers - 1):
            d, dn = drv[l], drv[l + 1]
            dk, dnk = 'drv%d' % l, 'drv%d' % (l + 1)
            TT(d[:, 48:56], lnp[l][:, 16:24], dn[:, 0:8], ALU.mult, ['lnp%d' % l, dnk, dk], [dk])
            TT(d[:, 56:64], lnp[l][:, 24:32], dn[:, 0:8], ALU.mult, ['lnp%d' % l, dnk, dk], [dk])
            TT(d[:, 56:64], d[:, 56:64], modT[l + 1][:, 0:8], ALU.add, [dk, 'modT%d' % (l + 1)], [dk])

        S.barrier(relay=RELAY)
        AR.reset()
        xst = [AR.get([1024], F32) for _ in range(8)]
        bi = 0
        for tb in (range(4) if dbg != 'skip_xload' else []):
            for q in range(4):
                tt = tb * 4 + q
                DMA('sp', xst[tt % 8], Dr['x'][tt * 128:(tt + 1) * 128, :], (), ['xst%d' % (tt % 8)])
            for c in range(8):
                b = bi % 8
                bi += 1
                for q in range(4):
                    tt = tb * 4 + q
                    TR(bank(b)[:, q * 128:(q + 1) * 128], xst[tt % 8][:, c * 128:(c + 1) * 128], ident_f[:],
                       ['xst%d' % (tt % 8), 'ident_f'], [BK[b]])
                ACT(xT[:, c, tb * 512:(tb + 1) * 512], bank(b), AF.Identity, [BK[b]], ['xT%d_%d' % (c, tb)])
                if dbg != 'no_ts':
                    TS(hT[:, c, tb * 512:(tb + 1) * 512], xT[:, c, tb * 512:(tb + 1) * 512], drv[0][:, c:c + 1], modT[0][:, c:c + 1],
                       ALU.mult, ALU.add, ['xT%d_%d' % (c, tb), 'drv0', 'modT0'], ['hT%d_%d' % (c, tb)])

        def out_proj(l, mixtm, mixkeys, nch, row0, mixT, alias=()):
            nonlocal_bi = [0]
            for c in range(nch):
                for tb in range(4):
                    b = 4 + (nonlocal_bi[0] % 4)
                    nonlocal_bi[0] += 1
                    pb = bank(b).bitcast(BF16)
                    for q in range(4):
                        tt = tb * 4 + q
                        TR(pb[:, q * 128:(q + 1) * 128], mixtm[:, tt, c * 128:(c + 1) * 128], ident_b[:],
                           [mixkeys[tt], 'ident_b'], [BK[b]])
                    CP(mixT[:, c, tb * 512:(tb + 1) * 512], pb[:, 0:512], [BK[b]], ['mixT%d_%d' % (c, tb)] + list(alias), eng=EV())
            wv, wk = wload(kchunks(Dr['w_out'][l][row0:row0 + nch * 128, :]), [nch, 1024])
            for fb in range(8):
                for tb in range(4):
                    b = nonlocal_bi[0] % 4
                    nonlocal_bi[0] += 1
                    for c in range(nch):
                        MM(bank(b), wv[:, c, fb * 128:(fb + 1) * 128], mixT[:, c, tb * 512:(tb + 1) * 512],
                           c == 0, c == nch - 1, [wk, 'mixT%d_%d' % (c, tb)], [BK[b]])
                    xs = xT[:, fb, tb * 512:(tb + 1) * 512]
                    STT(xs, bank(b), drv[l][:, 8 + fb:9 + fb], xs, ALU.mult, ALU.add,
                        [BK[b], 'drv%d' % l, 'xT%d_%d' % (fb, tb)], ['xT%d_%d' % (fb, tb)])

        def layer_norm(l, which, last):
            goff = 0 if which == 1 else 16
            aoff = 32 if which == 1 else 48
            AR.reset()
            xb = [AR.get([512], BF16) for _ in range(3)]
            sq = [AR.get([512], BF16) for _ in range(3)]
            rstd = [AR.get([512], F32) for _ in range(2)]
            nmr = [AR.get([512], F32) for _ in range(2)]
            tmp = [AR.get([512], F32) for _ in range(3)]
            n = 0
            for tb in range(4):
                bm, be = 0 + 2 * (tb % 2), 1 + 2 * (tb % 2)
                for c in range(8):
                    i = n % 3
                    n += 1
                    xs = xT[:, c, tb * 512:(tb + 1) * 512]
                    xk = 'xT%d_%d' % (c, tb)
                    ACT(sq[i], xs, AF.Square, [xk], ['lsq%d' % i])
                    CP(xb[i], xs, [xk], ['lxb%d' % i], eng='dve')
                    MM(bank(bm), onesm[:], xb[i], c == 0, c == 7, ['onesm', 'lxb%d' % i], [BK[bm]])
                    MM(bank(be), onesm[:], sq[i], c == 0, c == 7, ['onesm', 'lsq%d' % i], [BK[be]])
                r = tb % 2
                rk, nk = 'lrstd%d' % r, 'lnmr%d' % r
                ACT(nmr[r], bank(bm), AF.Square, [BK[bm]], [nk])
                TT(rstd[r], bank(be), nmr[r], ALU.subtract, [BK[be], nk], [rk])
                TS(rstd[r], rstd[r], 0.0, None, ALU.max, None, [rk], [rk])
                ACT(rstd[r], rstd[r], AF.Sqrt, [rk, 'epsA'], [rk], bias=epsA[:, 1:2])
                RCP(rstd[r], rstd[r], [rk], [rk])
                STT(nmr[r], bank(bm), -1.0, rstd[r], ALU.mult, ALU.mult, [BK[bm], rk], [nk])
                for c in range(8):
                    i = n % 3
                    n += 1
                    xs = xT[:, c, tb * 512:(tb + 1) * 512]
                    xk = 'xT%d_%d' % (c, tb)
                    tk = 'ltmp%d' % i
                    TT(tmp[i], xs, rstd[r], ALU.mult, [xk, rk], [tk])
                    TT(tmp[i], tmp[i], nmr[r], ALU.add, [tk, nk], [tk])
                    ACT(xs, tmp[i], AF.Identity, [tk, 'lnp%d' % l], [xk],
                        scale=lnp[l][:, goff + c:goff + c + 1], bias=lnp[l][:, goff + 8 + c:goff + 9 + c])
                    if not last:
                        ACT(hT[:, c, tb * 512:(tb + 1) * 512], tmp[i], AF.Identity, [tk, 'drv%d' % l], ['hT%d_%d' % (c, tb)],
                            scale=drv[l][:, aoff + c:aoff + c + 1], bias=drv[l][:, aoff + 8 + c:aoff + 9 + c])

        def dump_tm(src, c0, ncol, keys, dcol):
            dst = AR.get([ncol], F32)
            for tt in range(16):
                CP(dst, src[:, tt, c0:c0 + ncol], [keys[tt]], ['dst'])
                DMA('sp', dbg_d[tt * 128:(tt + 1) * 128, dcol:dcol + ncol], dst, ['dst'], ['dbg'])

        def group_ln_core(src, sqv, nb, rk, wk_sq, eps_ap):
            s1 = AR.get([nb], F32)
            s2 = AR.get([nb], F32)
            s3 = AR.get([nb], F32)
            TT(sqv, src, src, ALU.mult, rk, [wk_sq])
            RED(s1, src, ALU.add, rk, ['gs1'])
            RED(s2, sqv, ALU.add, [wk_sq], ['gs2'])
            TS(s1, s1, 1.0 / 64, None, ALU.mult, None, ['gs1'], ['gs1'])
            TT(s3, s1, s1, ALU.mult, ['gs1'], ['gs3'])
            STT(s2, s2, 1.0 / 64, s3, ALU.mult, ALU.subtract, ['gs2', 'gs3'], ['gs2'])
            TS(s2, s2, 0.0, None, ALU.max, None, ['gs2'], ['gs2'])
            ACT(s2, s2, AF.Sqrt, ['gs2', 'epsA'], ['gs2'], bias=eps_ap)
            RCP(s2, s2, ['gs2'], ['gs2'])
            TT(sqv, src, s1.unsqueeze(2).to_broadcast([128, nb, 64]), ALU.subtract, rk + ['gs1'], [wk_sq])
            TT(sqv, sqv, s2.unsqueeze(2).to_broadcast([128, nb, 64]), ALU.mult, [wk_sq, 'gs2'], [wk_sq])

        def mixer_B(l):
            hk = lambda k, tb: 'hT%d_%d' % (k, tb)
            S.barrier(relay=RELAY)
            AR.reset()
            qrT = AR.get([2, 2048], BF16)
            krT = AR.get([2, 2048], BF16)
            off_x = AR.off
            cosT = AR.get([2048], F32)
            sinT = AR.get([2048], F32)
            rt1 = [AR.get([512], F32) for _ in range(2)]
            rt2 = [AR.get([512], F32) for _ in range(2)]
            DMA('sp', cosT, Dr['cosT'], (), ['cosT'])
            DMA('sp', sinT, Dr['sinT'], (), ['sinT'])
            DMA('sp', lnbc[:, 2, :], Dr['b_gng'][l], (), ['lnbc'])
            DMA('sp', lnbc[:, 3, :], Dr['b_gnb'][l], (), ['lnbc'])
            n = 0
            for p in range(2):
                wv, wk = wload(kchunks(Dr['w_inBf'][l][:, p * 512:(p + 1) * 512]), [8, 512])
                for kind in range(2):
                    dst = qrT if kind == 0 else krT
                    for tb in range(4):
                        i = n % 2
                        b1, b2 = 2 * (n % 4), 2 * (n % 4) + 1
                        n += 1
                        for k in range(8):
                            MM(bank(b1), wv[:, k, (2 * kind) * 128:(2 * kind + 1) * 128], hT[:, k, tb * 512:(tb + 1) * 512],
                               k == 0, k == 7, [wk, hk(k, tb)], [BK[b1]])
                        for k in range(8):
                            MM(bank(b2), wv[:, k, (2 * kind + 1) * 128:(2 * kind + 2) * 128], hT[:, k, tb * 512:(tb + 1) * 512],
                               k == 0, k == 7, [wk, hk(k, tb)], [BK[b2]])
                        TT(rt1[i], bank(b1), cosT[:, tb * 512:(tb + 1) * 512], ALU.mult, [BK[b1], 'cosT'], ['rt1_%d' % i])
                        TT(rt2[i], bank(b2), sinT[:, tb * 512:(tb + 1) * 512], ALU.mult, [BK[b2], 'sinT'], ['rt2_%d' % i])
                        TT(dst[:, p, tb * 512:(tb + 1) * 512], rt1[i], rt2[i], ALU.add, ['rt1_%d' % i, 'rt2_%d' % i],
                           ['qk%d_%d' % (kind, p)])
            wvt, wkt = wload(kchunks(Dr['w_inBt'][l]), [8, 512])
            for p in range(2):
                S.barrier(relay=RELAY)
                AR.reset(off_x)
                vg = AR.get([16, 256], BF16)
                kvs = AR.get([16, 128], F32)
                s16 = AR.get([16, 128], BF16)
                oall = AR.get([16, 128], F32)
                kz = [AR.get([128], BF16) for _ in range(2)]
                qx = [AR.get([128], BF16) for _ in range(2)]
                sT = [AR.get([256], BF16) for _ in range(2)]
                mixT = AR.get([1, 2048], BF16)
                qkk = ['qk0_%d' % p, 'qk1_%d' % p]
                for tt in range(16):
                    b = tt % 4
                    for k in range(8):
                        MM(bank(b)[:, 0:256], hT[:, k, tt * 128:(tt + 1) * 128], wvt[:, k, p * 256:(p + 1) * 256],
                           k == 0, k == 7, [hk(k, tt // 4), wkt], [BK[b]])
                    CP(vg[:, tt, 0:128], bank(b)[:, 0:128], [BK[b]], ['vg%d' % tt], eng='dve')
                    ACT(vg[:, tt, 128:256], bank(b)[:, 128:256], AF.Silu, [BK[b]], ['vg%d' % tt])
                S.op('dve', lambda e: e.memset(kvs[:, 0, :], 0.0), (), ['kvs'])
                for c in range(15):
                    i = c % 2
                    bt = 4 + (c % 2)
                    bm = 6 + (c % 2)
                    pb = bank(bt).bitcast(BF16)
                    TR(pb[:, 0:128], krT[:, p, c * 128:(c + 1) * 128], ident_b[:], [qkk[1], 'ident_b'], [BK[bt]])
                    TT(kz[i].rearrange("p (h d) -> p h d", h=2), pb[:, 0:128].rearrange("p (h d) -> p h d", h=2),
                       zeta[:, 2 * p:2 * p + 2].unsqueeze(2).to_broadcast([128, 2, 64]), ALU.mult,
                       [BK[bt], 'zeta'], ['kz%d' % i])
                    MM(bank(bm)[:, 0:128], kz[i], vg[:, c, 0:128], True, True, ['kz%d' % i, 'vg%d' % c], [BK[bm]])
                    STT(kvs[:, c + 1, :], kvs[:, c, :], cdv[:, p:p + 1], bank(bm)[:, 0:128], ALU.mult, ALU.add,
                        ['kvs', 'cdv', BK[bm]], ['kvs'])
                CP(s16, kvs, ['kvs'], ['s16'], eng='dve')
                for c in range(16):
                    i = c % 2
                    bs0 = 2 * (c % 2)
                    bo0 = 4 + 2 * (c % 2)
                    cs = slice(c * 128, (c + 1) * 128)
                    if c > 0:
                        TT(qx[i], qrT[:, p, cs], xiT[:, p * 128:(p + 1) * 128], ALU.mult, [qkk[0], 'xiT'], ['qx%d' % i])
                    for hh in range(2):
                        ps_ = slice(hh * 64, (hh + 1) * 64)
                        MM(bank(bs0 + hh)[:, 0:128], krT[ps_, p, cs], qrT[ps_, p, cs], True, True,
                           qkk, [BK[bs0 + hh]])
                    TT(sT[i].rearrange("p (h l) -> p h l", h=2),
                       PSA[:, bs0 * 512:(bs0 + 2) * 512].rearrange("p (h x) -> p h x", h=2)[:, :, 0:128],
                       decayT[:, 2 * p * 128:(2 * p + 2) * 128].rearrange("p (h l) -> p h l", h=2), ALU.mult,
                       [BK[bs0], BK[bs0 + 1], 'decayT'], ['sT%d' % i])
                    for hh in range(2):
                        ps_ = slice(hh * 64, (hh + 1) * 64)
                        MM(bank(bo0 + hh)[:, 0:64], sT[i][:, hh * 128:(hh + 1) * 128], vg[:, c, hh * 64:(hh + 1) * 64],
                           True, c == 0, ['sT%d' % i, 'vg%d' % c], [BK[bo0 + hh]])
                        if c > 0:
                            MM(bank(bo0 + hh)[:, 0:64], qx[i][ps_, :], s16[ps_, c, hh * 64:(hh + 1) * 64],
                               False, True, ['qx%d' % i, 's16'], [BK[bo0 + hh]])
                    CP(oall[:, c, :].rearrange("p (h e) -> p h e", h=2),
                       PSB[:, (bo0 - 4) * 512:(bo0 - 2) * 512].rearrange("p (h x) -> p h x", h=2)[:, :, 0:64],
                       [BK[bo0], BK[bo0 + 1]], ['oall'], eng='act')
                o3 = oall.rearrange("p c (h e) -> p (c h) e", h=2)
                sq3 = kvs.rearrange("p c (h e) -> p (c h) e", h=2)
                gam_b = lnbc[:, 2, p * 128:(p + 1) * 128].rearrange("p (h e) -> p h e", h=2).unsqueeze(1).to_broadcast([128, 16, 2, 64])
                bet_b = lnbc[:, 3, p * 128:(p + 1) * 128].rearrange("p (h e) -> p h e", h=2).unsqueeze(1).to_broadcast([128, 16, 2, 64])
                sq4 = kvs.rearrange("p c (h e) -> p c h e", h=2)
                group_ln_core(o3, sq3, 32, ['oall'], 'kvs', epsA[:, 0:1])
                TT(sq4, sq4, gam_b, ALU.mult, ['kvs', 'lnbc'], ['kvs'])
                TT(sq4, sq4, bet_b, ALU.add, ['kvs', 'lnbc'], ['kvs'])
                vkeys = ['vg%d' % tt for tt in range(16)]
                TT(vg[:, :, 0:128], kvs, vg[:, :, 128:256], ALU.mult, ['kvs'] + vkeys, vkeys)
                if dbg == 'mixB%d_%d' % (l, p):
                    dump_tm(vg, 0, 128, vkeys, p * 128)
                out_proj(l, vg, vkeys, 1, 256 + p * 128, mixT)

        def mixer_C(l):
            hk = lambda k, tb: 'hT%d_%d' % (k, tb)
            DMA('pool', w1s[:].rearrange("p (a b) -> p a b", a=4), Dr['w1s'][l].rearrange("p (a b) -> p a b", a=4), (), ['w1s'])
            DMA('pool', w2k[:], Dr['w2k'][l], (), ['w2k'])
            DMA('pool', w2v[:], Dr['w2v'][l], (), ['w2v'])
            DMA('sp', posT[:], Dr['posT'][l], (), ['posT'])
            for g in range(2):
                S.barrier(relay=RELAY)
                AR.reset()
                qz = AR.get([4, 2048], BF16)
                KsT = AR.get([2048], BF16)
                KwT = AR.get([2048], BF16)
                Vaug = AR.get([16, 2, 65], BF16)
                gates = AR.get([16, 12], F32)
                mixC = AR.get([16, 256], BF16)
                kcmpT = AR.get([127], BF16)
                vcaug = AR.get([97], BF16)
                hidT = AR.get([2, 127], BF16)
                off_r = AR.off
                kvcT = AR.get([2048], BF16)
                kcp = AR.get([32, 127], BF16)
                wv, wk = wload(kchunks(Dr['w_inCf'][l][:, g * 640:g * 640 + 512]), [8, 512])
                wv2, wk2 = wload(kchunks(Dr['w_inCf'][l][:, g * 640 + 512:g * 640 + 640]), [8, 128])
                S.op('dve', lambda e: e.memset(qz, 0.0), (), ['qT'])
                dsts = [(None, 'qT', 0.125), (None, 'qT', 0.125), (KsT, 'KsT', 1.0), (KwT, 'KwT', 1.0),
                        (kvcT, 'kvcT', 1.0)]
                n = 0
                for bi_, (dst, dk, scl) in enumerate(dsts):
                    for tb in range(4):
                        b = n % 4
                        n += 1
                        for k in range(8):
                            lw = wv[:, k, bi_ * 128:(bi_ + 1) * 128] if bi_ < 4 else wv2[:, k, :]
                            MM(bank(b), lw, hT[:, k, tb * 512:(tb + 1) * 512], k == 0, k == 7,
                               [wk if bi_ < 4 else wk2, hk(k, tb)], [BK[b]])
                        if dst is None:
                            ACT(qz[0:64, 2 * bi_, tb * 512:(tb + 1) * 512], bank(b)[0:64, :], AF.Identity, [BK[b]], [dk], scale=scl)
                            TS(qz[64:128, 2 * bi_ + 1, tb * 512:(tb + 1) * 512], bank(b)[64:128, :], scl, None, ALU.mult, None, [BK[b]], [dk])
                        elif n % 2:
                            ACT(dst[:, tb * 512:(tb + 1) * 512], bank(b), AF.Identity, [BK[b]], [dk], scale=scl)
                        else:
                            TS(dst[:, tb * 512:(tb + 1) * 512], bank(b), scl, None, ALU.mult, None, [BK[b]], [dk])
                wv3, wk3 = wload(kchunks(Dr['w_inCt'][l][:, g * 140:(g + 1) * 140]), [8, 140])
                S.op('dve', lambda e: e.memset(Vaug[:, :, :, 64:65], 1.0), (), ['Vaug'])
                for tt in range(16):
                    b = 4 + tt % 4
                    for k in range(8):
                        MM(bank(b)[:, 0:140], hT[:, k, tt * 128:(tt + 1) * 128], wv3[:, k, :], k == 0, k == 7,
                           [hk(k, tt // 4), wk3], [BK[b]])
                    CP(Vaug[:, tt, :, 0:64], bank(b)[:, 0:128].rearrange("p (s d) -> p s d", s=2), [BK[b]], ['Vaug'], eng='dve')
                    ACT(gates[:, tt, :], bank(b)[:, 128:140], AF.Sigmoid, [BK[b]], ['gates'])
                kv_t = kvcT.tensor
                win = bass.AP(kv_t, kvcT.offset, [list(kvcT.ap[0]), [1, 32], [16, 127]])
                TT(kcp, win, posT[:, 0:32].unsqueeze(2).to_broadcast([128, 32, 127]), ALU.add, ['kvcT', 'posT'], ['kcp'])
                for kv in range(2):
                    ps_ = slice(kv * 64, (kv + 1) * 64)
                    for ll in range(32):
                        MM(bank(kv)[0:64, 0:127], w1s[ps_, ll * 64:(ll + 1) * 64], kcp[ps_, ll, :],
                           ll == 0, ll == 31, ['w1s', 'kcp'], [BK[kv]])
                    ACT(hidT[0:64, kv, :], bank(kv)[0:64, 0:127], AF.Gelu_apprx_tanh, [BK[kv]], ['hidT'])
                MM(bank(2)[:, 0:127], w2k[:], hidT[0:64, 0, :], True, True, ['w2k', 'hidT'], ['B2'])
                CP(kcmpT, bank(2)[:, 0:127], ['B2'], ['kcmpT'], eng='dve')
                MM(bank(3)[0:127, 0:64], hidT[0:64, 1, :], w2v[:], True, True, ['w2v', 'hidT'], ['B3'])
                S.op('dve', lambda e: e.memset(vcaug[:, 64:65], 1.0), (), ['vcaug'])
                CP(vcaug[0:127, 0:64], bank(3)[0:127, 0:64], ['B3'], ['vcaug'], eng='dve')
                CP(vcaug[:, 65:97], ovl[:, 0:32], ['ovl'], ['vcaug'], eng='dve')
                S.barrier(relay=RELAY)
                AR.reset(off_r)
                PT = [AR.get([512], BF16) for _ in range(3)]
                MbT = [AR.get([128], BF16) for _ in range(2)]
                for j_ in range(2):
                    S.op('dve', (lambda t_: (lambda e: e.memset(t_, 0.0)))(MbT[j_]), (), ['MbT%d' % j_])
                Mb = [AR.get([32], BF16) for _ in range(2)]
                ocg = [AR.get([4, 64], F32) for _ in range(2)]
                t1 = [AR.get([4, 64], F32) for _ in range(2)]
                t2 = [AR.get([4, 64], F32) for _ in range(2)]
                sm = [AR.get([96], F32) for _ in range(2)]
                impt = [AR.get([4, 32], F32) for _ in range(2)]
                pn = [0]

                def qslice(r, qt):
                    return qz[:, r, qt * 128:(qt + 1) * 128]

                def attend(qt, kts, KT, vsel, bS, bO, extra_fn):
                    def scores(ki):
                        kt = kts[ki]
                        bs_ = bS[ki % len(bS)]
                        extras = extra_fn(kt)
                        v4 = bank(bs_).rearrange("p (r t) -> p r t", r=4)
                        MM(v4, KT[:, kt * 128:(kt + 1) * 128], qz[:, :, qt * 128:(qt + 1) * 128],
                           True, not extras, ['KsT', 'KwT', 'qT'], [BK[bs_]])
                        for ei, (lh, rh, ks) in enumerate(extras):
                            MM(v4, lh, rh, False, ei == len(extras) - 1, ks, [BK[bs_]])

                    def rest(ki):
                        kt = kts[ki]
                        bs_ = bS[ki % len(bS)]
                        i = pn[0] % 3
                        pn[0] += 1
                        ACT(PT[i], bank(bs_), AF.Exp, [BK[bs_]], ['PT%d' % i])
                        for r in range(4):
                            MM(bank(bO)[:, r * 65:(r + 1) * 65], PT[i][:, r * 128:(r + 1) * 128], Vaug[:, kt, vsel, :],
                               ki == 0 and r == 0, ki == len(kts) - 1 and r == 3, ['PT%d' % i, 'Vaug'], [BK[bO]])

                    scores(0)
                    for ki in range(len(kts)):
                        if ki + 1 < len(kts):
                            scores(ki + 1)
                        rest(ki)

                for qt in range(16):
                    j = qt % 2
                    smj = sm[j]
                    sk = 'sm%d' % j
                    qs = slice(qt * 128, (qt + 1) * 128)
                    MM(bank(0)[0:127, :].rearrange("p (r t) -> p r t", r=4), kcmpT[:, :], qz[:, :, qs], True, False,
                       ['kcmpT', 'qT'], ['B0'])
                    MM(bank(0)[0:127, :].rearrange("p (r t) -> p r t", r=4), ident_b[0:127, 0:127],
                       cmpb[0:127, qs].unsqueeze(1).to_broadcast([127, 4, 128]), False, True, ['ident_b', 'cmpb'], ['B0'])
                    i = pn[0] % 3
                    pn[0] += 1
                    ACT(PT[i][0:127, :], bank(0)[0:127, :], AF.Exp, ['B0'], ['PT%d' % i])
                    for r in range(4):
                        MM(bank(1)[:, r * 97:(r + 1) * 97], PT[i][0:127, r * 128:(r + 1) * 128], vcaug[0:127, :],
                           r == 0, r == 3, ['PT%d' % i, 'vcaug'], ['B1'])
                    O = bank(1)[:, 0:388].rearrange("p (r c) -> p r c", r=4)
                    cb4 = causalb[:].unsqueeze(1).to_broadcast([128, 4, 128])
                    wb4 = winb[:].unsqueeze(1).to_broadcast([128, 4, 128])

                    def wmask(kt, qt=qt, cb4=cb4, wb4=wb4):
                        if kt == qt:
                            return [(ident_b[:], cb4, ['ident_b', 'causalb'])]
                        if kt == qt - 4:
                            return [(ident_b[:], wb4, ['ident_b', 'winb'])]
                        return []
                    attend(qt, list(range(max(0, qt - 4), qt + 1)), KwT, 1, [2, 3], 5, wmask)
                    TS(smj[:, 0:4], O[:, :, 64], 1e-30, None, ALU.max, None, ['B1'], [sk])
                    RCP(smj[:, 4:8], smj[:, 0:4], [sk], [sk])
                    TT(impt[j], O[:, :, 65:97], smj[:, 4:8].unsqueeze(2).to_broadcast([128, 4, 32]), ALU.mult, ['B1', sk], ['impt%d' % j])
                    RED(smj[:, 32:64], impt[j].rearrange("p r c -> p c r"), ALU.add, ['impt%d' % j], [sk])
                    TT(smj[:, 32:64], smj[:, 32:64], fbias[:, qt * 32:(qt + 1) * 32], ALU.add, [sk, 'fbias'], [sk])
                    S.op('dve', (lambda o_, i_: (lambda e: e.max(o_, i_)))(smj[:, 64:72], smj[:, 32:64]), [sk], [sk])
                    TS(Mb[j], smj[:, 32:64], smj[:, 71:72], -1.0, ALU.is_ge, ALU.add, [sk], ['Mb%d' % j])
                    pbt = bank(0).bitcast(BF16)
                    TR(pbt[0:32, 0:128], Mb[j], ident_b[:], ['Mb%d' % j, 'ident_b'], ['B0'])
                    CP(MbT[j][0:32, :], pbt[0:32, 0:128], ['B0'], ['MbT%d' % j], eng='dve')
                    gv = gates[:, qt, :].rearrange("p (r c) -> p r c", r=4)
                    TT(smj[:, 8:12], smj[:, 4:8], gv[:, :, 0], ALU.mult, [sk, 'gates'], [sk])
                    TT(ocg[j], O[:, :, 0:64], smj[:, 8:12].unsqueeze(2).to_broadcast([128, 4, 64]), ALU.mult, ['B1', sk], ['ocg%d' % j])

                    mb4 = MbT[j][:, :].unsqueeze(1).to_broadcast([128, 4, 128])

                    def smask(kt, qt=qt, j=j, cb4=cb4, mb4=mb4):
                        ex = [(expand[:, kt * 128:(kt + 1) * 128], mb4, ['expand', 'MbT%d' % j])]
                        if kt == qt:
                            ex.append((ident_b[:], cb4, ['ident_b', 'causalb']))
                        return ex
                    attend(qt, list(range(0, qt + 1)), KsT, 0, [6, 7], 4, smask)
                    Os = bank(4)[:, 0:260].rearrange("p (r c) -> p r c", r=4)
                    Ow = bank(5)[:, 0:260].rearrange("p (r c) -> p r c", r=4)
                    RCP(smj[:, 12:16], Os[:, :, 64], ['B4'], [sk])
                    TT(smj[:, 12:16], smj[:, 12:16], gv[:, :, 1], ALU.mult, [sk, 'gates'], [sk])
                    RCP(smj[:, 16:20], Ow[:, :, 64], ['B5'], [sk])
                    TT(smj[:, 16:20], smj[:, 16:20], gv[:, :, 2], ALU.mult, [sk, 'gates'], [sk])
                    TT(t1[j], Os[:, :, 0:64], smj[:, 12:16].unsqueeze(2).to_broadcast([128, 4, 64]), ALU.mult, ['B4', sk], ['t1_%d' % j])
                    TT(t2[j], Ow[:, :, 0:64], smj[:, 16:20].unsqueeze(2).to_broadcast([128, 4, 64]), ALU.mult, ['B5', sk], ['t2_%d' % j])
                    TT(t1[j], t1[j], ocg[j], ALU.add, ['t1_%d' % j, 'ocg%d' % j], ['t1_%d' % j])
                    TT(mixC[:, qt, :].rearrange("p (r d) -> p r d", r=4), t1[j], t2[j], ALU.add, ['t1_%d' % j, 't2_%d' % j], ['mixC%d' % qt])
                mkeys = ['mixC%d' % tt for tt in range(16)]
                if dbg == 'mixC%d_%d' % (l, g):
                    dump_tm(mixC, 0, 256, mkeys, g * 256)
                S.barrier(relay=RELAY)
                AR.reset(off_r)
                mixT = AR.get([2, 2048], BF16)
                out_proj(l, mixC, mkeys, 2, 512 + g * 256, mixT)

        for l in range(n_layers):
            hk = lambda k, tb: 'hT%d_%d' % (k, tb)
            if 'A' in mixers:
                S.barrier(relay=RELAY)
                AR.reset()
                zag = AR.get([16, 512], BF16)
                off_sqv = AR.off
                sqv = AR.get([16, 256], F32)
                st1 = AR.get([64], F32)
                st2 = AR.get([64], F32)
                st3 = AR.get([64], F32)
                mixA = AR.get([16, 256], BF16)
                atmp = [AR.get([256], F32) for _ in range(2)]
                wstage = AR.get([4, 128], F32)
                DMA('sp', wstage, Dr['WsT'][l].rearrange("p (g t) -> p g t", g=4), (), ['wstage'])
                TT(WsT[:].rearrange("p (g t) -> p g t", g=4), wstage,
                   causal01[:].unsqueeze(1).to_broadcast([128, 4, 128]), ALU.mult, ['wstage', 'causal01'], ['WsT'])
                DMA('sp', bsT[:], Dr['bsT'][l], (), ['bsT'])
                DMA('sp', lnbc[:, 0, :], Dr['a_lng'][l], (), ['lnbcA'])
                DMA('sp', lnbc[:, 1, :], Dr['a_lnb'][l], (), ['lnbcA'])
                wv, wk = wload(kchunks(Dr['w_inA'][l]), [8, 512])
                for tt in range(16):
                    b = tt % 4
                    for k in range(8):
                        MM(bank(b), hT[:, k, tt * 128:(tt + 1) * 128], wv[:, k, :], k == 0, k == 7,
                           [hk(k, tt // 4), wk], [BK[b]])
                    ACT(zag[:, tt, :], bank(b), AF.Gelu_apprx_tanh, [BK[b]], ['zag'])
                v4 = zag[:, :, 256:512].rearrange("p t (g d) -> p t g d", g=4)
                TT(sqv, zag[:, :, 256:512], zag[:, :, 256:512], ALU.mult, ['zag'], ['sqv'])
                RED(st1.rearrange("p (t g) -> p t g", t=16), v4, ALU.add, ['zag'], ['st1'])
                RED(st2.rearrange("p (t g) -> p t g", t=16), sqv.rearrange("p t (g d) -> p t g d", g=4), ALU.add, ['sqv'], ['st2'])
                TS(st1, st1, 1.0 / 64, None, ALU.mult, None, ['st1'], ['st1'])
                TT(st3, st1, st1, ALU.mult, ['st1'], ['st3'])
                STT(st2, st2, 1.0 / 64, st3, ALU.mult, ALU.subtract, ['st2', 'st3'], ['st2'])
                TS(st2, st2, 0.0, None, ALU.max, None, ['st2'], ['st2'])
                ACT(st2, st2, AF.Sqrt, ['st2', 'epsA'], ['st2'], bias=epsA[:, 0:1])
                RCP(st2, st2, ['st2'], ['st2'])
                mean_b = st1.rearrange("p (t g) -> p t g", t=16).unsqueeze(3).to_broadcast([128, 16, 4, 64])
                rstd_b = st2.rearrange("p (t g) -> p t g", t=16).unsqueeze(3).to_broadcast([128, 16, 4, 64])
                sq4 = sqv.rearrange("p t (g d) -> p t g d", g=4)
                TT(sq4, v4, mean_b, ALU.subtract, ['zag', 'st1'], ['sqv'])
                TT(sq4, sq4, rstd_b, ALU.mult, ['sqv', 'st2'], ['sqv'])
                gam_b = lnbc[:, 0, :].unsqueeze(1).to_broadcast([128, 16, 256])
                bet_b = lnbc[:, 1, :].unsqueeze(1).to_broadcast([128, 16, 256])
                TT(sqv, sqv, gam_b, ALU.mult, ['sqv', 'lnbcA'], ['sqv'])
                TT(zag[:, :, 256:512], sqv, bet_b, ALU.add, ['sqv', 'lnbcA'], ['zag'])
                bs_b = bsT[:, 0:4].unsqueeze(2).to_broadcast([128, 4, 64])
                for tt in range(16):
                    b = 4 + tt % 4
                    for g in range(4):
                        MM(bank(b)[:, g * 64:(g + 1) * 64], WsT[:, g * 128:(g + 1) * 128],
                           zag[:, tt, 256 + g * 64:256 + (g + 1) * 64], g == 0, g == 3, ['WsT', 'zag'], [BK[b]])
                    at = atmp[tt % 2]
                    ak = 'atmp%d' % (tt % 2)
                    TT(at.rearrange("p (g d) -> p g d", g=4), bank(b)[:, 0:256].rearrange("p (g d) -> p g d", g=4),
                       bs_b, ALU.add, [BK[b], 'bsT'], [ak])
                    TT(mixA[:, tt, :], at, zag[:, tt, 0:256], ALU.mult, [ak, 'zag'], ['mixA%d' % tt])
                if dbg == 'mixA%d' % l:
                    dst = AR.get([256], F32)
                    for tt in range(16):
                        CP(dst, mixA[:, tt, :], ['mixA%d' % tt], ['dst'])
                        DMA('sp', dbg_d[tt * 128:(tt + 1) * 128, 0:256], dst, ['dst'], ['dbg'])
                AR.reset(off_sqv)
                mixT = AR.get([2, 2048], BF16)
                out_proj(l, mixA, ['mixA%d' % tt for tt in range(16)], 2, 0, mixT, ['sqv'])

            if 'B' in mixers:
                mixer_B(l)
            if 'C' in mixers:
                mixer_C(l)

            S.barrier(relay=RELAY)
            layer_norm(l, 1, last=False)
            if dbg == 'ln1_%d' % l:
                break

            if do_ffn:
                S.barrier(relay=RELAY)
                AR.reset()
                DMA('sp', cwT[:], Dr['cwT'][l], (), ['cwT'])
                DMA('sp', cbT[:], Dr['cbT'][l], (), ['cbT'])
                gT = AR.get([max(FF_SPLIT), 2048], BF16)
                ctmp = [[AR.get([1024], F32) for _ in range(2)] for _ in range(2)]
                sgt = [AR.get([1024], BF16) for _ in range(2)]
                j0 = 0
                PS4 = [PSA, PSB]
                P4K = [BK[0:4], BK[4:8]]
                for part_n in FF_SPLIT:
                    wgrp = {}
                    for jl in range(part_n):
                        j = j0 + jl
                        if jl % 4 == 0:
                            ng = min(4, part_n - jl)
                            for part in range(2):
                                jj0 = part * 22 + j
                                wgrp[part] = wload(kchunks(Dr['w_up'][l][:, jj0 * 128:(jj0 + ng) * 128]), [8, ng * 128])
                        for part in range(2):
                            jj = part * 22 + j
                            wvf, wk = wgrp[part]
                            wv = wvf[:, :, (jl % 4) * 128:(jl % 4 + 1) * 128]
                            ps = PS4[part]
                            for tb in range(4):
                                for k in range(8):
                                    MM(ps[:, tb * 512:(tb + 1) * 512], wv[:, k, :], hT[:, k, tb * 512:(tb + 1) * 512],
                                       k == 0, k == 7, [wk, hk(k, tb)], [P4K[part][tb]])
                            for hf in range(2):
                                ct = ctmp[part][hf]
                                ck = 'ctmp%d_%d' % (part, hf)
                                o = hf * 1024
                                pk = P4K[part][2 * hf:2 * hf + 2]
                                pkp = P4K[part][max(0, 2 * hf - 1):2 * hf + 2]
                                ACT(ct, ps[:, o:o + 1024], AF.Identity, pk + ['cwT', 'cbT'], [ck],
                                    scale=cwT[:, jj * 3 + 2:jj * 3 + 3], bias=cbT[:, jj:jj + 1])
                                if hf == 0:
                                    STT(ct[:, 1:1024], ps[:, 0:1023], cwT[:, jj * 3 + 1:jj * 3 + 2], ct[:, 1:1024],
                                        ALU.mult, ALU.add, pk + ['cwT', ck], [ck])
                                    STT(ct[:, 2:1024], ps[:, 0:1022], cwT[:, jj * 3:jj * 3 + 1], ct[:, 2:1024],
                                        ALU.mult, ALU.add, pk + ['cwT', ck], [ck])
                                else:
                                    STT(ct, ps[:, o - 1:o + 1023], cwT[:, jj * 3 + 1:jj * 3 + 2], ct,
                                        ALU.mult, ALU.add, pkp + ['cwT', ck], [ck])
                                    STT(ct, ps[:, o - 2:o + 1022], cwT[:, jj * 3:jj * 3 + 1], ct,
                                        ALU.mult, ALU.add, pkp + ['cwT', ck], [ck])
                                if part == 0:
                                    ACT(sgt[hf], ct, AF.Silu, [ck], ['sgt%d' % hf])
                                else:
                                    TT(gT[:, jl, o:o + 1024], ct, sgt[hf], ALU.mult, [ck, 'sgt%d' % hf], ['gT%d' % jl])
                    nsl = (part_n + 3) // 4
                    wvs = []
                    for s in range(nsl):
                        r0 = (j0 + 4 * s) * 128
                        nr = min(4, part_n - 4 * s)
                        wvs.append(wload(kchunks(Dr['w_down'][l][r0:r0 + nr * 128, :]), [nr, 1024]))
                    bi2 = 0
                    for fb in range(8):
                        for tb in range(4):
                            b = bi2 % 8
                            bi2 += 1
                            for jl in range(part_n):
                                wv, wk = wvs[jl // 4]
                                MM(bank(b), wv[:, jl % 4, fb * 128:(fb + 1) * 128], gT[:, jl, tb * 512:(tb + 1) * 512],
                                   jl == 0, jl == part_n - 1, [wk, 'gT%d' % jl], [BK[b]])
                            xs = xT[:, fb, tb * 512:(tb + 1) * 512]
                            STT(xs, bank(b), drv[l][:, 24 + fb:25 + fb], xs, ALU.mult, ALU.add,
                                [BK[b], 'drv%d' % l, 'xT%d_%d' % (fb, tb)], ['xT%d_%d' % (fb, tb)])
                    j0 += part_n
                S.barrier(relay=RELAY)
            layer_norm(l, 2, last=(l == n_layers - 1))

        S.barrier(relay=RELAY)
        AR.reset()
        ost = [AR.get([1024], F32) for _ in range(4)]
        bi = 0
        for tt in range(16):
            o = ost[tt % 4]
            ok = 'ost%d' % (tt % 4)
            for cg in range(2):
                b = bi % 8
                bi += 1
                for q in range(4):
                    c = cg * 4 + q
                    TR(bank(b)[:, q * 128:(q + 1) * 128], xT[:, c, tt * 128:(tt + 1) * 128], ident_f[:],
                       ['xT%d_%d' % (c, tt // 4), 'ident_f'], [BK[b]])
                CP(o[:, cg * 512:(cg + 1) * 512], bank(b), [BK[b]], [ok], eng=EV())
            DMA('sp', out_d[tt * 128:(tt + 1) * 128, :], o, [ok], ['out'])
        S.finish()
        S.replay()
    return nc


_CACHE = {}


def kernel(**inputs):
    inp = {k: np.asarray(v) for k, v in inputs.items()}
    if 'nc' not in _CACHE:
        _CACHE['nc'] = build()
    nc = _CACHE['nc']
    w = _prep_weights(inp)
    tb = _tables()
    in_maps = []
    for b in range(8):
        m = dict(w)
        m.update(tb)
        m['x'] = np.ascontiguousarray(inp['x'][b], dtype=np.float32)
        m['cT'] = _pad64(inp['c'][b].reshape(8, 128).T)
        in_maps.append(m)
    res = run_bass_kernel_spmd(nc, in_maps, core_ids=list(range(8)))
    out = np.stack([np.asarray(r['out'], dtype=np.float32) for r in res.results], 0)
    return out
```

```python
import math
from contextlib import ExitStack
import numpy as np
import concourse.bass as bass
import concourse.mybir as mybir
from concourse.bass_utils import run_bass_kernel_spmd

F32 = mybir.dt.float32
BF16 = mybir.dt.bfloat16
AF = mybir.ActivationFunctionType
ALU = mybir.AluOpType
AX = mybir.AxisListType

ENGS = ['pe', 'dve', 'act', 'pool', 'sp']
NRING = 8
DEPTH = 2
SEQ = 2048
DM = 1024
ALPHA = (2 * DEPTH) ** 0.25
LN_EPS = 1e-5
NEGB = -30000.0
FF_SPLIT = [6, 6, 5, 5]


class Sched:
    def __init__(self, nc, stack):
        self.nc = nc
        self.prog = {e: [] for e in ENGS}
        self.cnt = {e: 0 for e in ENGS}
        self.seen = {e: {} for e in ENGS}
        self.lastw = {}
        self.readers = {}
        self.sems = {}
        self.semval = {}
        self.relay_fn = None
        for e in ENGS:
            self.sems[e] = stack.enter_context(nc.semaphore("s_" + e))
        self.dma_n = {}
        for q in ['sp', 'act', 'pool']:
            self.dma_n[q] = 0
            for j in range(NRING):
                nm = "d_%s%d" % (q, j)
                self.sems[nm] = stack.enter_context(nc.semaphore(nm))

    def _deps(self, eng, reads, writes, is_dma=False):
        deps = []
        for k in reads:
            t = self.lastw.get(k)
            if t is not None:
                deps.append((t, 'raw'))
        for k in writes:
            t = self.lastw.get(k)
            if t is not None:
                deps.append((t, 'waw'))
            for s, v in self.readers.get(k, {}).items():
                deps.append(((s, v), 'war'))
        need = {}
        for (s, v), kind in deps:
            if s == eng and not is_dma:
                if kind != 'raw' or eng == 'pe':
                    continue
            if self.seen[eng].get(s, 0) >= v:
                continue
            if need.get(s, 0) < v:
                need[s] = v
        return need

    def _emit_waits(self, eng, need):
        if eng in ('sp', 'pool') and 'pe' in need and self.relay_fn is not None:
            need = dict(need)
            v = need.pop('pe')
            self.seen[eng]['pe'] = v
            R = 'dve'
            if self.seen[R].get('pe', 0) < v:
                self.prog[R].append(('wait', 'pe', v))
                self.seen[R]['pe'] = v
            lr = getattr(self, 'last_relay', 0)
            if lr and self.seen[R].get(R, 0) < lr:
                self.prog[R].append(('wait', R, lr))
                self.seen[R][R] = lr
            self.cnt[R] += 1
            self.last_relay = self.cnt[R]
            self.semval[R] = self.cnt[R]
            self.prog[R].append(('op', self.relay_fn, R, 1))
            if self.seen[eng].get(R, 0) < self.cnt[R]:
                need[R] = max(need.get(R, 0), self.cnt[R])
        for s, v in need.items():
            self.prog[eng].append(('wait', s, v))
            self.seen[eng][s] = v

    def _commit(self, tok, reads, writes):
        for k in writes:
            self.lastw[k] = tok
            self.readers[k] = {}
        for k in reads:
            d = self.readers.setdefault(k, {})
            if d.get(tok[0], 0) < tok[1]:
                d[tok[0]] = tok[1]

    def relay_readers(self, keys):
        if self.relay_fn is None:
            return
        v = 0
        for k in keys:
            d = self.readers.get(k)
            if d and 'pe' in d:
                v = max(v, d['pe'])
        if v == 0:
            return
        R = 'dve'
        if self.seen[R].get('pe', 0) < v:
            self.prog[R].append(('wait', 'pe', v))
            self.seen[R]['pe'] = v
        lr = getattr(self, 'last_relay', 0)
        if lr and self.seen[R].get(R, 0) < lr:
            self.prog[R].append(('wait', R, lr))
            self.seen[R][R] = lr
        self.cnt[R] += 1
        self.semval[R] = self.cnt[R]
        self.last_relay = self.cnt[R]
        self.prog[R].append(('op', self.relay_fn, R, 1))
        for k in keys:
            d = self.readers.get(k)
            if d and 'pe' in d:
                d.pop('pe')
                d[R] = max(d.get(R, 0), self.cnt[R])

    def op(self, eng, fn, reads=(), writes=()):
        need = self._deps(eng, reads, writes)
        self._emit_waits(eng, need)
        self.cnt[eng] += 1
        tok = (eng, self.cnt[eng])
        self.semval[eng] = self.cnt[eng]
        self.prog[eng].append(('op', fn, eng, 1))
        self._commit(tok, reads, writes)
        return tok

    def dma_multi(self, q, pairs, reads=(), writes=()):
        i = self.dma_n[q]
        self.dma_n[q] += 1
        slot = "d_%s%d" % (q, i % NRING)
        prev = self.semval.get(slot, 0)
        val = prev + 16 * len(pairs)
        need = self._deps(q, reads, writes, is_dma=True)
        if prev > 0 and self.seen[q].get(slot, 0) < prev:
            need[slot] = max(need.get(slot, 0), prev)
        self._emit_waits(q, need)
        for out, in_ in pairs:
            self.prog[q].append(('op', (lambda o_, i_: (lambda e: e.dma_start(out=o_, in_=i_)))(out, in_), slot, 16))
        self.semval[slot] = val
        tok = (slot, val)
        self._commit(tok, reads, writes)
        return tok

    def dma(self, q, out, in_, reads=(), writes=()):
        return self.dma_multi(q, [(out, in_)], reads, writes)

    def _wait_all(self, e):
        need = {}
        for s_, v in self.semval.items():
            if s_ == e:
                continue
            if self.seen[e].get(s_, 0) < v:
                need[s_] = v
        self._emit_waits(e, need)

    def barrier(self, engs=('pe', 'dve', 'act', 'sp'), relay=None):
        if relay is None or 'sp' not in engs:
            for e in engs:
                self._wait_all(e)
            return
        snap = dict(self.semval)
        self._wait_all('sp')
        tok = self.dma('sp', relay[0], relay[1], (), ['__bar'])
        for e in engs:
            if e == 'sp':
                continue
            self._emit_waits(e, {tok[0]: tok[1]} if self.seen[e].get(tok[0], 0) < tok[1] else {})
            for s_, v in snap.items():
                if s_ != e and self.seen[e].get(s_, 0) < v:
                    self.seen[e][s_] = v

    def finish(self):
        self.barrier(engs=('sp',))

    def replay(self):
        nc = self.nc
        sems = self.sems
        prog = self.prog

        def run(e, name):
            for it in prog[name]:
                if it[0] == 'wait':
                    e.wait_ge(sems[it[1]], it[2])
                else:
                    it[1](e).then_inc(sems[it[2]], it[3])

        with nc.Block() as block:
            @block.tensor
            def _(e):
                run(e, 'pe')

            @block.vector
            def _(e):
                run(e, 'dve')

            @block.scalar
            def _(e):
                run(e, 'act')

            @block.gpsimd
            def _(e):
                run(e, 'pool')

            @block.sync
            def _(e):
                run(e, 'sp')


def _pad64(a):
    a = np.asarray(a, dtype=np.float32)
    out = np.zeros(a.shape[:-1] + (64,), np.float32)
    out[..., :a.shape[-1]] = a
    return out


def _tables():
    t = {}
    half = 32
    inv = np.power(np.float32(10000.0), -np.arange(half, dtype=np.float32) / np.float32(half)).astype(np.float32)
    pos = np.arange(SEQ, dtype=np.float32)
    ang = pos[:, None] * inv[None, :]
    cos = np.cos(ang).astype(np.float32).T
    sin = np.sin(ang).astype(np.float32).T
    cosT = np.concatenate([cos, cos, cos, cos], 0)
    sinT = np.concatenate([-sin, sin, -sin, sin], 0)
    t['cosT'] = np.ascontiguousarray(cosT)
    t['sinT'] = np.ascontiguousarray(sinT)
    H = 4
    L = 128
    lg = np.log1p(-np.exp2(-5.0 - np.arange(H, dtype=np.float32))).astype(np.float32)
    idx = np.arange(L, dtype=np.float32)
    diff = idx[:, None] - idx[None, :]
    dec = np.where(diff >= 0, np.exp(lg[:, None, None] * np.maximum(diff, 0.0)), 0.0).astype(np.float32)
    t['decayT'] = np.ascontiguousarray(np.transpose(dec, (2, 0, 1)) * np.float32(0.125)).reshape(128, 512)
    xi = np.exp(lg[:, None] * (idx + 1.0)).astype(np.float32)
    zeta = np.exp(lg[:, None] * (L - 1.0 - idx)).astype(np.float32)
    xiT = np.zeros((128, 2, 128), np.float32)
    for p in range(2):
        for hh in range(2):
            xiT[hh * 64:(hh + 1) * 64, p, :] = xi[2 * p + hh][None, :]
    t['xiT'] = xiT.reshape(128, 256)
    t['zeta'] = _pad64(zeta.T * np.float32(0.125))
    cd = np.exp(lg * L).astype(np.float32)
    cdv = np.zeros((128, 2), np.float32)
    for p in range(2):
        for hh in range(2):
            cdv[hh * 64:(hh + 1) * 64, p] = cd[2 * p + hh]
    t['cdv'] = _pad64(cdv)
    key = np.arange(SEQ)
    ex = np.zeros((128, SEQ), np.float32)
    ex[key // 64, key] = -NEGB
    t['expand'] = ex
    kk = np.arange(128)[:, None]
    tt = np.arange(128)[None, :]
    t['causalb'] = np.where(kk > tt, NEGB, 0.0).astype(np.float32)
    t['winb'] = np.where(kk <= tt, NEGB, 0.0).astype(np.float32)
    t['identf'] = np.eye(128, dtype=np.float32)
    t['causal01'] = np.where(tt >= kk, 1.0, 0.0).astype(np.float32)
    k127 = np.arange(128)[:, None]
    tpos = np.arange(SEQ)[None, :]
    t['cmpb'] = np.where(16 * k127 + 31 > tpos, NEGB, 0.0).astype(np.float32)
    fb = np.zeros((128, 16, 32), np.float32)
    for qt in range(16):
        tq = qt * 128 + np.arange(128)
        cur = tq // 64
        blk = np.arange(32)
        future = blk[None, :] > cur[:, None]
        forced = (blk[None, :] == 0) | (blk[None, :] == cur[:, None]) | (blk[None, :] == cur[:, None] - 1)
        fb[:, qt, :] = np.where(forced, 1e30, np.where(future, -1e30, 0.0))
    t['fbias'] = fb.reshape(128, 512)
    ov = np.zeros((128, 32), np.float32)
    c0 = np.arange(127)[:, None] * 16
    s0 = np.arange(32)[None, :] * 64
    ov[:127] = np.clip(np.minimum(c0 + 32, s0 + 64) - np.maximum(c0, s0), 0, None) / 32.0
    t['ovl'] = _pad64(ov)
    return t


TABLE_SHAPES = {'cosT': [128, 2048], 'sinT': [128, 2048], 'decayT': [128, 512], 'xiT': [128, 256],
                'zeta': [128, 64], 'cdv': [128, 64], 'expand': [128, 2048], 'causalb': [128, 128],
                'winb': [128, 128], 'causal01': [128, 128], 'identf': [128, 128], 'cmpb': [128, 2048], 'fbias': [128, 512], 'ovl': [128, 64]}


def _prep_weights(inp):
    w = {}
    f = lambda a: np.ascontiguousarray(a, dtype=np.float32)
    w_in = inp['w_in']
    L = DEPTH
    w['w_ada'] = f(inp['w_ada'])
    w['b_adaT'] = _pad64(inp['b_ada'].reshape(L, 48, 128).transpose(0, 2, 1))
    w['w_inA'] = f(w_in[:, :, 0:512])
    cols = []
    for p in range(2):
        for base in (512, 768):
            hd = np.arange(128)
            h = 2 * p + hd // 64
            d = hd % 64
            cols.append(base + h * 64 + d)
            cols.append(base + h * 64 + (d + 32) % 64)
    cols = np.concatenate(cols)
    w['w_inBf'] = f(w_in[:, :, cols])
    cols = []
    for p in range(2):
        cols.append(1024 + p * 128 + np.arange(128))
        cols.append(1280 + p * 128 + np.arange(128))
    w['w_inBt'] = f(w_in[:, :, np.concatenate(cols)])
    cols = []
    for g in range(2):
        cols.append(1536 + g * 256 + np.arange(256))
        ks = 2304 + g * 64 + np.arange(64)
        kw = 2560 + g * 64 + np.arange(64)
        cols += [ks, ks, kw, kw]
        cols.append(2048 + g * 64 + np.arange(64))
        cols.append(2176 + g * 64 + np.arange(64))
    w['w_inCf'] = f(w_in[:, :, np.concatenate(cols)])
    cols = []
    for g in range(2):
        cols.append(2432 + g * 64 + np.arange(64))
        cols.append(2688 + g * 64 + np.arange(64))
        cols.append(2816 + g * 12 + np.arange(12))
    w['w_inCt'] = f(w_in[:, :, np.concatenate(cols)])
    w['WsT'] = f(inp['a_ws'].transpose(0, 3, 1, 2).reshape(L, 128, 512))
    w['bsT'] = _pad64(inp['a_bs'].transpose(0, 2, 1))
    w['a_lng'] = f(np.broadcast_to(inp['a_ln_g'].reshape(L, 1, 256), (L, 128, 256)))
    w['a_lnb'] = f(np.broadcast_to(inp['a_ln_b'].reshape(L, 1, 256), (L, 128, 256)))
    w['b_gng'] = f(np.broadcast_to(inp['b_gn_g'].reshape(L, 1, 256), (L, 128, 256)))
    w['b_gnb'] = f(np.broadcast_to(inp['b_gn_b'].reshape(L, 1, 256), (L, 128, 256)))
    posT = np.concatenate([inp['c_pos_k'].transpose(0, 2, 1), inp['c_pos_v'].transpose(0, 2, 1)], 1)
    w['posT'] = _pad64(posT)
    w1k = inp['c_w1_k'].reshape(L, 32, 64, 64).transpose(0, 2, 1, 3)
    w1v = inp['c_w1_v'].reshape(L, 32, 64, 64).transpose(0, 2, 1, 3)
    w['w1s'] = f(np.concatenate([w1k, w1v], 1).reshape(L, 128, 2048))
    w['w2k'] = f(np.concatenate([inp['c_w2_k'], inp['c_w2_k']], 2))
    w['w2v'] = f(inp['c_w2_v'])
    w['w_out'] = f(inp['w_out'])
    w['lnpk'] = _pad64(np.concatenate([inp[k].reshape(L, 8, 128).transpose(0, 2, 1)
                                       for k in ('ln1_g', 'ln1_b', 'ln2_g', 'ln2_b')], 2))
    w['w_up'] = f(inp['w_up'])
    w['cwT'] = f(inp['conv_w'].reshape(L, 3, 44, 128).transpose(0, 3, 2, 1).reshape(L, 128, 132))
    w['cbT'] = _pad64(inp['conv_b'].reshape(L, 44, 128).transpose(0, 2, 1))
    w['w_down'] = f(inp['w_down'])
    return w


W_SHAPES = {'w_ada': [2, 1024, 6144], 'b_adaT': [2, 128, 64], 'w_inA': [2, 1024, 512], 'w_inBf': [2, 1024, 1024],
            'w_inBt': [2, 1024, 512], 'w_inCf': [2, 1024, 1280], 'w_inCt': [2, 1024, 280], 'WsT': [2, 128, 512],
            'bsT': [2, 128, 64], 'a_lng': [2, 128, 256], 'a_lnb': [2, 128, 256], 'b_gng': [2, 128, 256], 'b_gnb': [2, 128, 256],
            'posT': [2, 128, 64], 'w1s': [2, 128, 2048], 'w2k': [2, 64, 128], 'w2v': [2, 64, 64],
            'w_out': [2, 1024, 1024], 'lnpk': [2, 128, 64],
            'w_up': [2, 1024, 5632], 'cwT': [2, 128, 132], 'cbT': [2, 128, 64], 'w_down': [2, 2816, 1024]}


def build(n_layers=DEPTH, mixers=('A', 'B', 'C'), do_ffn=True, dbg=None):
    nc = bass.Bass("TRN2", target_bir_lowering=False)
    Dr = {}
    Dr['x'] = nc.dram_tensor("x", [SEQ, DM], F32, kind="ExternalInput").ap()
    Dr['cT'] = nc.dram_tensor("cT", [128, 64], F32, kind="ExternalInput").ap()
    for k, shp in W_SHAPES.items():
        Dr[k] = nc.dram_tensor(k, shp, F32, kind="ExternalInput").ap()
    for k, shp in TABLE_SHAPES.items():
        Dr[k] = nc.dram_tensor(k, shp, F32, kind="ExternalInput").ap()
    out_d = nc.dram_tensor("out", [SEQ, DM], F32, kind="ExternalOutput").ap()
    dbg_d = None
    if dbg is not None:
        dbg_d = nc.dram_tensor("dbg", [SEQ, DM], F32, kind="ExternalOutput").ap()

    st = ExitStack()
    with st:
        S = Sched(nc, st)
        T = lambda name, shape, dt=F32: st.enter_context(nc.sbuf_tensor("s_" + name, shape, dt))
        xT = T("xT", [128, 8, SEQ], F32)
        hT = T("hT", [128, 8, SEQ], BF16)
        WR = T("WR", [128, 4, 4096], BF16)
        AW = 51 * 256
        ARENA = T("ARENA", [128, AW], F32)
        PSA = st.enter_context(nc.psum_tensor("PSA", [128, 2048], F32))
        PSB = st.enter_context(nc.psum_tensor("PSB", [128, 2048], F32))

        def bank(i):
            t = PSA if i < 4 else PSB
            return t[:, (i % 4) * 512:(i % 4 + 1) * 512]

        BK = ['B%d' % i for i in range(8)]

        class Arena:
            def __init__(self):
                self.off = 0

            def reset(self, off=0):
                self.off = off

            def get(self, shape, dt=F32):
                n = int(np.prod(shape))
                nb = n * (2 if dt == BF16 else 4)
                w0 = self.off // 4
                w1 = w0 + (nb + 3) // 4
                assert w1 <= AW, ("arena overflow", w1 * 4, AW * 4)
                self.off = w1 * 4
                ap = ARENA[:, w0:w1]
                if dt == BF16:
                    ap = ap.bitcast(BF16)
                ap = ap[:, 0:n]
                if len(shape) == 2:
                    ap = ap.rearrange("p (a b) -> p a b", a=shape[0])
                elif len(shape) == 3:
                    ap = ap.rearrange("p (a b c) -> p a b c", a=shape[0], b=shape[1])
                return ap

        AR = Arena()

        def MM(out, lhsT, rhs, start, stop, rd, wr):
            S.op('pe', lambda e: e.matmul(out, lhsT=lhsT, rhs=rhs, start=start, stop=stop, skip_group_check=True), rd, wr)

        def TR(out, in_, ident, rd, wr):
            S.op('pe', lambda e: e.transpose(out, in_, ident), rd, wr)

        def ACT(out, in_, func, rd, wr, scale=1.0, bias=None):
            if bias is None:
                S.op('act', lambda e: e.activation(out, in_, func, scale=scale), rd, wr)
            else:
                S.op('act', lambda e: e.activation(out, in_, func, bias=bias, scale=scale), rd, wr)

        def TT(out, in0, in1, op, rd, wr, eng='dve'):
            S.op(eng, lambda e: e.tensor_tensor(out, in0, in1, op), rd, wr)

        def TS(out, in0, s1, s2, op0, op1, rd, wr, eng='dve'):
            if s2 is None:
                S.op(eng, lambda e: e.tensor_scalar(out, in0, s1, None, op0=op0), rd, wr)
            else:
                S.op(eng, lambda e: e.tensor_scalar(out, in0, s1, s2, op0=op0, op1=op1), rd, wr)

        def STT(out, in0, scalar, in1, op0, op1, rd, wr):
            S.op('dve', lambda e: e.scalar_tensor_tensor(out, in0, scalar, in1, op0=op0, op1=op1), rd, wr)

        def CP(out, in_, rd, wr, eng='dve'):
            if eng == 'act':
                S.op('act', lambda e: e.activation(out, in_, AF.Identity), rd, wr)
            else:
                S.op(eng, lambda e: e.tensor_copy(out, in_), rd, wr)

        def RED(out, in_, op, rd, wr):
            S.op('dve', lambda e: e.tensor_reduce(out, in_, axis=AX.X, op=op), rd, wr)

        def RCP(out, in_, rd, wr):
            S.op('dve', lambda e: e.reciprocal(out, in_), rd, wr)

        evt = [0]

        def EV():
            evt[0] += 1
            return 'act' if evt[0] % 2 else 'dve'

        def DMA(q, out, in_, rd, wr):
            pieces = []

            def split(o, i):
                shp = tuple(o.shape)
                assert tuple(i.shape) == shp, (shp, i.shape)
                if len(shp) == 3:
                    for a in range(shp[1]):
                        split(o[:, a, :], i[:, a, :])
                elif len(shp) == 2 and shp[1] > 512:
                    for c0 in range(0, shp[1], 512):
                        c1 = min(shp[1], c0 + 512)
                        pieces.append((o[:, c0:c1], i[:, c0:c1]))
                else:
                    pieces.append((o, i))
            split(out, in_)
            S.dma_multi(q, pieces, rd, wr)

        ident_f = T("ident_f", [128, 128], F32)
        ident_b = T("ident_b", [128, 128], BF16)
        onesm = T("onesm", [128, 128], BF16)
        epsA = T("epsA", [128, 2], F32)
        condT = T("condT", [128, 64], F32)
        modT = [T("modT%d" % l, [128, 48], F32) for l in range(DEPTH)]
        drv = [T("drv%d" % l, [128, 64], F32) for l in range(DEPTH)]
        lnp = [T("lnp%d" % l, [128, 64], F32) for l in range(DEPTH)]
        cwT = T("cwT", [128, 132], F32)
        cbT = T("cbT", [128, 64], F32)
        decayT = T("decayT", [128, 512], F32)
        xiT = T("xiT", [128, 256], F32)
        zeta = T("zeta", [128, 64], F32)
        cdv = T("cdv", [128, 64], F32)
        expand = T("expand", [128, 2048], BF16)
        causalb = T("causalb", [128, 128], BF16)
        winb = T("winb", [128, 128], BF16)
        cmpb = T("cmpb", [128, 2048], BF16)
        fbias = T("fbias", [128, 512], F32)
        causal01 = T("causal01", [128, 128], F32)
        ovl = T("ovl", [128, 64], F32)
        WsT = T("WsT", [128, 512], BF16)
        bsT = T("bsT", [128, 64], F32)
        lnbc = T("lnbc", [128, 4, 256], F32)
        posT = T("posT", [128, 64], F32)
        w1s = T("w1s", [128, 2048], BF16)
        w2k = T("w2k", [64, 128], BF16)
        w2v = T("w2v", [64, 64], BF16)

        DMA('sp', ident_f[:], Dr['identf'], (), ['ident_f'])
        S.op('dve', lambda e: e.tensor_copy(ident_b[:], ident_f[:]), ['ident_f'], ['ident_b'])
        S.op('dve', lambda e: e.memset(onesm[:], 1.0 / 1024.0), (), ['onesm'])
        S.op('dve', lambda e: e.memset(epsA[:, 0:1], LN_EPS), (), ['epsA'])
        S.op('dve', lambda e: e.memset(epsA[:, 1:2], LN_EPS / (ALPHA * ALPHA)), (), ['epsA'])
        barscr = T("barscr", [128, 64], F32)
        RELAY = (barscr[:], Dr['zeta'])
        rlscr = T("rlscr", [128, 2], F32)
        S.relay_fn = lambda e: e.memset(rlscr[:, 0:1], 0.0)
        DMA('sp', condT[:], Dr['cT'], (), ['condT'])
        for nm, tl in (('decayT', decayT), ('xiT', xiT), ('zeta', zeta), ('cdv', cdv), ('fbias', fbias), ('causal01', causal01), ('ovl', ovl)):
            DMA('sp', tl[:], Dr[nm], (), [nm])
        for nm, tl in (('causalb', causalb), ('winb', winb)):
            DMA('pool', tl[:], Dr[nm], (), [nm])
        for nm, tl in (('expand', expand), ('cmpb', cmpb)):
            DMA('pool', tl[:].rearrange("p (a b) -> p a b", a=4), Dr[nm].rearrange("p (a b) -> p a b", a=4), (), [nm])
        ACT(condT[:], condT[:], AF.Silu, ['condT'], ['condT'])

        wr_n = [0]

        def wload(src_ap, shape):
            S.relay_readers(['WR%d' % j_ for j_ in range(4)])
            i = wr_n[0] % 4
            wr_n[0] += 1
            n = int(np.prod(shape))
            v = WR[:, i, 0:n]
            if len(shape) == 2:
                v = v.rearrange("p (a b) -> p a b", a=shape[0])
            key = 'WR%d' % i
            DMA('pool', v, src_ap, (), [key])
            return v, key

        def kchunks(ap2d):
            return ap2d.rearrange("(k p) n -> p k n", p=128)

        AR.reset()
        ada_buf = [AR.get([8, 512], BF16) for _ in range(2)]
        condTb = AR.get([8, 8], BF16)
        CP(condTb, condT[:, 0:8].unsqueeze(2).to_broadcast([128, 8, 8]), ['condT'], ['condTb'])
        for l in range(n_layers):
            for blk in range(12):
                buf = ada_buf[blk % 2]
                bk = 'ada%d' % (blk % 2)
                DMA('pool', buf, kchunks(Dr['w_ada'][l][:, blk * 512:(blk + 1) * 512]), (), [bk])
                for jj in range(4):
                    j = blk * 4 + jj
                    for k in range(8):
                        MM(bank(0)[:, j * 8:j * 8 + 8], buf[:, k, jj * 128:(jj + 1) * 128], condTb[:, k, :],
                           k == 0, k == 7, [bk, 'condTb'], ['B0'])
            btmp = AR.get([64], F32) if l == 0 else btmp
            DMA('sp', btmp, Dr['b_adaT'][l], (), ['btmp'])
            TT(modT[l][:], bank(0)[:, 0:384].rearrange('p (j r) -> p j r', r=8)[:, :, 0], btmp[:, 0:48], ALU.add, ['B0', 'btmp'], ['modT%d' % l])
            DMA('sp', lnp[l][:], Dr['lnpk'][l], (), ['lnp%d' % l])
        for l in range(n_layers):
            m = modT[l]
            d = drv[l]
            mk, dk = 'modT%d' % l, 'drv%d' % l
            TS(d[:, 0:8], m[:, 8:16], 1.0, None, ALU.add, None, [mk], [dk])
            TS(d[:, 8:16], m[:, 16:24], 1.0 / ALPHA, None, ALU.mult, None, [mk], [dk])
            TS(d[:, 16:24], m[:, 32:40], 1.0, None, ALU.add, None, [mk], [dk])
            TS(d[:, 24:32], m[:, 40:48], 1.0 / ALPHA, None, ALU.mult, None, [mk], [dk])
            TT(d[:, 32:40], lnp[l][:, 0:8], d[:, 16:24], ALU.mult, ['lnp%d' % l, dk], [dk])
            TT(d[:, 40:48], lnp[l][:, 8:16], d[:, 16:24], ALU.mult, ['lnp%d' % l, dk], [dk])
            TT(d[:, 40:48], d[:, 40:48], m[:, 24:32], ALU.add, [dk, mk], [dk])
        for l in range(n_layers - 1):
            d, dn = drv[l], drv[l + 1]
            dk, dnk = 'drv%d' % l, 'drv%d' % (l + 1)
            TT(d[:, 48:56], lnp[l][:, 16:24], dn[:, 0:8], ALU.mult, ['lnp%d' % l, dnk, dk], [dk])
            TT(d[:, 56:64], lnp[l][:, 24:32], dn[:, 0:8], ALU.mult, ['lnp%d' % l, dnk, dk], [dk])
            TT(d[:, 56:64], d[:, 56:64], modT[l + 1][:, 0:8], ALU.add, [dk, 'modT%d' % (l + 1)], [dk])

        S.barrier(relay=RELAY)
        AR.reset()
        xst = [AR.get([1024], F32) for _ in range(8)]
        bi = 0
        for tb in (range(4) if dbg != 'skip_xload' else []):
            for q in range(4):
                tt = tb * 4 + q
                DMA('sp', xst[tt % 8], Dr['x'][tt * 128:(tt + 1) * 128, :], (), ['xst%d' % (tt % 8)])
            for c in range(8):
                b = bi % 8
                bi += 1
                for q in range(4):
                    tt = tb * 4 + q
                    TR(bank(b)[:, q * 128:(q + 1) * 128], xst[tt % 8][:, c * 128:(c + 1) * 128], ident_f[:],
                       ['xst%d' % (tt % 8), 'ident_f'], [BK[b]])
                ACT(xT[:, c, tb * 512:(tb + 1) * 512], bank(b), AF.Identity, [BK[b]], ['xT%d_%d' % (c, tb)])
                if dbg != 'no_ts':
                    TS(hT[:, c, tb * 512:(tb + 1) * 512], xT[:, c, tb * 512:(tb + 1) * 512], drv[0][:, c:c + 1], modT[0][:, c:c + 1],
                       ALU.mult, ALU.add, ['xT%d_%d' % (c, tb), 'drv0', 'modT0'], ['hT%d_%d' % (c, tb)])

        def out_proj(l, mixtm, mixkeys, nch, row0, mixT, alias=()):
            nonlocal_bi = [0]
            for c in range(nch):
                for tb in range(4):
                    b = 4 + (nonlocal_bi[0] % 4)
                    nonlocal_bi[0] += 1
                    pb = bank(b).bitcast(BF16)
                    for q in range(4):
                        tt = tb * 4 + q
                        TR(pb[:, q * 128:(q + 1) * 128], mixtm[:, tt, c * 128:(c + 1) * 128], ident_b[:],
                           [mixkeys[tt], 'ident_b'], [BK[b]])
                    CP(mixT[:, c, tb * 512:(tb + 1) * 512], pb[:, 0:512], [BK[b]], ['mixT%d_%d' % (c, tb)] + list(alias), eng=EV())
            wv, wk = wload(kchunks(Dr['w_out'][l][row0:row0 + nch * 128, :]), [nch, 1024])
            for fb in range(8):
                for tb in range(4):
                    b = nonlocal_bi[0] % 4
                    nonlocal_bi[0] += 1
                    for c in range(nch):
                        MM(bank(b), wv[:, c, fb * 128:(fb + 1) * 128], mixT[:, c, tb * 512:(tb + 1) * 512],
                           c == 0, c == nch - 1, [wk, 'mixT%d_%d' % (c, tb)], [BK[b]])
                    xs = xT[:, fb, tb * 512:(tb + 1) * 512]
                    STT(xs, bank(b), drv[l][:, 8 + fb:9 + fb], xs, ALU.mult, ALU.add,
                        [BK[b], 'drv%d' % l, 'xT%d_%d' % (fb, tb)], ['xT%d_%d' % (fb, tb)])

        def layer_norm(l, which, last):
            goff = 0 if which == 1 else 16
            aoff = 32 if which == 1 else 48
            AR.reset()
            xb = [AR.get([512], BF16) for _ in range(3)]
            sq = [AR.get([512], BF16) for _ in range(3)]
            rstd = [AR.get([512], F32) for _ in range(2)]
            nmr = [AR.get([512], F32) for _ in range(2)]
            tmp = [AR.get([512], F32) for _ in range(3)]
            n = 0
            for tb in range(4):
                bm, be = 0 + 2 * (tb % 2), 1 + 2 * (tb % 2)
                for c in range(8):
                    i = n % 3
                    n += 1
                    xs = xT[:, c, tb * 512:(tb + 1) * 512]
                    xk = 'xT%d_%d' % (c, tb)
                    ACT(sq[i], xs, AF.Square, [xk], ['lsq%d' % i])
                    CP(xb[i], xs, [xk], ['lxb%d' % i], eng='dve')
                    MM(bank(bm), onesm[:], xb[i], c == 0, c == 7, ['onesm', 'lxb%d' % i], [BK[bm]])
                    MM(bank(be), onesm[:], sq[i], c == 0, c == 7, ['onesm', 'lsq%d' % i], [BK[be]])
                r = tb % 2
                rk, nk = 'lrstd%d' % r, 'lnmr%d' % r
                ACT(nmr[r], bank(bm), AF.Square, [BK[bm]], [nk])
                TT(rstd[r], bank(be), nmr[r], ALU.subtract, [BK[be], nk], [rk])
                TS(rstd[r], rstd[r], 0.0, None, ALU.max, None, [rk], [rk])
                ACT(rstd[r], rstd[r], AF.Sqrt, [rk, 'epsA'], [rk], bias=epsA[:, 1:2])
                RCP(rstd[r], rstd[r], [rk], [rk])
                STT(nmr[r], bank(bm), -1.0, rstd[r], ALU.mult, ALU.mult, [BK[bm], rk], [nk])
                for c in range(8):
                    i = n % 3
                    n += 1
                    xs = xT[:, c, tb * 512:(tb + 1) * 512]
                    xk = 'xT%d_%d' % (c, tb)
                    tk = 'ltmp%d' % i
                    TT(tmp[i], xs, rstd[r], ALU.mult, [xk, rk], [tk])
                    TT(tmp[i], tmp[i], nmr[r], ALU.add, [tk, nk], [tk])
                    ACT(xs, tmp[i], AF.Identity, [tk, 'lnp%d' % l], [xk],
                        scale=lnp[l][:, goff + c:goff + c + 1], bias=lnp[l][:, goff + 8 + c:goff + 9 + c])
                    if not last:
                        ACT(hT[:, c, tb * 512:(tb + 1) * 512], tmp[i], AF.Identity, [tk, 'drv%d' % l], ['hT%d_%d' % (c, tb)],
                            scale=drv[l][:, aoff + c:aoff + c + 1], bias=drv[l][:, aoff + 8 + c:aoff + 9 + c])

        def dump_tm(src, c0, ncol, keys, dcol):
            dst = AR.get([ncol], F32)
            for tt in range(16):
                CP(dst, src[:, tt, c0:c0 + ncol], [keys[tt]], ['dst'])
                DMA('sp', dbg_d[tt * 128:(tt + 1) * 128, dcol:dcol + ncol], dst, ['dst'], ['dbg'])

        def group_ln_core(src, sqv, nb, rk, wk_sq, eps_ap):
            s1 = AR.get([nb], F32)
            s2 = AR.get([nb], F32)
            s3 = AR.get([nb], F32)
            TT(sqv, src, src, ALU.mult, rk, [wk_sq])
            RED(s1, src, ALU.add, rk, ['gs1'])
            RED(s2, sqv, ALU.add, [wk_sq], ['gs2'])
            TS(s1, s1, 1.0 / 64, None, ALU.mult, None, ['gs1'], ['gs1'])
            TT(s3, s1, s1, ALU.mult, ['gs1'], ['gs3'])
            STT(s2, s2, 1.0 / 64, s3, ALU.mult, ALU.subtract, ['gs2', 'gs3'], ['gs2'])
            TS(s2, s2, 0.0, None, ALU.max, None, ['gs2'], ['gs2'])
            ACT(s2, s2, AF.Sqrt, ['gs2', 'epsA'], ['gs2'], bias=eps_ap)
            RCP(s2, s2, ['gs2'], ['gs2'])
            TT(sqv, src, s1.unsqueeze(2).to_broadcast([128, nb, 64]), ALU.subtract, rk + ['gs1'], [wk_sq])
            TT(sqv, sqv, s2.unsqueeze(2).to_broadcast([128, nb, 64]), ALU.mult, [wk_sq, 'gs2'], [wk_sq])

        def mixer_B(l):
            hk = lambda k, tb: 'hT%d_%d' % (k, tb)
            S.barrier(relay=RELAY)
            AR.reset()
            qrT = AR.get([2, 2048], BF16)
            krT = AR.get([2, 2048], BF16)
            off_x = AR.off
            cosT = AR.get([2048], F32)
            sinT = AR.get([2048], F32)
            rt1 = [AR.get([512], F32) for _ in range(2)]
            rt2 = [AR.get([512], F32) for _ in range(2)]
            DMA('sp', cosT, Dr['cosT'], (), ['cosT'])
            DMA('sp', sinT, Dr['sinT'], (), ['sinT'])
            DMA('sp', lnbc[:, 2, :], Dr['b_gng'][l], (), ['lnbc'])
            DMA('sp', lnbc[:, 3, :], Dr['b_gnb'][l], (), ['lnbc'])
            n = 0
            for p in range(2):
                wv, wk = wload(kchunks(Dr['w_inBf'][l][:, p * 512:(p + 1) * 512]), [8, 512])
                for kind in range(2):
                    dst = qrT if kind == 0 else krT
                    for tb in range(4):
                        i = n % 2
                        b1, b2 = 2 * (n % 4), 2 * (n % 4) + 1
                        n += 1
                        for k in range(8):
                            MM(bank(b1), wv[:, k, (2 * kind) * 128:(2 * kind + 1) * 128], hT[:, k, tb * 512:(tb + 1) * 512],
                               k == 0, k == 7, [wk, hk(k, tb)], [BK[b1]])
                        for k in range(8):
                            MM(bank(b2), wv[:, k, (2 * kind + 1) * 128:(2 * kind + 2) * 128], hT[:, k, tb * 512:(tb + 1) * 512],
                               k == 0, k == 7, [wk, hk(k, tb)], [BK[b2]])
                        TT(rt1[i], bank(b1), cosT[:, tb * 512:(tb + 1) * 512], ALU.mult, [BK[b1], 'cosT'], ['rt1_%d' % i])
                        TT(rt2[i], bank(b2), sinT[:, tb * 512:(tb + 1) * 512], ALU.mult, [BK[b2], 'sinT'], ['rt2_%d' % i])
                        TT(dst[:, p, tb * 512:(tb + 1) * 512], rt1[i], rt2[i], ALU.add, ['rt1_%d' % i, 'rt2_%d' % i],
                           ['qk%d_%d' % (kind, p)])
            wvt, wkt = wload(kchunks(Dr['w_inBt'][l]), [8, 512])
            for p in range(2):
                S.barrier(relay=RELAY)
                AR.reset(off_x)
                vg = AR.get([16, 256], BF16)
                kvs = AR.get([16, 128], F32)
                s16 = AR.get([16, 128], BF16)
                oall = AR.get([16, 128], F32)
                kz = [AR.get([128], BF16) for _ in range(2)]
                qx = [AR.get([128], BF16) for _ in range(2)]
                sT = [AR.get([256], BF16) for _ in range(2)]
                mixT = AR.get([1, 2048], BF16)
                qkk = ['qk0_%d' % p, 'qk1_%d' % p]
                for tt in range(16):
                    b = tt % 4
                    for k in range(8):
                        MM(bank(b)[:, 0:256], hT[:, k, tt * 128:(tt + 1) * 128], wvt[:, k, p * 256:(p + 1) * 256],
                           k == 0, k == 7, [hk(k, tt // 4), wkt], [BK[b]])
                    CP(vg[:, tt, 0:128], bank(b)[:, 0:128], [BK[b]], ['vg%d' % tt], eng='dve')
                    ACT(vg[:, tt, 128:256], bank(b)[:, 128:256], AF.Silu, [BK[b]], ['vg%d' % tt])
                S.op('dve', lambda e: e.memset(kvs[:, 0, :], 0.0), (), ['kvs'])
                for c in range(15):
                    i = c % 2
                    bt = 4 + (c % 2)
                    bm = 6 + (c % 2)
                    pb = bank(bt).bitcast(BF16)
                    TR(pb[:, 0:128], krT[:, p, c * 128:(c + 1) * 128], ident_b[:], [qkk[1], 'ident_b'], [BK[bt]])
                    TT(kz[i].rearrange("p (h d) -> p h d", h=2), pb[:, 0:128].rearrange("p (h d) -> p h d", h=2),
                       zeta[:, 2 * p:2 * p + 2].unsqueeze(2).to_broadcast([128, 2, 64]), ALU.mult,
                       [BK[bt], 'zeta'], ['kz%d' % i])
                    MM(bank(bm)[:, 0:128], kz[i], vg[:, c, 0:128], True, True, ['kz%d' % i, 'vg%d' % c], [BK[bm]])
                    STT(kvs[:, c + 1, :], kvs[:, c, :], cdv[:, p:p + 1], bank(bm)[:, 0:128], ALU.mult, ALU.add,
                        ['kvs', 'cdv', BK[bm]], ['kvs'])
                CP(s16, kvs, ['kvs'], ['s16'], eng='dve')
                def b_scores(c):
                    i = c % 2
                    bs0 = 2 * (c % 2)
                    cs = slice(c * 128, (c + 1) * 128)
                    if c > 0:
                        TT(qx[i], qrT[:, p, cs], xiT[:, p * 128:(p + 1) * 128], ALU.mult, [qkk[0], 'xiT'], ['qx%d' % i])
                    for hh in range(2):
                        ps_ = slice(hh * 64, (hh + 1) * 64)
                        MM(bank(bs0 + hh)[:, 0:128], krT[ps_, p, cs], qrT[ps_, p, cs], True, True,
                           qkk, [BK[bs0 + hh]])

                def b_rest(c):
                    i = c % 2
                    bs0 = 2 * (c % 2)
                    bo0 = 4 + 2 * (c % 2)
                    TT(sT[i].rearrange("p (h l) -> p h l", h=2),
                       PSA[:, bs0 * 512:(bs0 + 2) * 512].rearrange("p (h x) -> p h x", h=2)[:, :, 0:128],
                       decayT[:, 2 * p * 128:(2 * p + 2) * 128].rearrange("p (h l) -> p h l", h=2), ALU.mult,
                       [BK[bs0], BK[bs0 + 1], 'decayT'], ['sT%d' % i])
                    for hh in range(2):
                        ps_ = slice(hh * 64, (hh + 1) * 64)
                        MM(bank(bo0 + hh)[:, 0:64], sT[i][:, hh * 128:(hh + 1) * 128], vg[:, c, hh * 64:(hh + 1) * 64],
                           True, c == 0, ['sT%d' % i, 'vg%d' % c], [BK[bo0 + hh]])
                        if c > 0:
                            MM(bank(bo0 + hh)[:, 0:64], qx[i][ps_, :], s16[ps_, c, hh * 64:(hh + 1) * 64],
                               False, True, ['qx%d' % i, 's16'], [BK[bo0 + hh]])
                    CP(oall[:, c, :].rearrange("p (h e) -> p h e", h=2),
                       PSB[:, (bo0 - 4) * 512:(bo0 - 2) * 512].rearrange("p (h x) -> p h x", h=2)[:, :, 0:64],
                       [BK[bo0], BK[bo0 + 1]], ['oall'], eng='act')

                b_scores(0)
                for c in range(16):
                    if c + 1 < 16:
                        b_scores(c + 1)
                    b_rest(c)
                o3 = oall.rearrange("p c (h e) -> p (c h) e", h=2)
                sq3 = kvs.rearrange("p c (h e) -> p (c h) e", h=2)
                gam_b = lnbc[:, 2, p * 128:(p + 1) * 128].rearrange("p (h e) -> p h e", h=2).unsqueeze(1).to_broadcast([128, 16, 2, 64])
                bet_b = lnbc[:, 3, p * 128:(p + 1) * 128].rearrange("p (h e) -> p h e", h=2).unsqueeze(1).to_broadcast([128, 16, 2, 64])
                sq4 = kvs.rearrange("p c (h e) -> p c h e", h=2)
                group_ln_core(o3, sq3, 32, ['oall'], 'kvs', epsA[:, 0:1])
                TT(sq4, sq4, gam_b, ALU.mult, ['kvs', 'lnbc'], ['kvs'])
                TT(sq4, sq4, bet_b, ALU.add, ['kvs', 'lnbc'], ['kvs'])
                vkeys = ['vg%d' % tt for tt in range(16)]
                TT(vg[:, :, 0:128], kvs, vg[:, :, 128:256], ALU.mult, ['kvs'] + vkeys, vkeys)
                if dbg == 'mixB%d_%d' % (l, p):
                    dump_tm(vg, 0, 128, vkeys, p * 128)
                out_proj(l, vg, vkeys, 1, 256 + p * 128, mixT)

        def mixer_C(l):
            hk = lambda k, tb: 'hT%d_%d' % (k, tb)
            DMA('pool', w1s[:].rearrange("p (a b) -> p a b", a=4), Dr['w1s'][l].rearrange("p (a b) -> p a b", a=4), (), ['w1s'])
            DMA('pool', w2k[:], Dr['w2k'][l], (), ['w2k'])
            DMA('pool', w2v[:], Dr['w2v'][l], (), ['w2v'])
            DMA('sp', posT[:], Dr['posT'][l], (), ['posT'])
            for g in range(2):
                S.barrier(relay=RELAY)
                AR.reset()
                qz = AR.get([4, 2048], BF16)
                KsT = AR.get([2048], BF16)
                KwT = AR.get([2048], BF16)
                Vaug = AR.get([16, 2, 65], BF16)
                gates = AR.get([16, 12], F32)
                mixC = AR.get([16, 256], BF16)
                kcmpT = AR.get([127], BF16)
                vcaug = AR.get([97], BF16)
                hidT = AR.get([2, 127], BF16)
                off_r = AR.off
                kvcT = AR.get([2048], BF16)
                kcp = AR.get([32, 127], BF16)
                wv, wk = wload(kchunks(Dr['w_inCf'][l][:, g * 640:g * 640 + 512]), [8, 512])
                wv2, wk2 = wload(kchunks(Dr['w_inCf'][l][:, g * 640 + 512:g * 640 + 640]), [8, 128])
                S.op('dve', lambda e: e.memset(qz, 0.0), (), ['qT'])
                dsts = [(None, 'qT', 0.125), (None, 'qT', 0.125), (KsT, 'KsT', 1.0), (KwT, 'KwT', 1.0),
                        (kvcT, 'kvcT', 1.0)]
                n = 0
                for bi_, (dst, dk, scl) in enumerate(dsts):
                    for tb in range(4):
                        b = n % 4
                        n += 1
                        for k in range(8):
                            lw = wv[:, k, bi_ * 128:(bi_ + 1) * 128] if bi_ < 4 else wv2[:, k, :]
                            MM(bank(b), lw, hT[:, k, tb * 512:(tb + 1) * 512], k == 0, k == 7,
                               [wk if bi_ < 4 else wk2, hk(k, tb)], [BK[b]])
                        if dst is None:
                            ACT(qz[0:64, 2 * bi_, tb * 512:(tb + 1) * 512], bank(b)[0:64, :], AF.Identity, [BK[b]], [dk], scale=scl)
                            TS(qz[64:128, 2 * bi_ + 1, tb * 512:(tb + 1) * 512], bank(b)[64:128, :], scl, None, ALU.mult, None, [BK[b]], [dk])
                        elif n % 2:
                            ACT(dst[:, tb * 512:(tb + 1) * 512], bank(b), AF.Identity, [BK[b]], [dk], scale=scl)
                        else:
                            TS(dst[:, tb * 512:(tb + 1) * 512], bank(b), scl, None, ALU.mult, None, [BK[b]], [dk])
                wv3, wk3 = wload(kchunks(Dr['w_inCt'][l][:, g * 140:(g + 1) * 140]), [8, 140])
                S.op('dve', lambda e: e.memset(Vaug[:, :, :, 64:65], 1.0), (), ['Vaug'])
                for tt in range(16):
                    b = 4 + tt % 4
                    for k in range(8):
                        MM(bank(b)[:, 0:140], hT[:, k, tt * 128:(tt + 1) * 128], wv3[:, k, :], k == 0, k == 7,
                           [hk(k, tt // 4), wk3], [BK[b]])
                    CP(Vaug[:, tt, :, 0:64], bank(b)[:, 0:128].rearrange("p (s d) -> p s d", s=2), [BK[b]], ['Vaug'], eng='dve')
                    ACT(gates[:, tt, :], bank(b)[:, 128:140], AF.Sigmoid, [BK[b]], ['gates'])
                kv_t = kvcT.tensor
                win = bass.AP(kv_t, kvcT.offset, [list(kvcT.ap[0]), [1, 32], [16, 127]])
                TT(kcp, win, posT[:, 0:32].unsqueeze(2).to_broadcast([128, 32, 127]), ALU.add, ['kvcT', 'posT'], ['kcp'])
                for kv in range(2):
                    ps_ = slice(kv * 64, (kv + 1) * 64)
                    for ll in range(32):
                        MM(bank(kv)[0:64, 0:127], w1s[ps_, ll * 64:(ll + 1) * 64], kcp[ps_, ll, :],
                           ll == 0, ll == 31, ['w1s', 'kcp'], [BK[kv]])
                    ACT(hidT[0:64, kv, :], bank(kv)[0:64, 0:127], AF.Gelu_apprx_tanh, [BK[kv]], ['hidT'])
                MM(bank(2)[:, 0:127], w2k[:], hidT[0:64, 0, :], True, True, ['w2k', 'hidT'], ['B2'])
                CP(kcmpT, bank(2)[:, 0:127], ['B2'], ['kcmpT'], eng='dve')
                MM(bank(3)[0:127, 0:64], hidT[0:64, 1, :], w2v[:], True, True, ['w2v', 'hidT'], ['B3'])
                S.op('dve', lambda e: e.memset(vcaug[:, 64:65], 1.0), (), ['vcaug'])
                CP(vcaug[0:127, 0:64], bank(3)[0:127, 0:64], ['B3'], ['vcaug'], eng='dve')
                CP(vcaug[:, 65:97], ovl[:, 0:32], ['ovl'], ['vcaug'], eng='dve')
                S.barrier(relay=RELAY)
                AR.reset(off_r)
                PT = [AR.get([512], BF16) for _ in range(4)]
                MbT = [AR.get([128], BF16) for _ in range(2)]
                for j_ in range(2):
                    S.op('dve', (lambda t_: (lambda e: e.memset(t_, 0.0)))(MbT[j_]), (), ['MbT%d' % j_])
                Mb = [AR.get([32], BF16) for _ in range(2)]
                ocg = [AR.get([4, 64], F32) for _ in range(2)]
                t1 = [AR.get([4, 64], F32) for _ in range(2)]
                t2 = [AR.get([4, 64], F32) for _ in range(2)]
                sm = [AR.get([96], F32) for _ in range(2)]
                impt = [AR.get([4, 32], F32) for _ in range(2)]
                pn = [0]

                def qslice(r, qt):
                    return qz[:, r, qt * 128:(qt + 1) * 128]

                def attend(qt, kts, KT, vsel, bS, bO, extra_fn):
                    def scores(ki):
                        kt = kts[ki]
                        bs_ = bS[ki % len(bS)]
                        extras = extra_fn(kt)
                        v4 = bank(bs_).rearrange("p (r t) -> p r t", r=4)
                        MM(v4, KT[:, kt * 128:(kt + 1) * 128], qz[:, :, qt * 128:(qt + 1) * 128],
                           True, not extras, ['KsT', 'KwT', 'qT'], [BK[bs_]])
                        for ei, (lh, rh, ks) in enumerate(extras):
                            MM(v4, lh, rh, False, ei == len(extras) - 1, ks, [BK[bs_]])

                    def rest(ki):
                        kt = kts[ki]
                        bs_ = bS[ki % len(bS)]
                        i = pn[0] % 4
                        pn[0] += 1
                        ACT(PT[i], bank(bs_), AF.Exp, [BK[bs_]], ['PT%d' % i])
                        for r in range(4):
                            MM(bank(bO)[:, r * 65:(r + 1) * 65], PT[i][:, r * 128:(r + 1) * 128], Vaug[:, kt, vsel, :],
                               ki == 0 and r == 0, ki == len(kts) - 1 and r == 3, ['PT%d' % i, 'Vaug'], [BK[bO]])

                    LA = 2
                    for ki in range(min(LA, len(kts))):
                        scores(ki)
                    for ki in range(len(kts)):
                        if ki + LA < len(kts):
                            scores(ki + LA)
                        rest(ki)

                for qt in range(16):
                    j = qt % 2
                    smj = sm[j]
                    sk = 'sm%d' % j
                    qs = slice(qt * 128, (qt + 1) * 128)
                    MM(bank(0)[0:127, :].rearrange("p (r t) -> p r t", r=4), kcmpT[:, :], qz[:, :, qs], True, False,
                       ['kcmpT', 'qT'], ['B0'])
                    MM(bank(0)[0:127, :].rearrange("p (r t) -> p r t", r=4), ident_b[0:127, 0:127],
                       cmpb[0:127, qs].unsqueeze(1).to_broadcast([127, 4, 128]), False, True, ['ident_b', 'cmpb'], ['B0'])
                    i = pn[0] % 4
                    pn[0] += 1
                    ACT(PT[i][0:127, :], bank(0)[0:127, :], AF.Exp, ['B0'], ['PT%d' % i])
                    for r in range(4):
                        MM(bank(1)[:, r * 97:(r + 1) * 97], PT[i][0:127, r * 128:(r + 1) * 128], vcaug[0:127, :],
                           r == 0, r == 3, ['PT%d' % i, 'vcaug'], ['B1'])
                    O = bank(1)[:, 0:388].rearrange("p (r c) -> p r c", r=4)
                    cb4 = causalb[:].unsqueeze(1).to_broadcast([128, 4, 128])
                    wb4 = winb[:].unsqueeze(1).to_broadcast([128, 4, 128])

                    def wmask(kt, qt=qt, cb4=cb4, wb4=wb4):
                        if kt == qt:
                            return [(ident_b[:], cb4, ['ident_b', 'causalb'])]
                        if kt == qt - 4:
                            return [(ident_b[:], wb4, ['ident_b', 'winb'])]
                        return []
                    attend(qt, list(range(max(0, qt - 4), qt + 1)), KwT, 1, [2, 3, 6, 7], 5, wmask)
                    TS(smj[:, 0:4], O[:, :, 64], 1e-30, None, ALU.max, None, ['B1'], [sk])
                    RCP(smj[:, 4:8], smj[:, 0:4], [sk], [sk])
                    TT(impt[j], O[:, :, 65:97], smj[:, 4:8].unsqueeze(2).to_broadcast([128, 4, 32]), ALU.mult, ['B1', sk], ['impt%d' % j])
                    RED(smj[:, 32:64], impt[j].rearrange("p r c -> p c r"), ALU.add, ['impt%d' % j], [sk])
                    TT(smj[:, 32:64], smj[:, 32:64], fbias[:, qt * 32:(qt + 1) * 32], ALU.add, [sk, 'fbias'], [sk])
                    S.op('dve', (lambda o_, i_: (lambda e: e.max(o_, i_)))(smj[:, 64:72], smj[:, 32:64]), [sk], [sk])
                    TS(Mb[j], smj[:, 32:64], smj[:, 71:72], -1.0, ALU.is_ge, ALU.add, [sk], ['Mb%d' % j])
                    pbt = bank(0).bitcast(BF16)
                    TR(pbt[0:32, 0:128], Mb[j], ident_b[:], ['Mb%d' % j, 'ident_b'], ['B0'])
                    CP(MbT[j][0:32, :], pbt[0:32, 0:128], ['B0'], ['MbT%d' % j], eng='dve')
                    gv = gates[:, qt, :].rearrange("p (r c) -> p r c", r=4)
                    TT(smj[:, 8:12], smj[:, 4:8], gv[:, :, 0], ALU.mult, [sk, 'gates'], [sk])
                    TT(ocg[j], O[:, :, 0:64], smj[:, 8:12].unsqueeze(2).to_broadcast([128, 4, 64]), ALU.mult, ['B1', sk], ['ocg%d' % j])

                    mb4 = MbT[j][:, :].unsqueeze(1).to_broadcast([128, 4, 128])

                    def smask(kt, qt=qt, j=j, cb4=cb4, mb4=mb4):
                        ex = [(expand[:, kt * 128:(kt + 1) * 128], mb4, ['expand', 'MbT%d' % j])]
                        if kt == qt:
                            ex.append((ident_b[:], cb4, ['ident_b', 'causalb']))
                        return ex
                    attend(qt, list(range(0, qt + 1)), KsT, 0, [2, 3, 6, 7], 4, smask)
                    Os = bank(4)[:, 0:260].rearrange("p (r c) -> p r c", r=4)
                    Ow = bank(5)[:, 0:260].rearrange("p (r c) -> p r c", r=4)
                    RCP(smj[:, 12:16], Os[:, :, 64], ['B4'], [sk])
                    TT(smj[:, 12:16], smj[:, 12:16], gv[:, :, 1], ALU.mult, [sk, 'gates'], [sk])
                    RCP(smj[:, 16:20], Ow[:, :, 64], ['B5'], [sk])
                    TT(smj[:, 16:20], smj[:, 16:20], gv[:, :, 2], ALU.mult, [sk, 'gates'], [sk])
                    TT(t1[j], Os[:, :, 0:64], smj[:, 12:16].unsqueeze(2).to_broadcast([128, 4, 64]), ALU.mult, ['B4', sk], ['t1_%d' % j])
                    TT(t2[j], Ow[:, :, 0:64], smj[:, 16:20].unsqueeze(2).to_broadcast([128, 4, 64]), ALU.mult, ['B5', sk], ['t2_%d' % j])
                    TT(t1[j], t1[j], ocg[j], ALU.add, ['t1_%d' % j, 'ocg%d' % j], ['t1_%d' % j])
                    TT(mixC[:, qt, :].rearrange("p (r d) -> p r d", r=4), t1[j], t2[j], ALU.add, ['t1_%d' % j, 't2_%d' % j], ['mixC%d' % qt])
                mkeys = ['mixC%d' % tt for tt in range(16)]
                if dbg == 'mixC%d_%d' % (l, g):
                    dump_tm(mixC, 0, 256, mkeys, g * 256)
                S.barrier(relay=RELAY)
                AR.reset(off_r)
                mixT = AR.get([2, 2048], BF16)
                out_proj(l, mixC, mkeys, 2, 512 + g * 256, mixT)

        for l in range(n_layers):
            hk = lambda k, tb: 'hT%d_%d' % (k, tb)
            if 'A' in mixers:
                S.barrier(relay=RELAY)
                AR.reset()
                zag = AR.get([16, 512], BF16)
                off_sqv = AR.off
                sqv = AR.get([16, 256], F32)
                st1 = AR.get([64], F32)
                st2 = AR.get([64], F32)
                st3 = AR.get([64], F32)
                mixA = AR.get([16, 256], BF16)
                atmp = [AR.get([256], F32) for _ in range(2)]
                wstage = AR.get([4, 128], F32)
                DMA('sp', wstage, Dr['WsT'][l].rearrange("p (g t) -> p g t", g=4), (), ['wstage'])
                TT(WsT[:].rearrange("p (g t) -> p g t", g=4), wstage,
                   causal01[:].unsqueeze(1).to_broadcast([128, 4, 128]), ALU.mult, ['wstage', 'causal01'], ['WsT'])
                DMA('sp', bsT[:], Dr['bsT'][l], (), ['bsT'])
                DMA('sp', lnbc[:, 0, :], Dr['a_lng'][l], (), ['lnbcA'])
                DMA('sp', lnbc[:, 1, :], Dr['a_lnb'][l], (), ['lnbcA'])
                wv, wk = wload(kchunks(Dr['w_inA'][l]), [8, 512])
                for tt in range(16):
                    b = tt % 4
                    for k in range(8):
                        MM(bank(b), hT[:, k, tt * 128:(tt + 1) * 128], wv[:, k, :], k == 0, k == 7,
                           [hk(k, tt // 4), wk], [BK[b]])
                    ACT(zag[:, tt, :], bank(b), AF.Gelu_apprx_tanh, [BK[b]], ['zag'])
                v4 = zag[:, :, 256:512].rearrange("p t (g d) -> p t g d", g=4)
                TT(sqv, zag[:, :, 256:512], zag[:, :, 256:512], ALU.mult, ['zag'], ['sqv'])
                RED(st1.rearrange("p (t g) -> p t g", t=16), v4, ALU.add, ['zag'], ['st1'])
                RED(st2.rearrange("p (t g) -> p t g", t=16), sqv.rearrange("p t (g d) -> p t g d", g=4), ALU.add, ['sqv'], ['st2'])
                TS(st1, st1, 1.0 / 64, None, ALU.mult, None, ['st1'], ['st1'])
                TT(st3, st1, st1, ALU.mult, ['st1'], ['st3'])
                STT(st2, st2, 1.0 / 64, st3, ALU.mult, ALU.subtract, ['st2', 'st3'], ['st2'])
                TS(st2, st2, 0.0, None, ALU.max, None, ['st2'], ['st2'])
                ACT(st2, st2, AF.Sqrt, ['st2', 'epsA'], ['st2'], bias=epsA[:, 0:1])
                RCP(st2, st2, ['st2'], ['st2'])
                mean_b = st1.rearrange("p (t g) -> p t g", t=16).unsqueeze(3).to_broadcast([128, 16, 4, 64])
                rstd_b = st2.rearrange("p (t g) -> p t g", t=16).unsqueeze(3).to_broadcast([128, 16, 4, 64])
                sq4 = sqv.rearrange("p t (g d) -> p t g d", g=4)
                TT(sq4, v4, mean_b, ALU.subtract, ['zag', 'st1'], ['sqv'])
                TT(sq4, sq4, rstd_b, ALU.mult, ['sqv', 'st2'], ['sqv'])
                gam_b = lnbc[:, 0, :].unsqueeze(1).to_broadcast([128, 16, 256])
                bet_b = lnbc[:, 1, :].unsqueeze(1).to_broadcast([128, 16, 256])
                TT(sqv, sqv, gam_b, ALU.mult, ['sqv', 'lnbcA'], ['sqv'])
                TT(zag[:, :, 256:512], sqv, bet_b, ALU.add, ['sqv', 'lnbcA'], ['zag'])
                bs_b = bsT[:, 0:4].unsqueeze(2).to_broadcast([128, 4, 64])
                for tt in range(16):
                    b = 4 + tt % 4
                    for g in range(4):
                        MM(bank(b)[:, g * 64:(g + 1) * 64], WsT[:, g * 128:(g + 1) * 128],
                           zag[:, tt, 256 + g * 64:256 + (g + 1) * 64], g == 0, g == 3, ['WsT', 'zag'], [BK[b]])
                    at = atmp[tt % 2]
                    ak = 'atmp%d' % (tt % 2)
                    TT(at.rearrange("p (g d) -> p g d", g=4), bank(b)[:, 0:256].rearrange("p (g d) -> p g d", g=4),
                       bs_b, ALU.add, [BK[b], 'bsT'], [ak])
                    TT(mixA[:, tt, :], at, zag[:, tt, 0:256], ALU.mult, [ak, 'zag'], ['mixA%d' % tt])
                if dbg == 'mixA%d' % l:
                    dst = AR.get([256], F32)
                    for tt in range(16):
                        CP(dst, mixA[:, tt, :], ['mixA%d' % tt], ['dst'])
                        DMA('sp', dbg_d[tt * 128:(tt + 1) * 128, 0:256], dst, ['dst'], ['dbg'])
                AR.reset(off_sqv)
                mixT = AR.get([2, 2048], BF16)
                out_proj(l, mixA, ['mixA%d' % tt for tt in range(16)], 2, 0, mixT, ['sqv'])

            if 'B' in mixers:
                mixer_B(l)
            if 'C' in mixers:
                mixer_C(l)

            S.barrier(relay=RELAY)
            layer_norm(l, 1, last=False)
            if dbg == 'ln1_%d' % l:
                break

            if do_ffn:
                S.barrier(relay=RELAY)
                AR.reset()
                DMA('sp', cwT[:], Dr['cwT'][l], (), ['cwT'])
                DMA('sp', cbT[:], Dr['cbT'][l], (), ['cbT'])
                gT = AR.get([max(FF_SPLIT), 2048], BF16)
                ctmp = [[AR.get([1024], F32) for _ in range(2)] for _ in range(2)]
                sgt = [AR.get([1024], BF16) for _ in range(2)]
                j0 = 0
                PS4 = [PSA, PSB]
                P4K = [BK[0:4], BK[4:8]]
                for part_n in FF_SPLIT:
                    wgrp = {}
                    for jl in range(part_n):
                        j = j0 + jl
                        if jl % 4 == 0:
                            ng = min(4, part_n - jl)
                            for part in range(2):
                                jj0 = part * 22 + j
                                wgrp[part] = wload(kchunks(Dr['w_up'][l][:, jj0 * 128:(jj0 + ng) * 128]), [8, ng * 128])
                        for part in range(2):
                            jj = part * 22 + j
                            wvf, wk = wgrp[part]
                            wv = wvf[:, :, (jl % 4) * 128:(jl % 4 + 1) * 128]
                            ps = PS4[part]
                            for tb in range(4):
                                for k in range(8):
                                    MM(ps[:, tb * 512:(tb + 1) * 512], wv[:, k, :], hT[:, k, tb * 512:(tb + 1) * 512],
                                       k == 0, k == 7, [wk, hk(k, tb)], [P4K[part][tb]])
                            for hf in range(2):
                                ct = ctmp[part][hf]
                                ck = 'ctmp%d_%d' % (part, hf)
                                o = hf * 1024
                                pk = P4K[part][2 * hf:2 * hf + 2]
                                pkp = P4K[part][max(0, 2 * hf - 1):2 * hf + 2]
                                ACT(ct, ps[:, o:o + 1024], AF.Identity, pk + ['cwT', 'cbT'], [ck],
                                    scale=cwT[:, jj * 3 + 2:jj * 3 + 3], bias=cbT[:, jj:jj + 1])
                                if hf == 0:
                                    STT(ct[:, 1:1024], ps[:, 0:1023], cwT[:, jj * 3 + 1:jj * 3 + 2], ct[:, 1:1024],
                                        ALU.mult, ALU.add, pk + ['cwT', ck], [ck])
                                    STT(ct[:, 2:1024], ps[:, 0:1022], cwT[:, jj * 3:jj * 3 + 1], ct[:, 2:1024],
                                        ALU.mult, ALU.add, pk + ['cwT', ck], [ck])
                                else:
                                    STT(ct, ps[:, o - 1:o + 1023], cwT[:, jj * 3 + 1:jj * 3 + 2], ct,
                                        ALU.mult, ALU.add, pkp + ['cwT', ck], [ck])
                                    STT(ct, ps[:, o - 2:o + 1022], cwT[:, jj * 3:jj * 3 + 1], ct,
                                        ALU.mult, ALU.add, pkp + ['cwT', ck], [ck])
                                if part == 0:
                                    ACT(sgt[hf], ct, AF.Silu, [ck], ['sgt%d' % hf])
                                else:
                                    TT(gT[:, jl, o:o + 1024], ct, sgt[hf], ALU.mult, [ck, 'sgt%d' % hf], ['gT%d' % jl])
                    nsl = (part_n + 3) // 4
                    wvs = []
                    for s in range(nsl):
                        r0 = (j0 + 4 * s) * 128
                        nr = min(4, part_n - 4 * s)
                        wvs.append(wload(kchunks(Dr['w_down'][l][r0:r0 + nr * 128, :]), [nr, 1024]))
                    bi2 = 0
                    for fb in range(8):
                        for tb in range(4):
                            b = bi2 % 8
                            bi2 += 1
                            for jl in range(part_n):
                                wv, wk = wvs[jl // 4]
                                MM(bank(b), wv[:, jl % 4, fb * 128:(fb + 1) * 128], gT[:, jl, tb * 512:(tb + 1) * 512],
                                   jl == 0, jl == part_n - 1, [wk, 'gT%d' % jl], [BK[b]])
                            xs = xT[:, fb, tb * 512:(tb + 1) * 512]
                            STT(xs, bank(b), drv[l][:, 24 + fb:25 + fb], xs, ALU.mult, ALU.add,
                                [BK[b], 'drv%d' % l, 'xT%d_%d' % (fb, tb)], ['xT%d_%d' % (fb, tb)])
                    j0 += part_n
                S.barrier(relay=RELAY)
            layer_norm(l, 2, last=(l == n_layers - 1))

        S.barrier(relay=RELAY)
        AR.reset()
        ost = [AR.get([1024], F32) for _ in range(4)]
        bi = 0
        for tt in range(16):
            o = ost[tt % 4]
            ok = 'ost%d' % (tt % 4)
            for cg in range(2):
                b = bi % 8
                bi += 1
                for q in range(4):
                    c = cg * 4 + q
                    TR(bank(b)[:, q * 128:(q + 1) * 128], xT[:, c, tt * 128:(tt + 1) * 128], ident_f[:],
                       ['xT%d_%d' % (c, tt // 4), 'ident_f'], [BK[b]])
                CP(o[:, cg * 512:(cg + 1) * 512], bank(b), [BK[b]], [ok], eng=EV())
            DMA('sp', out_d[tt * 128:(tt + 1) * 128, :], o, [ok], ['out'])
        S.finish()
        S.replay()
    return nc


_CACHE = {}


def kernel(**inputs):
    inp = {k: np.asarray(v) for k, v in inputs.items()}
    if 'nc' not in _CACHE:
        _CACHE['nc'] = build()
    nc = _CACHE['nc']
    w = _prep_weights(inp)
    tb = _tables()
    in_maps = []
    for b in range(8):
        m = dict(w)
        m.update(tb)
        m['x'] = np.ascontiguousarray(inp['x'][b], dtype=np.float32)
        m['cT'] = _pad64(inp['c'][b].reshape(8, 128).T)
        in_maps.append(m)
    res = run_bass_kernel_spmd(nc, in_maps, core_ids=list(range(8)))
    out = np.stack([np.asarray(r['out'], dtype=np.float32) for r in res.results], 0)
    return out
```

```python
import math
from contextlib import ExitStack
import numpy as np
import concourse.bass as bass
import concourse.mybir as mybir
from concourse.bass_utils import run_bass_kernel_spmd

F32 = mybir.dt.float32
BF16 = mybir.dt.bfloat16
AF = mybir.ActivationFunctionType
ALU = mybir.AluOpType
AX = mybir.AxisListType

ENGS = ['pe', 'dve', 'act', 'pool', 'sp']
NRING = 8
DEPTH = 2
SEQ = 2048
DM = 1024
ALPHA = (2 * DEPTH) ** 0.25
LN_EPS = 1e-5
NEGB = -30000.0
FF_SPLIT = [6, 6, 5, 5]


class Sched:
    def __init__(self, nc, stack):
        self.nc = nc
        self.prog = {e: [] for e in ENGS}
        self.cnt = {e: 0 for e in ENGS}
        self.seen = {e: {} for e in ENGS}
        self.lastw = {}
        self.readers = {}
        self.sems = {}
        self.semval = {}
        self.relay_fn = None
        for e in ENGS:
            self.sems[e] = stack.enter_context(nc.semaphore("s_" + e))
        self.dma_n = {}
        for q in ['sp', 'act', 'pool']:
            self.dma_n[q] = 0
            for j in range(NRING):
                nm = "d_%s%d" % (q, j)
                self.sems[nm] = stack.enter_context(nc.semaphore(nm))

    def _deps(self, eng, reads, writes, is_dma=False):
        deps = []
        for k in reads:
            t = self.lastw.get(k)
            if t is not None:
                deps.append((t, 'raw'))
        for k in writes:
            t = self.lastw.get(k)
            if t is not None:
                deps.append((t, 'waw'))
            for s, v in self.readers.get(k, {}).items():
                deps.append(((s, v), 'war'))
        need = {}
        for (s, v), kind in deps:
            if s == eng and not is_dma:
                if kind != 'raw' or eng == 'pe':
                    continue
            if self.seen[eng].get(s, 0) >= v:
                continue
            if need.get(s, 0) < v:
                need[s] = v
        return need

    def _emit_waits(self, eng, need):
        if eng in ('sp', 'pool') and 'pe' in need and self.relay_fn is not None:
            need = dict(need)
            v = need.pop('pe')
            self.seen[eng]['pe'] = v
            R = 'dve'
            if self.seen[R].get('pe', 0) < v:
                self.prog[R].append(('wait', 'pe', v))
                self.seen[R]['pe'] = v
            lr = getattr(self, 'last_relay', 0)
            if lr and self.seen[R].get(R, 0) < lr:
                self.prog[R].append(('wait', R, lr))
                self.seen[R][R] = lr
            self.cnt[R] += 1
            self.last_relay = self.cnt[R]
            self.semval[R] = self.cnt[R]
            self.prog[R].append(('op', self.relay_fn, R, 1))
            if self.seen[eng].get(R, 0) < self.cnt[R]:
                need[R] = max(need.get(R, 0), self.cnt[R])
        for s, v in need.items():
            self.prog[eng].append(('wait', s, v))
            self.seen[eng][s] = v

    def _commit(self, tok, reads, writes):
        for k in writes:
            self.lastw[k] = tok
            self.readers[k] = {}
        for k in reads:
            d = self.readers.setdefault(k, {})
            if d.get(tok[0], 0) < tok[1]:
                d[tok[0]] = tok[1]

    def relay_readers(self, keys):
        if self.relay_fn is None:
            return
        v = 0
        for k in keys:
            d = self.readers.get(k)
            if d and 'pe' in d:
                v = max(v, d['pe'])
        if v == 0:
            return
        R = 'dve'
        if self.seen[R].get('pe', 0) < v:
            self.prog[R].append(('wait', 'pe', v))
            self.seen[R]['pe'] = v
        lr = getattr(self, 'last_relay', 0)
        if lr and self.seen[R].get(R, 0) < lr:
            self.prog[R].append(('wait', R, lr))
            self.seen[R][R] = lr
        self.cnt[R] += 1
        self.semval[R] = self.cnt[R]
        self.last_relay = self.cnt[R]
        self.prog[R].append(('op', self.relay_fn, R, 1))
        for k in keys:
            d = self.readers.get(k)
            if d and 'pe' in d:
                d.pop('pe')
                d[R] = max(d.get(R, 0), self.cnt[R])

    def op(self, eng, fn, reads=(), writes=()):
        need = self._deps(eng, reads, writes)
        self._emit_waits(eng, need)
        self.cnt[eng] += 1
        tok = (eng, self.cnt[eng])
        self.semval[eng] = self.cnt[eng]
        self.prog[eng].append(('op', fn, eng, 1))
        self._commit(tok, reads, writes)
        return tok

    def dma_multi(self, q, pairs, reads=(), writes=()):
        i = self.dma_n[q]
        self.dma_n[q] += 1
        slot = "d_%s%d" % (q, i % NRING)
        prev = self.semval.get(slot, 0)
        val = prev + 16 * len(pairs)
        need = self._deps(q, reads, writes, is_dma=True)
        if prev > 0 and self.seen[q].get(slot, 0) < prev:
            need[slot] = max(need.get(slot, 0), prev)
        self._emit_waits(q, need)
        for out, in_ in pairs:
            self.prog[q].append(('op', (lambda o_, i_: (lambda e: e.dma_start(out=o_, in_=i_)))(out, in_), slot, 16))
        self.semval[slot] = val
        tok = (slot, val)
        self._commit(tok, reads, writes)
        return tok

    def dma(self, q, out, in_, reads=(), writes=()):
        return self.dma_multi(q, [(out, in_)], reads, writes)

    def _wait_all(self, e):
        need = {}
        for s_, v in self.semval.items():
            if s_ == e:
                continue
            if self.seen[e].get(s_, 0) < v:
                need[s_] = v
        self._emit_waits(e, need)

    def barrier(self, engs=('pe', 'dve', 'act', 'sp'), relay=None):
        if relay is None or 'sp' not in engs:
            for e in engs:
                self._wait_all(e)
            return
        snap = dict(self.semval)
        self._wait_all('sp')
        tok = self.dma('sp', relay[0], relay[1], (), ['__bar'])
        for e in engs:
            if e == 'sp':
                continue
            self._emit_waits(e, {tok[0]: tok[1]} if self.seen[e].get(tok[0], 0) < tok[1] else {})
            for s_, v in snap.items():
                if s_ != e and self.seen[e].get(s_, 0) < v:
                    self.seen[e][s_] = v

    def finish(self):
        self.barrier(engs=('sp',))

    def replay(self):
        nc = self.nc
        sems = self.sems
        prog = self.prog

        def run(e, name):
            for it in prog[name]:
                if it[0] == 'wait':
                    e.wait_ge(sems[it[1]], it[2])
                else:
                    it[1](e).then_inc(sems[it[2]], it[3])

        with nc.Block() as block:
            @block.tensor
            def _(e):
                run(e, 'pe')

            @block.vector
            def _(e):
                run(e, 'dve')

            @block.scalar
            def _(e):
                run(e, 'act')

            @block.gpsimd
            def _(e):
                run(e, 'pool')

            @block.sync
            def _(e):
                run(e, 'sp')


def _pad64(a):
    a = np.asarray(a, dtype=np.float32)
    out = np.zeros(a.shape[:-1] + (64,), np.float32)
    out[..., :a.shape[-1]] = a
    return out


def _tables():
    t = {}
    half = 32
    inv = np.power(np.float32(10000.0), -np.arange(half, dtype=np.float32) / np.float32(half)).astype(np.float32)
    pos = np.arange(SEQ, dtype=np.float32)
    ang = pos[:, None] * inv[None, :]
    cos = np.cos(ang).astype(np.float32).T
    sin = np.sin(ang).astype(np.float32).T
    cosT = np.concatenate([cos, cos, cos, cos], 0)
    sinT = np.concatenate([-sin, sin, -sin, sin], 0)
    t['cosT'] = np.ascontiguousarray(cosT)
    t['sinT'] = np.ascontiguousarray(sinT)
    H = 4
    L = 128
    lg = np.log1p(-np.exp2(-5.0 - np.arange(H, dtype=np.float32))).astype(np.float32)
    idx = np.arange(L, dtype=np.float32)
    diff = idx[:, None] - idx[None, :]
    dec = np.where(diff >= 0, np.exp(lg[:, None, None] * np.maximum(diff, 0.0)), 0.0).astype(np.float32)
    t['decayT'] = np.ascontiguousarray(np.transpose(dec, (2, 0, 1)) * np.float32(0.125)).reshape(128, 512)
    xi = np.exp(lg[:, None] * (idx + 1.0)).astype(np.float32)
    zeta = np.exp(lg[:, None] * (L - 1.0 - idx)).astype(np.float32)
    xiT = np.zeros((128, 2, 128), np.float32)
    for p in range(2):
        for hh in range(2):
            xiT[hh * 64:(hh + 1) * 64, p, :] = xi[2 * p + hh][None, :]
    t['xiT'] = xiT.reshape(128, 256)
    t['zeta'] = _pad64(zeta.T * np.float32(0.125))
    cd = np.exp(lg * L).astype(np.float32)
    cdv = np.zeros((128, 2), np.float32)
    for p in range(2):
        for hh in range(2):
            cdv[hh * 64:(hh + 1) * 64, p] = cd[2 * p + hh]
    t['cdv'] = _pad64(cdv)
    key = np.arange(SEQ)
    ex = np.zeros((128, SEQ), np.float32)
    ex[key // 64, key] = -NEGB
    t['expand'] = ex
    kk = np.arange(128)[:, None]
    tt = np.arange(128)[None, :]
    t['causalb'] = np.where(kk > tt, NEGB, 0.0).astype(np.float32)
    t['winb'] = np.where(kk <= tt, NEGB, 0.0).astype(np.float32)
    t['identf'] = np.eye(128, dtype=np.float32)
    t['causal01'] = np.where(tt >= kk, 1.0, 0.0).astype(np.float32)
    k127 = np.arange(128)[:, None]
    tpos = np.arange(SEQ)[None, :]
    t['cmpb'] = np.where(16 * k127 + 31 > tpos, NEGB, 0.0).astype(np.float32)
    fb = np.zeros((128, 16, 32), np.float32)
    for qt in range(16):
        tq = qt * 128 + np.arange(128)
        cur = tq // 64
        blk = np.arange(32)
        future = blk[None, :] > cur[:, None]
        forced = (blk[None, :] == 0) | (blk[None, :] == cur[:, None]) | (blk[None, :] == cur[:, None] - 1)
        fb[:, qt, :] = np.where(forced, 1e30, np.where(future, -1e30, 0.0))
    t['fbias'] = fb.reshape(128, 512)
    ov = np.zeros((128, 32), np.float32)
    c0 = np.arange(127)[:, None] * 16
    s0 = np.arange(32)[None, :] * 64
    ov[:127] = np.clip(np.minimum(c0 + 32, s0 + 64) - np.maximum(c0, s0), 0, None) / 32.0
    t['ovl'] = _pad64(ov)
    return t


TABLE_SHAPES = {'cosT': [128, 2048], 'sinT': [128, 2048], 'decayT': [128, 512], 'xiT': [128, 256],
                'zeta': [128, 64], 'cdv': [128, 64], 'expand': [128, 2048], 'causalb': [128, 128],
                'winb': [128, 128], 'causal01': [128, 128], 'identf': [128, 128], 'cmpb': [128, 2048], 'fbias': [128, 512], 'ovl': [128, 64]}


def _prep_weights(inp):
    w = {}
    f = lambda a: np.ascontiguousarray(a, dtype=np.float32)
    w_in = inp['w_in']
    L = DEPTH
    w['w_ada'] = f(inp['w_ada'])
    w['b_adaT'] = _pad64(inp['b_ada'].reshape(L, 48, 128).transpose(0, 2, 1))
    w['w_inA'] = f(w_in[:, :, 0:512])
    cols = []
    for p in range(2):
        for base in (512, 768):
            hd = np.arange(128)
            h = 2 * p + hd // 64
            d = hd % 64
            cols.append(base + h * 64 + d)
            cols.append(base + h * 64 + (d + 32) % 64)
    cols = np.concatenate(cols)
    w['w_inBf'] = f(w_in[:, :, cols])
    cols = []
    for p in range(2):
        cols.append(1024 + p * 128 + np.arange(128))
        cols.append(1280 + p * 128 + np.arange(128))
    w['w_inBt'] = f(w_in[:, :, np.concatenate(cols)])
    cols = []
    for g in range(2):
        cols.append(1536 + g * 256 + np.arange(256))
        ks = 2304 + g * 64 + np.arange(64)
        kw = 2560 + g * 64 + np.arange(64)
        cols += [ks, ks, kw, kw]
        cols.append(2048 + g * 64 + np.arange(64))
        cols.append(2176 + g * 64 + np.arange(64))
    w['w_inCf'] = f(w_in[:, :, np.concatenate(cols)])
    cols = []
    for g in range(2):
        cols.append(2432 + g * 64 + np.arange(64))
        cols.append(2688 + g * 64 + np.arange(64))
        cols.append(2816 + g * 12 + np.arange(12))
    w['w_inCt'] = f(w_in[:, :, np.concatenate(cols)])
    w['WsT'] = f(inp['a_ws'].transpose(0, 3, 1, 2).reshape(L, 128, 512))
    w['bsT'] = _pad64(inp['a_bs'].transpose(0, 2, 1))
    w['a_lng'] = f(np.broadcast_to(inp['a_ln_g'].reshape(L, 1, 256), (L, 128, 256)))
    w['a_lnb'] = f(np.broadcast_to(inp['a_ln_b'].reshape(L, 1, 256), (L, 128, 256)))
    w['b_gng'] = f(np.broadcast_to(inp['b_gn_g'].reshape(L, 1, 256), (L, 128, 256)))
    w['b_gnb'] = f(np.broadcast_to(inp['b_gn_b'].reshape(L, 1, 256), (L, 128, 256)))
    posT = np.concatenate([inp['c_pos_k'].transpose(0, 2, 1), inp['c_pos_v'].transpose(0, 2, 1)], 1)
    w['posT'] = _pad64(posT)
    w1k = inp['c_w1_k'].reshape(L, 32, 64, 64).transpose(0, 2, 1, 3)
    w1v = inp['c_w1_v'].reshape(L, 32, 64, 64).transpose(0, 2, 1, 3)
    w['w1s'] = f(np.concatenate([w1k, w1v], 1).reshape(L, 128, 2048))
    w['w2k'] = f(np.concatenate([inp['c_w2_k'], inp['c_w2_k']], 2))
    w['w2v'] = f(inp['c_w2_v'])
    w['w_out'] = f(inp['w_out'])
    w['lnpk'] = _pad64(np.concatenate([inp[k].reshape(L, 8, 128).transpose(0, 2, 1)
                                       for k in ('ln1_g', 'ln1_b', 'ln2_g', 'ln2_b')], 2))
    w['w_up'] = f(inp['w_up'])
    w['cwT'] = f(inp['conv_w'].reshape(L, 3, 44, 128).transpose(0, 3, 2, 1).reshape(L, 128, 132))
    w['cbT'] = _pad64(inp['conv_b'].reshape(L, 44, 128).transpose(0, 2, 1))
    w['w_down'] = f(inp['w_down'])
    return w


W_SHAPES = {'w_ada': [2, 1024, 6144], 'b_adaT': [2, 128, 64], 'w_inA': [2, 1024, 512], 'w_inBf': [2, 1024, 1024],
            'w_inBt': [2, 1024, 512], 'w_inCf': [2, 1024, 1280], 'w_inCt': [2, 1024, 280], 'WsT': [2, 128, 512],
            'bsT': [2, 128, 64], 'a_lng': [2, 128, 256], 'a_lnb': [2, 128, 256], 'b_gng': [2, 128, 256], 'b_gnb': [2, 128, 256],
            'posT': [2, 128, 64], 'w1s': [2, 128, 2048], 'w2k': [2, 64, 128], 'w2v': [2, 64, 64],
            'w_out': [2, 1024, 1024], 'lnpk': [2, 128, 64],
            'w_up': [2, 1024, 5632], 'cwT': [2, 128, 132], 'cbT': [2, 128, 64], 'w_down': [2, 2816, 1024]}


def build(n_layers=DEPTH, mixers=('A', 'B', 'C'), do_ffn=True, dbg=None):
    nc = bass.Bass("TRN2", target_bir_lowering=False)
    Dr = {}
    Dr['x'] = nc.dram_tensor("x", [SEQ, DM], F32, kind="ExternalInput").ap()
    Dr['cT'] = nc.dram_tensor("cT", [128, 64], F32, kind="ExternalInput").ap()
    for k, shp in W_SHAPES.items():
        Dr[k] = nc.dram_tensor(k, shp, F32, kind="ExternalInput").ap()
    for k, shp in TABLE_SHAPES.items():
        Dr[k] = nc.dram_tensor(k, shp, F32, kind="ExternalInput").ap()
    out_d = nc.dram_tensor("out", [SEQ, DM], F32, kind="ExternalOutput").ap()
    dbg_d = None
    if dbg is not None:
        dbg_d = nc.dram_tensor("dbg", [SEQ, DM], F32, kind="ExternalOutput").ap()

    st = ExitStack()
    with st:
        S = Sched(nc, st)
        T = lambda name, shape, dt=F32: st.enter_context(nc.sbuf_tensor("s_" + name, shape, dt))
        xT = T("xT", [128, 8, SEQ], F32)
        hT = T("hT", [128, 8, SEQ], BF16)
        WR = T("WR", [128, 4, 4096], BF16)
        AW = 51 * 256 - 128
        ARENA = T("ARENA", [128, AW], F32)
        PSA = st.enter_context(nc.psum_tensor("PSA", [128, 2048], F32))
        PSB = st.enter_context(nc.psum_tensor("PSB", [128, 2048], F32))

        def bank(i):
            t = PSA if i < 4 else PSB
            return t[:, (i % 4) * 512:(i % 4 + 1) * 512]

        BK = ['B%d' % i for i in range(8)]

        class Arena:
            def __init__(self):
                self.off = 0

            def reset(self, off=0):
                self.off = off

            def get(self, shape, dt=F32):
                n = int(np.prod(shape))
                nb = n * (2 if dt == BF16 else 4)
                w0 = self.off // 4
                w1 = w0 + (nb + 3) // 4
                assert w1 <= AW, ("arena overflow", w1 * 4, AW * 4)
                self.off = w1 * 4
                ap = ARENA[:, w0:w1]
                if dt == BF16:
                    ap = ap.bitcast(BF16)
                ap = ap[:, 0:n]
                if len(shape) == 2:
                    ap = ap.rearrange("p (a b) -> p a b", a=shape[0])
                elif len(shape) == 3:
                    ap = ap.rearrange("p (a b c) -> p a b c", a=shape[0], b=shape[1])
                return ap

        AR = Arena()

        def MM(out, lhsT, rhs, start, stop, rd, wr):
            S.op('pe', lambda e: e.matmul(out, lhsT=lhsT, rhs=rhs, start=start, stop=stop, skip_group_check=True), rd, wr)

        def TR(out, in_, ident, rd, wr):
            S.op('pe', lambda e: e.transpose(out, in_, ident), rd, wr)

        def ACT(out, in_, func, rd, wr, scale=1.0, bias=None):
            if bias is None:
                S.op('act', lambda e: e.activation(out, in_, func, scale=scale), rd, wr)
            else:
                S.op('act', lambda e: e.activation(out, in_, func, bias=bias, scale=scale), rd, wr)

        def TT(out, in0, in1, op, rd, wr, eng='dve'):
            S.op(eng, lambda e: e.tensor_tensor(out, in0, in1, op), rd, wr)

        def TS(out, in0, s1, s2, op0, op1, rd, wr, eng='dve'):
            if s2 is None:
                S.op(eng, lambda e: e.tensor_scalar(out, in0, s1, None, op0=op0), rd, wr)
            else:
                S.op(eng, lambda e: e.tensor_scalar(out, in0, s1, s2, op0=op0, op1=op1), rd, wr)

        def STT(out, in0, scalar, in1, op0, op1, rd, wr):
            S.op('dve', lambda e: e.scalar_tensor_tensor(out, in0, scalar, in1, op0=op0, op1=op1), rd, wr)

        def CP(out, in_, rd, wr, eng='dve'):
            if eng == 'act':
                S.op('act', lambda e: e.activation(out, in_, AF.Identity), rd, wr)
            else:
                S.op(eng, lambda e: e.tensor_copy(out, in_), rd, wr)

        def RED(out, in_, op, rd, wr):
            S.op('dve', lambda e: e.tensor_reduce(out, in_, axis=AX.X, op=op), rd, wr)

        def RCP(out, in_, rd, wr):
            S.op('dve', lambda e: e.reciprocal(out, in_), rd, wr)

        evt = [0]

        def EV():
            evt[0] += 1
            return 'act' if evt[0] % 2 else 'dve'

        def DMA(q, out, in_, rd, wr):
            pieces = []

            def split(o, i):
                shp = tuple(o.shape)
                assert tuple(i.shape) == shp, (shp, i.shape)
                if len(shp) == 3:
                    for a in range(shp[1]):
                        split(o[:, a, :], i[:, a, :])
                elif len(shp) == 2 and shp[1] > 512:
                    for c0 in range(0, shp[1], 512):
                        c1 = min(shp[1], c0 + 512)
                        pieces.append((o[:, c0:c1], i[:, c0:c1]))
                else:
                    pieces.append((o, i))
            split(out, in_)
            S.dma_multi(q, pieces, rd, wr)

        ident_f = T("ident_f", [128, 128], F32)
        ident_b = T("ident_b", [128, 128], BF16)
        onesm = T("onesm", [128, 128], BF16)
        epsA = T("epsA", [128, 2], F32)
        condT = T("condT", [128, 64], F32)
        condTb = T("condTb", [128, 8, 8], BF16)
        badaT = [T("badaT%d" % l_, [128, 64], F32) for l_ in range(DEPTH)]
        modT = [T("modT%d" % l, [128, 48], F32) for l in range(DEPTH)]
        drv = [T("drv%d" % l, [128, 64], F32) for l in range(DEPTH)]
        lnp = [T("lnp%d" % l, [128, 64], F32) for l in range(DEPTH)]
        cwT = T("cwT", [128, 132], F32)
        cbT = T("cbT", [128, 64], F32)
        decayT = T("decayT", [128, 512], F32)
        xiT = T("xiT", [128, 256], F32)
        zeta = T("zeta", [128, 64], F32)
        cdv = T("cdv", [128, 64], F32)
        expand = T("expand", [128, 2048], BF16)
        causalb = T("causalb", [128, 128], BF16)
        winb = T("winb", [128, 128], BF16)
        cmpb = T("cmpb", [128, 2048], BF16)
        fbias = T("fbias", [128, 512], F32)
        causal01 = T("causal01", [128, 128], F32)
        ovl = T("ovl", [128, 64], F32)
        WsT = T("WsT", [128, 512], BF16)
        bsT = T("bsT", [128, 64], F32)
        lnbc = T("lnbc", [128, 4, 256], F32)
        posT = T("posT", [128, 64], F32)
        w1s = T("w1s", [128, 2048], BF16)
        w2k = T("w2k", [64, 128], BF16)
        w2v = T("w2v", [64, 64], BF16)

        DMA('sp', ident_f[:], Dr['identf'], (), ['ident_f'])
        S.op('dve', lambda e: e.tensor_copy(ident_b[:], ident_f[:]), ['ident_f'], ['ident_b'])
        S.op('dve', lambda e: e.memset(onesm[:], 1.0 / 1024.0), (), ['onesm'])
        S.op('dve', lambda e: e.memset(epsA[:, 0:1], LN_EPS), (), ['epsA'])
        S.op('dve', lambda e: e.memset(epsA[:, 1:2], LN_EPS / (ALPHA * ALPHA)), (), ['epsA'])
        barscr = T("barscr", [128, 64], F32)
        RELAY = (barscr[:], Dr['zeta'])
        rlscr = T("rlscr", [128, 2], F32)
        S.relay_fn = lambda e: e.memset(rlscr[:, 0:1], 0.0)
        DMA('sp', condT[:], Dr['cT'], (), ['condT'])
        for nm, tl in (('decayT', decayT), ('xiT', xiT), ('zeta', zeta), ('cdv', cdv), ('fbias', fbias), ('causal01', causal01), ('ovl', ovl)):
            DMA('sp', tl[:], Dr[nm], (), [nm])
        for nm, tl in (('causalb', causalb), ('winb', winb)):
            DMA('pool', tl[:], Dr[nm], (), [nm])
        for nm, tl in (('expand', expand), ('cmpb', cmpb)):
            DMA('pool', tl[:].rearrange("p (a b) -> p a b", a=4), Dr[nm].rearrange("p (a b) -> p a b", a=4), (), [nm])
        ACT(condT[:], condT[:], AF.Silu, ['condT'], ['condT'])

        wr_n = [0]

        def wload(src_ap, shape):
            S.relay_readers(['WR%d' % j_ for j_ in range(4)])
            i = wr_n[0] % 4
            wr_n[0] += 1
            n = int(np.prod(shape))
            v = WR[:, i, 0:n]
            if len(shape) == 2:
                v = v.rearrange("p (a b) -> p a b", a=shape[0])
            key = 'WR%d' % i
            DMA('pool', v, src_ap, (), [key])
            return v, key

        def kchunks(ap2d):
            return ap2d.rearrange("(k p) n -> p k n", p=128)

        AR.reset()
        ada_buf = [AR.get([8, 512], BF16) for _ in range(2)]
        CP(condTb[:], condT[:, 0:8].unsqueeze(2).to_broadcast([128, 8, 8]), ['condT'], ['condTb'])
        for l in range(n_layers):
            DMA('sp', badaT[l][:], Dr['b_adaT'][l], (), ['bada%d' % l])
            DMA('sp', lnp[l][:], Dr['lnpk'][l], (), ['lnp%d' % l])
        for l in range(min(1, n_layers)):
            for blk in range(12):
                buf = ada_buf[blk % 2]
                bk = 'ada%d' % (blk % 2)
                DMA('pool', buf, kchunks(Dr['w_ada'][l][:, blk * 512:(blk + 1) * 512]), (), [bk])
                for jj in range(4):
                    j = blk * 4 + jj
                    for k in range(8):
                        MM(bank(0)[:, j * 8:j * 8 + 8], buf[:, k, jj * 128:(jj + 1) * 128], condTb[:, k, :],
                           k == 0, k == 7, [bk, 'condTb'], ['B0'])
            TT(modT[l][:], bank(0)[:, 0:384].rearrange('p (j r) -> p j r', r=8)[:, :, 0], badaT[l][:, 0:48], ALU.add, ['B0', 'bada%d' % l], ['modT%d' % l])

        def derive(l):
            m = modT[l]
            d = drv[l]
            mk, dk = 'modT%d' % l, 'drv%d' % l
            TS(d[:, 0:8], m[:, 8:16], 1.0, None, ALU.add, None, [mk], [dk])
            TS(d[:, 8:16], m[:, 16:24], 1.0 / ALPHA, None, ALU.mult, None, [mk], [dk])
            TS(d[:, 16:24], m[:, 32:40], 1.0, None, ALU.add, None, [mk], [dk])
            TS(d[:, 24:32], m[:, 40:48], 1.0 / ALPHA, None, ALU.mult, None, [mk], [dk])
            TT(d[:, 32:40], lnp[l][:, 0:8], d[:, 16:24], ALU.mult, ['lnp%d' % l, dk], [dk])
            TT(d[:, 40:48], lnp[l][:, 8:16], d[:, 16:24], ALU.mult, ['lnp%d' % l, dk], [dk])
            TT(d[:, 40:48], d[:, 40:48], m[:, 24:32], ALU.add, [dk, mk], [dk])

        def derive_cross(l):
            d, dn = drv[l], drv[l + 1]
            dk, dnk = 'drv%d' % l, 'drv%d' % (l + 1)
            TT(d[:, 48:56], lnp[l][:, 16:24], dn[:, 0:8], ALU.mult, ['lnp%d' % l, dnk, dk], [dk])
            TT(d[:, 56:64], lnp[l][:, 24:32], dn[:, 0:8], ALU.mult, ['lnp%d' % l, dnk, dk], [dk])
            TT(d[:, 56:64], d[:, 56:64], modT[l + 1][:, 0:8], ALU.add, [dk, 'modT%d' % (l + 1)], [dk])

        def ada_block_deferred(l, blk):
            wv_, wk_ = wload(kchunks(Dr['w_ada'][l][:, blk * 512:(blk + 1) * 512]), [8, 512])
            for jj in range(4):
                for k in range(8):
                    MM(bank(7)[:, jj * 8:jj * 8 + 8], wv_[:, k, jj * 128:(jj + 1) * 128], condTb[:, k, :],
                       k == 0, k == 7, [wk_, 'condTb'], ['B7'])
            TT(modT[l][:, blk * 4:(blk + 1) * 4], bank(7)[:, 0:32].rearrange('p (j r) -> p j r', r=8)[:, :, 0],
               badaT[l][:, blk * 4:(blk + 1) * 4], ALU.add, ['B7', 'bada%d' % l], ['modT%d' % l])
            if blk == 11:
                derive(l)
                derive_cross(l - 1)

        if n_layers >= 1:
            derive(0)

        S.barrier(relay=RELAY)
        AR.reset()
        xst = [AR.get([1024], F32) for _ in range(8)]
        bi = 0
        for tb in (range(4) if dbg != 'skip_xload' else []):
            for q in range(4):
                tt = tb * 4 + q
                DMA('sp', xst[tt % 8], Dr['x'][tt * 128:(tt + 1) * 128, :], (), ['xst%d' % (tt % 8)])
            for c in range(8):
                b = bi % 8
                bi += 1
                for q in range(4):
                    tt = tb * 4 + q
                    TR(bank(b)[:, q * 128:(q + 1) * 128], xst[tt % 8][:, c * 128:(c + 1) * 128], ident_f[:],
                       ['xst%d' % (tt % 8), 'ident_f'], [BK[b]])
                ACT(xT[:, c, tb * 512:(tb + 1) * 512], bank(b), AF.Identity, [BK[b]], ['xT%d_%d' % (c, tb)])
                if dbg != 'no_ts':
                    TS(hT[:, c, tb * 512:(tb + 1) * 512], xT[:, c, tb * 512:(tb + 1) * 512], drv[0][:, c:c + 1], modT[0][:, c:c + 1],
                       ALU.mult, ALU.add, ['xT%d_%d' % (c, tb), 'drv0', 'modT0'], ['hT%d_%d' % (c, tb)])

        def out_proj(l, mixtm, mixkeys, nch, row0, mixT, alias=()):
            nonlocal_bi = [0]
            for c in range(nch):
                for tb in range(4):
                    b = 4 + (nonlocal_bi[0] % 4)
                    nonlocal_bi[0] += 1
                    pb = bank(b).bitcast(BF16)
                    for q in range(4):
                        tt = tb * 4 + q
                        TR(pb[:, q * 128:(q + 1) * 128], mixtm[:, tt, c * 128:(c + 1) * 128], ident_b[:],
                           [mixkeys[tt], 'ident_b'], [BK[b]])
                    CP(mixT[:, c, tb * 512:(tb + 1) * 512], pb[:, 0:512], [BK[b]], ['mixT%d_%d' % (c, tb)] + list(alias), eng=EV())
            wv, wk = wload(kchunks(Dr['w_out'][l][row0:row0 + nch * 128, :]), [nch, 1024])
            for fb in range(8):
                for tb in range(4):
                    b = nonlocal_bi[0] % 4
                    nonlocal_bi[0] += 1
                    for c in range(nch):
                        MM(bank(b), wv[:, c, fb * 128:(fb + 1) * 128], mixT[:, c, tb * 512:(tb + 1) * 512],
                           c == 0, c == nch - 1, [wk, 'mixT%d_%d' % (c, tb)], [BK[b]])
                    xs = xT[:, fb, tb * 512:(tb + 1) * 512]
                    STT(xs, bank(b), drv[l][:, 8 + fb:9 + fb], xs, ALU.mult, ALU.add,
                        [BK[b], 'drv%d' % l, 'xT%d_%d' % (fb, tb)], ['xT%d_%d' % (fb, tb)])

        def layer_norm(l, which, last):
            goff = 0 if which == 1 else 16
            aoff = 32 if which == 1 else 48
            AR.reset()
            xb = [AR.get([512], BF16) for _ in range(3)]
            sq = [AR.get([512], BF16) for _ in range(3)]
            rstd = [AR.get([512], F32) for _ in range(2)]
            nmr = [AR.get([512], F32) for _ in range(2)]
            tmp = [AR.get([512], F32) for _ in range(3)]
            n = 0
            for tb in range(4):
                bm, be = 0 + 2 * (tb % 2), 1 + 2 * (tb % 2)
                for c in range(8):
                    i = n % 3
                    n += 1
                    xs = xT[:, c, tb * 512:(tb + 1) * 512]
                    xk = 'xT%d_%d' % (c, tb)
                    ACT(sq[i], xs, AF.Square, [xk], ['lsq%d' % i])
                    CP(xb[i], xs, [xk], ['lxb%d' % i], eng='dve')
                    MM(bank(bm), onesm[:], xb[i], c == 0, c == 7, ['onesm', 'lxb%d' % i], [BK[bm]])
                    MM(bank(be), onesm[:], sq[i], c == 0, c == 7, ['onesm', 'lsq%d' % i], [BK[be]])
                r = tb % 2
                rk, nk = 'lrstd%d' % r, 'lnmr%d' % r
                ACT(nmr[r], bank(bm), AF.Square, [BK[bm]], [nk])
                TT(rstd[r], bank(be), nmr[r], ALU.subtract, [BK[be], nk], [rk])
                TS(rstd[r], rstd[r], 0.0, None, ALU.max, None, [rk], [rk])
                ACT(rstd[r], rstd[r], AF.Sqrt, [rk, 'epsA'], [rk], bias=epsA[:, 1:2])
                RCP(rstd[r], rstd[r], [rk], [rk])
                STT(nmr[r], bank(bm), -1.0, rstd[r], ALU.mult, ALU.mult, [BK[bm], rk], [nk])
                for c in range(8):
                    i = n % 3
                    n += 1
                    xs = xT[:, c, tb * 512:(tb + 1) * 512]
                    xk = 'xT%d_%d' % (c, tb)
                    tk = 'ltmp%d' % i
                    TT(tmp[i], xs, rstd[r], ALU.mult, [xk, rk], [tk])
                    TT(tmp[i], tmp[i], nmr[r], ALU.add, [tk, nk], [tk])
                    ACT(xs, tmp[i], AF.Identity, [tk, 'lnp%d' % l], [xk],
                        scale=lnp[l][:, goff + c:goff + c + 1], bias=lnp[l][:, goff + 8 + c:goff + 9 + c])
                    if not last:
                        ACT(hT[:, c, tb * 512:(tb + 1) * 512], tmp[i], AF.Identity, [tk, 'drv%d' % l], ['hT%d_%d' % (c, tb)],
                            scale=drv[l][:, aoff + c:aoff + c + 1], bias=drv[l][:, aoff + 8 + c:aoff + 9 + c])

        def dump_tm(src, c0, ncol, keys, dcol):
            dst = AR.get([ncol], F32)
            for tt in range(16):
                CP(dst, src[:, tt, c0:c0 + ncol], [keys[tt]], ['dst'])
                DMA('sp', dbg_d[tt * 128:(tt + 1) * 128, dcol:dcol + ncol], dst, ['dst'], ['dbg'])

        def group_ln_core(src, sqv, nb, rk, wk_sq, eps_ap):
            s1 = AR.get([nb], F32)
            s2 = AR.get([nb], F32)
            s3 = AR.get([nb], F32)
            TT(sqv, src, src, ALU.mult, rk, [wk_sq])
            RED(s1, src, ALU.add, rk, ['gs1'])
            RED(s2, sqv, ALU.add, [wk_sq], ['gs2'])
            TS(s1, s1, 1.0 / 64, None, ALU.mult, None, ['gs1'], ['gs1'])
            TT(s3, s1, s1, ALU.mult, ['gs1'], ['gs3'])
            STT(s2, s2, 1.0 / 64, s3, ALU.mult, ALU.subtract, ['gs2', 'gs3'], ['gs2'])
            TS(s2, s2, 0.0, None, ALU.max, None, ['gs2'], ['gs2'])
            ACT(s2, s2, AF.Sqrt, ['gs2', 'epsA'], ['gs2'], bias=eps_ap)
            RCP(s2, s2, ['gs2'], ['gs2'])
            TT(sqv, src, s1.unsqueeze(2).to_broadcast([128, nb, 64]), ALU.subtract, rk + ['gs1'], [wk_sq])
            TT(sqv, sqv, s2.unsqueeze(2).to_broadcast([128, nb, 64]), ALU.mult, [wk_sq, 'gs2'], [wk_sq])

        def mixer_B(l):
            hk = lambda k, tb: 'hT%d_%d' % (k, tb)
            S.barrier(relay=RELAY)
            AR.reset()
            qrT = AR.get([2, 2048], BF16)
            krT = AR.get([2, 2048], BF16)
            off_x = AR.off
            cosT = AR.get([2048], F32)
            sinT = AR.get([2048], F32)
            rt1 = [AR.get([512], F32) for _ in range(2)]
            rt2 = [AR.get([512], F32) for _ in range(2)]
            DMA('sp', cosT, Dr['cosT'], (), ['cosT'])
            DMA('sp', sinT, Dr['sinT'], (), ['sinT'])
            DMA('sp', lnbc[:, 2, :], Dr['b_gng'][l], (), ['lnbc'])
            DMA('sp', lnbc[:, 3, :], Dr['b_gnb'][l], (), ['lnbc'])
            n = 0
            for p in range(2):
                wv, wk = wload(kchunks(Dr['w_inBf'][l][:, p * 512:(p + 1) * 512]), [8, 512])
                for kind in range(2):
                    dst = qrT if kind == 0 else krT
                    for tb in range(4):
                        i = n % 2
                        b1, b2 = 2 * (n % 4), 2 * (n % 4) + 1
                        n += 1
                        for k in range(8):
                            MM(bank(b1), wv[:, k, (2 * kind) * 128:(2 * kind + 1) * 128], hT[:, k, tb * 512:(tb + 1) * 512],
                               k == 0, k == 7, [wk, hk(k, tb)], [BK[b1]])
                        for k in range(8):
                            MM(bank(b2), wv[:, k, (2 * kind + 1) * 128:(2 * kind + 2) * 128], hT[:, k, tb * 512:(tb + 1) * 512],
                               k == 0, k == 7, [wk, hk(k, tb)], [BK[b2]])
                        TT(rt1[i], bank(b1), cosT[:, tb * 512:(tb + 1) * 512], ALU.mult, [BK[b1], 'cosT'], ['rt1_%d' % i])
                        TT(rt2[i], bank(b2), sinT[:, tb * 512:(tb + 1) * 512], ALU.mult, [BK[b2], 'sinT'], ['rt2_%d' % i])
                        TT(dst[:, p, tb * 512:(tb + 1) * 512], rt1[i], rt2[i], ALU.add, ['rt1_%d' % i, 'rt2_%d' % i],
                           ['qk%d_%d' % (kind, p)])
            wvt, wkt = wload(kchunks(Dr['w_inBt'][l]), [8, 512])
            for p in range(2):
                S.barrier(relay=RELAY)
                AR.reset(off_x)
                vg = AR.get([16, 256], BF16)
                kvs = AR.get([16, 128], F32)
                s16 = AR.get([16, 128], BF16)
                oall = AR.get([16, 128], F32)
                kz = [AR.get([128], BF16) for _ in range(2)]
                qx = [AR.get([128], BF16) for _ in range(2)]
                sT = [AR.get([256], BF16) for _ in range(2)]
                mixT = AR.get([1, 2048], BF16)
                qkk = ['qk0_%d' % p, 'qk1_%d' % p]
                for tt in range(16):
                    b = tt % 4
                    for k in range(8):
                        MM(bank(b)[:, 0:256], hT[:, k, tt * 128:(tt + 1) * 128], wvt[:, k, p * 256:(p + 1) * 256],
                           k == 0, k == 7, [hk(k, tt // 4), wkt], [BK[b]])
                    CP(vg[:, tt, 0:128], bank(b)[:, 0:128], [BK[b]], ['vg%d' % tt], eng='dve')
                    ACT(vg[:, tt, 128:256], bank(b)[:, 128:256], AF.Silu, [BK[b]], ['vg%d' % tt])
                S.op('dve', lambda e: e.memset(kvs[:, 0, :], 0.0), (), ['kvs'])
                def st_a(c):
                    i = c % 2
                    bt = 4 + (c % 2)
                    pb = bank(bt).bitcast(BF16)
                    TR(pb[:, 0:128], krT[:, p, c * 128:(c + 1) * 128], ident_b[:], [qkk[1], 'ident_b'], [BK[bt]])
                    TT(kz[i].rearrange("p (h d) -> p h d", h=2), pb[:, 0:128].rearrange("p (h d) -> p h d", h=2),
                       zeta[:, 2 * p:2 * p + 2].unsqueeze(2).to_broadcast([128, 2, 64]), ALU.mult,
                       [BK[bt], 'zeta'], ['kz%d' % i])

                def st_b(c):
                    i = c % 2
                    bm = 6 + (c % 2)
                    MM(bank(bm)[:, 0:128], kz[i], vg[:, c, 0:128], True, True, ['kz%d' % i, 'vg%d' % c], [BK[bm]])
                    STT(kvs[:, c + 1, :], kvs[:, c, :], cdv[:, p:p + 1], bank(bm)[:, 0:128], ALU.mult, ALU.add,
                        ['kvs', 'cdv', BK[bm]], ['kvs'])

                st_a(0)
                for c in range(15):
                    if c + 1 < 15:
                        st_a(c + 1)
                    st_b(c)
                CP(s16, kvs, ['kvs'], ['s16'], eng='dve')
                def b_scores(c):
                    i = c % 2
                    bs0 = 2 * (c % 2)
                    cs = slice(c * 128, (c + 1) * 128)
                    if c > 0:
                        TT(qx[i], qrT[:, p, cs], xiT[:, p * 128:(p + 1) * 128], ALU.mult, [qkk[0], 'xiT'], ['qx%d' % i])
                    for hh in range(2):
                        ps_ = slice(hh * 64, (hh + 1) * 64)
                        MM(bank(bs0 + hh)[:, 0:128], krT[ps_, p, cs], qrT[ps_, p, cs], True, True,
                           qkk, [BK[bs0 + hh]])

                def b_rest(c):
                    i = c % 2
                    bs0 = 2 * (c % 2)
                    bo0 = 4 + 2 * (c % 2)
                    TT(sT[i].rearrange("p (h l) -> p h l", h=2),
                       PSA[:, bs0 * 512:(bs0 + 2) * 512].rearrange("p (h x) -> p h x", h=2)[:, :, 0:128],
                       decayT[:, 2 * p * 128:(2 * p + 2) * 128].rearrange("p (h l) -> p h l", h=2), ALU.mult,
                       [BK[bs0], BK[bs0 + 1], 'decayT'], ['sT%d' % i])
                    for hh in range(2):
                        ps_ = slice(hh * 64, (hh + 1) * 64)
                        MM(bank(bo0 + hh)[:, 0:64], sT[i][:, hh * 128:(hh + 1) * 128], vg[:, c, hh * 64:(hh + 1) * 64],
                           True, c == 0, ['sT%d' % i, 'vg%d' % c], [BK[bo0 + hh]])
                        if c > 0:
                            MM(bank(bo0 + hh)[:, 0:64], qx[i][ps_, :], s16[ps_, c, hh * 64:(hh + 1) * 64],
                               False, True, ['qx%d' % i, 's16'], [BK[bo0 + hh]])
                    CP(oall[:, c, :].rearrange("p (h e) -> p h e", h=2),
                       PSB[:, (bo0 - 4) * 512:(bo0 - 2) * 512].rearrange("p (h x) -> p h x", h=2)[:, :, 0:64],
                       [BK[bo0], BK[bo0 + 1]], ['oall'], eng='act')

                b_scores(0)
                for c in range(16):
                    if c + 1 < 16:
                        b_scores(c + 1)
                    b_rest(c)
                o3 = oall.rearrange("p c (h e) -> p (c h) e", h=2)
                sq3 = kvs.rearrange("p c (h e) -> p (c h) e", h=2)
                gam_b = lnbc[:, 2, p * 128:(p + 1) * 128].rearrange("p (h e) -> p h e", h=2).unsqueeze(1).to_broadcast([128, 16, 2, 64])
                bet_b = lnbc[:, 3, p * 128:(p + 1) * 128].rearrange("p (h e) -> p h e", h=2).unsqueeze(1).to_broadcast([128, 16, 2, 64])
                sq4 = kvs.rearrange("p c (h e) -> p c h e", h=2)
                group_ln_core(o3, sq3, 32, ['oall'], 'kvs', epsA[:, 0:1])
                TT(sq4, sq4, gam_b, ALU.mult, ['kvs', 'lnbc'], ['kvs'])
                TT(sq4, sq4, bet_b, ALU.add, ['kvs', 'lnbc'], ['kvs'])
                vkeys = ['vg%d' % tt for tt in range(16)]
                TT(vg[:, :, 0:128], kvs, vg[:, :, 128:256], ALU.mult, ['kvs'] + vkeys, vkeys)
                if dbg == 'mixB%d_%d' % (l, p):
                    dump_tm(vg, 0, 128, vkeys, p * 128)
                out_proj(l, vg, vkeys, 1, 256 + p * 128, mixT)

        def mixer_C(l):
            hk = lambda k, tb: 'hT%d_%d' % (k, tb)
            DMA('pool', w1s[:].rearrange("p (a b) -> p a b", a=4), Dr['w1s'][l].rearrange("p (a b) -> p a b", a=4), (), ['w1s'])
            DMA('pool', w2k[:], Dr['w2k'][l], (), ['w2k'])
            DMA('pool', w2v[:], Dr['w2v'][l], (), ['w2v'])
            DMA('sp', posT[:], Dr['posT'][l], (), ['posT'])
            for g in range(2):
                S.barrier(relay=RELAY)
                AR.reset()
                qz = AR.get([4, 2048], BF16)
                KsT = AR.get([2048], BF16)
                KwT = AR.get([2048], BF16)
                Vaug = AR.get([16, 2, 65], BF16)
                gates = AR.get([16, 12], F32)
                mixC = AR.get([16, 256], BF16)
                kcmpT = AR.get([127], BF16)
                vcaug = AR.get([97], BF16)
                hidT = AR.get([2, 127], BF16)
                off_r = AR.off
                kvcT = AR.get([2048], BF16)
                kcp = AR.get([32, 127], BF16)
                wv, wk = wload(kchunks(Dr['w_inCf'][l][:, g * 640:g * 640 + 512]), [8, 512])
                wv2, wk2 = wload(kchunks(Dr['w_inCf'][l][:, g * 640 + 512:g * 640 + 640]), [8, 128])
                S.op('dve', lambda e: e.memset(qz, 0.0), (), ['qT'])
                dsts = [(None, 'qT', 0.125), (None, 'qT', 0.125), (KsT, 'KsT', 1.0), (KwT, 'KwT', 1.0),
                        (kvcT, 'kvcT', 1.0)]
                n = 0
                for bi_, (dst, dk, scl) in enumerate(dsts):
                    for tb in range(4):
                        b = n % 4
                        n += 1
                        for k in range(8):
                            lw = wv[:, k, bi_ * 128:(bi_ + 1) * 128] if bi_ < 4 else wv2[:, k, :]
                            MM(bank(b), lw, hT[:, k, tb * 512:(tb + 1) * 512], k == 0, k == 7,
                               [wk if bi_ < 4 else wk2, hk(k, tb)], [BK[b]])
                        if dst is None:
                            ACT(qz[0:64, 2 * bi_, tb * 512:(tb + 1) * 512], bank(b)[0:64, :], AF.Identity, [BK[b]], [dk], scale=scl)
                            TS(qz[64:128, 2 * bi_ + 1, tb * 512:(tb + 1) * 512], bank(b)[64:128, :], scl, None, ALU.mult, None, [BK[b]], [dk])
                        elif n % 2:
                            ACT(dst[:, tb * 512:(tb + 1) * 512], bank(b), AF.Identity, [BK[b]], [dk], scale=scl)
                        else:
                            TS(dst[:, tb * 512:(tb + 1) * 512], bank(b), scl, None, ALU.mult, None, [BK[b]], [dk])
                wv3, wk3 = wload(kchunks(Dr['w_inCt'][l][:, g * 140:(g + 1) * 140]), [8, 140])
                S.op('dve', lambda e: e.memset(Vaug[:, :, :, 64:65], 1.0), (), ['Vaug'])
                for tt in range(16):
                    b = 4 + tt % 4
                    for k in range(8):
                        MM(bank(b)[:, 0:140], hT[:, k, tt * 128:(tt + 1) * 128], wv3[:, k, :], k == 0, k == 7,
                           [hk(k, tt // 4), wk3], [BK[b]])
                    CP(Vaug[:, tt, :, 0:64], bank(b)[:, 0:128].rearrange("p (s d) -> p s d", s=2), [BK[b]], ['Vaug'], eng='dve')
                    ACT(gates[:, tt, :], bank(b)[:, 128:140], AF.Sigmoid, [BK[b]], ['gates'])
                kv_t = kvcT.tensor
                win = bass.AP(kv_t, kvcT.offset, [list(kvcT.ap[0]), [1, 32], [16, 127]])
                TT(kcp, win, posT[:, 0:32].unsqueeze(2).to_broadcast([128, 32, 127]), ALU.add, ['kvcT', 'posT'], ['kcp'])
                for kv in range(2):
                    ps_ = slice(kv * 64, (kv + 1) * 64)
                    for ll in range(32):
                        MM(bank(kv)[0:64, 0:127], w1s[ps_, ll * 64:(ll + 1) * 64], kcp[ps_, ll, :],
                           ll == 0, ll == 31, ['w1s', 'kcp'], [BK[kv]])
                    ACT(hidT[0:64, kv, :], bank(kv)[0:64, 0:127], AF.Gelu_apprx_tanh, [BK[kv]], ['hidT'])
                MM(bank(2)[:, 0:127], w2k[:], hidT[0:64, 0, :], True, True, ['w2k', 'hidT'], ['B2'])
                CP(kcmpT, bank(2)[:, 0:127], ['B2'], ['kcmpT'], eng='dve')
                MM(bank(3)[0:127, 0:64], hidT[0:64, 1, :], w2v[:], True, True, ['w2v', 'hidT'], ['B3'])
                S.op('dve', lambda e: e.memset(vcaug[:, 64:65], 1.0), (), ['vcaug'])
                CP(vcaug[0:127, 0:64], bank(3)[0:127, 0:64], ['B3'], ['vcaug'], eng='dve')
                CP(vcaug[:, 65:97], ovl[:, 0:32], ['ovl'], ['vcaug'], eng='dve')
                S.barrier(relay=RELAY)
                AR.reset(off_r)
                PT = [AR.get([512], BF16) for _ in range(4)]
                MbT = [AR.get([128], BF16) for _ in range(2)]
                for j_ in range(2):
                    S.op('dve', (lambda t_: (lambda e: e.memset(t_, 0.0)))(MbT[j_]), (), ['MbT%d' % j_])
                Mb = [AR.get([32], BF16) for _ in range(2)]
                ocg = [AR.get([4, 64], F32) for _ in range(2)]
                t1 = [AR.get([4, 64], F32) for _ in range(2)]
                t2 = [AR.get([4, 64], F32) for _ in range(2)]
                sm = [AR.get([96], F32) for _ in range(2)]
                impt = [AR.get([4, 32], F32) for _ in range(2)]
                pn = [0]

                def qslice(r, qt):
                    return qz[:, r, qt * 128:(qt + 1) * 128]

                def attend(qt, kts, KT, vsel, bS, bO, extra_fn):
                    def scores(ki):
                        kt = kts[ki]
                        bs_ = bS[ki % len(bS)]
                        extras = extra_fn(kt)
                        v4 = bank(bs_).rearrange("p (r t) -> p r t", r=4)
                        MM(v4, KT[:, kt * 128:(kt + 1) * 128], qz[:, :, qt * 128:(qt + 1) * 128],
                           True, not extras, ['KsT', 'KwT', 'qT'], [BK[bs_]])
                        for ei, (lh, rh, ks) in enumerate(extras):
                            MM(v4, lh, rh, False, ei == len(extras) - 1, ks, [BK[bs_]])

                    def rest(ki):
                        kt = kts[ki]
                        bs_ = bS[ki % len(bS)]
                        i = pn[0] % 4
                        pn[0] += 1
                        ACT(PT[i], bank(bs_), AF.Exp, [BK[bs_]], ['PT%d' % i])
                        for r in range(4):
                            MM(bank(bO)[:, r * 65:(r + 1) * 65], PT[i][:, r * 128:(r + 1) * 128], Vaug[:, kt, vsel, :],
                               ki == 0 and r == 0, ki == len(kts) - 1 and r == 3, ['PT%d' % i, 'Vaug'], [BK[bO]])

                    LA = 2
                    for ki in range(min(LA, len(kts))):
                        scores(ki)
                    for ki in range(len(kts)):
                        if ki + LA < len(kts):
                            scores(ki + LA)
                        rest(ki)

                for qt in range(16):
                    j = qt % 2
                    smj = sm[j]
                    sk = 'sm%d' % j
                    qs = slice(qt * 128, (qt + 1) * 128)
                    MM(bank(0)[0:127, :].rearrange("p (r t) -> p r t", r=4), kcmpT[:, :], qz[:, :, qs], True, False,
                       ['kcmpT', 'qT'], ['B0'])
                    MM(bank(0)[0:127, :].rearrange("p (r t) -> p r t", r=4), ident_b[0:127, 0:127],
                       cmpb[0:127, qs].unsqueeze(1).to_broadcast([127, 4, 128]), False, True, ['ident_b', 'cmpb'], ['B0'])
                    i = pn[0] % 4
                    pn[0] += 1
                    ACT(PT[i][0:127, :], bank(0)[0:127, :], AF.Exp, ['B0'], ['PT%d' % i])
                    for r in range(4):
                        MM(bank(1)[:, r * 97:(r + 1) * 97], PT[i][0:127, r * 128:(r + 1) * 128], vcaug[0:127, :],
                           r == 0, r == 3, ['PT%d' % i, 'vcaug'], ['B1'])
                    O = bank(1)[:, 0:388].rearrange("p (r c) -> p r c", r=4)
                    cb4 = causalb[:].unsqueeze(1).to_broadcast([128, 4, 128])
                    wb4 = winb[:].unsqueeze(1).to_broadcast([128, 4, 128])

                    def wmask(kt, qt=qt, cb4=cb4, wb4=wb4):
                        if kt == qt:
                            return [(ident_b[:], cb4, ['ident_b', 'causalb'])]
                        if kt == qt - 4:
                            return [(ident_b[:], wb4, ['ident_b', 'winb'])]
                        return []
                    attend(qt, list(range(max(0, qt - 4), qt + 1)), KwT, 1, [2, 3, 6, 7], 5, wmask)
                    TS(smj[:, 0:4], O[:, :, 64], 1e-30, None, ALU.max, None, ['B1'], [sk])
                    RCP(smj[:, 4:8], smj[:, 0:4], [sk], [sk])
                    TT(impt[j], O[:, :, 65:97], smj[:, 4:8].unsqueeze(2).to_broadcast([128, 4, 32]), ALU.mult, ['B1', sk], ['impt%d' % j])
                    RED(smj[:, 32:64], impt[j].rearrange("p r c -> p c r"), ALU.add, ['impt%d' % j], [sk])
                    TT(smj[:, 32:64], smj[:, 32:64], fbias[:, qt * 32:(qt + 1) * 32], ALU.add, [sk, 'fbias'], [sk])
                    S.op('dve', (lambda o_, i_: (lambda e: e.max(o_, i_)))(smj[:, 64:72], smj[:, 32:64]), [sk], [sk])
                    TS(Mb[j], smj[:, 32:64], smj[:, 71:72], -1.0, ALU.is_ge, ALU.add, [sk], ['Mb%d' % j])
                    pbt = bank(0).bitcast(BF16)
                    TR(pbt[0:32, 0:128], Mb[j], ident_b[:], ['Mb%d' % j, 'ident_b'], ['B0'])
                    CP(MbT[j][0:32, :], pbt[0:32, 0:128], ['B0'], ['MbT%d' % j], eng='dve')
                    gv = gates[:, qt, :].rearrange("p (r c) -> p r c", r=4)
                    TT(smj[:, 8:12], smj[:, 4:8], gv[:, :, 0], ALU.mult, [sk, 'gates'], [sk])
                    TT(ocg[j], O[:, :, 0:64], smj[:, 8:12].unsqueeze(2).to_broadcast([128, 4, 64]), ALU.mult, ['B1', sk], ['ocg%d' % j])

                    mb4 = MbT[j][:, :].unsqueeze(1).to_broadcast([128, 4, 128])

                    def smask(kt, qt=qt, j=j, cb4=cb4, mb4=mb4):
                        ex = [(expand[:, kt * 128:(kt + 1) * 128], mb4, ['expand', 'MbT%d' % j])]
                        if kt == qt:
                            ex.append((ident_b[:], cb4, ['ident_b', 'causalb']))
                        return ex
                    attend(qt, list(range(0, qt + 1)), KsT, 0, [2, 3, 6, 7], 4, smask)
                    Os = bank(4)[:, 0:260].rearrange("p (r c) -> p r c", r=4)
                    Ow = bank(5)[:, 0:260].rearrange("p (r c) -> p r c", r=4)
                    RCP(smj[:, 12:16], Os[:, :, 64], ['B4'], [sk])
                    TT(smj[:, 12:16], smj[:, 12:16], gv[:, :, 1], ALU.mult, [sk, 'gates'], [sk])
                    RCP(smj[:, 16:20], Ow[:, :, 64], ['B5'], [sk])
                    TT(smj[:, 16:20], smj[:, 16:20], gv[:, :, 2], ALU.mult, [sk, 'gates'], [sk])
                    TT(t1[j], Os[:, :, 0:64], smj[:, 12:16].unsqueeze(2).to_broadcast([128, 4, 64]), ALU.mult, ['B4', sk], ['t1_%d' % j])
                    TT(t2[j], Ow[:, :, 0:64], smj[:, 16:20].unsqueeze(2).to_broadcast([128, 4, 64]), ALU.mult, ['B5', sk], ['t2_%d' % j])
                    TT(t1[j], t1[j], ocg[j], ALU.add, ['t1_%d' % j, 'ocg%d' % j], ['t1_%d' % j])
                    TT(mixC[:, qt, :].rearrange("p (r d) -> p r d", r=4), t1[j], t2[j], ALU.add, ['t1_%d' % j, 't2_%d' % j], ['mixC%d' % qt])
                mkeys = ['mixC%d' % tt for tt in range(16)]
                if dbg == 'mixC%d_%d' % (l, g):
                    dump_tm(mixC, 0, 256, mkeys, g * 256)
                S.barrier(relay=RELAY)
                AR.reset(off_r)
                mixT = AR.get([2, 2048], BF16)
                out_proj(l, mixC, mkeys, 2, 512 + g * 256, mixT)

        for l in range(n_layers):
            hk = lambda k, tb: 'hT%d_%d' % (k, tb)
            if 'A' in mixers:
                S.barrier(relay=RELAY)
                AR.reset()
                zag = AR.get([16, 512], BF16)
                off_sqv = AR.off
                sqv = AR.get([16, 256], F32)
                st1 = AR.get([64], F32)
                st2 = AR.get([64], F32)
                st3 = AR.get([64], F32)
                mixA = AR.get([16, 256], BF16)
                atmp = [AR.get([256], F32) for _ in range(2)]
                wstage = AR.get([4, 128], F32)
                DMA('sp', wstage, Dr['WsT'][l].rearrange("p (g t) -> p g t", g=4), (), ['wstage'])
                TT(WsT[:].rearrange("p (g t) -> p g t", g=4), wstage,
                   causal01[:].unsqueeze(1).to_broadcast([128, 4, 128]), ALU.mult, ['wstage', 'causal01'], ['WsT'])
                DMA('sp', bsT[:], Dr['bsT'][l], (), ['bsT'])
                DMA('sp', lnbc[:, 0, :], Dr['a_lng'][l], (), ['lnbcA'])
                DMA('sp', lnbc[:, 1, :], Dr['a_lnb'][l], (), ['lnbcA'])
                wv, wk = wload(kchunks(Dr['w_inA'][l]), [8, 512])
                for tt in range(16):
                    b = tt % 4
                    for k in range(8):
                        MM(bank(b), hT[:, k, tt * 128:(tt + 1) * 128], wv[:, k, :], k == 0, k == 7,
                           [hk(k, tt // 4), wk], [BK[b]])
                    ACT(zag[:, tt, :], bank(b), AF.Gelu_apprx_tanh, [BK[b]], ['zag'])
                v4 = zag[:, :, 256:512].rearrange("p t (g d) -> p t g d", g=4)
                TT(sqv, zag[:, :, 256:512], zag[:, :, 256:512], ALU.mult, ['zag'], ['sqv'])
                RED(st1.rearrange("p (t g) -> p t g", t=16), v4, ALU.add, ['zag'], ['st1'])
                RED(st2.rearrange("p (t g) -> p t g", t=16), sqv.rearrange("p t (g d) -> p t g d", g=4), ALU.add, ['sqv'], ['st2'])
                TS(st1, st1, 1.0 / 64, None, ALU.mult, None, ['st1'], ['st1'])
                TT(st3, st1, st1, ALU.mult, ['st1'], ['st3'])
                STT(st2, st2, 1.0 / 64, st3, ALU.mult, ALU.subtract, ['st2', 'st3'], ['st2'])
                TS(st2, st2, 0.0, None, ALU.max, None, ['st2'], ['st2'])
                ACT(st2, st2, AF.Sqrt, ['st2', 'epsA'], ['st2'], bias=epsA[:, 0:1])
                RCP(st2, st2, ['st2'], ['st2'])
                mean_b = st1.rearrange("p (t g) -> p t g", t=16).unsqueeze(3).to_broadcast([128, 16, 4, 64])
                rstd_b = st2.rearrange("p (t g) -> p t g", t=16).unsqueeze(3).to_broadcast([128, 16, 4, 64])
                sq4 = sqv.rearrange("p t (g d) -> p t g d", g=4)
                TT(sq4, v4, mean_b, ALU.subtract, ['zag', 'st1'], ['sqv'])
                TT(sq4, sq4, rstd_b, ALU.mult, ['sqv', 'st2'], ['sqv'])
                gam_b = lnbc[:, 0, :].unsqueeze(1).to_broadcast([128, 16, 256])
                bet_b = lnbc[:, 1, :].unsqueeze(1).to_broadcast([128, 16, 256])
                TT(sqv, sqv, gam_b, ALU.mult, ['sqv', 'lnbcA'], ['sqv'])
                TT(zag[:, :, 256:512], sqv, bet_b, ALU.add, ['sqv', 'lnbcA'], ['zag'])
                bs_b = bsT[:, 0:4].unsqueeze(2).to_broadcast([128, 4, 64])
                for tt in range(16):
                    b = 4 + tt % 4
                    for g in range(4):
                        MM(bank(b)[:, g * 64:(g + 1) * 64], WsT[:, g * 128:(g + 1) * 128],
                           zag[:, tt, 256 + g * 64:256 + (g + 1) * 64], g == 0, g == 3, ['WsT', 'zag'], [BK[b]])
                    at = atmp[tt % 2]
                    ak = 'atmp%d' % (tt % 2)
                    TT(at.rearrange("p (g d) -> p g d", g=4), bank(b)[:, 0:256].rearrange("p (g d) -> p g d", g=4),
                       bs_b, ALU.add, [BK[b], 'bsT'], [ak])
                    TT(mixA[:, tt, :], at, zag[:, tt, 0:256], ALU.mult, [ak, 'zag'], ['mixA%d' % tt])
                if dbg == 'mixA%d' % l:
                    dst = AR.get([256], F32)
                    for tt in range(16):
                        CP(dst, mixA[:, tt, :], ['mixA%d' % tt], ['dst'])
                        DMA('sp', dbg_d[tt * 128:(tt + 1) * 128, 0:256], dst, ['dst'], ['dbg'])
                AR.reset(off_sqv)
                mixT = AR.get([2, 2048], BF16)
                out_proj(l, mixA, ['mixA%d' % tt for tt in range(16)], 2, 0, mixT, ['sqv'])

            if 'B' in mixers:
                mixer_B(l)
            if 'C' in mixers:
                mixer_C(l)

            S.barrier(relay=RELAY)
            layer_norm(l, 1, last=False)
            if dbg == 'ln1_%d' % l:
                break

            if do_ffn:
                S.barrier(relay=RELAY)
                AR.reset()
                DMA('sp', cwT[:], Dr['cwT'][l], (), ['cwT'])
                DMA('sp', cbT[:], Dr['cbT'][l], (), ['cbT'])
                gT = AR.get([max(FF_SPLIT), 2048], BF16)
                ctmp = [[AR.get([1024], F32) for _ in range(2)] for _ in range(2)]
                sgt = [AR.get([1024], BF16) for _ in range(2)]
                j0 = 0
                PS4 = [PSA, PSB]
                P4K = [BK[0:4], BK[4:8]]
                for pi_, part_n in enumerate(FF_SPLIT):
                    if l + 1 < n_layers:
                        for blk_ in range(3 * pi_, 3 * pi_ + 3):
                            ada_block_deferred(l + 1, blk_)
                    wgrp = {}
                    for jl in range(part_n):
                        j = j0 + jl
                        if jl % 4 == 0:
                            ng = min(4, part_n - jl)
                            for part in range(2):
                                jj0 = part * 22 + j
                                wgrp[part] = wload(kchunks(Dr['w_up'][l][:, jj0 * 128:(jj0 + ng) * 128]), [8, ng * 128])
                        for part in range(2):
                            jj = part * 22 + j
                            wvf, wk = wgrp[part]
                            wv = wvf[:, :, (jl % 4) * 128:(jl % 4 + 1) * 128]
                            ps = PS4[part]
                            for tb in range(4):
                                for k in range(8):
                                    MM(ps[:, tb * 512:(tb + 1) * 512], wv[:, k, :], hT[:, k, tb * 512:(tb + 1) * 512],
                                       k == 0, k == 7, [wk, hk(k, tb)], [P4K[part][tb]])
                            for hf in range(2):
                                ct = ctmp[part][hf]
                                ck = 'ctmp%d_%d' % (part, hf)
                                o = hf * 1024
                                pk = P4K[part][2 * hf:2 * hf + 2]
                                pkp = P4K[part][max(0, 2 * hf - 1):2 * hf + 2]
                                ACT(ct, ps[:, o:o + 1024], AF.Identity, pk + ['cwT', 'cbT'], [ck],
                                    scale=cwT[:, jj * 3 + 2:jj * 3 + 3], bias=cbT[:, jj:jj + 1])
                                if hf == 0:
                                    STT(ct[:, 1:1024], ps[:, 0:1023], cwT[:, jj * 3 + 1:jj * 3 + 2], ct[:, 1:1024],
                                        ALU.mult, ALU.add, pk + ['cwT', ck], [ck])
                                    STT(ct[:, 2:1024], ps[:, 0:1022], cwT[:, jj * 3:jj * 3 + 1], ct[:, 2:1024],
                                        ALU.mult, ALU.add, pk + ['cwT', ck], [ck])
                                else:
                                    STT(ct, ps[:, o - 1:o + 1023], cwT[:, jj * 3 + 1:jj * 3 + 2], ct,
                                        ALU.mult, ALU.add, pkp + ['cwT', ck], [ck])
                                    STT(ct, ps[:, o - 2:o + 1022], cwT[:, jj * 3:jj * 3 + 1], ct,
                                        ALU.mult, ALU.add, pkp + ['cwT', ck], [ck])
                                if part == 0:
                                    ACT(sgt[hf], ct, AF.Silu, [ck], ['sgt%d' % hf])
                                else:
                                    TT(gT[:, jl, o:o + 1024], ct, sgt[hf], ALU.mult, [ck, 'sgt%d' % hf], ['gT%d' % jl])
                    nsl = (part_n + 3) // 4
                    wvs = []
                    for s in range(nsl):
                        r0 = (j0 + 4 * s) * 128
                        nr = min(4, part_n - 4 * s)
                        wvs.append(wload(kchunks(Dr['w_down'][l][r0:r0 + nr * 128, :]), [nr, 1024]))
                    bi2 = 0
                    for fb in range(8):
                        for tb in range(4):
                            b = bi2 % 8
                            bi2 += 1
                            for jl in range(part_n):
                                wv, wk = wvs[jl // 4]
                                MM(bank(b), wv[:, jl % 4, fb * 128:(fb + 1) * 128], gT[:, jl, tb * 512:(tb + 1) * 512],
                                   jl == 0, jl == part_n - 1, [wk, 'gT%d' % jl], [BK[b]])
                            xs = xT[:, fb, tb * 512:(tb + 1) * 512]
                            STT(xs, bank(b), drv[l][:, 24 + fb:25 + fb], xs, ALU.mult, ALU.add,
                                [BK[b], 'drv%d' % l, 'xT%d_%d' % (fb, tb)], ['xT%d_%d' % (fb, tb)])
                    j0 += part_n
                S.barrier(relay=RELAY)
            layer_norm(l, 2, last=(l == n_layers - 1))

        S.barrier(relay=RELAY)
        AR.reset()
        ost = [AR.get([1024], F32) for _ in range(4)]
        bi = 0
        for tt in range(16):
            o = ost[tt % 4]
            ok = 'ost%d' % (tt % 4)
            for cg in range(2):
                b = bi % 8
                bi += 1
                for q in range(4):
                    c = cg * 4 + q
                    TR(bank(b)[:, q * 128:(q + 1) * 128], xT[:, c, tt * 128:(tt + 1) * 128], ident_f[:],
                       ['xT%d_%d' % (c, tt // 4), 'ident_f'], [BK[b]])
                CP(o[:, cg * 512:(cg + 1) * 512], bank(b), [BK[b]], [ok], eng=EV())
            DMA('sp', out_d[tt * 128:(tt + 1) * 128, :], o, [ok], ['out'])
        S.finish()
        S.replay()
    return nc


_CACHE = {}


def kernel(**inputs):
    inp = {k: np.asarray(v) for k, v in inputs.items()}
    if 'nc' not in _CACHE:
        _CACHE['nc'] = build()
    nc = _CACHE['nc']
    w = _prep_weights(inp)
    tb = _tables()
    in_maps = []
    for b in range(8):
        m = dict(w)
        m.update(tb)
        m['x'] = np.ascontiguousarray(inp['x'][b], dtype=np.float32)
        m['cT'] = _pad64(inp['c'][b].reshape(8, 128).T)
        in_maps.append(m)
    res = run_bass_kernel_spmd(nc, in_maps, core_ids=list(range(8)))
    out = np.stack([np.asarray(r['out'], dtype=np.float32) for r in res.results], 0)
    return out
```

```python
import math
from contextlib import ExitStack
import numpy as np
import concourse.bass as bass
import concourse.mybir as mybir
from concourse.bass_utils import run_bass_kernel_spmd

F32 = mybir.dt.float32
BF16 = mybir.dt.bfloat16
AF = mybir.ActivationFunctionType
ALU = mybir.AluOpType
AX = mybir.AxisListType

ENGS = ['pe', 'dve', 'act', 'pool', 'sp']
NRING = 8
DEPTH = 2
SEQ = 2048
DM = 1024
ALPHA = (2 * DEPTH) ** 0.25
LN_EPS = 1e-5
NEGB = -30000.0
FF_SPLIT = [6, 6, 5, 5]


class Sched:
    def __init__(self, nc, stack):
        self.nc = nc
        self.prog = {e: [] for e in ENGS}
        self.cnt = {e: 0 for e in ENGS}
        self.seen = {e: {} for e in ENGS}
        self.lastw = {}
        self.readers = {}
        self.sems = {}
        self.semval = {}
        self.relay_fn = None
        for e in ENGS:
            self.sems[e] = stack.enter_context(nc.semaphore("s_" + e))
        self.dma_n = {}
        for q in ['sp', 'act', 'pool']:
            self.dma_n[q] = 0
            for j in range(NRING):
                nm = "d_%s%d" % (q, j)
                self.sems[nm] = stack.enter_context(nc.semaphore(nm))

    def _deps(self, eng, reads, writes, is_dma=False):
        deps = []
        for k in reads:
            t = self.lastw.get(k)
            if t is not None:
                deps.append((t, 'raw'))
        for k in writes:
            t = self.lastw.get(k)
            if t is not None:
                deps.append((t, 'waw'))
            for s, v in self.readers.get(k, {}).items():
                deps.append(((s, v), 'war'))
        need = {}
        for (s, v), kind in deps:
            if s == eng and not is_dma:
                if kind != 'raw' or eng == 'pe':
                    continue
            if self.seen[eng].get(s, 0) >= v:
                continue
            if need.get(s, 0) < v:
                need[s] = v
        return need

    def _emit_waits(self, eng, need):
        if eng in ('sp', 'pool') and 'pe' in need and self.relay_fn is not None:
            need = dict(need)
            v = need.pop('pe')
            self.seen[eng]['pe'] = v
            R = 'dve'
            if self.seen[R].get('pe', 0) < v:
                self.prog[R].append(('wait', 'pe', v))
                self.seen[R]['pe'] = v
            lr = getattr(self, 'last_relay', 0)
            if lr and self.seen[R].get(R, 0) < lr:
                self.prog[R].append(('wait', R, lr))
                self.seen[R][R] = lr
            self.cnt[R] += 1
            self.last_relay = self.cnt[R]
            self.semval[R] = self.cnt[R]
            self.prog[R].append(('op', self.relay_fn, R, 1))
            if self.seen[eng].get(R, 0) < self.cnt[R]:
                need[R] = max(need.get(R, 0), self.cnt[R])
        for s, v in need.items():
            self.prog[eng].append(('wait', s, v))
            self.seen[eng][s] = v

    def _commit(self, tok, reads, writes):
        for k in writes:
            self.lastw[k] = tok
            self.readers[k] = {}
        for k in reads:
            d = self.readers.setdefault(k, {})
            if d.get(tok[0], 0) < tok[1]:
                d[tok[0]] = tok[1]

    def relay_readers(self, keys):
        if self.relay_fn is None:
            return
        v = 0
        for k in keys:
            d = self.readers.get(k)
            if d and 'pe' in d:
                v = max(v, d['pe'])
        if v == 0:
            return
        R = 'dve'
        if self.seen[R].get('pe', 0) < v:
            self.prog[R].append(('wait', 'pe', v))
            self.seen[R]['pe'] = v
        lr = getattr(self, 'last_relay', 0)
        if lr and self.seen[R].get(R, 0) < lr:
            self.prog[R].append(('wait', R, lr))
            self.seen[R][R] = lr
        self.cnt[R] += 1
        self.semval[R] = self.cnt[R]
        self.last_relay = self.cnt[R]
        self.prog[R].append(('op', self.relay_fn, R, 1))
        for k in keys:
            d = self.readers.get(k)
            if d and 'pe' in d:
                d.pop('pe')
                d[R] = max(d.get(R, 0), self.cnt[R])

    def op(self, eng, fn, reads=(), writes=()):
        need = self._deps(eng, reads, writes)
        self._emit_waits(eng, need)
        self.cnt[eng] += 1
        tok = (eng, self.cnt[eng])
        self.semval[eng] = self.cnt[eng]
        self.prog[eng].append(('op', fn, eng, 1))
        self._commit(tok, reads, writes)
        return tok

    def dma_multi(self, q, pairs, reads=(), writes=()):
        i = self.dma_n[q]
        self.dma_n[q] += 1
        slot = "d_%s%d" % (q, i % NRING)
        prev = self.semval.get(slot, 0)
        val = prev + 16 * len(pairs)
        need = self._deps(q, reads, writes, is_dma=True)
        if prev > 0 and self.seen[q].get(slot, 0) < prev:
            need[slot] = max(need.get(slot, 0), prev)
        self._emit_waits(q, need)
        for out, in_ in pairs:
            self.prog[q].append(('op', (lambda o_, i_: (lambda e: e.dma_start(out=o_, in_=i_)))(out, in_), slot, 16))
        self.semval[slot] = val
        tok = (slot, val)
        self._commit(tok, reads, writes)
        return tok

    def dma(self, q, out, in_, reads=(), writes=()):
        return self.dma_multi(q, [(out, in_)], reads, writes)

    def _wait_all(self, e):
        need = {}
        for s_, v in self.semval.items():
            if s_ == e:
                continue
            if self.seen[e].get(s_, 0) < v:
                need[s_] = v
        self._emit_waits(e, need)

    def barrier(self, engs=('pe', 'dve', 'act', 'sp'), relay=None):
        if relay is None or 'sp' not in engs:
            for e in engs:
                self._wait_all(e)
            return
        snap = dict(self.semval)
        self._wait_all('sp')
        tok = self.dma('sp', relay[0], relay[1], (), ['__bar'])
        for e in engs:
            if e == 'sp':
                continue
            self._emit_waits(e, {tok[0]: tok[1]} if self.seen[e].get(tok[0], 0) < tok[1] else {})
            for s_, v in snap.items():
                if s_ != e and self.seen[e].get(s_, 0) < v:
                    self.seen[e][s_] = v

    def finish(self):
        self.barrier(engs=('sp',))

    def replay(self):
        nc = self.nc
        sems = self.sems
        prog = self.prog

        def run(e, name):
            for it in prog[name]:
                if it[0] == 'wait':
                    e.wait_ge(sems[it[1]], it[2])
                else:
                    it[1](e).then_inc(sems[it[2]], it[3])

        with nc.Block() as block:
            @block.tensor
            def _(e):
                run(e, 'pe')

            @block.vector
            def _(e):
                run(e, 'dve')

            @block.scalar
            def _(e):
                run(e, 'act')

            @block.gpsimd
            def _(e):
                run(e, 'pool')

            @block.sync
            def _(e):
                run(e, 'sp')


def _pad64(a):
    a = np.asarray(a, dtype=np.float32)
    out = np.zeros(a.shape[:-1] + (64,), np.float32)
    out[..., :a.shape[-1]] = a
    return out


def _tables():
    t = {}
    half = 32
    inv = np.power(np.float32(10000.0), -np.arange(half, dtype=np.float32) / np.float32(half)).astype(np.float32)
    pos = np.arange(SEQ, dtype=np.float32)
    ang = pos[:, None] * inv[None, :]
    cos = np.cos(ang).astype(np.float32).T
    sin = np.sin(ang).astype(np.float32).T
    cosT = np.concatenate([cos, cos, cos, cos], 0)
    sinT = np.concatenate([-sin, sin, -sin, sin], 0)
    t['cosT'] = np.ascontiguousarray(cosT)
    t['sinT'] = np.ascontiguousarray(sinT)
    H = 4
    L = 128
    lg = np.log1p(-np.exp2(-5.0 - np.arange(H, dtype=np.float32))).astype(np.float32)
    idx = np.arange(L, dtype=np.float32)
    diff = idx[:, None] - idx[None, :]
    dec = np.where(diff >= 0, np.exp(lg[:, None, None] * np.maximum(diff, 0.0)), 0.0).astype(np.float32)
    t['decayT'] = np.ascontiguousarray(np.transpose(dec, (2, 0, 1)) * np.float32(0.125)).reshape(128, 512)
    xi = np.exp(lg[:, None] * (idx + 1.0)).astype(np.float32)
    zeta = np.exp(lg[:, None] * (L - 1.0 - idx)).astype(np.float32)
    xiT = np.zeros((128, 2, 128), np.float32)
    for p in range(2):
        for hh in range(2):
            xiT[hh * 64:(hh + 1) * 64, p, :] = xi[2 * p + hh][None, :]
    t['xiT'] = xiT.reshape(128, 256)
    t['zeta'] = _pad64(zeta.T * np.float32(0.125))
    cd = np.exp(lg * L).astype(np.float32)
    cdv = np.zeros((128, 2), np.float32)
    for p in range(2):
        for hh in range(2):
            cdv[hh * 64:(hh + 1) * 64, p] = cd[2 * p + hh]
    t['cdv'] = _pad64(cdv)
    key = np.arange(SEQ)
    ex = np.zeros((128, SEQ), np.float32)
    ex[key // 64, key] = -NEGB
    t['expand'] = ex
    kk = np.arange(128)[:, None]
    tt = np.arange(128)[None, :]
    t['causalb'] = np.where(kk > tt, NEGB, 0.0).astype(np.float32)
    t['winb'] = np.where(kk <= tt, NEGB, 0.0).astype(np.float32)
    t['identf'] = np.eye(128, dtype=np.float32)
    t['causal01'] = np.where(tt >= kk, 1.0, 0.0).astype(np.float32)
    k127 = np.arange(128)[:, None]
    tpos = np.arange(SEQ)[None, :]
    t['cmpb'] = np.where(16 * k127 + 31 > tpos, NEGB, 0.0).astype(np.float32)
    fb = np.zeros((128, 16, 32), np.float32)
    for qt in range(16):
        tq = qt * 128 + np.arange(128)
        cur = tq // 64
        blk = np.arange(32)
        future = blk[None, :] > cur[:, None]
        forced = (blk[None, :] == 0) | (blk[None, :] == cur[:, None]) | (blk[None, :] == cur[:, None] - 1)
        fb[:, qt, :] = np.where(forced, 1e30, np.where(future, -1e30, 0.0))
    t['fbias'] = fb.reshape(128, 512)
    ov = np.zeros((128, 32), np.float32)
    c0 = np.arange(127)[:, None] * 16
    s0 = np.arange(32)[None, :] * 64
    ov[:127] = np.clip(np.minimum(c0 + 32, s0 + 64) - np.maximum(c0, s0), 0, None) / 32.0
    t['ovl'] = _pad64(ov)
    return t


TABLE_SHAPES = {'cosT': [128, 2048], 'sinT': [128, 2048], 'decayT': [128, 512], 'xiT': [128, 256],
                'zeta': [128, 64], 'cdv': [128, 64], 'expand': [128, 2048], 'causalb': [128, 128],
                'winb': [128, 128], 'causal01': [128, 128], 'identf': [128, 128], 'cmpb': [128, 2048], 'fbias': [128, 512], 'ovl': [128, 64]}


def _prep_weights(inp):
    w = {}
    f = lambda a: np.ascontiguousarray(a, dtype=np.float32)
    w_in = inp['w_in']
    L = DEPTH
    w['w_ada'] = f(inp['w_ada'])
    w['b_adaT'] = _pad64(inp['b_ada'].reshape(L, 48, 128).transpose(0, 2, 1))
    w['w_inA'] = f(w_in[:, :, 0:512])
    cols = []
    for p in range(2):
        for base in (512, 768):
            hd = np.arange(128)
            h = 2 * p + hd // 64
            d = hd % 64
            cols.append(base + h * 64 + d)
            cols.append(base + h * 64 + (d + 32) % 64)
    cols = np.concatenate(cols)
    w['w_inBf'] = f(w_in[:, :, cols])
    cols = []
    for p in range(2):
        cols.append(1024 + p * 128 + np.arange(128))
        cols.append(1280 + p * 128 + np.arange(128))
    w['w_inBt'] = f(w_in[:, :, np.concatenate(cols)])
    cols = []
    for g in range(2):
        cols.append(1536 + g * 256 + np.arange(256))
        ks = 2304 + g * 64 + np.arange(64)
        kw = 2560 + g * 64 + np.arange(64)
        cols += [ks, ks, kw, kw]
        cols.append(2048 + g * 64 + np.arange(64))
        cols.append(2176 + g * 64 + np.arange(64))
    w['w_inCf'] = f(w_in[:, :, np.concatenate(cols)])
    cols = []
    for g in range(2):
        cols.append(2432 + g * 64 + np.arange(64))
        cols.append(2688 + g * 64 + np.arange(64))
        cols.append(2816 + g * 12 + np.arange(12))
    w['w_inCt'] = f(w_in[:, :, np.concatenate(cols)])
    w['WsT'] = f(inp['a_ws'].transpose(0, 3, 1, 2).reshape(L, 128, 512))
    w['bsT'] = _pad64(inp['a_bs'].transpose(0, 2, 1))
    w['a_lng'] = f(np.broadcast_to(inp['a_ln_g'].reshape(L, 1, 256), (L, 128, 256)))
    w['a_lnb'] = f(np.broadcast_to(inp['a_ln_b'].reshape(L, 1, 256), (L, 128, 256)))
    w['b_gng'] = f(np.broadcast_to(inp['b_gn_g'].reshape(L, 1, 256), (L, 128, 256)))
    w['b_gnb'] = f(np.broadcast_to(inp['b_gn_b'].reshape(L, 1, 256), (L, 128, 256)))
    posT = np.concatenate([inp['c_pos_k'].transpose(0, 2, 1), inp['c_pos_v'].transpose(0, 2, 1)], 1)
    w['posT'] = _pad64(posT)
    w1k = inp['c_w1_k'].reshape(L, 32, 64, 64).transpose(0, 2, 1, 3)
    w1v = inp['c_w1_v'].reshape(L, 32, 64, 64).transpose(0, 2, 1, 3)
    w['w1s'] = f(np.concatenate([w1k, w1v], 1).reshape(L, 128, 2048))
    w['w2k'] = f(np.concatenate([inp['c_w2_k'], inp['c_w2_k']], 2))
    w['w2v'] = f(inp['c_w2_v'])
    w['w_out'] = f(inp['w_out'])
    w['lnpk'] = _pad64(np.concatenate([inp[k].reshape(L, 8, 128).transpose(0, 2, 1)
                                       for k in ('ln1_g', 'ln1_b', 'ln2_g', 'ln2_b')], 2))
    w['w_up'] = f(inp['w_up'])
    w['cwT'] = f(inp['conv_w'].reshape(L, 3, 44, 128).transpose(0, 3, 2, 1).reshape(L, 128, 132))
    w['cbT'] = _pad64(inp['conv_b'].reshape(L, 44, 128).transpose(0, 2, 1))
    w['w_down'] = f(inp['w_down'])
    return w


W_SHAPES = {'w_ada': [2, 1024, 6144], 'b_adaT': [2, 128, 64], 'w_inA': [2, 1024, 512], 'w_inBf': [2, 1024, 1024],
            'w_inBt': [2, 1024, 512], 'w_inCf': [2, 1024, 1280], 'w_inCt': [2, 1024, 280], 'WsT': [2, 128, 512],
            'bsT': [2, 128, 64], 'a_lng': [2, 128, 256], 'a_lnb': [2, 128, 256], 'b_gng': [2, 128, 256], 'b_gnb': [2, 128, 256],
            'posT': [2, 128, 64], 'w1s': [2, 128, 2048], 'w2k': [2, 64, 128], 'w2v': [2, 64, 64],
            'w_out': [2, 1024, 1024], 'lnpk': [2, 128, 64],
            'w_up': [2, 1024, 5632], 'cwT': [2, 128, 132], 'cbT': [2, 128, 64], 'w_down': [2, 2816, 1024]}


def build(n_layers=DEPTH, mixers=('A', 'B', 'C'), do_ffn=True, dbg=None):
    nc = bass.Bass("TRN2", target_bir_lowering=False)
    Dr = {}
    Dr['x'] = nc.dram_tensor("x", [SEQ, DM], F32, kind="ExternalInput").ap()
    Dr['cT'] = nc.dram_tensor("cT", [128, 64], F32, kind="ExternalInput").ap()
    for k, shp in W_SHAPES.items():
        Dr[k] = nc.dram_tensor(k, shp, F32, kind="ExternalInput").ap()
    for k, shp in TABLE_SHAPES.items():
        Dr[k] = nc.dram_tensor(k, shp, F32, kind="ExternalInput").ap()
    out_d = nc.dram_tensor("out", [SEQ, DM], F32, kind="ExternalOutput").ap()
    dbg_d = None
    if dbg is not None:
        dbg_d = nc.dram_tensor("dbg", [SEQ, DM], F32, kind="ExternalOutput").ap()

    st = ExitStack()
    with st:
        S = Sched(nc, st)
        T = lambda name, shape, dt=F32: st.enter_context(nc.sbuf_tensor("s_" + name, shape, dt))
        xT = T("xT", [128, 8, SEQ], F32)
        hT = T("hT", [128, 8, SEQ], BF16)
        WR = T("WR", [128, 4, 4096], BF16)
        AW = 51 * 256 - 128
        ARENA = T("ARENA", [128, AW], F32)
        PSA = st.enter_context(nc.psum_tensor("PSA", [128, 2048], F32))
        PSB = st.enter_context(nc.psum_tensor("PSB", [128, 2048], F32))

        def bank(i):
            t = PSA if i < 4 else PSB
            return t[:, (i % 4) * 512:(i % 4 + 1) * 512]

        BK = ['B%d' % i for i in range(8)]

        class Arena:
            def __init__(self):
                self.off = 0

            def reset(self, off=0):
                self.off = off

            def get(self, shape, dt=F32):
                n = int(np.prod(shape))
                nb = n * (2 if dt == BF16 else 4)
                w0 = self.off // 4
                w1 = w0 + (nb + 3) // 4
                assert w1 <= AW, ("arena overflow", w1 * 4, AW * 4)
                self.off = w1 * 4
                ap = ARENA[:, w0:w1]
                if dt == BF16:
                    ap = ap.bitcast(BF16)
                ap = ap[:, 0:n]
                if len(shape) == 2:
                    ap = ap.rearrange("p (a b) -> p a b", a=shape[0])
                elif len(shape) == 3:
                    ap = ap.rearrange("p (a b c) -> p a b c", a=shape[0], b=shape[1])
                return ap

        AR = Arena()

        def MM(out, lhsT, rhs, start, stop, rd, wr):
            S.op('pe', lambda e: e.matmul(out, lhsT=lhsT, rhs=rhs, start=start, stop=stop, skip_group_check=True), rd, wr)

        def TR(out, in_, ident, rd, wr):
            S.op('pe', lambda e: e.transpose(out, in_, ident), rd, wr)

        def ACT(out, in_, func, rd, wr, scale=1.0, bias=None):
            if bias is None:
                S.op('act', lambda e: e.activation(out, in_, func, scale=scale), rd, wr)
            else:
                S.op('act', lambda e: e.activation(out, in_, func, bias=bias, scale=scale), rd, wr)

        def TT(out, in0, in1, op, rd, wr, eng='dve'):
            S.op(eng, lambda e: e.tensor_tensor(out, in0, in1, op), rd, wr)

        def TS(out, in0, s1, s2, op0, op1, rd, wr, eng='dve'):
            if s2 is None:
                S.op(eng, lambda e: e.tensor_scalar(out, in0, s1, None, op0=op0), rd, wr)
            else:
                S.op(eng, lambda e: e.tensor_scalar(out, in0, s1, s2, op0=op0, op1=op1), rd, wr)

        def STT(out, in0, scalar, in1, op0, op1, rd, wr):
            S.op('dve', lambda e: e.scalar_tensor_tensor(out, in0, scalar, in1, op0=op0, op1=op1), rd, wr)

        def CP(out, in_, rd, wr, eng='dve'):
            if eng == 'act':
                S.op('act', lambda e: e.activation(out, in_, AF.Identity), rd, wr)
            else:
                S.op(eng, lambda e: e.tensor_copy(out, in_), rd, wr)

        def RED(out, in_, op, rd, wr):
            S.op('dve', lambda e: e.tensor_reduce(out, in_, axis=AX.X, op=op), rd, wr)

        def RCP(out, in_, rd, wr):
            S.op('dve', lambda e: e.reciprocal(out, in_), rd, wr)

        evt = [0]

        def EV():
            evt[0] += 1
            return 'act' if evt[0] % 2 else 'dve'

        def DMA(q, out, in_, rd, wr):
            pieces = []

            def split(o, i):
                shp = tuple(o.shape)
                assert tuple(i.shape) == shp, (shp, i.shape)
                if len(shp) == 3:
                    for a in range(shp[1]):
                        split(o[:, a, :], i[:, a, :])
                elif len(shp) == 2 and shp[1] > 512:
                    for c0 in range(0, shp[1], 512):
                        c1 = min(shp[1], c0 + 512)
                        pieces.append((o[:, c0:c1], i[:, c0:c1]))
                else:
                    pieces.append((o, i))
            split(out, in_)
            S.dma_multi(q, pieces, rd, wr)

        ident_f = T("ident_f", [128, 128], F32)
        ident_b = T("ident_b", [128, 128], BF16)
        onesm = T("onesm", [128, 128], BF16)
        epsA = T("epsA", [128, 2], F32)
        condT = T("condT", [128, 64], F32)
        condTb = T("condTb", [128, 8, 8], BF16)
        badaT = [T("badaT%d" % l_, [128, 64], F32) for l_ in range(DEPTH)]
        modT = [T("modT%d" % l, [128, 48], F32) for l in range(DEPTH)]
        drv = [T("drv%d" % l, [128, 64], F32) for l in range(DEPTH)]
        lnp = [T("lnp%d" % l, [128, 64], F32) for l in range(DEPTH)]
        cwT = T("cwT", [128, 132], F32)
        cbT = T("cbT", [128, 64], F32)
        decayT = T("decayT", [128, 512], F32)
        xiT = T("xiT", [128, 256], F32)
        zeta = T("zeta", [128, 64], F32)
        cdv = T("cdv", [128, 64], F32)
        expand = T("expand", [128, 2048], BF16)
        causalb = T("causalb", [128, 128], BF16)
        winb = T("winb", [128, 128], BF16)
        cmpb = T("cmpb", [128, 2048], BF16)
        fbias = T("fbias", [128, 512], F32)
        causal01 = T("causal01", [128, 128], F32)
        ovl = T("ovl", [128, 64], F32)
        WsT = T("WsT", [128, 512], BF16)
        bsT = T("bsT", [128, 64], F32)
        lnbc = T("lnbc", [128, 4, 256], F32)
        posT = T("posT", [128, 64], F32)
        w1s = T("w1s", [128, 2048], BF16)
        w2k = T("w2k", [64, 128], BF16)
        w2v = T("w2v", [64, 64], BF16)

        DMA('sp', ident_f[:], Dr['identf'], (), ['ident_f'])
        S.op('dve', lambda e: e.tensor_copy(ident_b[:], ident_f[:]), ['ident_f'], ['ident_b'])
        S.op('dve', lambda e: e.memset(onesm[:], 1.0 / 1024.0), (), ['onesm'])
        S.op('dve', lambda e: e.memset(epsA[:, 0:1], LN_EPS), (), ['epsA'])
        S.op('dve', lambda e: e.memset(epsA[:, 1:2], LN_EPS / (ALPHA * ALPHA)), (), ['epsA'])
        barscr = T("barscr", [128, 64], F32)
        RELAY = (barscr[:], Dr['zeta'])
        rlscr = T("rlscr", [128, 2], F32)
        S.relay_fn = lambda e: e.memset(rlscr[:, 0:1], 0.0)
        DMA('sp', condT[:], Dr['cT'], (), ['condT'])
        for nm, tl in (('decayT', decayT), ('xiT', xiT), ('zeta', zeta), ('cdv', cdv), ('fbias', fbias), ('causal01', causal01), ('ovl', ovl)):
            DMA('sp', tl[:], Dr[nm], (), [nm])
        for nm, tl in (('causalb', causalb), ('winb', winb)):
            DMA('pool', tl[:], Dr[nm], (), [nm])
        for nm, tl in (('expand', expand), ('cmpb', cmpb)):
            DMA('pool', tl[:].rearrange("p (a b) -> p a b", a=4), Dr[nm].rearrange("p (a b) -> p a b", a=4), (), [nm])
        ACT(condT[:], condT[:], AF.Silu, ['condT'], ['condT'])

        wr_n = [0]

        def wload(src_ap, shape):
            S.relay_readers(['WR%d' % j_ for j_ in range(4)])
            i = wr_n[0] % 4
            wr_n[0] += 1
            n = int(np.prod(shape))
            v = WR[:, i, 0:n]
            if len(shape) == 2:
                v = v.rearrange("p (a b) -> p a b", a=shape[0])
            key = 'WR%d' % i
            DMA('pool', v, src_ap, (), [key])
            return v, key

        def kchunks(ap2d):
            return ap2d.rearrange("(k p) n -> p k n", p=128)

        AR.reset()
        ada_buf = [AR.get([8, 512], BF16) for _ in range(2)]
        CP(condTb[:], condT[:, 0:8].unsqueeze(2).to_broadcast([128, 8, 8]), ['condT'], ['condTb'])
        for l in range(n_layers):
            DMA('sp', badaT[l][:], Dr['b_adaT'][l], (), ['bada%d' % l])
            DMA('sp', lnp[l][:], Dr['lnpk'][l], (), ['lnp%d' % l])
        for l in range(min(1, n_layers)):
            for blk in range(12):
                buf = ada_buf[blk % 2]
                bk = 'ada%d' % (blk % 2)
                DMA('pool', buf, kchunks(Dr['w_ada'][l][:, blk * 512:(blk + 1) * 512]), (), [bk])
                for jj in range(4):
                    j = blk * 4 + jj
                    for k in range(8):
                        MM(bank(0)[:, j * 8:j * 8 + 8], buf[:, k, jj * 128:(jj + 1) * 128], condTb[:, k, :],
                           k == 0, k == 7, [bk, 'condTb'], ['B0'])
            TT(modT[l][:], bank(0)[:, 0:384].rearrange('p (j r) -> p j r', r=8)[:, :, 0], badaT[l][:, 0:48], ALU.add, ['B0', 'bada%d' % l], ['modT%d' % l])

        def derive(l):
            m = modT[l]
            d = drv[l]
            mk, dk = 'modT%d' % l, 'drv%d' % l
            TS(d[:, 0:8], m[:, 8:16], 1.0, None, ALU.add, None, [mk], [dk])
            TS(d[:, 8:16], m[:, 16:24], 1.0 / ALPHA, None, ALU.mult, None, [mk], [dk])
            TS(d[:, 16:24], m[:, 32:40], 1.0, None, ALU.add, None, [mk], [dk])
            TS(d[:, 24:32], m[:, 40:48], 1.0 / ALPHA, None, ALU.mult, None, [mk], [dk])
            TT(d[:, 32:40], lnp[l][:, 0:8], d[:, 16:24], ALU.mult, ['lnp%d' % l, dk], [dk])
            TT(d[:, 40:48], lnp[l][:, 8:16], d[:, 16:24], ALU.mult, ['lnp%d' % l, dk], [dk])
            TT(d[:, 40:48], d[:, 40:48], m[:, 24:32], ALU.add, [dk, mk], [dk])

        def derive_cross(l):
            d, dn = drv[l], drv[l + 1]
            dk, dnk = 'drv%d' % l, 'drv%d' % (l + 1)
            TT(d[:, 48:56], lnp[l][:, 16:24], dn[:, 0:8], ALU.mult, ['lnp%d' % l, dnk, dk], [dk])
            TT(d[:, 56:64], lnp[l][:, 24:32], dn[:, 0:8], ALU.mult, ['lnp%d' % l, dnk, dk], [dk])
            TT(d[:, 56:64], d[:, 56:64], modT[l + 1][:, 0:8], ALU.add, [dk, 'modT%d' % (l + 1)], [dk])

        def ada_block_deferred(l, blk):
            wv_, wk_ = wload(kchunks(Dr['w_ada'][l][:, blk * 512:(blk + 1) * 512]), [8, 512])
            for jj in range(4):
                for k in range(8):
                    MM(bank(7)[:, jj * 8:jj * 8 + 8], wv_[:, k, jj * 128:(jj + 1) * 128], condTb[:, k, :],
                       k == 0, k == 7, [wk_, 'condTb'], ['B7'])
            TT(modT[l][:, blk * 4:(blk + 1) * 4], bank(7)[:, 0:32].rearrange('p (j r) -> p j r', r=8)[:, :, 0],
               badaT[l][:, blk * 4:(blk + 1) * 4], ALU.add, ['B7', 'bada%d' % l], ['modT%d' % l])
            if blk == 11:
                derive(l)
                derive_cross(l - 1)

        if n_layers >= 1:
            derive(0)

        S.barrier(relay=RELAY)
        AR.reset()
        xst = [AR.get([1024], F32) for _ in range(8)]
        bi = 0
        for tb in (range(4) if dbg != 'skip_xload' else []):
            for q in range(4):
                tt = tb * 4 + q
                DMA('sp', xst[tt % 8], Dr['x'][tt * 128:(tt + 1) * 128, :], (), ['xst%d' % (tt % 8)])
            for c in range(8):
                b = bi % 8
                bi += 1
                for q in range(4):
                    tt = tb * 4 + q
                    TR(bank(b)[:, q * 128:(q + 1) * 128], xst[tt % 8][:, c * 128:(c + 1) * 128], ident_f[:],
                       ['xst%d' % (tt % 8), 'ident_f'], [BK[b]])
                ACT(xT[:, c, tb * 512:(tb + 1) * 512], bank(b), AF.Identity, [BK[b]], ['xT%d_%d' % (c, tb)])
                if dbg != 'no_ts':
                    TS(hT[:, c, tb * 512:(tb + 1) * 512], xT[:, c, tb * 512:(tb + 1) * 512], drv[0][:, c:c + 1], modT[0][:, c:c + 1],
                       ALU.mult, ALU.add, ['xT%d_%d' % (c, tb), 'drv0', 'modT0'], ['hT%d_%d' % (c, tb)])

        def out_proj(l, mixtm, mixkeys, nch, row0, mixT, alias=()):
            nonlocal_bi = [0]
            for c in range(nch):
                for tb in range(4):
                    b = 4 + (nonlocal_bi[0] % 4)
                    nonlocal_bi[0] += 1
                    pb = bank(b).bitcast(BF16)
                    for q in range(4):
                        tt = tb * 4 + q
                        TR(pb[:, q * 128:(q + 1) * 128], mixtm[:, tt, c * 128:(c + 1) * 128], ident_b[:],
                           [mixkeys[tt], 'ident_b'], [BK[b]])
                    CP(mixT[:, c, tb * 512:(tb + 1) * 512], pb[:, 0:512], [BK[b]], ['mixT%d_%d' % (c, tb)] + list(alias), eng=EV())
            wv, wk = wload(kchunks(Dr['w_out'][l][row0:row0 + nch * 128, :]), [nch, 1024])
            for fb in range(8):
                for tb in range(4):
                    b = nonlocal_bi[0] % 4
                    nonlocal_bi[0] += 1
                    for c in range(nch):
                        MM(bank(b), wv[:, c, fb * 128:(fb + 1) * 128], mixT[:, c, tb * 512:(tb + 1) * 512],
                           c == 0, c == nch - 1, [wk, 'mixT%d_%d' % (c, tb)], [BK[b]])
                    xs = xT[:, fb, tb * 512:(tb + 1) * 512]
                    STT(xs, bank(b), drv[l][:, 8 + fb:9 + fb], xs, ALU.mult, ALU.add,
                        [BK[b], 'drv%d' % l, 'xT%d_%d' % (fb, tb)], ['xT%d_%d' % (fb, tb)])

        def layer_norm(l, which, last):
            goff = 0 if which == 1 else 16
            aoff = 32 if which == 1 else 48
            AR.reset()
            xb = [AR.get([512], BF16) for _ in range(3)]
            sq = [AR.get([512], BF16) for _ in range(3)]
            rstd = [AR.get([512], F32) for _ in range(2)]
            nmr = [AR.get([512], F32) for _ in range(2)]
            tmp = [AR.get([512], F32) for _ in range(3)]
            n1 = [0]
            n3 = [0]

            def p_stats(tb):
                bm, be = 0 + 2 * (tb % 2), 1 + 2 * (tb % 2)
                for c in range(8):
                    i = n1[0] % 3
                    n1[0] += 1
                    xs = xT[:, c, tb * 512:(tb + 1) * 512]
                    xk = 'xT%d_%d' % (c, tb)
                    ACT(sq[i], xs, AF.Square, [xk], ['lsq%d' % i])
                    CP(xb[i], xs, [xk], ['lxb%d' % i], eng='dve')
                    MM(bank(bm), onesm[:], xb[i], c == 0, c == 7, ['onesm', 'lxb%d' % i], [BK[bm]])
                    MM(bank(be), onesm[:], sq[i], c == 0, c == 7, ['onesm', 'lsq%d' % i], [BK[be]])

            def p_apply(tb):
                bm, be = 0 + 2 * (tb % 2), 1 + 2 * (tb % 2)
                r = tb % 2
                rk, nk = 'lrstd%d' % r, 'lnmr%d' % r
                ACT(nmr[r], bank(bm), AF.Square, [BK[bm]], [nk])
                TT(rstd[r], bank(be), nmr[r], ALU.subtract, [BK[be], nk], [rk])
                TS(rstd[r], rstd[r], 0.0, None, ALU.max, None, [rk], [rk])
                ACT(rstd[r], rstd[r], AF.Sqrt, [rk, 'epsA'], [rk], bias=epsA[:, 1:2])
                RCP(rstd[r], rstd[r], [rk], [rk])
                STT(nmr[r], bank(bm), -1.0, rstd[r], ALU.mult, ALU.mult, [BK[bm], rk], [nk])
                for c in range(8):
                    i = n3[0] % 3
                    n3[0] += 1
                    xs = xT[:, c, tb * 512:(tb + 1) * 512]
                    xk = 'xT%d_%d' % (c, tb)
                    tk = 'ltmp%d' % i
                    TT(tmp[i], xs, rstd[r], ALU.mult, [xk, rk], [tk])
                    TT(tmp[i], tmp[i], nmr[r], ALU.add, [tk, nk], [tk])
                    ACT(xs, tmp[i], AF.Identity, [tk, 'lnp%d' % l], [xk],
                        scale=lnp[l][:, goff + c:goff + c + 1], bias=lnp[l][:, goff + 8 + c:goff + 9 + c])
                    if not last:
                        ACT(hT[:, c, tb * 512:(tb + 1) * 512], tmp[i], AF.Identity, [tk, 'drv%d' % l], ['hT%d_%d' % (c, tb)],
                            scale=drv[l][:, aoff + c:aoff + c + 1], bias=drv[l][:, aoff + 8 + c:aoff + 9 + c])

            p_stats(0)
            for tb in range(4):
                if tb + 1 < 4:
                    p_stats(tb + 1)
                p_apply(tb)

        def dump_tm(src, c0, ncol, keys, dcol):
            dst = AR.get([ncol], F32)
            for tt in range(16):
                CP(dst, src[:, tt, c0:c0 + ncol], [keys[tt]], ['dst'])
                DMA('sp', dbg_d[tt * 128:(tt + 1) * 128, dcol:dcol + ncol], dst, ['dst'], ['dbg'])

        def group_ln_core(src, sqv, nb, rk, wk_sq, eps_ap):
            s1 = AR.get([nb], F32)
            s2 = AR.get([nb], F32)
            s3 = AR.get([nb], F32)
            TT(sqv, src, src, ALU.mult, rk, [wk_sq])
            RED(s1, src, ALU.add, rk, ['gs1'])
            RED(s2, sqv, ALU.add, [wk_sq], ['gs2'])
            TS(s1, s1, 1.0 / 64, None, ALU.mult, None, ['gs1'], ['gs1'])
            TT(s3, s1, s1, ALU.mult, ['gs1'], ['gs3'])
            STT(s2, s2, 1.0 / 64, s3, ALU.mult, ALU.subtract, ['gs2', 'gs3'], ['gs2'])
            TS(s2, s2, 0.0, None, ALU.max, None, ['gs2'], ['gs2'])
            ACT(s2, s2, AF.Sqrt, ['gs2', 'epsA'], ['gs2'], bias=eps_ap)
            RCP(s2, s2, ['gs2'], ['gs2'])
            TT(sqv, src, s1.unsqueeze(2).to_broadcast([128, nb, 64]), ALU.subtract, rk + ['gs1'], [wk_sq])
            TT(sqv, sqv, s2.unsqueeze(2).to_broadcast([128, nb, 64]), ALU.mult, [wk_sq, 'gs2'], [wk_sq])

        def mixer_B(l):
            hk = lambda k, tb: 'hT%d_%d' % (k, tb)
            S.barrier(relay=RELAY)
            AR.reset()
            qrT = AR.get([2, 2048], BF16)
            krT = AR.get([2, 2048], BF16)
            off_x = AR.off
            cosT = AR.get([2048], F32)
            sinT = AR.get([2048], F32)
            rt1 = [AR.get([512], F32) for _ in range(2)]
            rt2 = [AR.get([512], F32) for _ in range(2)]
            DMA('sp', cosT, Dr['cosT'], (), ['cosT'])
            DMA('sp', sinT, Dr['sinT'], (), ['sinT'])
            DMA('sp', lnbc[:, 2, :], Dr['b_gng'][l], (), ['lnbc'])
            DMA('sp', lnbc[:, 3, :], Dr['b_gnb'][l], (), ['lnbc'])
            n = 0
            for p in range(2):
                wv, wk = wload(kchunks(Dr['w_inBf'][l][:, p * 512:(p + 1) * 512]), [8, 512])
                for kind in range(2):
                    dst = qrT if kind == 0 else krT
                    for tb in range(4):
                        i = n % 2
                        b1, b2 = 2 * (n % 4), 2 * (n % 4) + 1
                        n += 1
                        for k in range(8):
                            MM(bank(b1), wv[:, k, (2 * kind) * 128:(2 * kind + 1) * 128], hT[:, k, tb * 512:(tb + 1) * 512],
                               k == 0, k == 7, [wk, hk(k, tb)], [BK[b1]])
                        for k in range(8):
                            MM(bank(b2), wv[:, k, (2 * kind + 1) * 128:(2 * kind + 2) * 128], hT[:, k, tb * 512:(tb + 1) * 512],
                               k == 0, k == 7, [wk, hk(k, tb)], [BK[b2]])
                        TT(rt1[i], bank(b1), cosT[:, tb * 512:(tb + 1) * 512], ALU.mult, [BK[b1], 'cosT'], ['rt1_%d' % i])
                        TT(rt2[i], bank(b2), sinT[:, tb * 512:(tb + 1) * 512], ALU.mult, [BK[b2], 'sinT'], ['rt2_%d' % i])
                        TT(dst[:, p, tb * 512:(tb + 1) * 512], rt1[i], rt2[i], ALU.add, ['rt1_%d' % i, 'rt2_%d' % i],
                           ['qk%d_%d' % (kind, p)])
            wvt, wkt = wload(kchunks(Dr['w_inBt'][l]), [8, 512])
            for p in range(2):
                S.barrier(relay=RELAY)
                AR.reset(off_x)
                vg = AR.get([16, 256], BF16)
                kvs = AR.get([16, 128], F32)
                s16 = AR.get([16, 128], BF16)
                oall = AR.get([16, 128], F32)
                kz = [AR.get([128], BF16) for _ in range(2)]
                qx = [AR.get([128], BF16) for _ in range(2)]
                sT = [AR.get([256], BF16) for _ in range(2)]
                mixT = AR.get([1, 2048], BF16)
                qkk = ['qk0_%d' % p, 'qk1_%d' % p]
                for tt in range(16):
                    b = tt % 4
                    for k in range(8):
                        MM(bank(b)[:, 0:256], hT[:, k, tt * 128:(tt + 1) * 128], wvt[:, k, p * 256:(p + 1) * 256],
                           k == 0, k == 7, [hk(k, tt // 4), wkt], [BK[b]])
                    CP(vg[:, tt, 0:128], bank(b)[:, 0:128], [BK[b]], ['vg%d' % tt], eng='dve')
                    ACT(vg[:, tt, 128:256], bank(b)[:, 128:256], AF.Silu, [BK[b]], ['vg%d' % tt])
                S.op('dve', lambda e: e.memset(kvs[:, 0, :], 0.0), (), ['kvs'])
                def st_a(c):
                    i = c % 2
                    bt = 4 + (c % 2)
                    pb = bank(bt).bitcast(BF16)
                    TR(pb[:, 0:128], krT[:, p, c * 128:(c + 1) * 128], ident_b[:], [qkk[1], 'ident_b'], [BK[bt]])
                    TT(kz[i].rearrange("p (h d) -> p h d", h=2), pb[:, 0:128].rearrange("p (h d) -> p h d", h=2),
                       zeta[:, 2 * p:2 * p + 2].unsqueeze(2).to_broadcast([128, 2, 64]), ALU.mult,
                       [BK[bt], 'zeta'], ['kz%d' % i])

                def st_b(c):
                    i = c % 2
                    bm = 6 + (c % 2)
                    MM(bank(bm)[:, 0:128], kz[i], vg[:, c, 0:128], True, True, ['kz%d' % i, 'vg%d' % c], [BK[bm]])
                    STT(kvs[:, c + 1, :], kvs[:, c, :], cdv[:, p:p + 1], bank(bm)[:, 0:128], ALU.mult, ALU.add,
                        ['kvs', 'cdv', BK[bm]], ['kvs'])

                st_a(0)
                for c in range(15):
                    if c + 1 < 15:
                        st_a(c + 1)
                    st_b(c)
                CP(s16, kvs, ['kvs'], ['s16'], eng='dve')
                def b_scores(c):
                    i = c % 2
                    bs0 = 2 * (c % 2)
                    cs = slice(c * 128, (c + 1) * 128)
                    if c > 0:
                        TT(qx[i], qrT[:, p, cs], xiT[:, p * 128:(p + 1) * 128], ALU.mult, [qkk[0], 'xiT'], ['qx%d' % i])
                    for hh in range(2):
                        ps_ = slice(hh * 64, (hh + 1) * 64)
                        MM(bank(bs0 + hh)[:, 0:128], krT[ps_, p, cs], qrT[ps_, p, cs], True, True,
                           qkk, [BK[bs0 + hh]])

                def b_rest(c):
                    i = c % 2
                    bs0 = 2 * (c % 2)
                    bo0 = 4 + 2 * (c % 2)
                    TT(sT[i].rearrange("p (h l) -> p h l", h=2),
                       PSA[:, bs0 * 512:(bs0 + 2) * 512].rearrange("p (h x) -> p h x", h=2)[:, :, 0:128],
                       decayT[:, 2 * p * 128:(2 * p + 2) * 128].rearrange("p (h l) -> p h l", h=2), ALU.mult,
                       [BK[bs0], BK[bs0 + 1], 'decayT'], ['sT%d' % i])
                    for hh in range(2):
                        ps_ = slice(hh * 64, (hh + 1) * 64)
                        MM(bank(bo0 + hh)[:, 0:64], sT[i][:, hh * 128:(hh + 1) * 128], vg[:, c, hh * 64:(hh + 1) * 64],
                           True, c == 0, ['sT%d' % i, 'vg%d' % c], [BK[bo0 + hh]])
                        if c > 0:
                            MM(bank(bo0 + hh)[:, 0:64], qx[i][ps_, :], s16[ps_, c, hh * 64:(hh + 1) * 64],
                               False, True, ['qx%d' % i, 's16'], [BK[bo0 + hh]])
                    CP(oall[:, c, :].rearrange("p (h e) -> p h e", h=2),
                       PSB[:, (bo0 - 4) * 512:(bo0 - 2) * 512].rearrange("p (h x) -> p h x", h=2)[:, :, 0:64],
                       [BK[bo0], BK[bo0 + 1]], ['oall'], eng='act')

                b_scores(0)
                for c in range(16):
                    if c + 1 < 16:
                        b_scores(c + 1)
                    b_rest(c)
                o3 = oall.rearrange("p c (h e) -> p (c h) e", h=2)
                sq3 = kvs.rearrange("p c (h e) -> p (c h) e", h=2)
                gam_b = lnbc[:, 2, p * 128:(p + 1) * 128].rearrange("p (h e) -> p h e", h=2).unsqueeze(1).to_broadcast([128, 16, 2, 64])
                bet_b = lnbc[:, 3, p * 128:(p + 1) * 128].rearrange("p (h e) -> p h e", h=2).unsqueeze(1).to_broadcast([128, 16, 2, 64])
                sq4 = kvs.rearrange("p c (h e) -> p c h e", h=2)
                group_ln_core(o3, sq3, 32, ['oall'], 'kvs', epsA[:, 0:1])
                TT(sq4, sq4, gam_b, ALU.mult, ['kvs', 'lnbc'], ['kvs'])
                TT(sq4, sq4, bet_b, ALU.add, ['kvs', 'lnbc'], ['kvs'])
                vkeys = ['vg%d' % tt for tt in range(16)]
                TT(vg[:, :, 0:128], kvs, vg[:, :, 128:256], ALU.mult, ['kvs'] + vkeys, vkeys)
                if dbg == 'mixB%d_%d' % (l, p):
                    dump_tm(vg, 0, 128, vkeys, p * 128)
                out_proj(l, vg, vkeys, 1, 256 + p * 128, mixT)

        def mixer_C(l):
            hk = lambda k, tb: 'hT%d_%d' % (k, tb)
            DMA('pool', w1s[:].rearrange("p (a b) -> p a b", a=4), Dr['w1s'][l].rearrange("p (a b) -> p a b", a=4), (), ['w1s'])
            DMA('pool', w2k[:], Dr['w2k'][l], (), ['w2k'])
            DMA('pool', w2v[:], Dr['w2v'][l], (), ['w2v'])
            DMA('sp', posT[:], Dr['posT'][l], (), ['posT'])
            for g in range(2):
                S.barrier(relay=RELAY)
                AR.reset()
                qz = AR.get([4, 2048], BF16)
                KsT = AR.get([2048], BF16)
                KwT = AR.get([2048], BF16)
                Vaug = AR.get([16, 2, 65], BF16)
                gates = AR.get([16, 12], F32)
                mixC = AR.get([16, 256], BF16)
                kcmpT = AR.get([127], BF16)
                vcaug = AR.get([97], BF16)
                hidT = AR.get([2, 127], BF16)
                off_r = AR.off
                kvcT = AR.get([2048], BF16)
                kcp = AR.get([32, 127], BF16)
                wv, wk = wload(kchunks(Dr['w_inCf'][l][:, g * 640:g * 640 + 512]), [8, 512])
                wv2, wk2 = wload(kchunks(Dr['w_inCf'][l][:, g * 640 + 512:g * 640 + 640]), [8, 128])
                S.op('dve', lambda e: e.memset(qz, 0.0), (), ['qT'])
                dsts = [(None, 'qT', 0.125), (None, 'qT', 0.125), (KsT, 'KsT', 1.0), (KwT, 'KwT', 1.0),
                        (kvcT, 'kvcT', 1.0)]
                n = 0
                for bi_, (dst, dk, scl) in enumerate(dsts):
                    for tb in range(4):
                        b = n % 4
                        n += 1
                        for k in range(8):
                            lw = wv[:, k, bi_ * 128:(bi_ + 1) * 128] if bi_ < 4 else wv2[:, k, :]
                            MM(bank(b), lw, hT[:, k, tb * 512:(tb + 1) * 512], k == 0, k == 7,
                               [wk if bi_ < 4 else wk2, hk(k, tb)], [BK[b]])
                        if dst is None:
                            ACT(qz[0:64, 2 * bi_, tb * 512:(tb + 1) * 512], bank(b)[0:64, :], AF.Identity, [BK[b]], [dk], scale=scl)
                            TS(qz[64:128, 2 * bi_ + 1, tb * 512:(tb + 1) * 512], bank(b)[64:128, :], scl, None, ALU.mult, None, [BK[b]], [dk])
                        elif n % 2:
                            ACT(dst[:, tb * 512:(tb + 1) * 512], bank(b), AF.Identity, [BK[b]], [dk], scale=scl)
                        else:
                            TS(dst[:, tb * 512:(tb + 1) * 512], bank(b), scl, None, ALU.mult, None, [BK[b]], [dk])
                wv3, wk3 = wload(kchunks(Dr['w_inCt'][l][:, g * 140:(g + 1) * 140]), [8, 140])
                S.op('dve', lambda e: e.memset(Vaug[:, :, :, 64:65], 1.0), (), ['Vaug'])
                for tt in range(16):
                    b = 4 + tt % 4
                    for k in range(8):
                        MM(bank(b)[:, 0:140], hT[:, k, tt * 128:(tt + 1) * 128], wv3[:, k, :], k == 0, k == 7,
                           [hk(k, tt // 4), wk3], [BK[b]])
                    CP(Vaug[:, tt, :, 0:64], bank(b)[:, 0:128].rearrange("p (s d) -> p s d", s=2), [BK[b]], ['Vaug'], eng='dve')
                    ACT(gates[:, tt, :], bank(b)[:, 128:140], AF.Sigmoid, [BK[b]], ['gates'])
                kv_t = kvcT.tensor
                win = bass.AP(kv_t, kvcT.offset, [list(kvcT.ap[0]), [1, 32], [16, 127]])
                TT(kcp, win, posT[:, 0:32].unsqueeze(2).to_broadcast([128, 32, 127]), ALU.add, ['kvcT', 'posT'], ['kcp'])
                for kv in range(2):
                    ps_ = slice(kv * 64, (kv + 1) * 64)
                    for ll in range(32):
                        MM(bank(kv)[0:64, 0:127], w1s[ps_, ll * 64:(ll + 1) * 64], kcp[ps_, ll, :],
                           ll == 0, ll == 31, ['w1s', 'kcp'], [BK[kv]])
                    ACT(hidT[0:64, kv, :], bank(kv)[0:64, 0:127], AF.Gelu_apprx_tanh, [BK[kv]], ['hidT'])
                MM(bank(2)[:, 0:127], w2k[:], hidT[0:64, 0, :], True, True, ['w2k', 'hidT'], ['B2'])
                CP(kcmpT, bank(2)[:, 0:127], ['B2'], ['kcmpT'], eng='dve')
                MM(bank(3)[0:127, 0:64], hidT[0:64, 1, :], w2v[:], True, True, ['w2v', 'hidT'], ['B3'])
                S.op('dve', lambda e: e.memset(vcaug[:, 64:65], 1.0), (), ['vcaug'])
                CP(vcaug[0:127, 0:64], bank(3)[0:127, 0:64], ['B3'], ['vcaug'], eng='dve')
                CP(vcaug[:, 65:97], ovl[:, 0:32], ['ovl'], ['vcaug'], eng='dve')
                S.barrier(relay=RELAY)
                AR.reset(off_r)
                PT = [AR.get([512], BF16) for _ in range(4)]
                MbT = [AR.get([128], BF16) for _ in range(2)]
                for j_ in range(2):
                    S.op('dve', (lambda t_: (lambda e: e.memset(t_, 0.0)))(MbT[j_]), (), ['MbT%d' % j_])
                Mb = [AR.get([32], BF16) for _ in range(2)]
                ocg = [AR.get([4, 64], F32) for _ in range(2)]
                t1 = [AR.get([4, 64], F32) for _ in range(2)]
                t2 = [AR.get([4, 64], F32) for _ in range(2)]
                sm = [AR.get([96], F32) for _ in range(2)]
                impt = [AR.get([4, 32], F32) for _ in range(2)]
                pn = [0]

                def qslice(r, qt):
                    return qz[:, r, qt * 128:(qt + 1) * 128]

                def attend(qt, kts, KT, vsel, bS, bO, extra_fn):
                    def scores(ki):
                        kt = kts[ki]
                        bs_ = bS[ki % len(bS)]
                        extras = extra_fn(kt)
                        v4 = bank(bs_).rearrange("p (r t) -> p r t", r=4)
                        MM(v4, KT[:, kt * 128:(kt + 1) * 128], qz[:, :, qt * 128:(qt + 1) * 128],
                           True, not extras, ['KsT', 'KwT', 'qT'], [BK[bs_]])
                        for ei, (lh, rh, ks) in enumerate(extras):
                            MM(v4, lh, rh, False, ei == len(extras) - 1, ks, [BK[bs_]])

                    def rest(ki):
                        kt = kts[ki]
                        bs_ = bS[ki % len(bS)]
                        i = pn[0] % 4
                        pn[0] += 1
                        ACT(PT[i], bank(bs_), AF.Exp, [BK[bs_]], ['PT%d' % i])
                        for r in range(4):
                            MM(bank(bO)[:, r * 65:(r + 1) * 65], PT[i][:, r * 128:(r + 1) * 128], Vaug[:, kt, vsel, :],
                               ki == 0 and r == 0, ki == len(kts) - 1 and r == 3, ['PT%d' % i, 'Vaug'], [BK[bO]])

                    LA = 2
                    for ki in range(min(LA, len(kts))):
                        scores(ki)
                    for ki in range(len(kts)):
                        if ki + LA < len(kts):
                            scores(ki + LA)
                        rest(ki)

                for qt in range(16):
                    j = qt % 2
                    smj = sm[j]
                    sk = 'sm%d' % j
                    qs = slice(qt * 128, (qt + 1) * 128)
                    MM(bank(0)[0:127, :].rearrange("p (r t) -> p r t", r=4), kcmpT[:, :], qz[:, :, qs], True, False,
                       ['kcmpT', 'qT'], ['B0'])
                    MM(bank(0)[0:127, :].rearrange("p (r t) -> p r t", r=4), ident_b[0:127, 0:127],
                       cmpb[0:127, qs].unsqueeze(1).to_broadcast([127, 4, 128]), False, True, ['ident_b', 'cmpb'], ['B0'])
                    i = pn[0] % 4
                    pn[0] += 1
                    ACT(PT[i][0:127, :], bank(0)[0:127, :], AF.Exp, ['B0'], ['PT%d' % i])
                    for r in range(4):
                        MM(bank(1)[:, r * 97:(r + 1) * 97], PT[i][0:127, r * 128:(r + 1) * 128], vcaug[0:127, :],
                           r == 0, r == 3, ['PT%d' % i, 'vcaug'], ['B1'])
                    O = bank(1)[:, 0:388].rearrange("p (r c) -> p r c", r=4)
                    cb4 = causalb[:].unsqueeze(1).to_broadcast([128, 4, 128])
                    wb4 = winb[:].unsqueeze(1).to_broadcast([128, 4, 128])

                    def wmask(kt, qt=qt, cb4=cb4, wb4=wb4):
                        if kt == qt:
                            return [(ident_b[:], cb4, ['ident_b', 'causalb'])]
                        if kt == qt - 4:
                            return [(ident_b[:], wb4, ['ident_b', 'winb'])]
                        return []
                    attend(qt, list(range(max(0, qt - 4), qt + 1)), KwT, 1, [2, 3, 6, 7], 5, wmask)
                    TS(smj[:, 0:4], O[:, :, 64], 1e-30, None, ALU.max, None, ['B1'], [sk])
                    RCP(smj[:, 4:8], smj[:, 0:4], [sk], [sk])
                    TT(impt[j], O[:, :, 65:97], smj[:, 4:8].unsqueeze(2).to_broadcast([128, 4, 32]), ALU.mult, ['B1', sk], ['impt%d' % j])
                    RED(smj[:, 32:64], impt[j].rearrange("p r c -> p c r"), ALU.add, ['impt%d' % j], [sk])
                    TT(smj[:, 32:64], smj[:, 32:64], fbias[:, qt * 32:(qt + 1) * 32], ALU.add, [sk, 'fbias'], [sk])
                    S.op('dve', (lambda o_, i_: (lambda e: e.max(o_, i_)))(smj[:, 64:72], smj[:, 32:64]), [sk], [sk])
                    TS(Mb[j], smj[:, 32:64], smj[:, 71:72], -1.0, ALU.is_ge, ALU.add, [sk], ['Mb%d' % j])
                    pbt = bank(0).bitcast(BF16)
                    TR(pbt[0:32, 0:128], Mb[j], ident_b[:], ['Mb%d' % j, 'ident_b'], ['B0'])
                    CP(MbT[j][0:32, :], pbt[0:32, 0:128], ['B0'], ['MbT%d' % j], eng='dve')
                    gv = gates[:, qt, :].rearrange("p (r c) -> p r c", r=4)
                    TT(smj[:, 8:12], smj[:, 4:8], gv[:, :, 0], ALU.mult, [sk, 'gates'], [sk])
                    TT(ocg[j], O[:, :, 0:64], smj[:, 8:12].unsqueeze(2).to_broadcast([128, 4, 64]), ALU.mult, ['B1', sk], ['ocg%d' % j])

                    mb4 = MbT[j][:, :].unsqueeze(1).to_broadcast([128, 4, 128])

                    def smask(kt, qt=qt, j=j, cb4=cb4, mb4=mb4):
                        ex = [(expand[:, kt * 128:(kt + 1) * 128], mb4, ['expand', 'MbT%d' % j])]
                        if kt == qt:
                            ex.append((ident_b[:], cb4, ['ident_b', 'causalb']))
                        return ex
                    attend(qt, list(range(0, qt + 1)), KsT, 0, [2, 3, 6, 7], 4, smask)
                    Os = bank(4)[:, 0:260].rearrange("p (r c) -> p r c", r=4)
                    Ow = bank(5)[:, 0:260].rearrange("p (r c) -> p r c", r=4)
                    RCP(smj[:, 12:16], Os[:, :, 64], ['B4'], [sk])
                    TT(smj[:, 12:16], smj[:, 12:16], gv[:, :, 1], ALU.mult, [sk, 'gates'], [sk])
                    RCP(smj[:, 16:20], Ow[:, :, 64], ['B5'], [sk])
                    TT(smj[:, 16:20], smj[:, 16:20], gv[:, :, 2], ALU.mult, [sk, 'gates'], [sk])
                    TT(t1[j], Os[:, :, 0:64], smj[:, 12:16].unsqueeze(2).to_broadcast([128, 4, 64]), ALU.mult, ['B4', sk], ['t1_%d' % j])
                    TT(t2[j], Ow[:, :, 0:64], smj[:, 16:20].unsqueeze(2).to_broadcast([128, 4, 64]), ALU.mult, ['B5', sk], ['t2_%d' % j])
                    TT(t1[j], t1[j], ocg[j], ALU.add, ['t1_%d' % j, 'ocg%d' % j], ['t1_%d' % j])
                    TT(mixC[:, qt, :].rearrange("p (r d) -> p r d", r=4), t1[j], t2[j], ALU.add, ['t1_%d' % j, 't2_%d' % j], ['mixC%d' % qt])
                mkeys = ['mixC%d' % tt for tt in range(16)]
                if dbg == 'mixC%d_%d' % (l, g):
                    dump_tm(mixC, 0, 256, mkeys, g * 256)
                S.barrier(relay=RELAY)
                AR.reset(off_r)
                mixT = AR.get([2, 2048], BF16)
                out_proj(l, mixC, mkeys, 2, 512 + g * 256, mixT)

        for l in range(n_layers):
            hk = lambda k, tb: 'hT%d_%d' % (k, tb)
            if 'A' in mixers:
                S.barrier(relay=RELAY)
                AR.reset()
                zag = AR.get([16, 512], BF16)
                off_sqv = AR.off
                sqv = AR.get([16, 256], F32)
                st1 = AR.get([64], F32)
                st2 = AR.get([64], F32)
                st3 = AR.get([64], F32)
                mixA = AR.get([16, 256], BF16)
                atmp = [AR.get([256], F32) for _ in range(2)]
                wstage = AR.get([4, 128], F32)
                DMA('sp', wstage, Dr['WsT'][l].rearrange("p (g t) -> p g t", g=4), (), ['wstage'])
                TT(WsT[:].rearrange("p (g t) -> p g t", g=4), wstage,
                   causal01[:].unsqueeze(1).to_broadcast([128, 4, 128]), ALU.mult, ['wstage', 'causal01'], ['WsT'])
                DMA('sp', bsT[:], Dr['bsT'][l], (), ['bsT'])
                DMA('sp', lnbc[:, 0, :], Dr['a_lng'][l], (), ['lnbcA'])
                DMA('sp', lnbc[:, 1, :], Dr['a_lnb'][l], (), ['lnbcA'])
                wv, wk = wload(kchunks(Dr['w_inA'][l]), [8, 512])
                for tt in range(16):
                    b = tt % 4
                    for k in range(8):
                        MM(bank(b), hT[:, k, tt * 128:(tt + 1) * 128], wv[:, k, :], k == 0, k == 7,
                           [hk(k, tt // 4), wk], [BK[b]])
                    ACT(zag[:, tt, :], bank(b), AF.Gelu_apprx_tanh, [BK[b]], ['zag'])
                v4 = zag[:, :, 256:512].rearrange("p t (g d) -> p t g d", g=4)
                TT(sqv, zag[:, :, 256:512], zag[:, :, 256:512], ALU.mult, ['zag'], ['sqv'])
                RED(st1.rearrange("p (t g) -> p t g", t=16), v4, ALU.add, ['zag'], ['st1'])
                RED(st2.rearrange("p (t g) -> p t g", t=16), sqv.rearrange("p t (g d) -> p t g d", g=4), ALU.add, ['sqv'], ['st2'])
                TS(st1, st1, 1.0 / 64, None, ALU.mult, None, ['st1'], ['st1'])
                TT(st3, st1, st1, ALU.mult, ['st1'], ['st3'])
                STT(st2, st2, 1.0 / 64, st3, ALU.mult, ALU.subtract, ['st2', 'st3'], ['st2'])
                TS(st2, st2, 0.0, None, ALU.max, None, ['st2'], ['st2'])
                ACT(st2, st2, AF.Sqrt, ['st2', 'epsA'], ['st2'], bias=epsA[:, 0:1])
                RCP(st2, st2, ['st2'], ['st2'])
                mean_b = st1.rearrange("p (t g) -> p t g", t=16).unsqueeze(3).to_broadcast([128, 16, 4, 64])
                rstd_b = st2.rearrange("p (t g) -> p t g", t=16).unsqueeze(3).to_broadcast([128, 16, 4, 64])
                sq4 = sqv.rearrange("p t (g d) -> p t g d", g=4)
                TT(sq4, v4, mean_b, ALU.subtract, ['zag', 'st1'], ['sqv'])
                TT(sq4, sq4, rstd_b, ALU.mult, ['sqv', 'st2'], ['sqv'])
                gam_b = lnbc[:, 0, :].unsqueeze(1).to_broadcast([128, 16, 256])
                bet_b = lnbc[:, 1, :].unsqueeze(1).to_broadcast([128, 16, 256])
                TT(sqv, sqv, gam_b, ALU.mult, ['sqv', 'lnbcA'], ['sqv'])
                TT(zag[:, :, 256:512], sqv, bet_b, ALU.add, ['sqv', 'lnbcA'], ['zag'])
                bs_b = bsT[:, 0:4].unsqueeze(2).to_broadcast([128, 4, 64])
                for tt in range(16):
                    b = 4 + tt % 4
                    for g in range(4):
                        MM(bank(b)[:, g * 64:(g + 1) * 64], WsT[:, g * 128:(g + 1) * 128],
                           zag[:, tt, 256 + g * 64:256 + (g + 1) * 64], g == 0, g == 3, ['WsT', 'zag'], [BK[b]])
                    at = atmp[tt % 2]
                    ak = 'atmp%d' % (tt % 2)
                    TT(at.rearrange("p (g d) -> p g d", g=4), bank(b)[:, 0:256].rearrange("p (g d) -> p g d", g=4),
                       bs_b, ALU.add, [BK[b], 'bsT'], [ak])
                    TT(mixA[:, tt, :], at, zag[:, tt, 0:256], ALU.mult, [ak, 'zag'], ['mixA%d' % tt])
                if dbg == 'mixA%d' % l:
                    dst = AR.get([256], F32)
                    for tt in range(16):
                        CP(dst, mixA[:, tt, :], ['mixA%d' % tt], ['dst'])
                        DMA('sp', dbg_d[tt * 128:(tt + 1) * 128, 0:256], dst, ['dst'], ['dbg'])
                AR.reset(off_sqv)
                mixT = AR.get([2, 2048], BF16)
                out_proj(l, mixA, ['mixA%d' % tt for tt in range(16)], 2, 0, mixT, ['sqv'])

            if 'B' in mixers:
                mixer_B(l)
            if 'C' in mixers:
                mixer_C(l)

            S.barrier(relay=RELAY)
            layer_norm(l, 1, last=False)
            if dbg == 'ln1_%d' % l:
                break

            if do_ffn:
                S.barrier(relay=RELAY)
                AR.reset()
                DMA('sp', cwT[:], Dr['cwT'][l], (), ['cwT'])
                DMA('sp', cbT[:], Dr['cbT'][l], (), ['cbT'])
                gT = AR.get([max(FF_SPLIT), 2048], BF16)
                ctmp = [[AR.get([1024], F32) for _ in range(2)] for _ in range(2)]
                sgt = [AR.get([1024], BF16) for _ in range(2)]
                j0 = 0
                PS4 = [PSA, PSB]
                P4K = [BK[0:4], BK[4:8]]
                for pi_, part_n in enumerate(FF_SPLIT):
                    if l + 1 < n_layers:
                        for blk_ in range(3 * pi_, 3 * pi_ + 3):
                            ada_block_deferred(l + 1, blk_)
                    wgrp = {}
                    for jl in range(part_n):
                        j = j0 + jl
                        if jl % 4 == 0:
                            ng = min(4, part_n - jl)
                            for part in range(2):
                                jj0 = part * 22 + j
                                wgrp[part] = wload(kchunks(Dr['w_up'][l][:, jj0 * 128:(jj0 + ng) * 128]), [8, ng * 128])
                        for part in range(2):
                            jj = part * 22 + j
                            wvf, wk = wgrp[part]
                            wv = wvf[:, :, (jl % 4) * 128:(jl % 4 + 1) * 128]
                            ps = PS4[part]
                            for tb in range(4):
                                for k in range(8):
                                    MM(ps[:, tb * 512:(tb + 1) * 512], wv[:, k, :], hT[:, k, tb * 512:(tb + 1) * 512],
                                       k == 0, k == 7, [wk, hk(k, tb)], [P4K[part][tb]])
                            for hf in range(2):
                                ct = ctmp[part][hf]
                                ck = 'ctmp%d_%d' % (part, hf)
                                o = hf * 1024
                                pk = P4K[part][2 * hf:2 * hf + 2]
                                pkp = P4K[part][max(0, 2 * hf - 1):2 * hf + 2]
                                ACT(ct, ps[:, o:o + 1024], AF.Identity, pk + ['cwT', 'cbT'], [ck],
                                    scale=cwT[:, jj * 3 + 2:jj * 3 + 3], bias=cbT[:, jj:jj + 1])
                                if hf == 0:
                                    STT(ct[:, 1:1024], ps[:, 0:1023], cwT[:, jj * 3 + 1:jj * 3 + 2], ct[:, 1:1024],
                                        ALU.mult, ALU.add, pk + ['cwT', ck], [ck])
                                    STT(ct[:, 2:1024], ps[:, 0:1022], cwT[:, jj * 3:jj * 3 + 1], ct[:, 2:1024],
                                        ALU.mult, ALU.add, pk + ['cwT', ck], [ck])
                                else:
                                    STT(ct, ps[:, o - 1:o + 1023], cwT[:, jj * 3 + 1:jj * 3 + 2], ct,
                                        ALU.mult, ALU.add, pkp + ['cwT', ck], [ck])
                                    STT(ct, ps[:, o - 2:o + 1022], cwT[:, jj * 3:jj * 3 + 1], ct,
                                        ALU.mult, ALU.add, pkp + ['cwT', ck], [ck])
                                if part == 0:
                                    ACT(sgt[hf], ct, AF.Silu, [ck], ['sgt%d' % hf])
                                else:
                                    TT(gT[:, jl, o:o + 1024], ct, sgt[hf], ALU.mult, [ck, 'sgt%d' % hf], ['gT%d' % jl])
                    nsl = (part_n + 3) // 4
                    wvs = []
                    for s in range(nsl):
                        r0 = (j0 + 4 * s) * 128
                        nr = min(4, part_n - 4 * s)
                        wvs.append(wload(kchunks(Dr['w_down'][l][r0:r0 + nr * 128, :]), [nr, 1024]))
                    bi2 = 0
                    for fb in range(8):
                        for tb in range(4):
                            b = bi2 % 8
                            bi2 += 1
                            for jl in range(part_n):
                                wv, wk = wvs[jl // 4]
                                MM(bank(b), wv[:, jl % 4, fb * 128:(fb + 1) * 128], gT[:, jl, tb * 512:(tb + 1) * 512],
                                   jl == 0, jl == part_n - 1, [wk, 'gT%d' % jl], [BK[b]])
                            xs = xT[:, fb, tb * 512:(tb + 1) * 512]
                            STT(xs, bank(b), drv[l][:, 24 + fb:25 + fb], xs, ALU.mult, ALU.add,
                                [BK[b], 'drv%d' % l, 'xT%d_%d' % (fb, tb)], ['xT%d_%d' % (fb, tb)])
                    j0 += part_n
                S.barrier(relay=RELAY)
            layer_norm(l, 2, last=(l == n_layers - 1))

        S.barrier(relay=RELAY)
        AR.reset()
        ost = [AR.get([1024], F32) for _ in range(4)]
        bi = 0
        for tt in range(16):
            o = ost[tt % 4]
            ok = 'ost%d' % (tt % 4)
            for cg in range(2):
                b = bi % 8
                bi += 1
                for q in range(4):
                    c = cg * 4 + q
                    TR(bank(b)[:, q * 128:(q + 1) * 128], xT[:, c, tt * 128:(tt + 1) * 128], ident_f[:],
                       ['xT%d_%d' % (c, tt // 4), 'ident_f'], [BK[b]])
                CP(o[:, cg * 512:(cg + 1) * 512], bank(b), [BK[b]], [ok], eng=EV())
            DMA('sp', out_d[tt * 128:(tt + 1) * 128, :], o, [ok], ['out'])
        S.finish()
        S.replay()
    return nc


_CACHE = {}


def kernel(**inputs):
    inp = {k: np.asarray(v) for k, v in inputs.items()}
    if 'nc' not in _CACHE:
        _CACHE['nc'] = build()
    nc = _CACHE['nc']
    w = _prep_weights(inp)
    tb = _tables()
    in_maps = []
    for b in range(8):
        m = dict(w)
        m.update(tb)
        m['x'] = np.ascontiguousarray(inp['x'][b], dtype=np.float32)
        m['cT'] = _pad64(inp['c'][b].reshape(8, 128).T)
        in_maps.append(m)
    res = run_bass_kernel_spmd(nc, in_maps, core_ids=list(range(8)))
    out = np.stack([np.asarray(r['out'], dtype=np.float32) for r in res.results], 0)
    return out
```

```python
import math
from contextlib import ExitStack
import numpy as np
import concourse.bass as bass
import concourse.mybir as mybir
from concourse.bass_utils import run_bass_kernel_spmd

F32 = mybir.dt.float32
BF16 = mybir.dt.bfloat16
AF = mybir.ActivationFunctionType
ALU = mybir.AluOpType
AX = mybir.AxisListType

ENGS = ['pe', 'dve', 'act', 'pool', 'sp']
NRING = 8
DEPTH = 2
SEQ = 2048
DM = 1024
ALPHA = (2 * DEPTH) ** 0.25
LN_EPS = 1e-5
NEGB = -30000.0
FF_SPLIT = [6, 6, 5, 5]


class Sched:
    def __init__(self, nc, stack):
        self.nc = nc
        self.prog = {e: [] for e in ENGS}
        self.cnt = {e: 0 for e in ENGS}
        self.seen = {e: {} for e in ENGS}
        self.lastw = {}
        self.readers = {}
        self.sems = {}
        self.semval = {}
        self.relay_fn = None
        for e in ENGS:
            self.sems[e] = stack.enter_context(nc.semaphore("s_" + e))
        self.dma_n = {}
        for q in ['sp', 'act', 'pool']:
            self.dma_n[q] = 0
            for j in range(NRING):
                nm = "d_%s%d" % (q, j)
                self.sems[nm] = stack.enter_context(nc.semaphore(nm))

    def _deps(self, eng, reads, writes, is_dma=False):
        deps = []
        for k in reads:
            t = self.lastw.get(k)
            if t is not None:
                deps.append((t, 'raw'))
        for k in writes:
            t = self.lastw.get(k)
            if t is not None:
                deps.append((t, 'waw'))
            for s, v in self.readers.get(k, {}).items():
                deps.append(((s, v), 'war'))
        need = {}
        for (s, v), kind in deps:
            if s == eng and not is_dma:
                if kind != 'raw' or eng == 'pe':
                    continue
            if self.seen[eng].get(s, 0) >= v:
                continue
            if need.get(s, 0) < v:
                need[s] = v
        return need

    def _emit_waits(self, eng, need):
        if eng in ('sp', 'pool') and 'pe' in need and self.relay_fn is not None:
            need = dict(need)
            v = need.pop('pe')
            self.seen[eng]['pe'] = v
            R = 'dve'
            if self.seen[R].get('pe', 0) < v:
                self.prog[R].append(('wait', 'pe', v))
                self.seen[R]['pe'] = v
            lr = getattr(self, 'last_relay', 0)
            if lr and self.seen[R].get(R, 0) < lr:
                self.prog[R].append(('wait', R, lr))
                self.seen[R][R] = lr
            self.cnt[R] += 1
            self.last_relay = self.cnt[R]
            self.semval[R] = self.cnt[R]
            self.prog[R].append(('op', self.relay_fn, R, 1))
            if self.seen[eng].get(R, 0) < self.cnt[R]:
                need[R] = max(need.get(R, 0), self.cnt[R])
        for s, v in need.items():
            self.prog[eng].append(('wait', s, v))
            self.seen[eng][s] = v

    def _commit(self, tok, reads, writes):
        for k in writes:
            self.lastw[k] = tok
            self.readers[k] = {}
        for k in reads:
            d = self.readers.setdefault(k, {})
            if d.get(tok[0], 0) < tok[1]:
                d[tok[0]] = tok[1]

    def relay_readers(self, keys):
        if self.relay_fn is None:
            return
        v = 0
        for k in keys:
            d = self.readers.get(k)
            if d and 'pe' in d:
                v = max(v, d['pe'])
        if v == 0:
            return
        R = 'dve'
        if self.seen[R].get('pe', 0) < v:
            self.prog[R].append(('wait', 'pe', v))
            self.seen[R]['pe'] = v
        lr = getattr(self, 'last_relay', 0)
        if lr and self.seen[R].get(R, 0) < lr:
            self.prog[R].append(('wait', R, lr))
            self.seen[R][R] = lr
        self.cnt[R] += 1
        self.semval[R] = self.cnt[R]
        self.last_relay = self.cnt[R]
        self.prog[R].append(('op', self.relay_fn, R, 1))
        for k in keys:
            d = self.readers.get(k)
            if d and 'pe' in d:
                d.pop('pe')
                d[R] = max(d.get(R, 0), self.cnt[R])

    def op(self, eng, fn, reads=(), writes=()):
        need = self._deps(eng, reads, writes)
        self._emit_waits(eng, need)
        self.cnt[eng] += 1
        tok = (eng, self.cnt[eng])
        self.semval[eng] = self.cnt[eng]
        self.prog[eng].append(('op', fn, eng, 1))
        self._commit(tok, reads, writes)
        return tok

    def dma_multi(self, q, pairs, reads=(), writes=()):
        i = self.dma_n[q]
        self.dma_n[q] += 1
        slot = "d_%s%d" % (q, i % NRING)
        prev = self.semval.get(slot, 0)
        val = prev + 16 * len(pairs)
        need = self._deps(q, reads, writes, is_dma=True)
        if prev > 0 and self.seen[q].get(slot, 0) < prev:
            need[slot] = max(need.get(slot, 0), prev)
        self._emit_waits(q, need)
        for out, in_ in pairs:
            self.prog[q].append(('op', (lambda o_, i_: (lambda e: e.dma_start(out=o_, in_=i_)))(out, in_), slot, 16))
        self.semval[slot] = val
        tok = (slot, val)
        self._commit(tok, reads, writes)
        return tok

    def dma(self, q, out, in_, reads=(), writes=()):
        return self.dma_multi(q, [(out, in_)], reads, writes)

    def _wait_all(self, e):
        need = {}
        for s_, v in self.semval.items():
            if s_ == e:
                continue
            if self.seen[e].get(s_, 0) < v:
                need[s_] = v
        self._emit_waits(e, need)

    def barrier(self, engs=('pe', 'dve', 'act', 'sp'), relay=None):
        if relay is None or 'sp' not in engs:
            for e in engs:
                self._wait_all(e)
            return
        snap = dict(self.semval)
        self._wait_all('sp')
        tok = self.dma('sp', relay[0], relay[1], (), ['__bar'])
        for e in engs:
            if e == 'sp':
                continue
            self._emit_waits(e, {tok[0]: tok[1]} if self.seen[e].get(tok[0], 0) < tok[1] else {})
            for s_, v in snap.items():
                if s_ != e and self.seen[e].get(s_, 0) < v:
                    self.seen[e][s_] = v

    def finish(self):
        self.barrier(engs=('sp',))

    def replay(self):
        nc = self.nc
        sems = self.sems
        prog = self.prog

        def run(e, name):
            for it in prog[name]:
                if it[0] == 'wait':
                    e.wait_ge(sems[it[1]], it[2])
                else:
                    it[1](e).then_inc(sems[it[2]], it[3])

        with nc.Block() as block:
            @block.tensor
            def _(e):
                run(e, 'pe')

            @block.vector
            def _(e):
                run(e, 'dve')

            @block.scalar
            def _(e):
                run(e, 'act')

            @block.gpsimd
            def _(e):
                run(e, 'pool')

            @block.sync
            def _(e):
                run(e, 'sp')


def _pad64(a):
    a = np.asarray(a, dtype=np.float32)
    out = np.zeros(a.shape[:-1] + (64,), np.float32)
    out[..., :a.shape[-1]] = a
    return out


def _tables():
    t = {}
    half = 32
    inv = np.power(np.float32(10000.0), -np.arange(half, dtype=np.float32) / np.float32(half)).astype(np.float32)
    pos = np.arange(SEQ, dtype=np.float32)
    ang = pos[:, None] * inv[None, :]
    cos = np.cos(ang).astype(np.float32).T
    sin = np.sin(ang).astype(np.float32).T
    cosT = np.concatenate([cos, cos, cos, cos], 0)
    sinT = np.concatenate([-sin, sin, -sin, sin], 0)
    t['cosT'] = np.ascontiguousarray(cosT)
    t['sinT'] = np.ascontiguousarray(sinT)
    H = 4
    L = 128
    lg = np.log1p(-np.exp2(-5.0 - np.arange(H, dtype=np.float32))).astype(np.float32)
    idx = np.arange(L, dtype=np.float32)
    diff = idx[:, None] - idx[None, :]
    dec = np.where(diff >= 0, np.exp(lg[:, None, None] * np.maximum(diff, 0.0)), 0.0).astype(np.float32)
    t['decayT'] = np.ascontiguousarray(np.transpose(dec, (2, 0, 1)) * np.float32(0.125)).reshape(128, 512)
    xi = np.exp(lg[:, None] * (idx + 1.0)).astype(np.float32)
    zeta = np.exp(lg[:, None] * (L - 1.0 - idx)).astype(np.float32)
    xiT = np.zeros((128, 2, 128), np.float32)
    for p in range(2):
        for hh in range(2):
            xiT[hh * 64:(hh + 1) * 64, p, :] = xi[2 * p + hh][None, :]
    t['xiT'] = xiT.reshape(128, 256)
    t['zeta'] = _pad64(zeta.T * np.float32(0.125))
    cd = np.exp(lg * L).astype(np.float32)
    cdv = np.zeros((128, 2), np.float32)
    for p in range(2):
        for hh in range(2):
            cdv[hh * 64:(hh + 1) * 64, p] = cd[2 * p + hh]
    t['cdv'] = _pad64(cdv)
    key = np.arange(SEQ)
    ex = np.zeros((128, SEQ), np.float32)
    ex[key // 64, key] = -NEGB
    t['expand'] = ex
    kk = np.arange(128)[:, None]
    tt = np.arange(128)[None, :]
    t['causalb'] = np.where(kk > tt, NEGB, 0.0).astype(np.float32)
    t['winb'] = np.where(kk <= tt, NEGB, 0.0).astype(np.float32)
    t['identf'] = np.eye(128, dtype=np.float32)
    t['causal01'] = np.where(tt >= kk, 1.0, 0.0).astype(np.float32)
    k127 = np.arange(128)[:, None]
    tpos = np.arange(SEQ)[None, :]
    t['cmpb'] = np.where(16 * k127 + 31 > tpos, NEGB, 0.0).astype(np.float32)
    fb = np.zeros((128, 16, 32), np.float32)
    for qt in range(16):
        tq = qt * 128 + np.arange(128)
        cur = tq // 64
        blk = np.arange(32)
        future = blk[None, :] > cur[:, None]
        forced = (blk[None, :] == 0) | (blk[None, :] == cur[:, None]) | (blk[None, :] == cur[:, None] - 1)
        fb[:, qt, :] = np.where(forced, 1e30, np.where(future, -1e30, 0.0))
    t['fbias'] = fb.reshape(128, 512)
    ov = np.zeros((128, 32), np.float32)
    c0 = np.arange(127)[:, None] * 16
    s0 = np.arange(32)[None, :] * 64
    ov[:127] = np.clip(np.minimum(c0 + 32, s0 + 64) - np.maximum(c0, s0), 0, None) / 32.0
    t['ovl'] = _pad64(ov)
    return t


TABLE_SHAPES = {'cosT': [128, 2048], 'sinT': [128, 2048], 'decayT': [128, 512], 'xiT': [128, 256],
                'zeta': [128, 64], 'cdv': [128, 64], 'expand': [128, 2048], 'causalb': [128, 128],
                'winb': [128, 128], 'causal01': [128, 128], 'identf': [128, 128], 'cmpb': [128, 2048], 'fbias': [128, 512], 'ovl': [128, 64]}


def _prep_weights(inp):
    w = {}
    f = lambda a: np.ascontiguousarray(a, dtype=np.float32)
    w_in = inp['w_in']
    L = DEPTH
    w['w_ada'] = f(inp['w_ada'])
    w['b_adaT'] = _pad64(inp['b_ada'].reshape(L, 48, 128).transpose(0, 2, 1))
    w['w_inA'] = f(w_in[:, :, 0:512])
    cols = []
    for p in range(2):
        for base in (512, 768):
            hd = np.arange(128)
            h = 2 * p + hd // 64
            d = hd % 64
            cols.append(base + h * 64 + d)
            cols.append(base + h * 64 + (d + 32) % 64)
    cols = np.concatenate(cols)
    w['w_inBf'] = f(w_in[:, :, cols])
    cols = []
    for p in range(2):
        cols.append(1024 + p * 128 + np.arange(128))
        cols.append(1280 + p * 128 + np.arange(128))
    w['w_inBt'] = f(w_in[:, :, np.concatenate(cols)])
    cols = []
    for g in range(2):
        cols.append(1536 + g * 256 + np.arange(256))
        ks = 2304 + g * 64 + np.arange(64)
        kw = 2560 + g * 64 + np.arange(64)
        cols += [ks, ks, kw, kw]
        cols.append(2048 + g * 64 + np.arange(64))
        cols.append(2176 + g * 64 + np.arange(64))
    w['w_inCf'] = f(w_in[:, :, np.concatenate(cols)])
    cols = []
    for g in range(2):
        cols.append(2432 + g * 64 + np.arange(64))
        cols.append(2688 + g * 64 + np.arange(64))
        cols.append(2816 + g * 12 + np.arange(12))
    w['w_inCt'] = f(w_in[:, :, np.concatenate(cols)])
    w['WsT'] = f(inp['a_ws'].transpose(0, 3, 1, 2).reshape(L, 128, 512))
    w['bsT'] = _pad64(inp['a_bs'].transpose(0, 2, 1))
    w['a_lng'] = f(np.broadcast_to(inp['a_ln_g'].reshape(L, 1, 256), (L, 128, 256)))
    w['a_lnb'] = f(np.broadcast_to(inp['a_ln_b'].reshape(L, 1, 256), (L, 128, 256)))
    w['b_gng'] = f(np.broadcast_to(inp['b_gn_g'].reshape(L, 1, 256), (L, 128, 256)))
    w['b_gnb'] = f(np.broadcast_to(inp['b_gn_b'].reshape(L, 1, 256), (L, 128, 256)))
    posT = np.concatenate([inp['c_pos_k'].transpose(0, 2, 1), inp['c_pos_v'].transpose(0, 2, 1)], 1)
    w['posT'] = _pad64(posT)
    w1k = inp['c_w1_k'].reshape(L, 32, 64, 64).transpose(0, 2, 1, 3)
    w1v = inp['c_w1_v'].reshape(L, 32, 64, 64).transpose(0, 2, 1, 3)
    w['w1s'] = f(np.concatenate([w1k, w1v], 1).reshape(L, 128, 2048))
    w['w2k'] = f(np.concatenate([inp['c_w2_k'], inp['c_w2_k']], 2))
    w['w2v'] = f(inp['c_w2_v'])
    w['w_out'] = f(inp['w_out'])
    w['lnpk'] = _pad64(np.concatenate([inp[k].reshape(L, 8, 128).transpose(0, 2, 1)
                                       for k in ('ln1_g', 'ln1_b', 'ln2_g', 'ln2_b')], 2))
    w['w_up'] = f(inp['w_up'])
    w['cwT'] = f(inp['conv_w'].reshape(L, 3, 44, 128).transpose(0, 3, 2, 1).reshape(L, 128, 132))
    w['cbT'] = _pad64(inp['conv_b'].reshape(L, 44, 128).transpose(0, 2, 1))
    w['w_down'] = f(inp['w_down'])
    return w


W_SHAPES = {'w_ada': [2, 1024, 6144], 'b_adaT': [2, 128, 64], 'w_inA': [2, 1024, 512], 'w_inBf': [2, 1024, 1024],
            'w_inBt': [2, 1024, 512], 'w_inCf': [2, 1024, 1280], 'w_inCt': [2, 1024, 280], 'WsT': [2, 128, 512],
            'bsT': [2, 128, 64], 'a_lng': [2, 128, 256], 'a_lnb': [2, 128, 256], 'b_gng': [2, 128, 256], 'b_gnb': [2, 128, 256],
            'posT': [2, 128, 64], 'w1s': [2, 128, 2048], 'w2k': [2, 64, 128], 'w2v': [2, 64, 64],
            'w_out': [2, 1024, 1024], 'lnpk': [2, 128, 64],
            'w_up': [2, 1024, 5632], 'cwT': [2, 128, 132], 'cbT': [2, 128, 64], 'w_down': [2, 2816, 1024]}


def build(n_layers=DEPTH, mixers=('A', 'B', 'C'), do_ffn=True, dbg=None):
    nc = bass.Bass("TRN2", target_bir_lowering=False)
    Dr = {}
    Dr['x'] = nc.dram_tensor("x", [SEQ, DM], F32, kind="ExternalInput").ap()
    Dr['cT'] = nc.dram_tensor("cT", [128, 64], F32, kind="ExternalInput").ap()
    for k, shp in W_SHAPES.items():
        Dr[k] = nc.dram_tensor(k, shp, F32, kind="ExternalInput").ap()
    for k, shp in TABLE_SHAPES.items():
        Dr[k] = nc.dram_tensor(k, shp, F32, kind="ExternalInput").ap()
    out_d = nc.dram_tensor("out", [SEQ, DM], F32, kind="ExternalOutput").ap()
    dbg_d = None
    if dbg is not None:
        dbg_d = nc.dram_tensor("dbg", [SEQ, DM], F32, kind="ExternalOutput").ap()

    st = ExitStack()
    with st:
        S = Sched(nc, st)
        T = lambda name, shape, dt=F32: st.enter_context(nc.sbuf_tensor("s_" + name, shape, dt))
        xT = T("xT", [128, 8, SEQ], F32)
        hT = T("hT", [128, 8, SEQ], BF16)
        WR = T("WR", [128, 4, 4096], BF16)
        AW = 51 * 256 - 128
        ARENA = T("ARENA", [128, AW], F32)
        PSA = st.enter_context(nc.psum_tensor("PSA", [128, 2048], F32))
        PSB = st.enter_context(nc.psum_tensor("PSB", [128, 2048], F32))

        def bank(i):
            t = PSA if i < 4 else PSB
            return t[:, (i % 4) * 512:(i % 4 + 1) * 512]

        BK = ['B%d' % i for i in range(8)]

        class Arena:
            def __init__(self):
                self.off = 0

            def reset(self, off=0):
                self.off = off

            def get(self, shape, dt=F32):
                n = int(np.prod(shape))
                nb = n * (2 if dt == BF16 else 4)
                w0 = self.off // 4
                w1 = w0 + (nb + 3) // 4
                assert w1 <= AW, ("arena overflow", w1 * 4, AW * 4)
                self.off = w1 * 4
                ap = ARENA[:, w0:w1]
                if dt == BF16:
                    ap = ap.bitcast(BF16)
                ap = ap[:, 0:n]
                if len(shape) == 2:
                    ap = ap.rearrange("p (a b) -> p a b", a=shape[0])
                elif len(shape) == 3:
                    ap = ap.rearrange("p (a b c) -> p a b c", a=shape[0], b=shape[1])
                return ap

        AR = Arena()

        def MM(out, lhsT, rhs, start, stop, rd, wr):
            S.op('pe', lambda e: e.matmul(out, lhsT=lhsT, rhs=rhs, start=start, stop=stop, skip_group_check=True), rd, wr)

        def TR(out, in_, ident, rd, wr):
            S.op('pe', lambda e: e.transpose(out, in_, ident), rd, wr)

        def ACT(out, in_, func, rd, wr, scale=1.0, bias=None):
            if bias is None:
                S.op('act', lambda e: e.activation(out, in_, func, scale=scale), rd, wr)
            else:
                S.op('act', lambda e: e.activation(out, in_, func, bias=bias, scale=scale), rd, wr)

        def TT(out, in0, in1, op, rd, wr, eng='dve'):
            S.op(eng, lambda e: e.tensor_tensor(out, in0, in1, op), rd, wr)

        def TS(out, in0, s1, s2, op0, op1, rd, wr, eng='dve'):
            if s2 is None:
                S.op(eng, lambda e: e.tensor_scalar(out, in0, s1, None, op0=op0), rd, wr)
            else:
                S.op(eng, lambda e: e.tensor_scalar(out, in0, s1, s2, op0=op0, op1=op1), rd, wr)

        def STT(out, in0, scalar, in1, op0, op1, rd, wr):
            S.op('dve', lambda e: e.scalar_tensor_tensor(out, in0, scalar, in1, op0=op0, op1=op1), rd, wr)

        def CP(out, in_, rd, wr, eng='dve'):
            if eng == 'act':
                S.op('act', lambda e: e.activation(out, in_, AF.Identity), rd, wr)
            else:
                S.op(eng, lambda e: e.tensor_copy(out, in_), rd, wr)

        def RED(out, in_, op, rd, wr):
            S.op('dve', lambda e: e.tensor_reduce(out, in_, axis=AX.X, op=op), rd, wr)

        def RCP(out, in_, rd, wr):
            S.op('dve', lambda e: e.reciprocal(out, in_), rd, wr)

        evt = [0]

        def EV():
            evt[0] += 1
            return 'act' if evt[0] % 2 else 'dve'

        def DMA(q, out, in_, rd, wr):
            pieces = []

            def split(o, i):
                shp = tuple(o.shape)
                assert tuple(i.shape) == shp, (shp, i.shape)
                if len(shp) == 3:
                    for a in range(shp[1]):
                        split(o[:, a, :], i[:, a, :])
                elif len(shp) == 2 and shp[1] > 512:
                    for c0 in range(0, shp[1], 512):
                        c1 = min(shp[1], c0 + 512)
                        pieces.append((o[:, c0:c1], i[:, c0:c1]))
                else:
                    pieces.append((o, i))
            split(out, in_)
            S.dma_multi(q, pieces, rd, wr)

        ident_f = T("ident_f", [128, 128], F32)
        ident_b = T("ident_b", [128, 128], BF16)
        onesm = T("onesm", [128, 128], BF16)
        epsA = T("epsA", [128, 2], F32)
        condT = T("condT", [128, 64], F32)
        condTb = T("condTb", [128, 8, 8], BF16)
        badaT = [T("badaT%d" % l_, [128, 64], F32) for l_ in range(DEPTH)]
        modT = [T("modT%d" % l, [128, 48], F32) for l in range(DEPTH)]
        drv = [T("drv%d" % l, [128, 64], F32) for l in range(DEPTH)]
        lnp = [T("lnp%d" % l, [128, 64], F32) for l in range(DEPTH)]
        cwT = T("cwT", [128, 132], F32)
        cbT = T("cbT", [128, 64], F32)
        decayT = T("decayT", [128, 512], F32)
        xiT = T("xiT", [128, 256], F32)
        zeta = T("zeta", [128, 64], F32)
        cdv = T("cdv", [128, 64], F32)
        expand = T("expand", [128, 2048], BF16)
        causalb = T("causalb", [128, 128], BF16)
        winb = T("winb", [128, 128], BF16)
        cmpb = T("cmpb", [128, 2048], BF16)
        fbias = T("fbias", [128, 512], F32)
        causal01 = T("causal01", [128, 128], F32)
        ovl = T("ovl", [128, 64], F32)
        WsT = T("WsT", [128, 512], BF16)
        bsT = T("bsT", [128, 64], F32)
        lnbc = T("lnbc", [128, 4, 256], F32)
        posT = T("posT", [128, 64], F32)
        w1s = T("w1s", [128, 2048], BF16)
        w2k = T("w2k", [64, 128], BF16)
        w2v = T("w2v", [64, 64], BF16)

        DMA('sp', ident_f[:], Dr['identf'], (), ['ident_f'])
        S.op('dve', lambda e: e.tensor_copy(ident_b[:], ident_f[:]), ['ident_f'], ['ident_b'])
        S.op('dve', lambda e: e.memset(onesm[:], 1.0 / 1024.0), (), ['onesm'])
        S.op('dve', lambda e: e.memset(epsA[:, 0:1], LN_EPS), (), ['epsA'])
        S.op('dve', lambda e: e.memset(epsA[:, 1:2], LN_EPS / (ALPHA * ALPHA)), (), ['epsA'])
        barscr = T("barscr", [128, 64], F32)
        RELAY = (barscr[:], Dr['zeta'])
        rlscr = T("rlscr", [128, 2], F32)
        S.relay_fn = lambda e: e.memset(rlscr[:, 0:1], 0.0)
        DMA('sp', condT[:], Dr['cT'], (), ['condT'])
        for nm, tl in (('decayT', decayT), ('xiT', xiT), ('zeta', zeta), ('cdv', cdv), ('fbias', fbias), ('causal01', causal01), ('ovl', ovl)):
            DMA('sp', tl[:], Dr[nm], (), [nm])
        for nm, tl in (('causalb', causalb), ('winb', winb)):
            DMA('pool', tl[:], Dr[nm], (), [nm])
        for nm, tl in (('expand', expand), ('cmpb', cmpb)):
            DMA('pool', tl[:].rearrange("p (a b) -> p a b", a=4), Dr[nm].rearrange("p (a b) -> p a b", a=4), (), [nm])
        ACT(condT[:], condT[:], AF.Silu, ['condT'], ['condT'])

        wr_n = [0]

        def wload(src_ap, shape):
            S.relay_readers(['WR%d' % j_ for j_ in range(4)])
            i = wr_n[0] % 4
            wr_n[0] += 1
            n = int(np.prod(shape))
            v = WR[:, i, 0:n]
            if len(shape) == 2:
                v = v.rearrange("p (a b) -> p a b", a=shape[0])
            key = 'WR%d' % i
            DMA('pool', v, src_ap, (), [key])
            return v, key

        def kchunks(ap2d):
            return ap2d.rearrange("(k p) n -> p k n", p=128)

        AR.reset()
        ada_buf = [AR.get([8, 512], BF16) for _ in range(2)]
        CP(condTb[:], condT[:, 0:8].unsqueeze(2).to_broadcast([128, 8, 8]), ['condT'], ['condTb'])
        xst = [AR.get([1024], F32) for _ in range(8)]
        bi = 0
        for tb in range(4):
            for q in range(4):
                tt = tb * 4 + q
                DMA('sp', xst[tt % 8], Dr['x'][tt * 128:(tt + 1) * 128, :], (), ['xst%d' % (tt % 8)])
            for c in range(8):
                b = 1 + bi % 7
                bi += 1
                for q in range(4):
                    tt = tb * 4 + q
                    TR(bank(b)[:, q * 128:(q + 1) * 128], xst[tt % 8][:, c * 128:(c + 1) * 128], ident_f[:],
                       ['xst%d' % (tt % 8), 'ident_f'], [BK[b]])
                CP(xT[:, c, tb * 512:(tb + 1) * 512], bank(b), [BK[b]], ['xT%d_%d' % (c, tb)], eng=EV())
        for l in range(n_layers):
            DMA('sp', badaT[l][:], Dr['b_adaT'][l], (), ['bada%d' % l])
            DMA('sp', lnp[l][:], Dr['lnpk'][l], (), ['lnp%d' % l])
        for l in range(min(1, n_layers)):
            for blk in range(12):
                buf = ada_buf[blk % 2]
                bk = 'ada%d' % (blk % 2)
                DMA('pool', buf, kchunks(Dr['w_ada'][l][:, blk * 512:(blk + 1) * 512]), (), [bk])
                for jj in range(4):
                    j = blk * 4 + jj
                    for k in range(8):
                        MM(bank(0)[:, j * 8:j * 8 + 8], buf[:, k, jj * 128:(jj + 1) * 128], condTb[:, k, :],
                           k == 0, k == 7, [bk, 'condTb'], ['B0'])
            TT(modT[l][:], bank(0)[:, 0:384].rearrange('p (j r) -> p j r', r=8)[:, :, 0], badaT[l][:, 0:48], ALU.add, ['B0', 'bada%d' % l], ['modT%d' % l])

        def derive(l):
            m = modT[l]
            d = drv[l]
            mk, dk = 'modT%d' % l, 'drv%d' % l
            TS(d[:, 0:8], m[:, 8:16], 1.0, None, ALU.add, None, [mk], [dk])
            TS(d[:, 8:16], m[:, 16:24], 1.0 / ALPHA, None, ALU.mult, None, [mk], [dk])
            TS(d[:, 16:24], m[:, 32:40], 1.0, None, ALU.add, None, [mk], [dk])
            TS(d[:, 24:32], m[:, 40:48], 1.0 / ALPHA, None, ALU.mult, None, [mk], [dk])
            TT(d[:, 32:40], lnp[l][:, 0:8], d[:, 16:24], ALU.mult, ['lnp%d' % l, dk], [dk])
            TT(d[:, 40:48], lnp[l][:, 8:16], d[:, 16:24], ALU.mult, ['lnp%d' % l, dk], [dk])
            TT(d[:, 40:48], d[:, 40:48], m[:, 24:32], ALU.add, [dk, mk], [dk])

        def derive_cross(l):
            d, dn = drv[l], drv[l + 1]
            dk, dnk = 'drv%d' % l, 'drv%d' % (l + 1)
            TT(d[:, 48:56], lnp[l][:, 16:24], dn[:, 0:8], ALU.mult, ['lnp%d' % l, dnk, dk], [dk])
            TT(d[:, 56:64], lnp[l][:, 24:32], dn[:, 0:8], ALU.mult, ['lnp%d' % l, dnk, dk], [dk])
            TT(d[:, 56:64], d[:, 56:64], modT[l + 1][:, 0:8], ALU.add, [dk, 'modT%d' % (l + 1)], [dk])

        def ada_block_deferred(l, blk):
            wv_, wk_ = wload(kchunks(Dr['w_ada'][l][:, blk * 512:(blk + 1) * 512]), [8, 512])
            for jj in range(4):
                for k in range(8):
                    MM(bank(7)[:, jj * 8:jj * 8 + 8], wv_[:, k, jj * 128:(jj + 1) * 128], condTb[:, k, :],
                       k == 0, k == 7, [wk_, 'condTb'], ['B7'])
            TT(modT[l][:, blk * 4:(blk + 1) * 4], bank(7)[:, 0:32].rearrange('p (j r) -> p j r', r=8)[:, :, 0],
               badaT[l][:, blk * 4:(blk + 1) * 4], ALU.add, ['B7', 'bada%d' % l], ['modT%d' % l])
            if blk == 11:
                derive(l)
                derive_cross(l - 1)

        if n_layers >= 1:
            derive(0)

        for tb in range(4):
            for c in range(8):
                xk = 'xT%d_%d' % (c, tb)
                if (tb * 8 + c) % 2 == 0:
                    TS(hT[:, c, tb * 512:(tb + 1) * 512], xT[:, c, tb * 512:(tb + 1) * 512], drv[0][:, c:c + 1], modT[0][:, c:c + 1],
                       ALU.mult, ALU.add, [xk, 'drv0', 'modT0'], ['hT%d_%d' % (c, tb)])
                else:
                    ACT(hT[:, c, tb * 512:(tb + 1) * 512], xT[:, c, tb * 512:(tb + 1) * 512], AF.Identity,
                        [xk, 'drv0', 'modT0'], ['hT%d_%d' % (c, tb)], scale=drv[0][:, c:c + 1], bias=modT[0][:, c:c + 1])

        def out_proj(l, mixtm, mixkeys, nch, row0, mixT, alias=()):
            nonlocal_bi = [0]
            for c in range(nch):
                for tb in range(4):
                    b = 4 + (nonlocal_bi[0] % 4)
                    nonlocal_bi[0] += 1
                    pb = bank(b).bitcast(BF16)
                    for q in range(4):
                        tt = tb * 4 + q
                        TR(pb[:, q * 128:(q + 1) * 128], mixtm[:, tt, c * 128:(c + 1) * 128], ident_b[:],
                           [mixkeys[tt], 'ident_b'], [BK[b]])
                    CP(mixT[:, c, tb * 512:(tb + 1) * 512], pb[:, 0:512], [BK[b]], ['mixT%d_%d' % (c, tb)] + list(alias), eng=EV())
            wv, wk = wload(kchunks(Dr['w_out'][l][row0:row0 + nch * 128, :]), [nch, 1024])
            for fb in range(8):
                for tb in range(4):
                    b = nonlocal_bi[0] % 4
                    nonlocal_bi[0] += 1
                    for c in range(nch):
                        MM(bank(b), wv[:, c, fb * 128:(fb + 1) * 128], mixT[:, c, tb * 512:(tb + 1) * 512],
                           c == 0, c == nch - 1, [wk, 'mixT%d_%d' % (c, tb)], [BK[b]])
                    xs = xT[:, fb, tb * 512:(tb + 1) * 512]
                    STT(xs, bank(b), drv[l][:, 8 + fb:9 + fb], xs, ALU.mult, ALU.add,
                        [BK[b], 'drv%d' % l, 'xT%d_%d' % (fb, tb)], ['xT%d_%d' % (fb, tb)])

        def layer_norm(l, which, last):
            goff = 0 if which == 1 else 16
            aoff = 32 if which == 1 else 48
            AR.reset()
            xb = [AR.get([512], BF16) for _ in range(3)]
            sq = [AR.get([512], BF16) for _ in range(3)]
            rstd = [AR.get([512], F32) for _ in range(2)]
            nmr = [AR.get([512], F32) for _ in range(2)]
            tmp = [AR.get([512], F32) for _ in range(3)]
            n1 = [0]
            n3 = [0]

            def p_stats(tb):
                bm, be = 0 + 2 * (tb % 2), 1 + 2 * (tb % 2)
                for c in range(8):
                    i = n1[0] % 3
                    n1[0] += 1
                    xs = xT[:, c, tb * 512:(tb + 1) * 512]
                    xk = 'xT%d_%d' % (c, tb)
                    ACT(sq[i], xs, AF.Square, [xk], ['lsq%d' % i])
                    CP(xb[i], xs, [xk], ['lxb%d' % i], eng='dve')
                    MM(bank(bm), onesm[:], xb[i], c == 0, c == 7, ['onesm', 'lxb%d' % i], [BK[bm]])
                    MM(bank(be), onesm[:], sq[i], c == 0, c == 7, ['onesm', 'lsq%d' % i], [BK[be]])

            def p_apply(tb):
                bm, be = 0 + 2 * (tb % 2), 1 + 2 * (tb % 2)
                r = tb % 2
                rk, nk = 'lrstd%d' % r, 'lnmr%d' % r
                ACT(nmr[r], bank(bm), AF.Square, [BK[bm]], [nk])
                TT(rstd[r], bank(be), nmr[r], ALU.subtract, [BK[be], nk], [rk])
                TS(rstd[r], rstd[r], 0.0, None, ALU.max, None, [rk], [rk])
                ACT(rstd[r], rstd[r], AF.Sqrt, [rk, 'epsA'], [rk], bias=epsA[:, 1:2])
                RCP(rstd[r], rstd[r], [rk], [rk])
                STT(nmr[r], bank(bm), -1.0, rstd[r], ALU.mult, ALU.mult, [BK[bm], rk], [nk])
                for c in range(8):
                    i = n3[0] % 3
                    n3[0] += 1
                    xs = xT[:, c, tb * 512:(tb + 1) * 512]
                    xk = 'xT%d_%d' % (c, tb)
                    tk = 'ltmp%d' % i
                    TT(tmp[i], xs, rstd[r], ALU.mult, [xk, rk], [tk])
                    TT(tmp[i], tmp[i], nmr[r], ALU.add, [tk, nk], [tk])
                    ACT(xs, tmp[i], AF.Identity, [tk, 'lnp%d' % l], [xk],
                        scale=lnp[l][:, goff + c:goff + c + 1], bias=lnp[l][:, goff + 8 + c:goff + 9 + c])
                    if not last:
                        ACT(hT[:, c, tb * 512:(tb + 1) * 512], tmp[i], AF.Identity, [tk, 'drv%d' % l], ['hT%d_%d' % (c, tb)],
                            scale=drv[l][:, aoff + c:aoff + c + 1], bias=drv[l][:, aoff + 8 + c:aoff + 9 + c])

            p_stats(0)
            for tb in range(4):
                if tb + 1 < 4:
                    p_stats(tb + 1)
                p_apply(tb)

        def dump_tm(src, c0, ncol, keys, dcol):
            dst = AR.get([ncol], F32)
            for tt in range(16):
                CP(dst, src[:, tt, c0:c0 + ncol], [keys[tt]], ['dst'])
                DMA('sp', dbg_d[tt * 128:(tt + 1) * 128, dcol:dcol + ncol], dst, ['dst'], ['dbg'])

        def group_ln_core(src, sqv, nb, rk, wk_sq, eps_ap):
            s1 = AR.get([nb], F32)
            s2 = AR.get([nb], F32)
            s3 = AR.get([nb], F32)
            TT(sqv, src, src, ALU.mult, rk, [wk_sq])
            RED(s1, src, ALU.add, rk, ['gs1'])
            RED(s2, sqv, ALU.add, [wk_sq], ['gs2'])
            TS(s1, s1, 1.0 / 64, None, ALU.mult, None, ['gs1'], ['gs1'])
            TT(s3, s1, s1, ALU.mult, ['gs1'], ['gs3'])
            STT(s2, s2, 1.0 / 64, s3, ALU.mult, ALU.subtract, ['gs2', 'gs3'], ['gs2'])
            TS(s2, s2, 0.0, None, ALU.max, None, ['gs2'], ['gs2'])
            ACT(s2, s2, AF.Sqrt, ['gs2', 'epsA'], ['gs2'], bias=eps_ap)
            RCP(s2, s2, ['gs2'], ['gs2'])
            TT(sqv, src, s1.unsqueeze(2).to_broadcast([128, nb, 64]), ALU.subtract, rk + ['gs1'], [wk_sq])
            TT(sqv, sqv, s2.unsqueeze(2).to_broadcast([128, nb, 64]), ALU.mult, [wk_sq, 'gs2'], [wk_sq])

        def mixer_B(l):
            hk = lambda k, tb: 'hT%d_%d' % (k, tb)
            S.barrier(relay=RELAY)
            AR.reset()
            qrT = AR.get([2, 2048], BF16)
            krT = AR.get([2, 2048], BF16)
            off_x = AR.off
            cosT = AR.get([2048], F32)
            sinT = AR.get([2048], F32)
            rt1 = [AR.get([512], F32) for _ in range(2)]
            rt2 = [AR.get([512], F32) for _ in range(2)]
            DMA('sp', cosT, Dr['cosT'], (), ['cosT'])
            DMA('sp', sinT, Dr['sinT'], (), ['sinT'])
            DMA('sp', lnbc[:, 2, :], Dr['b_gng'][l], (), ['lnbc'])
            DMA('sp', lnbc[:, 3, :], Dr['b_gnb'][l], (), ['lnbc'])
            n = 0
            for p in range(2):
                wv, wk = wload(kchunks(Dr['w_inBf'][l][:, p * 512:(p + 1) * 512]), [8, 512])
                for kind in range(2):
                    dst = qrT if kind == 0 else krT
                    for tb in range(4):
                        i = n % 2
                        b1, b2 = 2 * (n % 4), 2 * (n % 4) + 1
                        n += 1
                        for k in range(8):
                            MM(bank(b1), wv[:, k, (2 * kind) * 128:(2 * kind + 1) * 128], hT[:, k, tb * 512:(tb + 1) * 512],
                               k == 0, k == 7, [wk, hk(k, tb)], [BK[b1]])
                        for k in range(8):
                            MM(bank(b2), wv[:, k, (2 * kind + 1) * 128:(2 * kind + 2) * 128], hT[:, k, tb * 512:(tb + 1) * 512],
                               k == 0, k == 7, [wk, hk(k, tb)], [BK[b2]])
                        TT(rt1[i], bank(b1), cosT[:, tb * 512:(tb + 1) * 512], ALU.mult, [BK[b1], 'cosT'], ['rt1_%d' % i])
                        TT(rt2[i], bank(b2), sinT[:, tb * 512:(tb + 1) * 512], ALU.mult, [BK[b2], 'sinT'], ['rt2_%d' % i])
                        TT(dst[:, p, tb * 512:(tb + 1) * 512], rt1[i], rt2[i], ALU.add, ['rt1_%d' % i, 'rt2_%d' % i],
                           ['qk%d_%d' % (kind, p)])
            wvt, wkt = wload(kchunks(Dr['w_inBt'][l]), [8, 512])
            for p in range(2):
                S.barrier(relay=RELAY)
                AR.reset(off_x)
                vg = AR.get([16, 256], BF16)
                kvs = AR.get([16, 128], F32)
                s16 = AR.get([16, 128], BF16)
                oall = AR.get([16, 128], F32)
                kz = [AR.get([128], BF16) for _ in range(2)]
                qx = [AR.get([128], BF16) for _ in range(2)]
                sT = [AR.get([256], BF16) for _ in range(2)]
                mixT = AR.get([1, 2048], BF16)
                qkk = ['qk0_%d' % p, 'qk1_%d' % p]
                for tt in range(16):
                    b = tt % 4
                    for k in range(8):
                        MM(bank(b)[:, 0:256], hT[:, k, tt * 128:(tt + 1) * 128], wvt[:, k, p * 256:(p + 1) * 256],
                           k == 0, k == 7, [hk(k, tt // 4), wkt], [BK[b]])
                    CP(vg[:, tt, 0:128], bank(b)[:, 0:128], [BK[b]], ['vg%d' % tt], eng='dve')
                    ACT(vg[:, tt, 128:256], bank(b)[:, 128:256], AF.Silu, [BK[b]], ['vg%d' % tt])
                S.op('dve', lambda e: e.memset(kvs[:, 0, :], 0.0), (), ['kvs'])
                def st_a(c):
                    i = c % 2
                    bt = 4 + (c % 2)
                    pb = bank(bt).bitcast(BF16)
                    TR(pb[:, 0:128], krT[:, p, c * 128:(c + 1) * 128], ident_b[:], [qkk[1], 'ident_b'], [BK[bt]])
                    TT(kz[i].rearrange("p (h d) -> p h d", h=2), pb[:, 0:128].rearrange("p (h d) -> p h d", h=2),
                       zeta[:, 2 * p:2 * p + 2].unsqueeze(2).to_broadcast([128, 2, 64]), ALU.mult,
                       [BK[bt], 'zeta'], ['kz%d' % i])

                def st_b(c):
                    i = c % 2
                    bm = 6 + (c % 2)
                    MM(bank(bm)[:, 0:128], kz[i], vg[:, c, 0:128], True, True, ['kz%d' % i, 'vg%d' % c], [BK[bm]])
                    STT(kvs[:, c + 1, :], kvs[:, c, :], cdv[:, p:p + 1], bank(bm)[:, 0:128], ALU.mult, ALU.add,
                        ['kvs', 'cdv', BK[bm]], ['kvs'])

                st_a(0)
                for c in range(15):
                    if c + 1 < 15:
                        st_a(c + 1)
                    st_b(c)
                CP(s16, kvs, ['kvs'], ['s16'], eng='dve')
                def b_scores(c):
                    i = c % 2
                    bs0 = 2 * (c % 2)
                    cs = slice(c * 128, (c + 1) * 128)
                    if c > 0:
                        TT(qx[i], qrT[:, p, cs], xiT[:, p * 128:(p + 1) * 128], ALU.mult, [qkk[0], 'xiT'], ['qx%d' % i])
                    for hh in range(2):
                        ps_ = slice(hh * 64, (hh + 1) * 64)
                        MM(bank(bs0 + hh)[:, 0:128], krT[ps_, p, cs], qrT[ps_, p, cs], True, True,
                           qkk, [BK[bs0 + hh]])

                def b_rest(c):
                    i = c % 2
                    bs0 = 2 * (c % 2)
                    bo0 = 4 + 2 * (c % 2)
                    TT(sT[i].rearrange("p (h l) -> p h l", h=2),
                       PSA[:, bs0 * 512:(bs0 + 2) * 512].rearrange("p (h x) -> p h x", h=2)[:, :, 0:128],
                       decayT[:, 2 * p * 128:(2 * p + 2) * 128].rearrange("p (h l) -> p h l", h=2), ALU.mult,
                       [BK[bs0], BK[bs0 + 1], 'decayT'], ['sT%d' % i])
                    for hh in range(2):
                        ps_ = slice(hh * 64, (hh + 1) * 64)
                        MM(bank(bo0 + hh)[:, 0:64], sT[i][:, hh * 128:(hh + 1) * 128], vg[:, c, hh * 64:(hh + 1) * 64],
                           True, c == 0, ['sT%d' % i, 'vg%d' % c], [BK[bo0 + hh]])
                        if c > 0:
                            MM(bank(bo0 + hh)[:, 0:64], qx[i][ps_, :], s16[ps_, c, hh * 64:(hh + 1) * 64],
                               False, True, ['qx%d' % i, 's16'], [BK[bo0 + hh]])
                    CP(oall[:, c, :].rearrange("p (h e) -> p h e", h=2),
                       PSB[:, (bo0 - 4) * 512:(bo0 - 2) * 512].rearrange("p (h x) -> p h x", h=2)[:, :, 0:64],
                       [BK[bo0], BK[bo0 + 1]], ['oall'], eng='act')

                b_scores(0)
                for c in range(16):
                    if c + 1 < 16:
                        b_scores(c + 1)
                    b_rest(c)
                o3 = oall.rearrange("p c (h e) -> p (c h) e", h=2)
                sq3 = kvs.rearrange("p c (h e) -> p (c h) e", h=2)
                gam_b = lnbc[:, 2, p * 128:(p + 1) * 128].rearrange("p (h e) -> p h e", h=2).unsqueeze(1).to_broadcast([128, 16, 2, 64])
                bet_b = lnbc[:, 3, p * 128:(p + 1) * 128].rearrange("p (h e) -> p h e", h=2).unsqueeze(1).to_broadcast([128, 16, 2, 64])
                sq4 = kvs.rearrange("p c (h e) -> p c h e", h=2)
                group_ln_core(o3, sq3, 32, ['oall'], 'kvs', epsA[:, 0:1])
                TT(sq4, sq4, gam_b, ALU.mult, ['kvs', 'lnbc'], ['kvs'])
                TT(sq4, sq4, bet_b, ALU.add, ['kvs', 'lnbc'], ['kvs'])
                vkeys = ['vg%d' % tt for tt in range(16)]
                TT(vg[:, :, 0:128], kvs, vg[:, :, 128:256], ALU.mult, ['kvs'] + vkeys, vkeys)
                if dbg == 'mixB%d_%d' % (l, p):
                    dump_tm(vg, 0, 128, vkeys, p * 128)
                out_proj(l, vg, vkeys, 1, 256 + p * 128, mixT)

        def mixer_C(l):
            hk = lambda k, tb: 'hT%d_%d' % (k, tb)
            DMA('pool', w1s[:].rearrange("p (a b) -> p a b", a=4), Dr['w1s'][l].rearrange("p (a b) -> p a b", a=4), (), ['w1s'])
            DMA('pool', w2k[:], Dr['w2k'][l], (), ['w2k'])
            DMA('pool', w2v[:], Dr['w2v'][l], (), ['w2v'])
            DMA('sp', posT[:], Dr['posT'][l], (), ['posT'])
            for g in range(2):
                S.barrier(relay=RELAY)
                AR.reset()
                qz = AR.get([4, 2048], BF16)
                KsT = AR.get([2048], BF16)
                KwT = AR.get([2048], BF16)
                Vaug = AR.get([16, 2, 65], BF16)
                gates = AR.get([16, 12], F32)
                mixC = AR.get([16, 256], BF16)
                kcmpT = AR.get([127], BF16)
                vcaug = AR.get([97], BF16)
                hidT = AR.get([2, 127], BF16)
                off_r = AR.off
                kvcT = AR.get([2048], BF16)
                kcp = AR.get([32, 127], BF16)
                wv, wk = wload(kchunks(Dr['w_inCf'][l][:, g * 640:g * 640 + 512]), [8, 512])
                wv2, wk2 = wload(kchunks(Dr['w_inCf'][l][:, g * 640 + 512:g * 640 + 640]), [8, 128])
                S.op('dve', lambda e: e.memset(qz, 0.0), (), ['qT'])
                dsts = [(None, 'qT', 0.125), (None, 'qT', 0.125), (KsT, 'KsT', 1.0), (KwT, 'KwT', 1.0),
                        (kvcT, 'kvcT', 1.0)]
                n = 0
                for bi_, (dst, dk, scl) in enumerate(dsts):
                    for tb in range(4):
                        b = n % 4
                        n += 1
                        for k in range(8):
                            lw = wv[:, k, bi_ * 128:(bi_ + 1) * 128] if bi_ < 4 else wv2[:, k, :]
                            MM(bank(b), lw, hT[:, k, tb * 512:(tb + 1) * 512], k == 0, k == 7,
                               [wk if bi_ < 4 else wk2, hk(k, tb)], [BK[b]])
                        if dst is None:
                            ACT(qz[0:64, 2 * bi_, tb * 512:(tb + 1) * 512], bank(b)[0:64, :], AF.Identity, [BK[b]], [dk], scale=scl)
                            TS(qz[64:128, 2 * bi_ + 1, tb * 512:(tb + 1) * 512], bank(b)[64:128, :], scl, None, ALU.mult, None, [BK[b]], [dk])
                        elif n % 2:
                            ACT(dst[:, tb * 512:(tb + 1) * 512], bank(b), AF.Identity, [BK[b]], [dk], scale=scl)
                        else:
                            TS(dst[:, tb * 512:(tb + 1) * 512], bank(b), scl, None, ALU.mult, None, [BK[b]], [dk])
                wv3, wk3 = wload(kchunks(Dr['w_inCt'][l][:, g * 140:(g + 1) * 140]), [8, 140])
                S.op('dve', lambda e: e.memset(Vaug[:, :, :, 64:65], 1.0), (), ['Vaug'])
                for tt in range(16):
                    b = 4 + tt % 4
                    for k in range(8):
                        MM(bank(b)[:, 0:140], hT[:, k, tt * 128:(tt + 1) * 128], wv3[:, k, :], k == 0, k == 7,
                           [hk(k, tt // 4), wk3], [BK[b]])
                    CP(Vaug[:, tt, :, 0:64], bank(b)[:, 0:128].rearrange("p (s d) -> p s d", s=2), [BK[b]], ['Vaug'], eng='dve')
                    ACT(gates[:, tt, :], bank(b)[:, 128:140], AF.Sigmoid, [BK[b]], ['gates'])
                kv_t = kvcT.tensor
                win = bass.AP(kv_t, kvcT.offset, [list(kvcT.ap[0]), [1, 32], [16, 127]])
                TT(kcp, win, posT[:, 0:32].unsqueeze(2).to_broadcast([128, 32, 127]), ALU.add, ['kvcT', 'posT'], ['kcp'])
                for kv in range(2):
                    ps_ = slice(kv * 64, (kv + 1) * 64)
                    for ll in range(32):
                        MM(bank(kv)[0:64, 0:127], w1s[ps_, ll * 64:(ll + 1) * 64], kcp[ps_, ll, :],
                           ll == 0, ll == 31, ['w1s', 'kcp'], [BK[kv]])
                    ACT(hidT[0:64, kv, :], bank(kv)[0:64, 0:127], AF.Gelu_apprx_tanh, [BK[kv]], ['hidT'])
                MM(bank(2)[:, 0:127], w2k[:], hidT[0:64, 0, :], True, True, ['w2k', 'hidT'], ['B2'])
                CP(kcmpT, bank(2)[:, 0:127], ['B2'], ['kcmpT'], eng='dve')
                MM(bank(3)[0:127, 0:64], hidT[0:64, 1, :], w2v[:], True, True, ['w2v', 'hidT'], ['B3'])
                S.op('dve', lambda e: e.memset(vcaug[:, 64:65], 1.0), (), ['vcaug'])
                CP(vcaug[0:127, 0:64], bank(3)[0:127, 0:64], ['B3'], ['vcaug'], eng='dve')
                CP(vcaug[:, 65:97], ovl[:, 0:32], ['ovl'], ['vcaug'], eng='dve')
                S.barrier(relay=RELAY)
                AR.reset(off_r)
                PT = [AR.get([512], BF16) for _ in range(4)]
                MbT = [AR.get([128], BF16) for _ in range(2)]
                for j_ in range(2):
                    S.op('dve', (lambda t_: (lambda e: e.memset(t_, 0.0)))(MbT[j_]), (), ['MbT%d' % j_])
                Mb = [AR.get([32], BF16) for _ in range(2)]
                ocg = [AR.get([4, 64], F32) for _ in range(2)]
                t1 = [AR.get([4, 64], F32) for _ in range(2)]
                t2 = [AR.get([4, 64], F32) for _ in range(2)]
                sm = [AR.get([96], F32) for _ in range(2)]
                impt = [AR.get([4, 32], F32) for _ in range(2)]
                pn = [0]

                def qslice(r, qt):
                    return qz[:, r, qt * 128:(qt + 1) * 128]

                def attend(qt, kts, KT, vsel, bS, bO, extra_fn):
                    def scores(ki):
                        kt = kts[ki]
                        bs_ = bS[ki % len(bS)]
                        extras = extra_fn(kt)
                        v4 = bank(bs_).rearrange("p (r t) -> p r t", r=4)
                        MM(v4, KT[:, kt * 128:(kt + 1) * 128], qz[:, :, qt * 128:(qt + 1) * 128],
                           True, not extras, ['KsT', 'KwT', 'qT'], [BK[bs_]])
                        for ei, (lh, rh, ks) in enumerate(extras):
                            MM(v4, lh, rh, False, ei == len(extras) - 1, ks, [BK[bs_]])

                    def rest(ki):
                        kt = kts[ki]
                        bs_ = bS[ki % len(bS)]
                        i = pn[0] % 4
                        pn[0] += 1
                        ACT(PT[i], bank(bs_), AF.Exp, [BK[bs_]], ['PT%d' % i])
                        for r in range(4):
                            MM(bank(bO)[:, r * 65:(r + 1) * 65], PT[i][:, r * 128:(r + 1) * 128], Vaug[:, kt, vsel, :],
                               ki == 0 and r == 0, ki == len(kts) - 1 and r == 3, ['PT%d' % i, 'Vaug'], [BK[bO]])

                    LA = 2
                    for ki in range(min(LA, len(kts))):
                        scores(ki)
                    for ki in range(len(kts)):
                        if ki + LA < len(kts):
                            scores(ki + LA)
                        rest(ki)

                for qt in range(16):
                    j = qt % 2
                    smj = sm[j]
                    sk = 'sm%d' % j
                    qs = slice(qt * 128, (qt + 1) * 128)
                    MM(bank(0)[0:127, :].rearrange("p (r t) -> p r t", r=4), kcmpT[:, :], qz[:, :, qs], True, False,
                       ['kcmpT', 'qT'], ['B0'])
                    MM(bank(0)[0:127, :].rearrange("p (r t) -> p r t", r=4), ident_b[0:127, 0:127],
                       cmpb[0:127, qs].unsqueeze(1).to_broadcast([127, 4, 128]), False, True, ['ident_b', 'cmpb'], ['B0'])
                    i = pn[0] % 4
                    pn[0] += 1
                    ACT(PT[i][0:127, :], bank(0)[0:127, :], AF.Exp, ['B0'], ['PT%d' % i])
                    for r in range(4):
                        MM(bank(1)[:, r * 97:(r + 1) * 97], PT[i][0:127, r * 128:(r + 1) * 128], vcaug[0:127, :],
                           r == 0, r == 3, ['PT%d' % i, 'vcaug'], ['B1'])
                    O = bank(1)[:, 0:388].rearrange("p (r c) -> p r c", r=4)
                    cb4 = causalb[:].unsqueeze(1).to_broadcast([128, 4, 128])
                    wb4 = winb[:].unsqueeze(1).to_broadcast([128, 4, 128])

                    def wmask(kt, qt=qt, cb4=cb4, wb4=wb4):
                        if kt == qt:
                            return [(ident_b[:], cb4, ['ident_b', 'causalb'])]
                        if kt == qt - 4:
                            return [(ident_b[:], wb4, ['ident_b', 'winb'])]
                        return []
                    attend(qt, list(range(max(0, qt - 4), qt + 1)), KwT, 1, [2, 3, 6, 7], 5, wmask)
                    TS(smj[:, 0:4], O[:, :, 64], 1e-30, None, ALU.max, None, ['B1'], [sk])
                    RCP(smj[:, 4:8], smj[:, 0:4], [sk], [sk])
                    TT(impt[j], O[:, :, 65:97], smj[:, 4:8].unsqueeze(2).to_broadcast([128, 4, 32]), ALU.mult, ['B1', sk], ['impt%d' % j])
                    RED(smj[:, 32:64], impt[j].rearrange("p r c -> p c r"), ALU.add, ['impt%d' % j], [sk])
                    TT(smj[:, 32:64], smj[:, 32:64], fbias[:, qt * 32:(qt + 1) * 32], ALU.add, [sk, 'fbias'], [sk])
                    S.op('dve', (lambda o_, i_: (lambda e: e.max(o_, i_)))(smj[:, 64:72], smj[:, 32:64]), [sk], [sk])
                    TS(Mb[j], smj[:, 32:64], smj[:, 71:72], -1.0, ALU.is_ge, ALU.add, [sk], ['Mb%d' % j])
                    pbt = bank(0).bitcast(BF16)
                    TR(pbt[0:32, 0:128], Mb[j], ident_b[:], ['Mb%d' % j, 'ident_b'], ['B0'])
                    CP(MbT[j][0:32, :], pbt[0:32, 0:128], ['B0'], ['MbT%d' % j], eng='dve')
                    gv = gates[:, qt, :].rearrange("p (r c) -> p r c", r=4)
                    TT(smj[:, 8:12], smj[:, 4:8], gv[:, :, 0], ALU.mult, [sk, 'gates'], [sk])
                    TT(ocg[j], O[:, :, 0:64], smj[:, 8:12].unsqueeze(2).to_broadcast([128, 4, 64]), ALU.mult, ['B1', sk], ['ocg%d' % j])

                    mb4 = MbT[j][:, :].unsqueeze(1).to_broadcast([128, 4, 128])

                    def smask(kt, qt=qt, j=j, cb4=cb4, mb4=mb4):
                        ex = [(expand[:, kt * 128:(kt + 1) * 128], mb4, ['expand', 'MbT%d' % j])]
                        if kt == qt:
                            ex.append((ident_b[:], cb4, ['ident_b', 'causalb']))
                        return ex
                    attend(qt, list(range(0, qt + 1)), KsT, 0, [2, 3, 6, 7], 4, smask)
                    Os = bank(4)[:, 0:260].rearrange("p (r c) -> p r c", r=4)
                    Ow = bank(5)[:, 0:260].rearrange("p (r c) -> p r c", r=4)
                    RCP(smj[:, 12:16], Os[:, :, 64], ['B4'], [sk])
                    TT(smj[:, 12:16], smj[:, 12:16], gv[:, :, 1], ALU.mult, [sk, 'gates'], [sk])
                    RCP(smj[:, 16:20], Ow[:, :, 64], ['B5'], [sk])
                    TT(smj[:, 16:20], smj[:, 16:20], gv[:, :, 2], ALU.mult, [sk, 'gates'], [sk])
                    TT(t1[j], Os[:, :, 0:64], smj[:, 12:16].unsqueeze(2).to_broadcast([128, 4, 64]), ALU.mult, ['B4', sk], ['t1_%d' % j])
                    TT(t2[j], Ow[:, :, 0:64], smj[:, 16:20].unsqueeze(2).to_broadcast([128, 4, 64]), ALU.mult, ['B5', sk], ['t2_%d' % j])
                    TT(t1[j], t1[j], ocg[j], ALU.add, ['t1_%d' % j, 'ocg%d' % j], ['t1_%d' % j])
                    TT(mixC[:, qt, :].rearrange("p (r d) -> p r d", r=4), t1[j], t2[j], ALU.add, ['t1_%d' % j, 't2_%d' % j], ['mixC%d' % qt])
                mkeys = ['mixC%d' % tt for tt in range(16)]
                if dbg == 'mixC%d_%d' % (l, g):
                    dump_tm(mixC, 0, 256, mkeys, g * 256)
                S.barrier(relay=RELAY)
                AR.reset(off_r)
                mixT = AR.get([2, 2048], BF16)
                out_proj(l, mixC, mkeys, 2, 512 + g * 256, mixT)

        for l in range(n_layers):
            hk = lambda k, tb: 'hT%d_%d' % (k, tb)
            if 'A' in mixers:
                S.barrier(relay=RELAY)
                AR.reset()
                zag = AR.get([16, 512], BF16)
                off_sqv = AR.off
                sqv = AR.get([16, 256], F32)
                st1 = AR.get([64], F32)
                st2 = AR.get([64], F32)
                st3 = AR.get([64], F32)
                mixA = AR.get([16, 256], BF16)
                atmp = [AR.get([256], F32) for _ in range(2)]
                wstage = AR.get([4, 128], F32)
                DMA('sp', wstage, Dr['WsT'][l].rearrange("p (g t) -> p g t", g=4), (), ['wstage'])
                TT(WsT[:].rearrange("p (g t) -> p g t", g=4), wstage,
                   causal01[:].unsqueeze(1).to_broadcast([128, 4, 128]), ALU.mult, ['wstage', 'causal01'], ['WsT'])
                DMA('sp', bsT[:], Dr['bsT'][l], (), ['bsT'])
                DMA('sp', lnbc[:, 0, :], Dr['a_lng'][l], (), ['lnbcA'])
                DMA('sp', lnbc[:, 1, :], Dr['a_lnb'][l], (), ['lnbcA'])
                wv, wk = wload(kchunks(Dr['w_inA'][l]), [8, 512])
                for tt in range(16):
                    b = tt % 4
                    for k in range(8):
                        MM(bank(b), hT[:, k, tt * 128:(tt + 1) * 128], wv[:, k, :], k == 0, k == 7,
                           [hk(k, tt // 4), wk], [BK[b]])
                    ACT(zag[:, tt, :], bank(b), AF.Gelu_apprx_tanh, [BK[b]], ['zag'])
                v4 = zag[:, :, 256:512].rearrange("p t (g d) -> p t g d", g=4)
                TT(sqv, zag[:, :, 256:512], zag[:, :, 256:512], ALU.mult, ['zag'], ['sqv'])
                RED(st1.rearrange("p (t g) -> p t g", t=16), v4, ALU.add, ['zag'], ['st1'])
                RED(st2.rearrange("p (t g) -> p t g", t=16), sqv.rearrange("p t (g d) -> p t g d", g=4), ALU.add, ['sqv'], ['st2'])
                TS(st1, st1, 1.0 / 64, None, ALU.mult, None, ['st1'], ['st1'])
                TT(st3, st1, st1, ALU.mult, ['st1'], ['st3'])
                STT(st2, st2, 1.0 / 64, st3, ALU.mult, ALU.subtract, ['st2', 'st3'], ['st2'])
                TS(st2, st2, 0.0, None, ALU.max, None, ['st2'], ['st2'])
                ACT(st2, st2, AF.Sqrt, ['st2', 'epsA'], ['st2'], bias=epsA[:, 0:1])
                RCP(st2, st2, ['st2'], ['st2'])
                mean_b = st1.rearrange("p (t g) -> p t g", t=16).unsqueeze(3).to_broadcast([128, 16, 4, 64])
                rstd_b = st2.rearrange("p (t g) -> p t g", t=16).unsqueeze(3).to_broadcast([128, 16, 4, 64])
                sq4 = sqv.rearrange("p t (g d) -> p t g d", g=4)
                TT(sq4, v4, mean_b, ALU.subtract, ['zag', 'st1'], ['sqv'])
                TT(sq4, sq4, rstd_b, ALU.mult, ['sqv', 'st2'], ['sqv'])
                gam_b = lnbc[:, 0, :].unsqueeze(1).to_broadcast([128, 16, 256])
                bet_b = lnbc[:, 1, :].unsqueeze(1).to_broadcast([128, 16, 256])
                TT(sqv, sqv, gam_b, ALU.mult, ['sqv', 'lnbcA'], ['sqv'])
                TT(zag[:, :, 256:512], sqv, bet_b, ALU.add, ['sqv', 'lnbcA'], ['zag'])
                bs_b = bsT[:, 0:4].unsqueeze(2).to_broadcast([128, 4, 64])
                for tt in range(16):
                    b = 4 + tt % 4
                    for g in range(4):
                        MM(bank(b)[:, g * 64:(g + 1) * 64], WsT[:, g * 128:(g + 1) * 128],
                           zag[:, tt, 256 + g * 64:256 + (g + 1) * 64], g == 0, g == 3, ['WsT', 'zag'], [BK[b]])
                    at = atmp[tt % 2]
                    ak = 'atmp%d' % (tt % 2)
                    TT(at.rearrange("p (g d) -> p g d", g=4), bank(b)[:, 0:256].rearrange("p (g d) -> p g d", g=4),
                       bs_b, ALU.add, [BK[b], 'bsT'], [ak])
                    TT(mixA[:, tt, :], at, zag[:, tt, 0:256], ALU.mult, [ak, 'zag'], ['mixA%d' % tt])
                if dbg == 'mixA%d' % l:
                    dst = AR.get([256], F32)
                    for tt in range(16):
                        CP(dst, mixA[:, tt, :], ['mixA%d' % tt], ['dst'])
                        DMA('sp', dbg_d[tt * 128:(tt + 1) * 128, 0:256], dst, ['dst'], ['dbg'])
                AR.reset(off_sqv)
                mixT = AR.get([2, 2048], BF16)
                out_proj(l, mixA, ['mixA%d' % tt for tt in range(16)], 2, 0, mixT, ['sqv'])

            if 'B' in mixers:
                mixer_B(l)
            if 'C' in mixers:
                mixer_C(l)

            S.barrier(relay=RELAY)
            layer_norm(l, 1, last=False)
            if dbg == 'ln1_%d' % l:
                break

            if do_ffn:
                S.barrier(relay=RELAY)
                AR.reset()
                DMA('sp', cwT[:], Dr['cwT'][l], (), ['cwT'])
                DMA('sp', cbT[:], Dr['cbT'][l], (), ['cbT'])
                gT = AR.get([max(FF_SPLIT), 2048], BF16)
                ctmp = [[AR.get([1024], F32) for _ in range(2)] for _ in range(2)]
                sgt = [AR.get([1024], BF16) for _ in range(2)]
                j0 = 0
                PS4 = [PSA, PSB]
                P4K = [BK[0:4], BK[4:8]]
                for pi_, part_n in enumerate(FF_SPLIT):
                    if l + 1 < n_layers:
                        for blk_ in range(3 * pi_, 3 * pi_ + 3):
                            ada_block_deferred(l + 1, blk_)
                    wgrp = {}
                    for jl in range(part_n):
                        j = j0 + jl
                        if jl % 4 == 0:
                            ng = min(4, part_n - jl)
                            for part in range(2):
                                jj0 = part * 22 + j
                                wgrp[part] = wload(kchunks(Dr['w_up'][l][:, jj0 * 128:(jj0 + ng) * 128]), [8, ng * 128])
                        for part in range(2):
                            jj = part * 22 + j
                            wvf, wk = wgrp[part]
                            wv = wvf[:, :, (jl % 4) * 128:(jl % 4 + 1) * 128]
                            ps = PS4[part]
                            for tb in range(4):
                                for k in range(8):
                                    MM(ps[:, tb * 512:(tb + 1) * 512], wv[:, k, :], hT[:, k, tb * 512:(tb + 1) * 512],
                                       k == 0, k == 7, [wk, hk(k, tb)], [P4K[part][tb]])
                            for hf in range(2):
                                ct = ctmp[part][hf]
                                ck = 'ctmp%d_%d' % (part, hf)
                                o = hf * 1024
                                pk = P4K[part][2 * hf:2 * hf + 2]
                                pkp = P4K[part][max(0, 2 * hf - 1):2 * hf + 2]
                                ACT(ct, ps[:, o:o + 1024], AF.Identity, pk + ['cwT', 'cbT'], [ck],
                                    scale=cwT[:, jj * 3 + 2:jj * 3 + 3], bias=cbT[:, jj:jj + 1])
                                if hf == 0:
                                    STT(ct[:, 1:1024], ps[:, 0:1023], cwT[:, jj * 3 + 1:jj * 3 + 2], ct[:, 1:1024],
                                        ALU.mult, ALU.add, pk + ['cwT', ck], [ck])
                                    STT(ct[:, 2:1024], ps[:, 0:1022], cwT[:, jj * 3:jj * 3 + 1], ct[:, 2:1024],
                                        ALU.mult, ALU.add, pk + ['cwT', ck], [ck])
                                else:
                                    STT(ct, ps[:, o - 1:o + 1023], cwT[:, jj * 3 + 1:jj * 3 + 2], ct,
                                        ALU.mult, ALU.add, pkp + ['cwT', ck], [ck])
                                    STT(ct, ps[:, o - 2:o + 1022], cwT[:, jj * 3:jj * 3 + 1], ct,
                                        ALU.mult, ALU.add, pkp + ['cwT', ck], [ck])
                                if part == 0:
                                    ACT(sgt[hf], ct, AF.Silu, [ck], ['sgt%d' % hf])
                                else:
                                    TT(gT[:, jl, o:o + 1024], ct, sgt[hf], ALU.mult, [ck, 'sgt%d' % hf], ['gT%d' % jl])
                    nsl = (part_n + 3) // 4
                    wvs = []
                    for s in range(nsl):
                        r0 = (j0 + 4 * s) * 128
                        nr = min(4, part_n - 4 * s)
                        wvs.append(wload(kchunks(Dr['w_down'][l][r0:r0 + nr * 128, :]), [nr, 1024]))
                    bi2 = 0
                    for fb in range(8):
                        for tb in range(4):
                            b = bi2 % 8
                            bi2 += 1
                            for jl in range(part_n):
                                wv, wk = wvs[jl // 4]
                                MM(bank(b), wv[:, jl % 4, fb * 128:(fb + 1) * 128], gT[:, jl, tb * 512:(tb + 1) * 512],
                                   jl == 0, jl == part_n - 1, [wk, 'gT%d' % jl], [BK[b]])
                            xs = xT[:, fb, tb * 512:(tb + 1) * 512]
                            STT(xs, bank(b), drv[l][:, 24 + fb:25 + fb], xs, ALU.mult, ALU.add,
                                [BK[b], 'drv%d' % l, 'xT%d_%d' % (fb, tb)], ['xT%d_%d' % (fb, tb)])
                    j0 += part_n
                S.barrier(relay=RELAY)
            layer_norm(l, 2, last=(l == n_layers - 1))

        S.barrier(relay=RELAY)
        AR.reset()
        ost = [AR.get([1024], F32) for _ in range(4)]
        bi = 0
        for tt in range(16):
            o = ost[tt % 4]
            ok = 'ost%d' % (tt % 4)
            for cg in range(2):
                b = bi % 8
                bi += 1
                for q in range(4):
                    c = cg * 4 + q
                    TR(bank(b)[:, q * 128:(q + 1) * 128], xT[:, c, tt * 128:(tt + 1) * 128], ident_f[:],
                       ['xT%d_%d' % (c, tt // 4), 'ident_f'], [BK[b]])
                CP(o[:, cg * 512:(cg + 1) * 512], bank(b), [BK[b]], [ok], eng=EV())
            DMA('sp', out_d[tt * 128:(tt + 1) * 128, :], o, [ok], ['out'])
        S.finish()
        S.replay()
    return nc


_CACHE = {}


def kernel(**inputs):
    inp = {k: np.asarray(v) for k, v in inputs.items()}
    if 'nc' not in _CACHE:
        _CACHE['nc'] = build()
    nc = _CACHE['nc']
    w = _prep_weights(inp)
    tb = _tables()
    in_maps = []
    for b in range(8):
        m = dict(w)
        m.update(tb)
        m['x'] = np.ascontiguousarray(inp['x'][b], dtype=np.float32)
        m['cT'] = _pad64(inp['c'][b].reshape(8, 128).T)
        in_maps.append(m)
    res = run_bass_kernel_spmd(nc, in_maps, core_ids=list(range(8)))
    out = np.stack([np.asarray(r['out'], dtype=np.float32) for r in res.results], 0)
    return out
```

```python
import math
from contextlib import ExitStack
import numpy as np
import concourse.bass as bass
import concourse.mybir as mybir
from concourse.bass_utils import run_bass_kernel_spmd

F32 = mybir.dt.float32
BF16 = mybir.dt.bfloat16
AF = mybir.ActivationFunctionType
ALU = mybir.AluOpType
AX = mybir.AxisListType

ENGS = ['pe', 'dve', 'act', 'pool', 'sp']
NRING = 8
DEPTH = 2
SEQ = 2048
DM = 1024
ALPHA = (2 * DEPTH) ** 0.25
LN_EPS = 1e-5
NEGB = -30000.0
FF_SPLIT = [6, 6, 5, 5]


class Sched:
    def __init__(self, nc, stack):
        self.nc = nc
        self.prog = {e: [] for e in ENGS}
        self.cnt = {e: 0 for e in ENGS}
        self.seen = {e: {} for e in ENGS}
        self.lastw = {}
        self.readers = {}
        self.sems = {}
        self.semval = {}
        self.relay_fn = None
        for e in ENGS:
            self.sems[e] = stack.enter_context(nc.semaphore("s_" + e))
        self.dma_n = {}
        for q in ['sp', 'act', 'pool']:
            self.dma_n[q] = 0
            for j in range(NRING):
                nm = "d_%s%d" % (q, j)
                self.sems[nm] = stack.enter_context(nc.semaphore(nm))

    def _deps(self, eng, reads, writes, is_dma=False):
        deps = []
        for k in reads:
            t = self.lastw.get(k)
            if t is not None:
                deps.append((t, 'raw'))
        for k in writes:
            t = self.lastw.get(k)
            if t is not None:
                deps.append((t, 'waw'))
            for s, v in self.readers.get(k, {}).items():
                deps.append(((s, v), 'war'))
        need = {}
        for (s, v), kind in deps:
            if s == eng and not is_dma:
                if kind != 'raw' or eng == 'pe':
                    continue
            if self.seen[eng].get(s, 0) >= v:
                continue
            if need.get(s, 0) < v:
                need[s] = v
        return need

    def _emit_waits(self, eng, need):
        if eng in ('sp', 'pool') and 'pe' in need and self.relay_fn is not None:
            need = dict(need)
            v = need.pop('pe')
            self.seen[eng]['pe'] = v
            R = 'dve'
            if self.seen[R].get('pe', 0) < v:
                self.prog[R].append(('wait', 'pe', v))
                self.seen[R]['pe'] = v
            lr = getattr(self, 'last_relay', 0)
            if lr and self.seen[R].get(R, 0) < lr:
                self.prog[R].append(('wait', R, lr))
                self.seen[R][R] = lr
            self.cnt[R] += 1
            self.last_relay = self.cnt[R]
            self.semval[R] = self.cnt[R]
            self.prog[R].append(('op', self.relay_fn, R, 1))
            if self.seen[eng].get(R, 0) < self.cnt[R]:
                need[R] = max(need.get(R, 0), self.cnt[R])
        for s, v in need.items():
            self.prog[eng].append(('wait', s, v))
            self.seen[eng][s] = v

    def _commit(self, tok, reads, writes):
        for k in writes:
            self.lastw[k] = tok
            self.readers[k] = {}
        for k in reads:
            d = self.readers.setdefault(k, {})
            if d.get(tok[0], 0) < tok[1]:
                d[tok[0]] = tok[1]

    def relay_readers(self, keys):
        if self.relay_fn is None:
            return
        v = 0
        for k in keys:
            d = self.readers.get(k)
            if d and 'pe' in d:
                v = max(v, d['pe'])
        if v == 0:
            return
        R = 'dve'
        if self.seen[R].get('pe', 0) < v:
            self.prog[R].append(('wait', 'pe', v))
            self.seen[R]['pe'] = v
        lr = getattr(self, 'last_relay', 0)
        if lr and self.seen[R].get(R, 0) < lr:
            self.prog[R].append(('wait', R, lr))
            self.seen[R][R] = lr
        self.cnt[R] += 1
        self.semval[R] = self.cnt[R]
        self.last_relay = self.cnt[R]
        self.prog[R].append(('op', self.relay_fn, R, 1))
        for k in keys:
            d = self.readers.get(k)
            if d and 'pe' in d:
                d.pop('pe')
                d[R] = max(d.get(R, 0), self.cnt[R])

    def op(self, eng, fn, reads=(), writes=()):
        need = self._deps(eng, reads, writes)
        self._emit_waits(eng, need)
        self.cnt[eng] += 1
        tok = (eng, self.cnt[eng])
        self.semval[eng] = self.cnt[eng]
        self.prog[eng].append(('op', fn, eng, 1))
        self._commit(tok, reads, writes)
        return tok

    def dma_multi(self, q, pairs, reads=(), writes=()):
        i = self.dma_n[q]
        self.dma_n[q] += 1
        slot = "d_%s%d" % (q, i % NRING)
        prev = self.semval.get(slot, 0)
        val = prev + 16 * len(pairs)
        need = self._deps(q, reads, writes, is_dma=True)
        if prev > 0 and self.seen[q].get(slot, 0) < prev:
            need[slot] = max(need.get(slot, 0), prev)
        self._emit_waits(q, need)
        for out, in_ in pairs:
            self.prog[q].append(('op', (lambda o_, i_: (lambda e: e.dma_start(out=o_, in_=i_)))(out, in_), slot, 16))
        self.semval[slot] = val
        tok = (slot, val)
        self._commit(tok, reads, writes)
        return tok

    def dma(self, q, out, in_, reads=(), writes=()):
        return self.dma_multi(q, [(out, in_)], reads, writes)

    def _wait_all(self, e):
        need = {}
        for s_, v in self.semval.items():
            if s_ == e:
                continue
            if self.seen[e].get(s_, 0) < v:
                need[s_] = v
        self._emit_waits(e, need)

    def barrier(self, engs=('pe', 'dve', 'act', 'sp'), relay=None):
        if relay is None or 'sp' not in engs:
            for e in engs:
                self._wait_all(e)
            return
        snap = dict(self.semval)
        self._wait_all('sp')
        tok = self.dma('sp', relay[0], relay[1], (), ['__bar'])
        for e in engs:
            if e == 'sp':
                continue
            self._emit_waits(e, {tok[0]: tok[1]} if self.seen[e].get(tok[0], 0) < tok[1] else {})
            for s_, v in snap.items():
                if s_ != e and self.seen[e].get(s_, 0) < v:
                    self.seen[e][s_] = v

    def finish(self):
        self.barrier(engs=('sp',))

    def replay(self):
        nc = self.nc
        sems = self.sems
        prog = self.prog

        def run(e, name):
            for it in prog[name]:
                if it[0] == 'wait':
                    e.wait_ge(sems[it[1]], it[2])
                else:
                    it[1](e).then_inc(sems[it[2]], it[3])

        with nc.Block() as block:
            @block.tensor
            def _(e):
                run(e, 'pe')

            @block.vector
            def _(e):
                run(e, 'dve')

            @block.scalar
            def _(e):
                run(e, 'act')

            @block.gpsimd
            def _(e):
                run(e, 'pool')

            @block.sync
            def _(e):
                run(e, 'sp')


def _pad64(a):
    a = np.asarray(a, dtype=np.float32)
    out = np.zeros(a.shape[:-1] + (64,), np.float32)
    out[..., :a.shape[-1]] = a
    return out


def _tables():
    t = {}
    half = 32
    inv = np.power(np.float32(10000.0), -np.arange(half, dtype=np.float32) / np.float32(half)).astype(np.float32)
    pos = np.arange(SEQ, dtype=np.float32)
    ang = pos[:, None] * inv[None, :]
    cos = np.cos(ang).astype(np.float32).T
    sin = np.sin(ang).astype(np.float32).T
    cosT = np.concatenate([cos, cos, cos, cos], 0)
    sinT = np.concatenate([-sin, sin, -sin, sin], 0)
    t['cosT'] = np.ascontiguousarray(cosT)
    t['sinT'] = np.ascontiguousarray(sinT)
    H = 4
    L = 128
    lg = np.log1p(-np.exp2(-5.0 - np.arange(H, dtype=np.float32))).astype(np.float32)
    idx = np.arange(L, dtype=np.float32)
    diff = idx[:, None] - idx[None, :]
    dec = np.where(diff >= 0, np.exp(lg[:, None, None] * np.maximum(diff, 0.0)), 0.0).astype(np.float32)
    t['decayT'] = np.ascontiguousarray(np.transpose(dec, (2, 0, 1)) * np.float32(0.125)).reshape(128, 512)
    xi = np.exp(lg[:, None] * (idx + 1.0)).astype(np.float32)
    zeta = np.exp(lg[:, None] * (L - 1.0 - idx)).astype(np.float32)
    xiT = np.zeros((128, 2, 128), np.float32)
    for p in range(2):
        for hh in range(2):
            xiT[hh * 64:(hh + 1) * 64, p, :] = xi[2 * p + hh][None, :]
    t['xiT'] = xiT.reshape(128, 256)
    t['zeta'] = _pad64(zeta.T * np.float32(0.125))
    cd = np.exp(lg * L).astype(np.float32)
    cdv = np.zeros((128, 2), np.float32)
    for p in range(2):
        for hh in range(2):
            cdv[hh * 64:(hh + 1) * 64, p] = cd[2 * p + hh]
    t['cdv'] = _pad64(cdv)
    key = np.arange(SEQ)
    ex = np.zeros((128, SEQ), np.float32)
    ex[key // 64, key] = -NEGB
    t['expand'] = ex
    kk = np.arange(128)[:, None]
    tt = np.arange(128)[None, :]
    t['causalb'] = np.where(kk > tt, NEGB, 0.0).astype(np.float32)
    t['winb'] = np.where(kk <= tt, NEGB, 0.0).astype(np.float32)
    t['identf'] = np.eye(128, dtype=np.float32)
    t['causal01'] = np.where(tt >= kk, 1.0, 0.0).astype(np.float32)
    k127 = np.arange(128)[:, None]
    tpos = np.arange(SEQ)[None, :]
    t['cmpb'] = np.where(16 * k127 + 31 > tpos, NEGB, 0.0).astype(np.float32)
    fb = np.zeros((128, 16, 32), np.float32)
    for qt in range(16):
        tq = qt * 128 + np.arange(128)
        cur = tq // 64
        blk = np.arange(32)
        future = blk[None, :] > cur[:, None]
        forced = (blk[None, :] == 0) | (blk[None, :] == cur[:, None]) | (blk[None, :] == cur[:, None] - 1)
        fb[:, qt, :] = np.where(forced, 1e30, np.where(future, -1e30, 0.0))
    t['fbias'] = fb.reshape(128, 512)
    ov = np.zeros((128, 32), np.float32)
    c0 = np.arange(127)[:, None] * 16
    s0 = np.arange(32)[None, :] * 64
    ov[:127] = np.clip(np.minimum(c0 + 32, s0 + 64) - np.maximum(c0, s0), 0, None) / 32.0
    t['ovl'] = _pad64(ov)
    return t


TABLE_SHAPES = {'cosT': [128, 2048], 'sinT': [128, 2048], 'decayT': [128, 512], 'xiT': [128, 256],
                'zeta': [128, 64], 'cdv': [128, 64], 'expand': [128, 2048], 'causalb': [128, 128],
                'winb': [128, 128], 'causal01': [128, 128], 'identf': [128, 128], 'cmpb': [128, 2048], 'fbias': [128, 512], 'ovl': [128, 64]}


def _prep_weights(inp):
    w = {}
    f = lambda a: np.ascontiguousarray(a, dtype=np.float32)
    w_in = inp['w_in']
    L = DEPTH
    w['w_ada'] = f(inp['w_ada'])
    w['b_adaT'] = _pad64(inp['b_ada'].reshape(L, 48, 128).transpose(0, 2, 1))
    w['w_inA'] = f(w_in[:, :, 0:512])
    cols = []
    for p in range(2):
        for base in (512, 768):
            hd = np.arange(128)
            h = 2 * p + hd // 64
            d = hd % 64
            cols.append(base + h * 64 + d)
            cols.append(base + h * 64 + (d + 32) % 64)
    cols = np.concatenate(cols)
    w['w_inBf'] = f(w_in[:, :, cols])
    cols = []
    for p in range(2):
        cols.append(1024 + p * 128 + np.arange(128))
        cols.append(1280 + p * 128 + np.arange(128))
    w['w_inBt'] = f(w_in[:, :, np.concatenate(cols)])
    cols = []
    for g in range(2):
        cols.append(1536 + g * 256 + np.arange(256))
        ks = 2304 + g * 64 + np.arange(64)
        kw = 2560 + g * 64 + np.arange(64)
        cols += [ks, ks, kw, kw]
        cols.append(2048 + g * 64 + np.arange(64))
        cols.append(2176 + g * 64 + np.arange(64))
    w['w_inCf'] = f(w_in[:, :, np.concatenate(cols)])
    cols = []
    for g in range(2):
        cols.append(2432 + g * 64 + np.arange(64))
        cols.append(2688 + g * 64 + np.arange(64))
        cols.append(2816 + g * 12 + np.arange(12))
    w['w_inCt'] = f(w_in[:, :, np.concatenate(cols)])
    w['WsT'] = f(inp['a_ws'].transpose(0, 3, 1, 2).reshape(L, 128, 512))
    w['bsT'] = _pad64(inp['a_bs'].transpose(0, 2, 1))
    w['a_lng'] = f(np.broadcast_to(inp['a_ln_g'].reshape(L, 1, 256), (L, 128, 256)))
    w['a_lnb'] = f(np.broadcast_to(inp['a_ln_b'].reshape(L, 1, 256), (L, 128, 256)))
    w['b_gng'] = f(np.broadcast_to(inp['b_gn_g'].reshape(L, 1, 256), (L, 128, 256)))
    w['b_gnb'] = f(np.broadcast_to(inp['b_gn_b'].reshape(L, 1, 256), (L, 128, 256)))
    posT = np.concatenate([inp['c_pos_k'].transpose(0, 2, 1), inp['c_pos_v'].transpose(0, 2, 1)], 1)
    w['posT'] = _pad64(posT)
    w1k = inp['c_w1_k'].reshape(L, 32, 64, 64).transpose(0, 2, 1, 3)
    w1v = inp['c_w1_v'].reshape(L, 32, 64, 64).transpose(0, 2, 1, 3)
    w['w1s'] = f(np.concatenate([w1k, w1v], 1).reshape(L, 128, 2048))
    w['w2k'] = f(np.concatenate([inp['c_w2_k'], inp['c_w2_k']], 2))
    w['w2v'] = f(inp['c_w2_v'])
    w['w_out'] = f(inp['w_out'])
    w['lnpk'] = _pad64(np.concatenate([inp[k].reshape(L, 8, 128).transpose(0, 2, 1)
                                       for k in ('ln1_g', 'ln1_b', 'ln2_g', 'ln2_b')], 2))
    w['w_up'] = f(inp['w_up'])
    w['cwT'] = f(inp['conv_w'].reshape(L, 3, 44, 128).transpose(0, 3, 2, 1).reshape(L, 128, 132))
    w['cbT'] = _pad64(inp['conv_b'].reshape(L, 44, 128).transpose(0, 2, 1))
    w['w_down'] = f(inp['w_down'])
    return w


W_SHAPES = {'w_ada': [2, 1024, 6144], 'b_adaT': [2, 128, 64], 'w_inA': [2, 1024, 512], 'w_inBf': [2, 1024, 1024],
            'w_inBt': [2, 1024, 512], 'w_inCf': [2, 1024, 1280], 'w_inCt': [2, 1024, 280], 'WsT': [2, 128, 512],
            'bsT': [2, 128, 64], 'a_lng': [2, 128, 256], 'a_lnb': [2, 128, 256], 'b_gng': [2, 128, 256], 'b_gnb': [2, 128, 256],
            'posT': [2, 128, 64], 'w1s': [2, 128, 2048], 'w2k': [2, 64, 128], 'w2v': [2, 64, 64],
            'w_out': [2, 1024, 1024], 'lnpk': [2, 128, 64],
            'w_up': [2, 1024, 5632], 'cwT': [2, 128, 132], 'cbT': [2, 128, 64], 'w_down': [2, 2816, 1024]}


def build(n_layers=DEPTH, mixers=('A', 'B', 'C'), do_ffn=True, dbg=None):
    nc = bass.Bass("TRN2", target_bir_lowering=False)
    Dr = {}
    Dr['x'] = nc.dram_tensor("x", [SEQ, DM], F32, kind="ExternalInput").ap()
    Dr['cT'] = nc.dram_tensor("cT", [128, 64], F32, kind="ExternalInput").ap()
    for k, shp in W_SHAPES.items():
        Dr[k] = nc.dram_tensor(k, shp, F32, kind="ExternalInput").ap()
    for k, shp in TABLE_SHAPES.items():
        Dr[k] = nc.dram_tensor(k, shp, F32, kind="ExternalInput").ap()
    out_d = nc.dram_tensor("out", [SEQ, DM], F32, kind="ExternalOutput").ap()
    dbg_d = None
    if dbg is not None:
        dbg_d = nc.dram_tensor("dbg", [SEQ, DM], F32, kind="ExternalOutput").ap()

    st = ExitStack()
    with st:
        S = Sched(nc, st)
        T = lambda name, shape, dt=F32: st.enter_context(nc.sbuf_tensor("s_" + name, shape, dt))
        xT = T("xT", [128, 8, SEQ], F32)
        hT = T("hT", [128, 8, SEQ], BF16)
        WR = T("WR", [128, 4, 4096], BF16)
        AW = 51 * 256 - 128
        ARENA = T("ARENA", [128, AW], F32)
        PSA = st.enter_context(nc.psum_tensor("PSA", [128, 2048], F32))
        PSB = st.enter_context(nc.psum_tensor("PSB", [128, 2048], F32))

        def bank(i):
            t = PSA if i < 4 else PSB
            return t[:, (i % 4) * 512:(i % 4 + 1) * 512]

        BK = ['B%d' % i for i in range(8)]

        class Arena:
            def __init__(self):
                self.off = 0

            def reset(self, off=0):
                self.off = off

            def get(self, shape, dt=F32):
                n = int(np.prod(shape))
                nb = n * (2 if dt == BF16 else 4)
                w0 = self.off // 4
                w1 = w0 + (nb + 3) // 4
                assert w1 <= AW, ("arena overflow", w1 * 4, AW * 4)
                self.off = w1 * 4
                ap = ARENA[:, w0:w1]
                if dt == BF16:
                    ap = ap.bitcast(BF16)
                ap = ap[:, 0:n]
                if len(shape) == 2:
                    ap = ap.rearrange("p (a b) -> p a b", a=shape[0])
                elif len(shape) == 3:
                    ap = ap.rearrange("p (a b c) -> p a b c", a=shape[0], b=shape[1])
                return ap

        AR = Arena()

        def MM(out, lhsT, rhs, start, stop, rd, wr):
            S.op('pe', lambda e: e.matmul(out, lhsT=lhsT, rhs=rhs, start=start, stop=stop, skip_group_check=True), rd, wr)

        def TR(out, in_, ident, rd, wr):
            S.op('pe', lambda e: e.transpose(out, in_, ident), rd, wr)

        def ACT(out, in_, func, rd, wr, scale=1.0, bias=None):
            if bias is None:
                S.op('act', lambda e: e.activation(out, in_, func, scale=scale), rd, wr)
            else:
                S.op('act', lambda e: e.activation(out, in_, func, bias=bias, scale=scale), rd, wr)

        def TT(out, in0, in1, op, rd, wr, eng='dve'):
            S.op(eng, lambda e: e.tensor_tensor(out, in0, in1, op), rd, wr)

        def TS(out, in0, s1, s2, op0, op1, rd, wr, eng='dve'):
            if s2 is None:
                S.op(eng, lambda e: e.tensor_scalar(out, in0, s1, None, op0=op0), rd, wr)
            else:
                S.op(eng, lambda e: e.tensor_scalar(out, in0, s1, s2, op0=op0, op1=op1), rd, wr)

        def STT(out, in0, scalar, in1, op0, op1, rd, wr):
            S.op('dve', lambda e: e.scalar_tensor_tensor(out, in0, scalar, in1, op0=op0, op1=op1), rd, wr)

        def CP(out, in_, rd, wr, eng='dve'):
            if eng == 'act':
                S.op('act', lambda e: e.activation(out, in_, AF.Identity), rd, wr)
            else:
                S.op(eng, lambda e: e.tensor_copy(out, in_), rd, wr)

        def RED(out, in_, op, rd, wr):
            S.op('dve', lambda e: e.tensor_reduce(out, in_, axis=AX.X, op=op), rd, wr)

        def RCP(out, in_, rd, wr):
            S.op('dve', lambda e: e.reciprocal(out, in_), rd, wr)

        evt = [0]

        def EV():
            evt[0] += 1
            return 'act' if evt[0] % 2 else 'dve'

        def DMA(q, out, in_, rd, wr):
            pieces = []

            def split(o, i):
                shp = tuple(o.shape)
                assert tuple(i.shape) == shp, (shp, i.shape)
                if len(shp) == 3:
                    for a in range(shp[1]):
                        split(o[:, a, :], i[:, a, :])
                elif len(shp) == 2 and shp[1] > 512:
                    for c0 in range(0, shp[1], 512):
                        c1 = min(shp[1], c0 + 512)
                        pieces.append((o[:, c0:c1], i[:, c0:c1]))
                else:
                    pieces.append((o, i))
            split(out, in_)
            S.dma_multi(q, pieces, rd, wr)

        ident_f = T("ident_f", [128, 128], F32)
        ident_b = T("ident_b", [128, 128], BF16)
        onesm = T("onesm", [128, 128], BF16)
        epsA = T("epsA", [128, 2], F32)
        condT = T("condT", [128, 64], F32)
        condTb = T("condTb", [128, 8, 8], BF16)
        badaT = [T("badaT%d" % l_, [128, 64], F32) for l_ in range(DEPTH)]
        modT = [T("modT%d" % l, [128, 48], F32) for l in range(DEPTH)]
        drv = [T("drv%d" % l, [128, 64], F32) for l in range(DEPTH)]
        lnp = [T("lnp%d" % l, [128, 64], F32) for l in range(DEPTH)]
        cwT = T("cwT", [128, 132], F32)
        cbT = T("cbT", [128, 64], F32)
        decayT = T("decayT", [128, 512], F32)
        xiT = T("xiT", [128, 256], F32)
        zeta = T("zeta", [128, 64], F32)
        cdv = T("cdv", [128, 64], F32)
        expand = T("expand", [128, 2048], BF16)
        causalb = T("causalb", [128, 128], BF16)
        winb = T("winb", [128, 128], BF16)
        cmpb = T("cmpb", [128, 2048], BF16)
        fbias = T("fbias", [128, 512], F32)
        causal01 = T("causal01", [128, 128], F32)
        ovl = T("ovl", [128, 64], F32)
        WsT = T("WsT", [128, 512], BF16)
        bsT = T("bsT", [128, 64], F32)
        lnbc = T("lnbc", [128, 4, 256], F32)
        posT = T("posT", [128, 64], F32)
        w1s = T("w1s", [128, 2048], BF16)
        w2k = T("w2k", [64, 128], BF16)
        w2v = T("w2v", [64, 64], BF16)

        DMA('sp', ident_f[:], Dr['identf'], (), ['ident_f'])
        S.op('dve', lambda e: e.tensor_copy(ident_b[:], ident_f[:]), ['ident_f'], ['ident_b'])
        S.op('dve', lambda e: e.memset(onesm[:], 1.0 / 1024.0), (), ['onesm'])
        S.op('dve', lambda e: e.memset(epsA[:, 0:1], LN_EPS), (), ['epsA'])
        S.op('dve', lambda e: e.memset(epsA[:, 1:2], LN_EPS / (ALPHA * ALPHA)), (), ['epsA'])
        barscr = T("barscr", [128, 64], F32)
        RELAY = (barscr[:], Dr['zeta'])
        rlscr = T("rlscr", [128, 2], F32)
        S.relay_fn = lambda e: e.memset(rlscr[:, 0:1], 0.0)
        DMA('sp', condT[:], Dr['cT'], (), ['condT'])
        for nm, tl in (('decayT', decayT), ('xiT', xiT), ('zeta', zeta), ('cdv', cdv), ('fbias', fbias), ('causal01', causal01), ('ovl', ovl)):
            DMA('sp', tl[:], Dr[nm], (), [nm])
        for nm, tl in (('causalb', causalb), ('winb', winb)):
            DMA('pool', tl[:], Dr[nm], (), [nm])
        for nm, tl in (('expand', expand), ('cmpb', cmpb)):
            DMA('pool', tl[:].rearrange("p (a b) -> p a b", a=4), Dr[nm].rearrange("p (a b) -> p a b", a=4), (), [nm])
        ACT(condT[:], condT[:], AF.Silu, ['condT'], ['condT'])

        wr_n = [0]

        def wload(src_ap, shape):
            S.relay_readers(['WR%d' % j_ for j_ in range(4)])
            i = wr_n[0] % 4
            wr_n[0] += 1
            n = int(np.prod(shape))
            v = WR[:, i, 0:n]
            if len(shape) == 2:
                v = v.rearrange("p (a b) -> p a b", a=shape[0])
            key = 'WR%d' % i
            DMA('pool', v, src_ap, (), [key])
            return v, key

        def kchunks(ap2d):
            return ap2d.rearrange("(k p) n -> p k n", p=128)

        AR.reset()
        ada_buf = [AR.get([8, 512], BF16) for _ in range(2)]
        CP(condTb[:], condT[:, 0:8].unsqueeze(2).to_broadcast([128, 8, 8]), ['condT'], ['condTb'])
        xst = [AR.get([1024], F32) for _ in range(8)]
        bi = 0
        for tb in range(4):
            for q in range(4):
                tt = tb * 4 + q
                DMA('sp', xst[tt % 8], Dr['x'][tt * 128:(tt + 1) * 128, :], (), ['xst%d' % (tt % 8)])
            for c in range(8):
                b = 1 + bi % 7
                bi += 1
                for q in range(4):
                    tt = tb * 4 + q
                    TR(bank(b)[:, q * 128:(q + 1) * 128], xst[tt % 8][:, c * 128:(c + 1) * 128], ident_f[:],
                       ['xst%d' % (tt % 8), 'ident_f'], [BK[b]])
                CP(xT[:, c, tb * 512:(tb + 1) * 512], bank(b), [BK[b]], ['xT%d_%d' % (c, tb)], eng=EV())
        for l in range(n_layers):
            DMA('sp', badaT[l][:], Dr['b_adaT'][l], (), ['bada%d' % l])
            DMA('sp', lnp[l][:], Dr['lnpk'][l], (), ['lnp%d' % l])
        for l in range(min(1, n_layers)):
            for blk in range(12):
                buf = ada_buf[blk % 2]
                bk = 'ada%d' % (blk % 2)
                DMA('pool', buf, kchunks(Dr['w_ada'][l][:, blk * 512:(blk + 1) * 512]), (), [bk])
                for jj in range(4):
                    j = blk * 4 + jj
                    for k in range(8):
                        MM(bank(0)[:, j * 8:j * 8 + 8], buf[:, k, jj * 128:(jj + 1) * 128], condTb[:, k, :],
                           k == 0, k == 7, [bk, 'condTb'], ['B0'])
            TT(modT[l][:], bank(0)[:, 0:384].rearrange('p (j r) -> p j r', r=8)[:, :, 0], badaT[l][:, 0:48], ALU.add, ['B0', 'bada%d' % l], ['modT%d' % l])

        def derive(l):
            m = modT[l]
            d = drv[l]
            mk, dk = 'modT%d' % l, 'drv%d' % l
            TS(d[:, 0:8], m[:, 8:16], 1.0, None, ALU.add, None, [mk], [dk])
            TS(d[:, 8:16], m[:, 16:24], 1.0 / ALPHA, None, ALU.mult, None, [mk], [dk])
            TS(d[:, 16:24], m[:, 32:40], 1.0, None, ALU.add, None, [mk], [dk])
            TS(d[:, 24:32], m[:, 40:48], 1.0 / ALPHA, None, ALU.mult, None, [mk], [dk])
            TT(d[:, 32:40], lnp[l][:, 0:8], d[:, 16:24], ALU.mult, ['lnp%d' % l, dk], [dk])
            TT(d[:, 40:48], lnp[l][:, 8:16], d[:, 16:24], ALU.mult, ['lnp%d' % l, dk], [dk])
            TT(d[:, 40:48], d[:, 40:48], m[:, 24:32], ALU.add, [dk, mk], [dk])

        def derive_cross(l):
            d, dn = drv[l], drv[l + 1]
            dk, dnk = 'drv%d' % l, 'drv%d' % (l + 1)
            TT(d[:, 48:56], lnp[l][:, 16:24], dn[:, 0:8], ALU.mult, ['lnp%d' % l, dnk, dk], [dk])
            TT(d[:, 56:64], lnp[l][:, 24:32], dn[:, 0:8], ALU.mult, ['lnp%d' % l, dnk, dk], [dk])
            TT(d[:, 56:64], d[:, 56:64], modT[l + 1][:, 0:8], ALU.add, [dk, 'modT%d' % (l + 1)], [dk])

        def ada_block_deferred(l, blk):
            wv_, wk_ = wload(kchunks(Dr['w_ada'][l][:, blk * 512:(blk + 1) * 512]), [8, 512])
            for jj in range(4):
                for k in range(8):
                    MM(bank(7)[:, jj * 8:jj * 8 + 8], wv_[:, k, jj * 128:(jj + 1) * 128], condTb[:, k, :],
                       k == 0, k == 7, [wk_, 'condTb'], ['B7'])
            TT(modT[l][:, blk * 4:(blk + 1) * 4], bank(7)[:, 0:32].rearrange('p (j r) -> p j r', r=8)[:, :, 0],
               badaT[l][:, blk * 4:(blk + 1) * 4], ALU.add, ['B7', 'bada%d' % l], ['modT%d' % l])
            if blk == 11:
                derive(l)
                derive_cross(l - 1)

        if n_layers >= 1:
            derive(0)

        for tb in range(4):
            for c in range(8):
                xk = 'xT%d_%d' % (c, tb)
                if (tb * 8 + c) % 2 == 0:
                    TS(hT[:, c, tb * 512:(tb + 1) * 512], xT[:, c, tb * 512:(tb + 1) * 512], drv[0][:, c:c + 1], modT[0][:, c:c + 1],
                       ALU.mult, ALU.add, [xk, 'drv0', 'modT0'], ['hT%d_%d' % (c, tb)])
                else:
                    ACT(hT[:, c, tb * 512:(tb + 1) * 512], xT[:, c, tb * 512:(tb + 1) * 512], AF.Identity,
                        [xk, 'drv0', 'modT0'], ['hT%d_%d' % (c, tb)], scale=drv[0][:, c:c + 1], bias=modT[0][:, c:c + 1])

        def out_proj(l, mixtm, mixkeys, nch, row0, mixT, alias=()):
            nonlocal_bi = [0]
            for c in range(nch):
                for tb in range(4):
                    b = 4 + (nonlocal_bi[0] % 4)
                    nonlocal_bi[0] += 1
                    pb = bank(b).bitcast(BF16)
                    for q in range(4):
                        tt = tb * 4 + q
                        TR(pb[:, q * 128:(q + 1) * 128], mixtm[:, tt, c * 128:(c + 1) * 128], ident_b[:],
                           [mixkeys[tt], 'ident_b'], [BK[b]])
                    CP(mixT[:, c, tb * 512:(tb + 1) * 512], pb[:, 0:512], [BK[b]], ['mixT%d_%d' % (c, tb)] + list(alias), eng=EV())
            wv, wk = wload(kchunks(Dr['w_out'][l][row0:row0 + nch * 128, :]), [nch, 1024])
            for fb in range(8):
                for tb in range(4):
                    b = nonlocal_bi[0] % 4
                    nonlocal_bi[0] += 1
                    for c in range(nch):
                        MM(bank(b), wv[:, c, fb * 128:(fb + 1) * 128], mixT[:, c, tb * 512:(tb + 1) * 512],
                           c == 0, c == nch - 1, [wk, 'mixT%d_%d' % (c, tb)], [BK[b]])
                    xs = xT[:, fb, tb * 512:(tb + 1) * 512]
                    STT(xs, bank(b), drv[l][:, 8 + fb:9 + fb], xs, ALU.mult, ALU.add,
                        [BK[b], 'drv%d' % l, 'xT%d_%d' % (fb, tb)], ['xT%d_%d' % (fb, tb)])

        def layer_norm(l, which, last):
            goff = 0 if which == 1 else 16
            aoff = 32 if which == 1 else 48
            AR.reset()
            xb = [AR.get([512], BF16) for _ in range(3)]
            sq = [AR.get([512], BF16) for _ in range(3)]
            rstd = [AR.get([512], F32) for _ in range(2)]
            nmr = [AR.get([512], F32) for _ in range(2)]
            tmp = [AR.get([512], F32) for _ in range(3)]
            n1 = [0]
            n3 = [0]

            def p_stats(tb):
                bm, be = 0 + 2 * (tb % 2), 1 + 2 * (tb % 2)
                for c in range(8):
                    i = n1[0] % 3
                    n1[0] += 1
                    xs = xT[:, c, tb * 512:(tb + 1) * 512]
                    xk = 'xT%d_%d' % (c, tb)
                    ACT(sq[i], xs, AF.Square, [xk], ['lsq%d' % i])
                    CP(xb[i], xs, [xk], ['lxb%d' % i], eng='dve')
                    MM(bank(bm), onesm[:], xb[i], c == 0, c == 7, ['onesm', 'lxb%d' % i], [BK[bm]])
                    MM(bank(be), onesm[:], sq[i], c == 0, c == 7, ['onesm', 'lsq%d' % i], [BK[be]])

            def p_apply(tb):
                bm, be = 0 + 2 * (tb % 2), 1 + 2 * (tb % 2)
                r = tb % 2
                rk, nk = 'lrstd%d' % r, 'lnmr%d' % r
                ACT(nmr[r], bank(bm), AF.Square, [BK[bm]], [nk])
                TT(rstd[r], bank(be), nmr[r], ALU.subtract, [BK[be], nk], [rk])
                TS(rstd[r], rstd[r], 0.0, None, ALU.max, None, [rk], [rk])
                ACT(rstd[r], rstd[r], AF.Sqrt, [rk, 'epsA'], [rk], bias=epsA[:, 1:2])
                RCP(rstd[r], rstd[r], [rk], [rk])
                STT(nmr[r], bank(bm), -1.0, rstd[r], ALU.mult, ALU.mult, [BK[bm], rk], [nk])
                for c in range(8):
                    i = n3[0] % 3
                    n3[0] += 1
                    xs = xT[:, c, tb * 512:(tb + 1) * 512]
                    xk = 'xT%d_%d' % (c, tb)
                    tk = 'ltmp%d' % i
                    TT(tmp[i], xs, rstd[r], ALU.mult, [xk, rk], [tk])
                    TT(tmp[i], tmp[i], nmr[r], ALU.add, [tk, nk], [tk])
                    ACT(xs, tmp[i], AF.Identity, [tk, 'lnp%d' % l], [xk],
                        scale=lnp[l][:, goff + c:goff + c + 1], bias=lnp[l][:, goff + 8 + c:goff + 9 + c])
                    if not last:
                        ACT(hT[:, c, tb * 512:(tb + 1) * 512], tmp[i], AF.Identity, [tk, 'drv%d' % l], ['hT%d_%d' % (c, tb)],
                            scale=drv[l][:, aoff + c:aoff + c + 1], bias=drv[l][:, aoff + 8 + c:aoff + 9 + c])

            p_stats(0)
            for tb in range(4):
                if tb + 1 < 4:
                    p_stats(tb + 1)
                p_apply(tb)

        def dump_tm(src, c0, ncol, keys, dcol):
            dst = AR.get([ncol], F32)
            for tt in range(16):
                CP(dst, src[:, tt, c0:c0 + ncol], [keys[tt]], ['dst'])
                DMA('sp', dbg_d[tt * 128:(tt + 1) * 128, dcol:dcol + ncol], dst, ['dst'], ['dbg'])

        def group_ln_core(src, sqv, nb, rk, wk_sq, eps_ap):
            s1 = AR.get([nb], F32)
            s2 = AR.get([nb], F32)
            s3 = AR.get([nb], F32)
            TT(sqv, src, src, ALU.mult, rk, [wk_sq])
            RED(s1, src, ALU.add, rk, ['gs1'])
            RED(s2, sqv, ALU.add, [wk_sq], ['gs2'])
            TS(s1, s1, 1.0 / 64, None, ALU.mult, None, ['gs1'], ['gs1'])
            TT(s3, s1, s1, ALU.mult, ['gs1'], ['gs3'])
            STT(s2, s2, 1.0 / 64, s3, ALU.mult, ALU.subtract, ['gs2', 'gs3'], ['gs2'])
            TS(s2, s2, 0.0, None, ALU.max, None, ['gs2'], ['gs2'])
            ACT(s2, s2, AF.Sqrt, ['gs2', 'epsA'], ['gs2'], bias=eps_ap)
            RCP(s2, s2, ['gs2'], ['gs2'])
            TT(sqv, src, s1.unsqueeze(2).to_broadcast([128, nb, 64]), ALU.subtract, rk + ['gs1'], [wk_sq])
            TT(sqv, sqv, s2.unsqueeze(2).to_broadcast([128, nb, 64]), ALU.mult, [wk_sq, 'gs2'], [wk_sq])

        def mixer_B(l):
            hk = lambda k, tb: 'hT%d_%d' % (k, tb)
            S.barrier()
            AR.reset()
            qrT = AR.get([2, 2048], BF16)
            krT = AR.get([2, 2048], BF16)
            off_x = AR.off
            cosT = AR.get([2048], F32)
            sinT = AR.get([2048], F32)
            rt1 = [AR.get([512], F32) for _ in range(2)]
            rt2 = [AR.get([512], F32) for _ in range(2)]
            DMA('sp', cosT, Dr['cosT'], (), ['cosT'])
            DMA('sp', sinT, Dr['sinT'], (), ['sinT'])
            DMA('sp', lnbc[:, 2, :], Dr['b_gng'][l], (), ['lnbc'])
            DMA('sp', lnbc[:, 3, :], Dr['b_gnb'][l], (), ['lnbc'])
            n = 0
            for p in range(2):
                wv, wk = wload(kchunks(Dr['w_inBf'][l][:, p * 512:(p + 1) * 512]), [8, 512])
                for kind in range(2):
                    dst = qrT if kind == 0 else krT
                    for tb in range(4):
                        i = n % 2
                        b1, b2 = 2 * (n % 4), 2 * (n % 4) + 1
                        n += 1
                        for k in range(8):
                            MM(bank(b1), wv[:, k, (2 * kind) * 128:(2 * kind + 1) * 128], hT[:, k, tb * 512:(tb + 1) * 512],
                               k == 0, k == 7, [wk, hk(k, tb)], [BK[b1]])
                        for k in range(8):
                            MM(bank(b2), wv[:, k, (2 * kind + 1) * 128:(2 * kind + 2) * 128], hT[:, k, tb * 512:(tb + 1) * 512],
                               k == 0, k == 7, [wk, hk(k, tb)], [BK[b2]])
                        TT(rt1[i], bank(b1), cosT[:, tb * 512:(tb + 1) * 512], ALU.mult, [BK[b1], 'cosT'], ['rt1_%d' % i])
                        TT(rt2[i], bank(b2), sinT[:, tb * 512:(tb + 1) * 512], ALU.mult, [BK[b2], 'sinT'], ['rt2_%d' % i])
                        TT(dst[:, p, tb * 512:(tb + 1) * 512], rt1[i], rt2[i], ALU.add, ['rt1_%d' % i, 'rt2_%d' % i],
                           ['qk%d_%d' % (kind, p)])
            wvt, wkt = wload(kchunks(Dr['w_inBt'][l]), [8, 512])
            for p in range(2):
                S.barrier()
                AR.reset(off_x)
                vg = AR.get([16, 256], BF16)
                kvs = AR.get([16, 128], F32)
                s16 = AR.get([16, 128], BF16)
                oall = AR.get([16, 128], F32)
                kz = [AR.get([128], BF16) for _ in range(2)]
                qx = [AR.get([128], BF16) for _ in range(2)]
                sT = [AR.get([256], BF16) for _ in range(2)]
                mixT = AR.get([1, 2048], BF16)
                qkk = ['qk0_%d' % p, 'qk1_%d' % p]
                for tt in range(16):
                    b = tt % 4
                    for k in range(8):
                        MM(bank(b)[:, 0:256], hT[:, k, tt * 128:(tt + 1) * 128], wvt[:, k, p * 256:(p + 1) * 256],
                           k == 0, k == 7, [hk(k, tt // 4), wkt], [BK[b]])
                    CP(vg[:, tt, 0:128], bank(b)[:, 0:128], [BK[b]], ['vg%d' % tt], eng='dve')
                    ACT(vg[:, tt, 128:256], bank(b)[:, 128:256], AF.Silu, [BK[b]], ['vg%d' % tt])
                S.op('dve', lambda e: e.memset(kvs[:, 0, :], 0.0), (), ['kvs'])
                def st_a(c):
                    i = c % 2
                    bt = 4 + (c % 2)
                    pb = bank(bt).bitcast(BF16)
                    TR(pb[:, 0:128], krT[:, p, c * 128:(c + 1) * 128], ident_b[:], [qkk[1], 'ident_b'], [BK[bt]])
                    TT(kz[i].rearrange("p (h d) -> p h d", h=2), pb[:, 0:128].rearrange("p (h d) -> p h d", h=2),
                       zeta[:, 2 * p:2 * p + 2].unsqueeze(2).to_broadcast([128, 2, 64]), ALU.mult,
                       [BK[bt], 'zeta'], ['kz%d' % i])

                def st_b(c):
                    i = c % 2
                    bm = 6 + (c % 2)
                    MM(bank(bm)[:, 0:128], kz[i], vg[:, c, 0:128], True, True, ['kz%d' % i, 'vg%d' % c], [BK[bm]])
                    STT(kvs[:, c + 1, :], kvs[:, c, :], cdv[:, p:p + 1], bank(bm)[:, 0:128], ALU.mult, ALU.add,
                        ['kvs', 'cdv', BK[bm]], ['kvs'])

                st_a(0)
                for c in range(15):
                    if c + 1 < 15:
                        st_a(c + 1)
                    st_b(c)
                CP(s16, kvs, ['kvs'], ['s16'], eng='dve')
                def b_scores(c):
                    i = c % 2
                    bs0 = 2 * (c % 2)
                    cs = slice(c * 128, (c + 1) * 128)
                    if c > 0:
                        TT(qx[i], qrT[:, p, cs], xiT[:, p * 128:(p + 1) * 128], ALU.mult, [qkk[0], 'xiT'], ['qx%d' % i])
                    for hh in range(2):
                        ps_ = slice(hh * 64, (hh + 1) * 64)
                        MM(bank(bs0 + hh)[:, 0:128], krT[ps_, p, cs], qrT[ps_, p, cs], True, True,
                           qkk, [BK[bs0 + hh]])

                def b_rest(c):
                    i = c % 2
                    bs0 = 2 * (c % 2)
                    bo0 = 4 + 2 * (c % 2)
                    TT(sT[i].rearrange("p (h l) -> p h l", h=2),
                       PSA[:, bs0 * 512:(bs0 + 2) * 512].rearrange("p (h x) -> p h x", h=2)[:, :, 0:128],
                       decayT[:, 2 * p * 128:(2 * p + 2) * 128].rearrange("p (h l) -> p h l", h=2), ALU.mult,
                       [BK[bs0], BK[bs0 + 1], 'decayT'], ['sT%d' % i])
                    for hh in range(2):
                        ps_ = slice(hh * 64, (hh + 1) * 64)
                        MM(bank(bo0 + hh)[:, 0:64], sT[i][:, hh * 128:(hh + 1) * 128], vg[:, c, hh * 64:(hh + 1) * 64],
                           True, c == 0, ['sT%d' % i, 'vg%d' % c], [BK[bo0 + hh]])
                        if c > 0:
                            MM(bank(bo0 + hh)[:, 0:64], qx[i][ps_, :], s16[ps_, c, hh * 64:(hh + 1) * 64],
                               False, True, ['qx%d' % i, 's16'], [BK[bo0 + hh]])
                    CP(oall[:, c, :].rearrange("p (h e) -> p h e", h=2),
                       PSB[:, (bo0 - 4) * 512:(bo0 - 2) * 512].rearrange("p (h x) -> p h x", h=2)[:, :, 0:64],
                       [BK[bo0], BK[bo0 + 1]], ['oall'], eng='act')

                b_scores(0)
                for c in range(16):
                    if c + 1 < 16:
                        b_scores(c + 1)
                    b_rest(c)
                o3 = oall.rearrange("p c (h e) -> p (c h) e", h=2)
                sq3 = kvs.rearrange("p c (h e) -> p (c h) e", h=2)
                gam_b = lnbc[:, 2, p * 128:(p + 1) * 128].rearrange("p (h e) -> p h e", h=2).unsqueeze(1).to_broadcast([128, 16, 2, 64])
                bet_b = lnbc[:, 3, p * 128:(p + 1) * 128].rearrange("p (h e) -> p h e", h=2).unsqueeze(1).to_broadcast([128, 16, 2, 64])
                sq4 = kvs.rearrange("p c (h e) -> p c h e", h=2)
                group_ln_core(o3, sq3, 32, ['oall'], 'kvs', epsA[:, 0:1])
                TT(sq4, sq4, gam_b, ALU.mult, ['kvs', 'lnbc'], ['kvs'])
                TT(sq4, sq4, bet_b, ALU.add, ['kvs', 'lnbc'], ['kvs'])
                vkeys = ['vg%d' % tt for tt in range(16)]
                TT(vg[:, :, 0:128], kvs, vg[:, :, 128:256], ALU.mult, ['kvs'] + vkeys, vkeys)
                if dbg == 'mixB%d_%d' % (l, p):
                    dump_tm(vg, 0, 128, vkeys, p * 128)
                out_proj(l, vg, vkeys, 1, 256 + p * 128, mixT)

        def mixer_C(l):
            hk = lambda k, tb: 'hT%d_%d' % (k, tb)
            DMA('pool', w1s[:].rearrange("p (a b) -> p a b", a=4), Dr['w1s'][l].rearrange("p (a b) -> p a b", a=4), (), ['w1s'])
            DMA('pool', w2k[:], Dr['w2k'][l], (), ['w2k'])
            DMA('pool', w2v[:], Dr['w2v'][l], (), ['w2v'])
            DMA('sp', posT[:], Dr['posT'][l], (), ['posT'])
            for g in range(2):
                S.barrier()
                AR.reset()
                qz = AR.get([4, 2048], BF16)
                KsT = AR.get([2048], BF16)
                KwT = AR.get([2048], BF16)
                Vaug = AR.get([16, 2, 65], BF16)
                gates = AR.get([16, 12], F32)
                mixC = AR.get([16, 256], BF16)
                kcmpT = AR.get([127], BF16)
                vcaug = AR.get([97], BF16)
                hidT = AR.get([2, 127], BF16)
                off_r = AR.off
                kvcT = AR.get([2048], BF16)
                kcp = AR.get([32, 127], BF16)
                wv, wk = wload(kchunks(Dr['w_inCf'][l][:, g * 640:g * 640 + 512]), [8, 512])
                wv2, wk2 = wload(kchunks(Dr['w_inCf'][l][:, g * 640 + 512:g * 640 + 640]), [8, 128])
                S.op('dve', lambda e: e.memset(qz, 0.0), (), ['qT'])
                dsts = [(None, 'qT', 0.125), (None, 'qT', 0.125), (KsT, 'KsT', 1.0), (KwT, 'KwT', 1.0),
                        (kvcT, 'kvcT', 1.0)]
                n = 0
                for bi_, (dst, dk, scl) in enumerate(dsts):
                    for tb in range(4):
                        b = n % 4
                        n += 1
                        for k in range(8):
                            lw = wv[:, k, bi_ * 128:(bi_ + 1) * 128] if bi_ < 4 else wv2[:, k, :]
                            MM(bank(b), lw, hT[:, k, tb * 512:(tb + 1) * 512], k == 0, k == 7,
                               [wk if bi_ < 4 else wk2, hk(k, tb)], [BK[b]])
                        if dst is None:
                            ACT(qz[0:64, 2 * bi_, tb * 512:(tb + 1) * 512], bank(b)[0:64, :], AF.Identity, [BK[b]], [dk], scale=scl)
                            TS(qz[64:128, 2 * bi_ + 1, tb * 512:(tb + 1) * 512], bank(b)[64:128, :], scl, None, ALU.mult, None, [BK[b]], [dk])
                        elif n % 2:
                            ACT(dst[:, tb * 512:(tb + 1) * 512], bank(b), AF.Identity, [BK[b]], [dk], scale=scl)
                        else:
                            TS(dst[:, tb * 512:(tb + 1) * 512], bank(b), scl, None, ALU.mult, None, [BK[b]], [dk])
                wv3, wk3 = wload(kchunks(Dr['w_inCt'][l][:, g * 140:(g + 1) * 140]), [8, 140])
                S.op('dve', lambda e: e.memset(Vaug[:, :, :, 64:65], 1.0), (), ['Vaug'])
                for tt in range(16):
                    b = 4 + tt % 4
                    for k in range(8):
                        MM(bank(b)[:, 0:140], hT[:, k, tt * 128:(tt + 1) * 128], wv3[:, k, :], k == 0, k == 7,
                           [hk(k, tt // 4), wk3], [BK[b]])
                    CP(Vaug[:, tt, :, 0:64], bank(b)[:, 0:128].rearrange("p (s d) -> p s d", s=2), [BK[b]], ['Vaug'], eng='dve')
                    ACT(gates[:, tt, :], bank(b)[:, 128:140], AF.Sigmoid, [BK[b]], ['gates'])
                kv_t = kvcT.tensor
                win = bass.AP(kv_t, kvcT.offset, [list(kvcT.ap[0]), [1, 32], [16, 127]])
                TT(kcp, win, posT[:, 0:32].unsqueeze(2).to_broadcast([128, 32, 127]), ALU.add, ['kvcT', 'posT'], ['kcp'])
                for kv in range(2):
                    ps_ = slice(kv * 64, (kv + 1) * 64)
                    for ll in range(32):
                        MM(bank(kv)[0:64, 0:127], w1s[ps_, ll * 64:(ll + 1) * 64], kcp[ps_, ll, :],
                           ll == 0, ll == 31, ['w1s', 'kcp'], [BK[kv]])
                    ACT(hidT[0:64, kv, :], bank(kv)[0:64, 0:127], AF.Gelu_apprx_tanh, [BK[kv]], ['hidT'])
                MM(bank(2)[:, 0:127], w2k[:], hidT[0:64, 0, :], True, True, ['w2k', 'hidT'], ['B2'])
                CP(kcmpT, bank(2)[:, 0:127], ['B2'], ['kcmpT'], eng='dve')
                MM(bank(3)[0:127, 0:64], hidT[0:64, 1, :], w2v[:], True, True, ['w2v', 'hidT'], ['B3'])
                S.op('dve', lambda e: e.memset(vcaug[:, 64:65], 1.0), (), ['vcaug'])
                CP(vcaug[0:127, 0:64], bank(3)[0:127, 0:64], ['B3'], ['vcaug'], eng='dve')
                CP(vcaug[:, 65:97], ovl[:, 0:32], ['ovl'], ['vcaug'], eng='dve')
                S.barrier()
                AR.reset(off_r)
                PT = [AR.get([512], BF16) for _ in range(4)]
                MbT = [AR.get([128], BF16) for _ in range(2)]
                for j_ in range(2):
                    S.op('dve', (lambda t_: (lambda e: e.memset(t_, 0.0)))(MbT[j_]), (), ['MbT%d' % j_])
                Mb = [AR.get([32], BF16) for _ in range(2)]
                ocg = [AR.get([4, 64], F32) for _ in range(2)]
                t1 = [AR.get([4, 64], F32) for _ in range(2)]
                t2 = [AR.get([4, 64], F32) for _ in range(2)]
                sm = [AR.get([96], F32) for _ in range(2)]
                impt = [AR.get([4, 32], F32) for _ in range(2)]
                pn = [0]

                def qslice(r, qt):
                    return qz[:, r, qt * 128:(qt + 1) * 128]

                def attend(qt, kts, KT, vsel, bS, bO, extra_fn):
                    def scores(ki):
                        kt = kts[ki]
                        bs_ = bS[ki % len(bS)]
                        extras = extra_fn(kt)
                        v4 = bank(bs_).rearrange("p (r t) -> p r t", r=4)
                        MM(v4, KT[:, kt * 128:(kt + 1) * 128], qz[:, :, qt * 128:(qt + 1) * 128],
                           True, not extras, ['KsT', 'KwT', 'qT'], [BK[bs_]])
                        for ei, (lh, rh, ks) in enumerate(extras):
                            MM(v4, lh, rh, False, ei == len(extras) - 1, ks, [BK[bs_]])

                    def rest(ki):
                        kt = kts[ki]
                        bs_ = bS[ki % len(bS)]
                        i = pn[0] % 4
                        pn[0] += 1
                        ACT(PT[i], bank(bs_), AF.Exp, [BK[bs_]], ['PT%d' % i])
                        for r in range(4):
                            MM(bank(bO)[:, r * 65:(r + 1) * 65], PT[i][:, r * 128:(r + 1) * 128], Vaug[:, kt, vsel, :],
                               ki == 0 and r == 0, ki == len(kts) - 1 and r == 3, ['PT%d' % i, 'Vaug'], [BK[bO]])

                    LA = 2
                    for ki in range(min(LA, len(kts))):
                        scores(ki)
                    for ki in range(len(kts)):
                        if ki + LA < len(kts):
                            scores(ki + LA)
                        rest(ki)

                for qt in range(16):
                    j = qt % 2
                    smj = sm[j]
                    sk = 'sm%d' % j
                    qs = slice(qt * 128, (qt + 1) * 128)
                    MM(bank(0)[0:127, :].rearrange("p (r t) -> p r t", r=4), kcmpT[:, :], qz[:, :, qs], True, False,
                       ['kcmpT', 'qT'], ['B0'])
                    MM(bank(0)[0:127, :].rearrange("p (r t) -> p r t", r=4), ident_b[0:127, 0:127],
                       cmpb[0:127, qs].unsqueeze(1).to_broadcast([127, 4, 128]), False, True, ['ident_b', 'cmpb'], ['B0'])
                    i = pn[0] % 4
                    pn[0] += 1
                    ACT(PT[i][0:127, :], bank(0)[0:127, :], AF.Exp, ['B0'], ['PT%d' % i])
                    for r in range(4):
                        MM(bank(1)[:, r * 97:(r + 1) * 97], PT[i][0:127, r * 128:(r + 1) * 128], vcaug[0:127, :],
                           r == 0, r == 3, ['PT%d' % i, 'vcaug'], ['B1'])
                    O = bank(1)[:, 0:388].rearrange("p (r c) -> p r c", r=4)
                    cb4 = causalb[:].unsqueeze(1).to_broadcast([128, 4, 128])
                    wb4 = winb[:].unsqueeze(1).to_broadcast([128, 4, 128])

                    def wmask(kt, qt=qt, cb4=cb4, wb4=wb4):
                        if kt == qt:
                            return [(ident_b[:], cb4, ['ident_b', 'causalb'])]
                        if kt == qt - 4:
                            return [(ident_b[:], wb4, ['ident_b', 'winb'])]
                        return []
                    attend(qt, list(range(max(0, qt - 4), qt + 1)), KwT, 1, [2, 3, 6, 7], 5, wmask)
                    TS(smj[:, 0:4], O[:, :, 64], 1e-30, None, ALU.max, None, ['B1'], [sk])
                    RCP(smj[:, 4:8], smj[:, 0:4], [sk], [sk])
                    TT(impt[j], O[:, :, 65:97], smj[:, 4:8].unsqueeze(2).to_broadcast([128, 4, 32]), ALU.mult, ['B1', sk], ['impt%d' % j])
                    RED(smj[:, 32:64], impt[j].rearrange("p r c -> p c r"), ALU.add, ['impt%d' % j], [sk])
                    TT(smj[:, 32:64], smj[:, 32:64], fbias[:, qt * 32:(qt + 1) * 32], ALU.add, [sk, 'fbias'], [sk])
                    S.op('dve', (lambda o_, i_: (lambda e: e.max(o_, i_)))(smj[:, 64:72], smj[:, 32:64]), [sk], [sk])
                    TS(Mb[j], smj[:, 32:64], smj[:, 71:72], -1.0, ALU.is_ge, ALU.add, [sk], ['Mb%d' % j])
                    pbt = bank(0).bitcast(BF16)
                    TR(pbt[0:32, 0:128], Mb[j], ident_b[:], ['Mb%d' % j, 'ident_b'], ['B0'])
                    CP(MbT[j][0:32, :], pbt[0:32, 0:128], ['B0'], ['MbT%d' % j], eng='dve')
                    gv = gates[:, qt, :].rearrange("p (r c) -> p r c", r=4)
                    TT(smj[:, 8:12], smj[:, 4:8], gv[:, :, 0], ALU.mult, [sk, 'gates'], [sk])
                    TT(ocg[j], O[:, :, 0:64], smj[:, 8:12].unsqueeze(2).to_broadcast([128, 4, 64]), ALU.mult, ['B1', sk], ['ocg%d' % j])

                    mb4 = MbT[j][:, :].unsqueeze(1).to_broadcast([128, 4, 128])

                    def smask(kt, qt=qt, j=j, cb4=cb4, mb4=mb4):
                        ex = [(expand[:, kt * 128:(kt + 1) * 128], mb4, ['expand', 'MbT%d' % j])]
                        if kt == qt:
                            ex.append((ident_b[:], cb4, ['ident_b', 'causalb']))
                        return ex
                    attend(qt, list(range(0, qt + 1)), KsT, 0, [2, 3, 6, 7], 4, smask)
                    Os = bank(4)[:, 0:260].rearrange("p (r c) -> p r c", r=4)
                    Ow = bank(5)[:, 0:260].rearrange("p (r c) -> p r c", r=4)
                    RCP(smj[:, 12:16], Os[:, :, 64], ['B4'], [sk])
                    TT(smj[:, 12:16], smj[:, 12:16], gv[:, :, 1], ALU.mult, [sk, 'gates'], [sk])
                    RCP(smj[:, 16:20], Ow[:, :, 64], ['B5'], [sk])
                    TT(smj[:, 16:20], smj[:, 16:20], gv[:, :, 2], ALU.mult, [sk, 'gates'], [sk])
                    TT(t1[j], Os[:, :, 0:64], smj[:, 12:16].unsqueeze(2).to_broadcast([128, 4, 64]), ALU.mult, ['B4', sk], ['t1_%d' % j])
                    TT(t2[j], Ow[:, :, 0:64], smj[:, 16:20].unsqueeze(2).to_broadcast([128, 4, 64]), ALU.mult, ['B5', sk], ['t2_%d' % j])
                    TT(t1[j], t1[j], ocg[j], ALU.add, ['t1_%d' % j, 'ocg%d' % j], ['t1_%d' % j])
                    TT(mixC[:, qt, :].rearrange("p (r d) -> p r d", r=4), t1[j], t2[j], ALU.add, ['t1_%d' % j, 't2_%d' % j], ['mixC%d' % qt])
                mkeys = ['mixC%d' % tt for tt in range(16)]
                if dbg == 'mixC%d_%d' % (l, g):
                    dump_tm(mixC, 0, 256, mkeys, g * 256)
                S.barrier()
                AR.reset(off_r)
                mixT = AR.get([2, 2048], BF16)
                out_proj(l, mixC, mkeys, 2, 512 + g * 256, mixT)

        for l in range(n_layers):
            hk = lambda k, tb: 'hT%d_%d' % (k, tb)
            if 'A' in mixers:
                S.barrier()
                AR.reset()
                zag = AR.get([16, 512], BF16)
                off_sqv = AR.off
                sqv = AR.get([16, 256], F32)
                st1 = AR.get([64], F32)
                st2 = AR.get([64], F32)
                st3 = AR.get([64], F32)
                mixA = AR.get([16, 256], BF16)
                atmp = [AR.get([256], F32) for _ in range(2)]
                wstage = AR.get([4, 128], F32)
                DMA('sp', wstage, Dr['WsT'][l].rearrange("p (g t) -> p g t", g=4), (), ['wstage'])
                TT(WsT[:].rearrange("p (g t) -> p g t", g=4), wstage,
                   causal01[:].unsqueeze(1).to_broadcast([128, 4, 128]), ALU.mult, ['wstage', 'causal01'], ['WsT'])
                DMA('sp', bsT[:], Dr['bsT'][l], (), ['bsT'])
                DMA('sp', lnbc[:, 0, :], Dr['a_lng'][l], (), ['lnbcA'])
                DMA('sp', lnbc[:, 1, :], Dr['a_lnb'][l], (), ['lnbcA'])
                wv, wk = wload(kchunks(Dr['w_inA'][l]), [8, 512])
                for tt in range(16):
                    b = tt % 4
                    for k in range(8):
                        MM(bank(b), hT[:, k, tt * 128:(tt + 1) * 128], wv[:, k, :], k == 0, k == 7,
                           [hk(k, tt // 4), wk], [BK[b]])
                    ACT(zag[:, tt, :], bank(b), AF.Gelu_apprx_tanh, [BK[b]], ['zag'])
                v4 = zag[:, :, 256:512].rearrange("p t (g d) -> p t g d", g=4)
                TT(sqv, zag[:, :, 256:512], zag[:, :, 256:512], ALU.mult, ['zag'], ['sqv'])
                RED(st1.rearrange("p (t g) -> p t g", t=16), v4, ALU.add, ['zag'], ['st1'])
                RED(st2.rearrange("p (t g) -> p t g", t=16), sqv.rearrange("p t (g d) -> p t g d", g=4), ALU.add, ['sqv'], ['st2'])
                TS(st1, st1, 1.0 / 64, None, ALU.mult, None, ['st1'], ['st1'])
                TT(st3, st1, st1, ALU.mult, ['st1'], ['st3'])
                STT(st2, st2, 1.0 / 64, st3, ALU.mult, ALU.subtract, ['st2', 'st3'], ['st2'])
                TS(st2, st2, 0.0, None, ALU.max, None, ['st2'], ['st2'])
                ACT(st2, st2, AF.Sqrt, ['st2', 'epsA'], ['st2'], bias=epsA[:, 0:1])
                RCP(st2, st2, ['st2'], ['st2'])
                mean_b = st1.rearrange("p (t g) -> p t g", t=16).unsqueeze(3).to_broadcast([128, 16, 4, 64])
                rstd_b = st2.rearrange("p (t g) -> p t g", t=16).unsqueeze(3).to_broadcast([128, 16, 4, 64])
                sq4 = sqv.rearrange("p t (g d) -> p t g d", g=4)
                TT(sq4, v4, mean_b, ALU.subtract, ['zag', 'st1'], ['sqv'])
                TT(sq4, sq4, rstd_b, ALU.mult, ['sqv', 'st2'], ['sqv'])
                gam_b = lnbc[:, 0, :].unsqueeze(1).to_broadcast([128, 16, 256])
                bet_b = lnbc[:, 1, :].unsqueeze(1).to_broadcast([128, 16, 256])
                TT(sqv, sqv, gam_b, ALU.mult, ['sqv', 'lnbcA'], ['sqv'])
                TT(zag[:, :, 256:512], sqv, bet_b, ALU.add, ['sqv', 'lnbcA'], ['zag'])
                bs_b = bsT[:, 0:4].unsqueeze(2).to_broadcast([128, 4, 64])
                for tt in range(16):
                    b = 4 + tt % 4
                    for g in range(4):
                        MM(bank(b)[:, g * 64:(g + 1) * 64], WsT[:, g * 128:(g + 1) * 128],
                           zag[:, tt, 256 + g * 64:256 + (g + 1) * 64], g == 0, g == 3, ['WsT', 'zag'], [BK[b]])
                    at = atmp[tt % 2]
                    ak = 'atmp%d' % (tt % 2)
                    TT(at.rearrange("p (g d) -> p g d", g=4), bank(b)[:, 0:256].rearrange("p (g d) -> p g d", g=4),
                       bs_b, ALU.add, [BK[b], 'bsT'], [ak])
                    TT(mixA[:, tt, :], at, zag[:, tt, 0:256], ALU.mult, [ak, 'zag'], ['mixA%d' % tt])
                if dbg == 'mixA%d' % l:
                    dst = AR.get([256], F32)
                    for tt in range(16):
                        CP(dst, mixA[:, tt, :], ['mixA%d' % tt], ['dst'])
                        DMA('sp', dbg_d[tt * 128:(tt + 1) * 128, 0:256], dst, ['dst'], ['dbg'])
                AR.reset(off_sqv)
                mixT = AR.get([2, 2048], BF16)
                out_proj(l, mixA, ['mixA%d' % tt for tt in range(16)], 2, 0, mixT, ['sqv'])

            if 'B' in mixers:
                mixer_B(l)
            if 'C' in mixers:
                mixer_C(l)

            S.barrier()
            layer_norm(l, 1, last=False)
            if dbg == 'ln1_%d' % l:
                break

            if do_ffn:
                S.barrier()
                AR.reset()
                DMA('sp', cwT[:], Dr['cwT'][l], (), ['cwT'])
                DMA('sp', cbT[:], Dr['cbT'][l], (), ['cbT'])
                gT = AR.get([max(FF_SPLIT), 2048], BF16)
                ctmp = [[AR.get([1024], F32) for _ in range(2)] for _ in range(2)]
                sgt = [AR.get([1024], BF16) for _ in range(2)]
                j0 = 0
                PS4 = [PSA, PSB]
                P4K = [BK[0:4], BK[4:8]]
                for pi_, part_n in enumerate(FF_SPLIT):
                    if l + 1 < n_layers:
                        for blk_ in range(3 * pi_, 3 * pi_ + 3):
                            ada_block_deferred(l + 1, blk_)
                    wgrp = {}
                    for jl in range(part_n):
                        j = j0 + jl
                        if jl % 4 == 0:
                            ng = min(4, part_n - jl)
                            for part in range(2):
                                jj0 = part * 22 + j
                                wgrp[part] = wload(kchunks(Dr['w_up'][l][:, jj0 * 128:(jj0 + ng) * 128]), [8, ng * 128])
                        for part in range(2):
                            jj = part * 22 + j
                            wvf, wk = wgrp[part]
                            wv = wvf[:, :, (jl % 4) * 128:(jl % 4 + 1) * 128]
                            ps = PS4[part]
                            for tb in range(4):
                                for k in range(8):
                                    MM(ps[:, tb * 512:(tb + 1) * 512], wv[:, k, :], hT[:, k, tb * 512:(tb + 1) * 512],
                                       k == 0, k == 7, [wk, hk(k, tb)], [P4K[part][tb]])
                            for hf in range(2):
                                ct = ctmp[part][hf]
                                ck = 'ctmp%d_%d' % (part, hf)
                                o = hf * 1024
                                pk = P4K[part][2 * hf:2 * hf + 2]
                                pkp = P4K[part][max(0, 2 * hf - 1):2 * hf + 2]
                                ACT(ct, ps[:, o:o + 1024], AF.Identity, pk + ['cwT', 'cbT'], [ck],
                                    scale=cwT[:, jj * 3 + 2:jj * 3 + 3], bias=cbT[:, jj:jj + 1])
                                if hf == 0:
                                    STT(ct[:, 1:1024], ps[:, 0:1023], cwT[:, jj * 3 + 1:jj * 3 + 2], ct[:, 1:1024],
                                        ALU.mult, ALU.add, pk + ['cwT', ck], [ck])
                                    STT(ct[:, 2:1024], ps[:, 0:1022], cwT[:, jj * 3:jj * 3 + 1], ct[:, 2:1024],
                                        ALU.mult, ALU.add, pk + ['cwT', ck], [ck])
                                else:
                                    STT(ct, ps[:, o - 1:o + 1023], cwT[:, jj * 3 + 1:jj * 3 + 2], ct,
                                        ALU.mult, ALU.add, pkp + ['cwT', ck], [ck])
                                    STT(ct, ps[:, o - 2:o + 1022], cwT[:, jj * 3:jj * 3 + 1], ct,
                                        ALU.mult, ALU.add, pkp + ['cwT', ck], [ck])
                                if part == 0:
                                    ACT(sgt[hf], ct, AF.Silu, [ck], ['sgt%d' % hf])
                                else:
                                    TT(gT[:, jl, o:o + 1024], ct, sgt[hf], ALU.mult, [ck, 'sgt%d' % hf], ['gT%d' % jl])
                    nsl = (part_n + 3) // 4
                    wvs = []
                    for s in range(nsl):
                        r0 = (j0 + 4 * s) * 128
                        nr = min(4, part_n - 4 * s)
                        wvs.append(wload(kchunks(Dr['w_down'][l][r0:r0 + nr * 128, :]), [nr, 1024]))
                    bi2 = 0
                    for fb in range(8):
                        for tb in range(4):
                            b = bi2 % 8
                            bi2 += 1
                            for jl in range(part_n):
                                wv, wk = wvs[jl // 4]
                                MM(bank(b), wv[:, jl % 4, fb * 128:(fb + 1) * 128], gT[:, jl, tb * 512:(tb + 1) * 512],
                                   jl == 0, jl == part_n - 1, [wk, 'gT%d' % jl], [BK[b]])
                            xs = xT[:, fb, tb * 512:(tb + 1) * 512]
                            STT(xs, bank(b), drv[l][:, 24 + fb:25 + fb], xs, ALU.mult, ALU.add,
                                [BK[b], 'drv%d' % l, 'xT%d_%d' % (fb, tb)], ['xT%d_%d' % (fb, tb)])
                    j0 += part_n
                S.barrier()
            layer_norm(l, 2, last=(l == n_layers - 1))

        S.barrier()
        AR.reset()
        ost = [AR.get([1024], F32) for _ in range(4)]
        bi = 0
        for tt in range(16):
            o = ost[tt % 4]
            ok = 'ost%d' % (tt % 4)
            for cg in range(2):
                b = bi % 8
                bi += 1
                for q in range(4):
                    c = cg * 4 + q
                    TR(bank(b)[:, q * 128:(q + 1) * 128], xT[:, c, tt * 128:(tt + 1) * 128], ident_f[:],
                       ['xT%d_%d' % (c, tt // 4), 'ident_f'], [BK[b]])
                CP(o[:, cg * 512:(cg + 1) * 512], bank(b), [BK[b]], [ok], eng=EV())
            DMA('sp', out_d[tt * 128:(tt + 1) * 128, :], o, [ok], ['out'])
        S.finish()
        S.replay()
    return nc


_CACHE = {}


def kernel(**inputs):
    inp = {k: np.asarray(v) for k, v in inputs.items()}
    if 'nc' not in _CACHE:
        _CACHE['nc'] = build()
    nc = _CACHE['nc']
    w = _prep_weights(inp)
    tb = _tables()
    in_maps = []
    for b in range(8):
        m = dict(w)
        m.update(tb)
        m['x'] = np.ascontiguousarray(inp['x'][b], dtype=np.float32)
        m['cT'] = _pad64(inp['c'][b].reshape(8, 128).T)
        in_maps.append(m)
    res = run_bass_kernel_spmd(nc, in_maps, core_ids=list(range(8)))
    out = np.stack([np.asarray(r['out'], dtype=np.float32) for r in res.results], 0)
    return out
```

```python
import math
from contextlib import ExitStack
import numpy as np
import concourse.bass as bass
import concourse.mybir as mybir
from concourse.bass_utils import run_bass_kernel_spmd

F32 = mybir.dt.float32
BF16 = mybir.dt.bfloat16
AF = mybir.ActivationFunctionType
ALU = mybir.AluOpType
AX = mybir.AxisListType

ENGS = ['pe', 'dve', 'act', 'pool', 'sp']
NRING = 8
DEPTH = 2
SEQ = 2048
DM = 1024
ALPHA = (2 * DEPTH) ** 0.25
LN_EPS = 1e-5
NEGB = -30000.0
FF_SPLIT = [6, 6, 5, 5]


class Sched:
    def __init__(self, nc, stack):
        self.nc = nc
        self.prog = {e: [] for e in ENGS}
        self.cnt = {e: 0 for e in ENGS}
        self.seen = {e: {} for e in ENGS}
        self.lastw = {}
        self.readers = {}
        self.sems = {}
        self.semval = {}
        self.relay_fn = None
        for e in ENGS:
            self.sems[e] = stack.enter_context(nc.semaphore("s_" + e))
        self.dma_n = {}
        for q in ['sp', 'act', 'pool']:
            self.dma_n[q] = 0
            for j in range(NRING):
                nm = "d_%s%d" % (q, j)
                self.sems[nm] = stack.enter_context(nc.semaphore(nm))

    def _deps(self, eng, reads, writes, is_dma=False):
        deps = []
        for k in reads:
            t = self.lastw.get(k)
            if t is not None:
                deps.append((t, 'raw'))
        for k in writes:
            t = self.lastw.get(k)
            if t is not None:
                deps.append((t, 'waw'))
            for s, v in self.readers.get(k, {}).items():
                deps.append(((s, v), 'war'))
        need = {}
        for (s, v), kind in deps:
            if s == eng and not is_dma:
                if kind != 'raw' or eng == 'pe':
                    continue
            if self.seen[eng].get(s, 0) >= v:
                continue
            if need.get(s, 0) < v:
                need[s] = v
        return need

    def _emit_waits(self, eng, need):
        if eng in ('sp', 'pool') and 'pe' in need and self.relay_fn is not None:
            need = dict(need)
            v = need.pop('pe')
            self.seen[eng]['pe'] = v
            R = 'dve'
            if self.seen[R].get('pe', 0) < v:
                self.prog[R].append(('wait', 'pe', v))
                self.seen[R]['pe'] = v
            lr = getattr(self, 'last_relay', 0)
            if lr and self.seen[R].get(R, 0) < lr:
                self.prog[R].append(('wait', R, lr))
                self.seen[R][R] = lr
            self.cnt[R] += 1
            self.last_relay = self.cnt[R]
            self.semval[R] = self.cnt[R]
            self.prog[R].append(('op', self.relay_fn, R, 1))
            if self.seen[eng].get(R, 0) < self.cnt[R]:
                need[R] = max(need.get(R, 0), self.cnt[R])
        for s, v in need.items():
            self.prog[eng].append(('wait', s, v))
            self.seen[eng][s] = v

    def _commit(self, tok, reads, writes):
        for k in writes:
            self.lastw[k] = tok
            self.readers[k] = {}
        for k in reads:
            d = self.readers.setdefault(k, {})
            if d.get(tok[0], 0) < tok[1]:
                d[tok[0]] = tok[1]

    def relay_readers(self, keys):
        if self.relay_fn is None:
            return
        v = 0
        for k in keys:
            d = self.readers.get(k)
            if d and 'pe' in d:
                v = max(v, d['pe'])
        if v == 0:
            return
        R = 'dve'
        if self.seen[R].get('pe', 0) < v:
            self.prog[R].append(('wait', 'pe', v))
            self.seen[R]['pe'] = v
        lr = getattr(self, 'last_relay', 0)
        if lr and self.seen[R].get(R, 0) < lr:
            self.prog[R].append(('wait', R, lr))
            self.seen[R][R] = lr
        self.cnt[R] += 1
        self.semval[R] = self.cnt[R]
        self.last_relay = self.cnt[R]
        self.prog[R].append(('op', self.relay_fn, R, 1))
        for k in keys:
            d = self.readers.get(k)
            if d and 'pe' in d:
                d.pop('pe')
                d[R] = max(d.get(R, 0), self.cnt[R])

    def op(self, eng, fn, reads=(), writes=()):
        need = self._deps(eng, reads, writes)
        self._emit_waits(eng, need)
        self.cnt[eng] += 1
        tok = (eng, self.cnt[eng])
        self.semval[eng] = self.cnt[eng]
        self.prog[eng].append(('op', fn, eng, 1))
        self._commit(tok, reads, writes)
        return tok

    def dma_multi(self, q, pairs, reads=(), writes=()):
        i = self.dma_n[q]
        self.dma_n[q] += 1
        slot = "d_%s%d" % (q, i % NRING)
        prev = self.semval.get(slot, 0)
        val = prev + 16 * len(pairs)
        need = self._deps(q, reads, writes, is_dma=True)
        if prev > 0 and self.seen[q].get(slot, 0) < prev:
            need[slot] = max(need.get(slot, 0), prev)
        self._emit_waits(q, need)
        for out, in_ in pairs:
            self.prog[q].append(('op', (lambda o_, i_: (lambda e: e.dma_start(out=o_, in_=i_)))(out, in_), slot, 16))
        self.semval[slot] = val
        tok = (slot, val)
        self._commit(tok, reads, writes)
        return tok

    def dma(self, q, out, in_, reads=(), writes=()):
        return self.dma_multi(q, [(out, in_)], reads, writes)

    def _wait_all(self, e):
        need = {}
        for s_, v in self.semval.items():
            if s_ == e:
                continue
            if self.seen[e].get(s_, 0) < v:
                need[s_] = v
        self._emit_waits(e, need)

    def barrier(self, engs=('pe', 'dve', 'act', 'sp'), relay=None):
        if relay is None or 'sp' not in engs:
            for e in engs:
                self._wait_all(e)
            return
        snap = dict(self.semval)
        self._wait_all('sp')
        tok = self.dma('sp', relay[0], relay[1], (), ['__bar'])
        for e in engs:
            if e == 'sp':
                continue
            self._emit_waits(e, {tok[0]: tok[1]} if self.seen[e].get(tok[0], 0) < tok[1] else {})
            for s_, v in snap.items():
                if s_ != e and self.seen[e].get(s_, 0) < v:
                    self.seen[e][s_] = v

    def finish(self):
        self.barrier(engs=('sp',))

    def replay(self):
        nc = self.nc
        sems = self.sems
        prog = self.prog

        def run(e, name):
            for it in prog[name]:
                if it[0] == 'wait':
                    e.wait_ge(sems[it[1]], it[2])
                else:
                    it[1](e).then_inc(sems[it[2]], it[3])

        with nc.Block() as block:
            @block.tensor
            def _(e):
                run(e, 'pe')

            @block.vector
            def _(e):
                run(e, 'dve')

            @block.scalar
            def _(e):
                run(e, 'act')

            @block.gpsimd
            def _(e):
                run(e, 'pool')

            @block.sync
            def _(e):
                run(e, 'sp')


def _pad64(a):
    a = np.asarray(a, dtype=np.float32)
    out = np.zeros(a.shape[:-1] + (64,), np.float32)
    out[..., :a.shape[-1]] = a
    return out


def _tables():
    t = {}
    half = 32
    inv = np.power(np.float32(10000.0), -np.arange(half, dtype=np.float32) / np.float32(half)).astype(np.float32)
    pos = np.arange(SEQ, dtype=np.float32)
    ang = pos[:, None] * inv[None, :]
    cos = np.cos(ang).astype(np.float32).T
    sin = np.sin(ang).astype(np.float32).T
    cosT = np.concatenate([cos, cos, cos, cos], 0)
    sinT = np.concatenate([-sin, sin, -sin, sin], 0)
    t['cosT'] = np.ascontiguousarray(cosT)
    t['sinT'] = np.ascontiguousarray(sinT)
    H = 4
    L = 128
    lg = np.log1p(-np.exp2(-5.0 - np.arange(H, dtype=np.float32))).astype(np.float32)
    idx = np.arange(L, dtype=np.float32)
    diff = idx[:, None] - idx[None, :]
    dec = np.where(diff >= 0, np.exp(lg[:, None, None] * np.maximum(diff, 0.0)), 0.0).astype(np.float32)
    t['decayT'] = np.ascontiguousarray(np.transpose(dec, (2, 0, 1)) * np.float32(0.125)).reshape(128, 512)
    xi = np.exp(lg[:, None] * (idx + 1.0)).astype(np.float32)
    zeta = np.exp(lg[:, None] * (L - 1.0 - idx)).astype(np.float32)
    xiT = np.zeros((128, 2, 128), np.float32)
    for p in range(2):
        for hh in range(2):
            xiT[hh * 64:(hh + 1) * 64, p, :] = xi[2 * p + hh][None, :]
    t['xiT'] = xiT.reshape(128, 256)
    t['zeta'] = _pad64(zeta.T * np.float32(0.125))
    cd = np.exp(lg * L).astype(np.float32)
    cdv = np.zeros((128, 2), np.float32)
    for p in range(2):
        for hh in range(2):
            cdv[hh * 64:(hh + 1) * 64, p] = cd[2 * p + hh]
    t['cdv'] = _pad64(cdv)
    key = np.arange(SEQ)
    ex = np.zeros((128, SEQ), np.float32)
    ex[key // 64, key] = -NEGB
    t['expand'] = ex
    kk = np.arange(128)[:, None]
    tt = np.arange(128)[None, :]
    t['causalb'] = np.where(kk > tt, NEGB, 0.0).astype(np.float32)
    t['winb'] = np.where(kk <= tt, NEGB, 0.0).astype(np.float32)
    t['identf'] = np.eye(128, dtype=np.float32)
    t['causal01'] = np.where(tt >= kk, 1.0, 0.0).astype(np.float32)
    k127 = np.arange(128)[:, None]
    tpos = np.arange(SEQ)[None, :]
    t['cmpb'] = np.where(16 * k127 + 31 > tpos, NEGB, 0.0).astype(np.float32)
    fb = np.zeros((128, 16, 32), np.float32)
    for qt in range(16):
        tq = qt * 128 + np.arange(128)
        cur = tq // 64
        blk = np.arange(32)
        future = blk[None, :] > cur[:, None]
        forced = (blk[None, :] == 0) | (blk[None, :] == cur[:, None]) | (blk[None, :] == cur[:, None] - 1)
        fb[:, qt, :] = np.where(forced, 1e30, np.where(future, -1e30, 0.0))
    t['fbias'] = fb.reshape(128, 512)
    ov = np.zeros((128, 32), np.float32)
    c0 = np.arange(127)[:, None] * 16
    s0 = np.arange(32)[None, :] * 64
    ov[:127] = np.clip(np.minimum(c0 + 32, s0 + 64) - np.maximum(c0, s0), 0, None) / 32.0
    t['ovl'] = _pad64(ov)
    return t


TABLE_SHAPES = {'cosT': [128, 2048], 'sinT': [128, 2048], 'decayT': [128, 512], 'xiT': [128, 256],
                'zeta': [128, 64], 'cdv': [128, 64], 'expand': [128, 2048], 'causalb': [128, 128],
                'winb': [128, 128], 'causal01': [128, 128], 'identf': [128, 128], 'cmpb': [128, 2048], 'fbias': [128, 512], 'ovl': [128, 64]}


def _prep_weights(inp):
    w = {}
    f = lambda a: np.ascontiguousarray(a, dtype=np.float32)
    w_in = inp['w_in']
    L = DEPTH
    w['w_ada'] = f(inp['w_ada'])
    w['b_adaT'] = _pad64(inp['b_ada'].reshape(L, 48, 128).transpose(0, 2, 1))
    w['w_inA'] = f(w_in[:, :, 0:512])
    cols = []
    for p in range(2):
        for base in (512, 768):
            hd = np.arange(128)
            h = 2 * p + hd // 64
            d = hd % 64
            cols.append(base + h * 64 + d)
            cols.append(base + h * 64 + (d + 32) % 64)
    cols = np.concatenate(cols)
    w['w_inBf'] = f(w_in[:, :, cols])
    cols = []
    for p in range(2):
        cols.append(1024 + p * 128 + np.arange(128))
        cols.append(1280 + p * 128 + np.arange(128))
    w['w_inBt'] = f(w_in[:, :, np.concatenate(cols)])
    cols = []
    for g in range(2):
        cols.append(1536 + g * 256 + np.arange(256))
        ks = 2304 + g * 64 + np.arange(64)
        kw = 2560 + g * 64 + np.arange(64)
        cols += [ks, ks, kw, kw]
        cols.append(2048 + g * 64 + np.arange(64))
        cols.append(2176 + g * 64 + np.arange(64))
    w['w_inCf'] = f(w_in[:, :, np.concatenate(cols)])
    cols = []
    for g in range(2):
        cols.append(2432 + g * 64 + np.arange(64))
        cols.append(2688 + g * 64 + np.arange(64))
        cols.append(2816 + g * 12 + np.arange(12))
    w['w_inCt'] = f(w_in[:, :, np.concatenate(cols)])
    w['WsT'] = f(inp['a_ws'].transpose(0, 3, 1, 2).reshape(L, 128, 512))
    w['bsT'] = _pad64(inp['a_bs'].transpose(0, 2, 1))
    w['a_lng'] = f(np.broadcast_to(inp['a_ln_g'].reshape(L, 1, 256), (L, 128, 256)))
    w['a_lnb'] = f(np.broadcast_to(inp['a_ln_b'].reshape(L, 1, 256), (L, 128, 256)))
    w['b_gng'] = f(np.broadcast_to(inp['b_gn_g'].reshape(L, 1, 256), (L, 128, 256)))
    w['b_gnb'] = f(np.broadcast_to(inp['b_gn_b'].reshape(L, 1, 256), (L, 128, 256)))
    posT = np.concatenate([inp['c_pos_k'].transpose(0, 2, 1), inp['c_pos_v'].transpose(0, 2, 1)], 1)
    w['posT'] = _pad64(posT)
    w1k = inp['c_w1_k'].reshape(L, 32, 64, 64).transpose(0, 2, 1, 3)
    w1v = inp['c_w1_v'].reshape(L, 32, 64, 64).transpose(0, 2, 1, 3)
    w['w1s'] = f(np.concatenate([w1k, w1v], 1).reshape(L, 128, 2048))
    w['w2k'] = f(np.concatenate([inp['c_w2_k'], inp['c_w2_k']], 2))
    w['w2v'] = f(inp['c_w2_v'])
    w['w_out'] = f(inp['w_out'])
    w['lnpk'] = _pad64(np.concatenate([inp[k].reshape(L, 8, 128).transpose(0, 2, 1)
                                       for k in ('ln1_g', 'ln1_b', 'ln2_g', 'ln2_b')], 2))
    w['w_up'] = f(inp['w_up'])
    w['cwT'] = f(inp['conv_w'].reshape(L, 3, 44, 128).transpose(0, 3, 2, 1).reshape(L, 128, 132))
    w['cbT'] = _pad64(inp['conv_b'].reshape(L, 44, 128).transpose(0, 2, 1))
    w['w_down'] = f(inp['w_down'])
    return w


W_SHAPES = {'w_ada': [2, 1024, 6144], 'b_adaT': [2, 128, 64], 'w_inA': [2, 1024, 512], 'w_inBf': [2, 1024, 1024],
            'w_inBt': [2, 1024, 512], 'w_inCf': [2, 1024, 1280], 'w_inCt': [2, 1024, 280], 'WsT': [2, 128, 512],
            'bsT': [2, 128, 64], 'a_lng': [2, 128, 256], 'a_lnb': [2, 128, 256], 'b_gng': [2, 128, 256], 'b_gnb': [2, 128, 256],
            'posT': [2, 128, 64], 'w1s': [2, 128, 2048], 'w2k': [2, 64, 128], 'w2v': [2, 64, 64],
            'w_out': [2, 1024, 1024], 'lnpk': [2, 128, 64],
            'w_up': [2, 1024, 5632], 'cwT': [2, 128, 132], 'cbT': [2, 128, 64], 'w_down': [2, 2816, 1024]}


def build(n_layers=DEPTH, mixers=('A', 'B', 'C'), do_ffn=True, dbg=None):
    nc = bass.Bass("TRN2", target_bir_lowering=False)
    Dr = {}
    Dr['x'] = nc.dram_tensor("x", [SEQ, DM], F32, kind="ExternalInput").ap()
    Dr['cT'] = nc.dram_tensor("cT", [128, 64], F32, kind="ExternalInput").ap()
    for k, shp in W_SHAPES.items():
        Dr[k] = nc.dram_tensor(k, shp, F32, kind="ExternalInput").ap()
    for k, shp in TABLE_SHAPES.items():
        Dr[k] = nc.dram_tensor(k, shp, F32, kind="ExternalInput").ap()
    out_d = nc.dram_tensor("out", [SEQ, DM], F32, kind="ExternalOutput").ap()
    dbg_d = None
    if dbg is not None:
        dbg_d = nc.dram_tensor("dbg", [SEQ, DM], F32, kind="ExternalOutput").ap()

    st = ExitStack()
    with st:
        S = Sched(nc, st)
        T = lambda name, shape, dt=F32: st.enter_context(nc.sbuf_tensor("s_" + name, shape, dt))
        xT = T("xT", [128, 8, SEQ], F32)
        hT = T("hT", [128, 8, SEQ], BF16)
        WR = T("WR", [128, 4, 4096], BF16)
        AW = 51 * 256 - 128
        ARENA = T("ARENA", [128, AW], F32)
        PSA = st.enter_context(nc.psum_tensor("PSA", [128, 2048], F32))
        PSB = st.enter_context(nc.psum_tensor("PSB", [128, 2048], F32))

        def bank(i):
            t = PSA if i < 4 else PSB
            return t[:, (i % 4) * 512:(i % 4 + 1) * 512]

        BK = ['B%d' % i for i in range(8)]

        class Arena:
            def __init__(self):
                self.off = 0

            def reset(self, off=0):
                self.off = off

            def get(self, shape, dt=F32):
                n = int(np.prod(shape))
                nb = n * (2 if dt == BF16 else 4)
                w0 = self.off // 4
                w1 = w0 + (nb + 3) // 4
                assert w1 <= AW, ("arena overflow", w1 * 4, AW * 4)
                self.off = w1 * 4
                ap = ARENA[:, w0:w1]
                if dt == BF16:
                    ap = ap.bitcast(BF16)
                ap = ap[:, 0:n]
                if len(shape) == 2:
                    ap = ap.rearrange("p (a b) -> p a b", a=shape[0])
                elif len(shape) == 3:
                    ap = ap.rearrange("p (a b c) -> p a b c", a=shape[0], b=shape[1])
                return ap

        AR = Arena()

        def MM(out, lhsT, rhs, start, stop, rd, wr):
            S.op('pe', lambda e: e.matmul(out, lhsT=lhsT, rhs=rhs, start=start, stop=stop, skip_group_check=True), rd, wr)

        def TR(out, in_, ident, rd, wr):
            S.op('pe', lambda e: e.transpose(out, in_, ident), rd, wr)

        def ACT(out, in_, func, rd, wr, scale=1.0, bias=None):
            if bias is None:
                S.op('act', lambda e: e.activation(out, in_, func, scale=scale), rd, wr)
            else:
                S.op('act', lambda e: e.activation(out, in_, func, bias=bias, scale=scale), rd, wr)

        def TT(out, in0, in1, op, rd, wr, eng='dve'):
            S.op(eng, lambda e: e.tensor_tensor(out, in0, in1, op), rd, wr)

        def TS(out, in0, s1, s2, op0, op1, rd, wr, eng='dve'):
            if s2 is None:
                S.op(eng, lambda e: e.tensor_scalar(out, in0, s1, None, op0=op0), rd, wr)
            else:
                S.op(eng, lambda e: e.tensor_scalar(out, in0, s1, s2, op0=op0, op1=op1), rd, wr)

        def STT(out, in0, scalar, in1, op0, op1, rd, wr):
            S.op('dve', lambda e: e.scalar_tensor_tensor(out, in0, scalar, in1, op0=op0, op1=op1), rd, wr)

        def CP(out, in_, rd, wr, eng='dve'):
            if eng == 'act':
                S.op('act', lambda e: e.activation(out, in_, AF.Identity), rd, wr)
            else:
                S.op(eng, lambda e: e.tensor_copy(out, in_), rd, wr)

        def RED(out, in_, op, rd, wr):
            S.op('dve', lambda e: e.tensor_reduce(out, in_, axis=AX.X, op=op), rd, wr)

        def RCP(out, in_, rd, wr):
            S.op('dve', lambda e: e.reciprocal(out, in_), rd, wr)

        evt = [0]

        def EV():
            evt[0] += 1
            return 'act' if evt[0] % 2 else 'dve'

        def DMA(q, out, in_, rd, wr):
            pieces = []

            def split(o, i):
                shp = tuple(o.shape)
                assert tuple(i.shape) == shp, (shp, i.shape)
                if len(shp) == 3:
                    for a in range(shp[1]):
                        split(o[:, a, :], i[:, a, :])
                elif len(shp) == 2 and shp[1] > 512:
                    for c0 in range(0, shp[1], 512):
                        c1 = min(shp[1], c0 + 512)
                        pieces.append((o[:, c0:c1], i[:, c0:c1]))
                else:
                    pieces.append((o, i))
            split(out, in_)
            S.dma_multi(q, pieces, rd, wr)

        ident_f = T("ident_f", [128, 128], F32)
        ident_b = T("ident_b", [128, 128], BF16)
        onesm = T("onesm", [128, 128], BF16)
        epsA = T("epsA", [128, 2], F32)
        condT = T("condT", [128, 64], F32)
        condTb = T("condTb", [128, 8, 8], BF16)
        badaT = [T("badaT%d" % l_, [128, 64], F32) for l_ in range(DEPTH)]
        modT = [T("modT%d" % l, [128, 48], F32) for l in range(DEPTH)]
        drv = [T("drv%d" % l, [128, 64], F32) for l in range(DEPTH)]
        lnp = [T("lnp%d" % l, [128, 64], F32) for l in range(DEPTH)]
        cwT = T("cwT", [128, 132], F32)
        cbT = T("cbT", [128, 64], F32)
        decayT = T("decayT", [128, 512], F32)
        xiT = T("xiT", [128, 256], F32)
        zeta = T("zeta", [128, 64], F32)
        cdv = T("cdv", [128, 64], F32)
        expand = T("expand", [128, 2048], BF16)
        causalb = T("causalb", [128, 128], BF16)
        winb = T("winb", [128, 128], BF16)
        cmpb = T("cmpb", [128, 2048], BF16)
        fbias = T("fbias", [128, 512], F32)
        causal01 = T("causal01", [128, 128], F32)
        ovl = T("ovl", [128, 64], F32)
        WsT = T("WsT", [128, 512], BF16)
        bsT = T("bsT", [128, 64], F32)
        lnbc = T("lnbc", [128, 4, 256], F32)
        posT = T("posT", [128, 64], F32)
        w1s = T("w1s", [128, 2048], BF16)
        w2k = T("w2k", [64, 128], BF16)
        w2v = T("w2v", [64, 64], BF16)

        DMA('sp', ident_f[:], Dr['identf'], (), ['ident_f'])
        S.op('dve', lambda e: e.tensor_copy(ident_b[:], ident_f[:]), ['ident_f'], ['ident_b'])
        S.op('dve', lambda e: e.memset(onesm[:], 1.0 / 1024.0), (), ['onesm'])
        S.op('dve', lambda e: e.memset(epsA[:, 0:1], LN_EPS), (), ['epsA'])
        S.op('dve', lambda e: e.memset(epsA[:, 1:2], LN_EPS / (ALPHA * ALPHA)), (), ['epsA'])
        barscr = T("barscr", [128, 64], F32)
        RELAY = (barscr[:], Dr['zeta'])
        rlscr = T("rlscr", [128, 2], F32)
        S.relay_fn = lambda e: e.memset(rlscr[:, 0:1], 0.0)
        DMA('sp', condT[:], Dr['cT'], (), ['condT'])
        for nm, tl in (('decayT', decayT), ('xiT', xiT), ('zeta', zeta), ('cdv', cdv), ('fbias', fbias), ('causal01', causal01), ('ovl', ovl)):
            DMA('sp', tl[:], Dr[nm], (), [nm])
        for nm, tl in (('causalb', causalb), ('winb', winb)):
            DMA('pool', tl[:], Dr[nm], (), [nm])
        for nm, tl in (('expand', expand), ('cmpb', cmpb)):
            DMA('pool', tl[:].rearrange("p (a b) -> p a b", a=4), Dr[nm].rearrange("p (a b) -> p a b", a=4), (), [nm])
        ACT(condT[:], condT[:], AF.Silu, ['condT'], ['condT'])

        wr_n = [0]

        def wload(src_ap, shape):
            S.relay_readers(['WR%d' % j_ for j_ in range(4)])
            i = wr_n[0] % 4
            wr_n[0] += 1
            n = int(np.prod(shape))
            v = WR[:, i, 0:n]
            if len(shape) == 2:
                v = v.rearrange("p (a b) -> p a b", a=shape[0])
            key = 'WR%d' % i
            DMA('pool', v, src_ap, (), [key])
            return v, key

        def kchunks(ap2d):
            return ap2d.rearrange("(k p) n -> p k n", p=128)

        AR.reset()
        ada_buf = [AR.get([8, 512], BF16) for _ in range(2)]
        CP(condTb[:], condT[:, 0:8].unsqueeze(2).to_broadcast([128, 8, 8]), ['condT'], ['condTb'])
        xst = [AR.get([1024], F32) for _ in range(8)]
        bi = 0
        for tb in range(4):
            for q in range(4):
                tt = tb * 4 + q
                DMA('sp', xst[tt % 8], Dr['x'][tt * 128:(tt + 1) * 128, :], (), ['xst%d' % (tt % 8)])
            for c in range(8):
                b = 1 + bi % 7
                bi += 1
                for q in range(4):
                    tt = tb * 4 + q
                    TR(bank(b)[:, q * 128:(q + 1) * 128], xst[tt % 8][:, c * 128:(c + 1) * 128], ident_f[:],
                       ['xst%d' % (tt % 8), 'ident_f'], [BK[b]])
                CP(xT[:, c, tb * 512:(tb + 1) * 512], bank(b), [BK[b]], ['xT%d_%d' % (c, tb)], eng=EV())
        for l in range(n_layers):
            DMA('sp', badaT[l][:], Dr['b_adaT'][l], (), ['bada%d' % l])
            DMA('sp', lnp[l][:], Dr['lnpk'][l], (), ['lnp%d' % l])
        for l in range(min(1, n_layers)):
            for blk in range(12):
                buf = ada_buf[blk % 2]
                bk = 'ada%d' % (blk % 2)
                DMA('pool', buf, kchunks(Dr['w_ada'][l][:, blk * 512:(blk + 1) * 512]), (), [bk])
                for jj in range(4):
                    j = blk * 4 + jj
                    for k in range(8):
                        MM(bank(0)[:, j * 8:j * 8 + 8], buf[:, k, jj * 128:(jj + 1) * 128], condTb[:, k, :],
                           k == 0, k == 7, [bk, 'condTb'], ['B0'])
            TT(modT[l][:], bank(0)[:, 0:384].rearrange('p (j r) -> p j r', r=8)[:, :, 0], badaT[l][:, 0:48], ALU.add, ['B0', 'bada%d' % l], ['modT%d' % l])

        def derive(l):
            m = modT[l]
            d = drv[l]
            mk, dk = 'modT%d' % l, 'drv%d' % l
            TS(d[:, 0:8], m[:, 8:16], 1.0, None, ALU.add, None, [mk], [dk])
            TS(d[:, 8:16], m[:, 16:24], 1.0 / ALPHA, None, ALU.mult, None, [mk], [dk])
            TS(d[:, 16:24], m[:, 32:40], 1.0, None, ALU.add, None, [mk], [dk])
            TS(d[:, 24:32], m[:, 40:48], 1.0 / ALPHA, None, ALU.mult, None, [mk], [dk])
            TT(d[:, 32:40], lnp[l][:, 0:8], d[:, 16:24], ALU.mult, ['lnp%d' % l, dk], [dk])
            TT(d[:, 40:48], lnp[l][:, 8:16], d[:, 16:24], ALU.mult, ['lnp%d' % l, dk], [dk])
            TT(d[:, 40:48], d[:, 40:48], m[:, 24:32], ALU.add, [dk, mk], [dk])

        def derive_cross(l):
            d, dn = drv[l], drv[l + 1]
            dk, dnk = 'drv%d' % l, 'drv%d' % (l + 1)
            TT(d[:, 48:56], lnp[l][:, 16:24], dn[:, 0:8], ALU.mult, ['lnp%d' % l, dnk, dk], [dk])
            TT(d[:, 56:64], lnp[l][:, 24:32], dn[:, 0:8], ALU.mult, ['lnp%d' % l, dnk, dk], [dk])
            TT(d[:, 56:64], d[:, 56:64], modT[l + 1][:, 0:8], ALU.add, [dk, 'modT%d' % (l + 1)], [dk])

        def ada_block_deferred(l, blk):
            wv_, wk_ = wload(kchunks(Dr['w_ada'][l][:, blk * 512:(blk + 1) * 512]), [8, 512])
            for jj in range(4):
                for k in range(8):
                    MM(bank(7)[:, jj * 8:jj * 8 + 8], wv_[:, k, jj * 128:(jj + 1) * 128], condTb[:, k, :],
                       k == 0, k == 7, [wk_, 'condTb'], ['B7'])
            TT(modT[l][:, blk * 4:(blk + 1) * 4], bank(7)[:, 0:32].rearrange('p (j r) -> p j r', r=8)[:, :, 0],
               badaT[l][:, blk * 4:(blk + 1) * 4], ALU.add, ['B7', 'bada%d' % l], ['modT%d' % l])
            if blk == 11:
                derive(l)
                derive_cross(l - 1)

        if n_layers >= 1:
            derive(0)

        for tb in range(4):
            for c in range(8):
                xk = 'xT%d_%d' % (c, tb)
                if (tb * 8 + c) % 2 == 0:
                    TS(hT[:, c, tb * 512:(tb + 1) * 512], xT[:, c, tb * 512:(tb + 1) * 512], drv[0][:, c:c + 1], modT[0][:, c:c + 1],
                       ALU.mult, ALU.add, [xk, 'drv0', 'modT0'], ['hT%d_%d' % (c, tb)])
                else:
                    ACT(hT[:, c, tb * 512:(tb + 1) * 512], xT[:, c, tb * 512:(tb + 1) * 512], AF.Identity,
                        [xk, 'drv0', 'modT0'], ['hT%d_%d' % (c, tb)], scale=drv[0][:, c:c + 1], bias=modT[0][:, c:c + 1])

        def out_proj(l, mixtm, mixkeys, nch, row0, mixT, alias=()):
            nonlocal_bi = [0]
            for c in range(nch):
                for tb in range(4):
                    b = 4 + (nonlocal_bi[0] % 4)
                    nonlocal_bi[0] += 1
                    pb = bank(b).bitcast(BF16)
                    for q in range(4):
                        tt = tb * 4 + q
                        TR(pb[:, q * 128:(q + 1) * 128], mixtm[:, tt, c * 128:(c + 1) * 128], ident_b[:],
                           [mixkeys[tt], 'ident_b'], [BK[b]])
                    CP(mixT[:, c, tb * 512:(tb + 1) * 512], pb[:, 0:512], [BK[b]], ['mixT%d_%d' % (c, tb)] + list(alias), eng=EV())
            wv, wk = wload(kchunks(Dr['w_out'][l][row0:row0 + nch * 128, :]), [nch, 1024])
            for fb in range(8):
                for tb in range(4):
                    b = nonlocal_bi[0] % 4
                    nonlocal_bi[0] += 1
                    for c in range(nch):
                        MM(bank(b), wv[:, c, fb * 128:(fb + 1) * 128], mixT[:, c, tb * 512:(tb + 1) * 512],
                           c == 0, c == nch - 1, [wk, 'mixT%d_%d' % (c, tb)], [BK[b]])
                    xs = xT[:, fb, tb * 512:(tb + 1) * 512]
                    STT(xs, bank(b), drv[l][:, 8 + fb:9 + fb], xs, ALU.mult, ALU.add,
                        [BK[b], 'drv%d' % l, 'xT%d_%d' % (fb, tb)], ['xT%d_%d' % (fb, tb)])

        def layer_norm(l, which, last):
            goff = 0 if which == 1 else 16
            aoff = 32 if which == 1 else 48
            AR.reset()
            xb = [AR.get([512], BF16) for _ in range(3)]
            sq = [AR.get([512], BF16) for _ in range(3)]
            rstd = [AR.get([512], F32) for _ in range(2)]
            nmr = [AR.get([512], F32) for _ in range(2)]
            tmp = [AR.get([512], F32) for _ in range(3)]
            n1 = [0]
            n3 = [0]

            def p_stats(tb):
                bm, be = 0 + 2 * (tb % 2), 1 + 2 * (tb % 2)
                for c in range(8):
                    i = n1[0] % 3
                    n1[0] += 1
                    xs = xT[:, c, tb * 512:(tb + 1) * 512]
                    xk = 'xT%d_%d' % (c, tb)
                    ACT(sq[i], xs, AF.Square, [xk], ['lsq%d' % i])
                    CP(xb[i], xs, [xk], ['lxb%d' % i], eng='dve')
                    MM(bank(bm), onesm[:], xb[i], c == 0, c == 7, ['onesm', 'lxb%d' % i], [BK[bm]])
                    MM(bank(be), onesm[:], sq[i], c == 0, c == 7, ['onesm', 'lsq%d' % i], [BK[be]])

            def p_apply(tb):
                bm, be = 0 + 2 * (tb % 2), 1 + 2 * (tb % 2)
                r = tb % 2
                rk, nk = 'lrstd%d' % r, 'lnmr%d' % r
                ACT(nmr[r], bank(bm), AF.Square, [BK[bm]], [nk])
                TT(rstd[r], bank(be), nmr[r], ALU.subtract, [BK[be], nk], [rk])
                TS(rstd[r], rstd[r], 0.0, None, ALU.max, None, [rk], [rk])
                ACT(rstd[r], rstd[r], AF.Sqrt, [rk, 'epsA'], [rk], bias=epsA[:, 1:2])
                RCP(rstd[r], rstd[r], [rk], [rk])
                STT(nmr[r], bank(bm), -1.0, rstd[r], ALU.mult, ALU.mult, [BK[bm], rk], [nk])
                for c in range(8):
                    i = n3[0] % 3
                    n3[0] += 1
                    xs = xT[:, c, tb * 512:(tb + 1) * 512]
                    xk = 'xT%d_%d' % (c, tb)
                    tk = 'ltmp%d' % i
                    TT(tmp[i], xs, rstd[r], ALU.mult, [xk, rk], [tk])
                    TT(tmp[i], tmp[i], nmr[r], ALU.add, [tk, nk], [tk])
                    ACT(xs, tmp[i], AF.Identity, [tk, 'lnp%d' % l], [xk],
                        scale=lnp[l][:, goff + c:goff + c + 1], bias=lnp[l][:, goff + 8 + c:goff + 9 + c])
                    if not last:
                        ACT(hT[:, c, tb * 512:(tb + 1) * 512], tmp[i], AF.Identity, [tk, 'drv%d' % l], ['hT%d_%d' % (c, tb)],
                            scale=drv[l][:, aoff + c:aoff + c + 1], bias=drv[l][:, aoff + 8 + c:aoff + 9 + c])

            p_stats(0)
            for tb in range(4):
                if tb + 1 < 4:
                    p_stats(tb + 1)
                p_apply(tb)

        def dump_tm(src, c0, ncol, keys, dcol):
            dst = AR.get([ncol], F32)
            for tt in range(16):
                CP(dst, src[:, tt, c0:c0 + ncol], [keys[tt]], ['dst'])
                DMA('sp', dbg_d[tt * 128:(tt + 1) * 128, dcol:dcol + ncol], dst, ['dst'], ['dbg'])

        def group_ln_core(src, sqv, nb, rk, wk_sq, eps_ap):
            s1 = AR.get([nb], F32)
            s2 = AR.get([nb], F32)
            s3 = AR.get([nb], F32)
            TT(sqv, src, src, ALU.mult, rk, [wk_sq])
            RED(s1, src, ALU.add, rk, ['gs1'])
            RED(s2, sqv, ALU.add, [wk_sq], ['gs2'])
            TS(s1, s1, 1.0 / 64, None, ALU.mult, None, ['gs1'], ['gs1'])
            TT(s3, s1, s1, ALU.mult, ['gs1'], ['gs3'])
            STT(s2, s2, 1.0 / 64, s3, ALU.mult, ALU.subtract, ['gs2', 'gs3'], ['gs2'])
            TS(s2, s2, 0.0, None, ALU.max, None, ['gs2'], ['gs2'])
            ACT(s2, s2, AF.Sqrt, ['gs2', 'epsA'], ['gs2'], bias=eps_ap)
            RCP(s2, s2, ['gs2'], ['gs2'])
            TT(sqv, src, s1.unsqueeze(2).to_broadcast([128, nb, 64]), ALU.subtract, rk + ['gs1'], [wk_sq])
            TT(sqv, sqv, s2.unsqueeze(2).to_broadcast([128, nb, 64]), ALU.mult, [wk_sq, 'gs2'], [wk_sq])

        def mixer_B(l):
            hk = lambda k, tb: 'hT%d_%d' % (k, tb)
            S.barrier()
            AR.reset()
            qrT = AR.get([2, 2048], BF16)
            krT = AR.get([2, 2048], BF16)
            off_x = AR.off
            cosT = AR.get([2048], F32)
            sinT = AR.get([2048], F32)
            rt1 = [AR.get([512], F32) for _ in range(2)]
            rt2 = [AR.get([512], F32) for _ in range(2)]
            DMA('sp', cosT, Dr['cosT'], (), ['cosT'])
            DMA('sp', sinT, Dr['sinT'], (), ['sinT'])
            DMA('sp', lnbc[:, 2, :], Dr['b_gng'][l], (), ['lnbc'])
            DMA('sp', lnbc[:, 3, :], Dr['b_gnb'][l], (), ['lnbc'])
            n = 0
            for p in range(2):
                wv, wk = wload(kchunks(Dr['w_inBf'][l][:, p * 512:(p + 1) * 512]), [8, 512])
                for kind in range(2):
                    dst = qrT if kind == 0 else krT
                    for tb in range(4):
                        i = n % 2
                        b1, b2 = 2 * (n % 4), 2 * (n % 4) + 1
                        n += 1
                        for k in range(8):
                            MM(bank(b1), wv[:, k, (2 * kind) * 128:(2 * kind + 1) * 128], hT[:, k, tb * 512:(tb + 1) * 512],
                               k == 0, k == 7, [wk, hk(k, tb)], [BK[b1]])
                        for k in range(8):
                            MM(bank(b2), wv[:, k, (2 * kind + 1) * 128:(2 * kind + 2) * 128], hT[:, k, tb * 512:(tb + 1) * 512],
                               k == 0, k == 7, [wk, hk(k, tb)], [BK[b2]])
                        TT(rt1[i], bank(b1), cosT[:, tb * 512:(tb + 1) * 512], ALU.mult, [BK[b1], 'cosT'], ['rt1_%d' % i])
                        TT(rt2[i], bank(b2), sinT[:, tb * 512:(tb + 1) * 512], ALU.mult, [BK[b2], 'sinT'], ['rt2_%d' % i])
                        TT(dst[:, p, tb * 512:(tb + 1) * 512], rt1[i], rt2[i], ALU.add, ['rt1_%d' % i, 'rt2_%d' % i],
                           ['qk%d_%d' % (kind, p)])
            wvt, wkt = wload(kchunks(Dr['w_inBt'][l]), [8, 512])
            for p in range(2):
                S.barrier()
                AR.reset(off_x)
                vg = AR.get([16, 256], BF16)
                kvs = AR.get([16, 128], F32)
                s16 = AR.get([16, 128], BF16)
                oall = AR.get([16, 128], F32)
                kz = [AR.get([128], BF16) for _ in range(2)]
                qx = [AR.get([128], BF16) for _ in range(2)]
                sT = [AR.get([256], BF16) for _ in range(2)]
                mixT = AR.get([1, 2048], BF16)
                qkk = ['qk0_%d' % p, 'qk1_%d' % p]
                for tt in range(16):
                    b = tt % 4
                    for k in range(8):
                        MM(bank(b)[:, 0:256], hT[:, k, tt * 128:(tt + 1) * 128], wvt[:, k, p * 256:(p + 1) * 256],
                           k == 0, k == 7, [hk(k, tt // 4), wkt], [BK[b]])
                    CP(vg[:, tt, 0:128], bank(b)[:, 0:128], [BK[b]], ['vg%d' % tt], eng='dve')
                    ACT(vg[:, tt, 128:256], bank(b)[:, 128:256], AF.Silu, [BK[b]], ['vg%d' % tt])
                S.op('dve', lambda e: e.memset(kvs[:, 0, :], 0.0), (), ['kvs'])
                def st_a(c):
                    i = c % 2
                    bt = 4 + (c % 2)
                    pb = bank(bt).bitcast(BF16)
                    TR(pb[:, 0:128], krT[:, p, c * 128:(c + 1) * 128], ident_b[:], [qkk[1], 'ident_b'], [BK[bt]])
                    TT(kz[i].rearrange("p (h d) -> p h d", h=2), pb[:, 0:128].rearrange("p (h d) -> p h d", h=2),
                       zeta[:, 2 * p:2 * p + 2].unsqueeze(2).to_broadcast([128, 2, 64]), ALU.mult,
                       [BK[bt], 'zeta'], ['kz%d' % i])

                def st_b(c):
                    i = c % 2
                    bm = 6 + (c % 2)
                    MM(bank(bm)[:, 0:128], kz[i], vg[:, c, 0:128], True, True, ['kz%d' % i, 'vg%d' % c], [BK[bm]])
                    STT(kvs[:, c + 1, :], kvs[:, c, :], cdv[:, p:p + 1], bank(bm)[:, 0:128], ALU.mult, ALU.add,
                        ['kvs', 'cdv', BK[bm]], ['kvs'])

                st_a(0)
                for c in range(15):
                    if c + 1 < 15:
                        st_a(c + 1)
                    st_b(c)
                CP(s16, kvs, ['kvs'], ['s16'], eng='dve')
                def b_scores(c):
                    i = c % 2
                    bs0 = 2 * (c % 2)
                    cs = slice(c * 128, (c + 1) * 128)
                    if c > 0:
                        TT(qx[i], qrT[:, p, cs], xiT[:, p * 128:(p + 1) * 128], ALU.mult, [qkk[0], 'xiT'], ['qx%d' % i])
                    for hh in range(2):
                        ps_ = slice(hh * 64, (hh + 1) * 64)
                        MM(bank(bs0 + hh)[:, 0:128], krT[ps_, p, cs], qrT[ps_, p, cs], True, True,
                           qkk, [BK[bs0 + hh]])

                def b_rest(c):
                    i = c % 2
                    bs0 = 2 * (c % 2)
                    bo0 = 4 + 2 * (c % 2)
                    TT(sT[i].rearrange("p (h l) -> p h l", h=2),
                       PSA[:, bs0 * 512:(bs0 + 2) * 512].rearrange("p (h x) -> p h x", h=2)[:, :, 0:128],
                       decayT[:, 2 * p * 128:(2 * p + 2) * 128].rearrange("p (h l) -> p h l", h=2), ALU.mult,
                       [BK[bs0], BK[bs0 + 1], 'decayT'], ['sT%d' % i])
                    for hh in range(2):
                        ps_ = slice(hh * 64, (hh + 1) * 64)
                        MM(bank(bo0 + hh)[:, 0:64], sT[i][:, hh * 128:(hh + 1) * 128], vg[:, c, hh * 64:(hh + 1) * 64],
                           True, c == 0, ['sT%d' % i, 'vg%d' % c], [BK[bo0 + hh]])
                        if c > 0:
                            MM(bank(bo0 + hh)[:, 0:64], qx[i][ps_, :], s16[ps_, c, hh * 64:(hh + 1) * 64],
                               False, True, ['qx%d' % i, 's16'], [BK[bo0 + hh]])
                    CP(oall[:, c, :].rearrange("p (h e) -> p h e", h=2),
                       PSB[:, (bo0 - 4) * 512:(bo0 - 2) * 512].rearrange("p (h x) -> p h x", h=2)[:, :, 0:64],
                       [BK[bo0], BK[bo0 + 1]], ['oall'], eng='act')

                b_scores(0)
                for c in range(16):
                    if c + 1 < 16:
                        b_scores(c + 1)
                    b_rest(c)
                o3 = oall.rearrange("p c (h e) -> p (c h) e", h=2)
                sq3 = kvs.rearrange("p c (h e) -> p (c h) e", h=2)
                gam_b = lnbc[:, 2, p * 128:(p + 1) * 128].rearrange("p (h e) -> p h e", h=2).unsqueeze(1).to_broadcast([128, 16, 2, 64])
                bet_b = lnbc[:, 3, p * 128:(p + 1) * 128].rearrange("p (h e) -> p h e", h=2).unsqueeze(1).to_broadcast([128, 16, 2, 64])
                sq4 = kvs.rearrange("p c (h e) -> p c h e", h=2)
                group_ln_core(o3, sq3, 32, ['oall'], 'kvs', epsA[:, 0:1])
                TT(sq4, sq4, gam_b, ALU.mult, ['kvs', 'lnbc'], ['kvs'])
                TT(sq4, sq4, bet_b, ALU.add, ['kvs', 'lnbc'], ['kvs'])
                vkeys = ['vg%d' % tt for tt in range(16)]
                TT(vg[:, :, 0:128], kvs, vg[:, :, 128:256], ALU.mult, ['kvs'] + vkeys, vkeys)
                if dbg == 'mixB%d_%d' % (l, p):
                    dump_tm(vg, 0, 128, vkeys, p * 128)
                out_proj(l, vg, vkeys, 1, 256 + p * 128, mixT)

        def mixer_C(l):
            hk = lambda k, tb: 'hT%d_%d' % (k, tb)
            DMA('pool', w1s[:].rearrange("p (a b) -> p a b", a=4), Dr['w1s'][l].rearrange("p (a b) -> p a b", a=4), (), ['w1s'])
            DMA('pool', w2k[:], Dr['w2k'][l], (), ['w2k'])
            DMA('pool', w2v[:], Dr['w2v'][l], (), ['w2v'])
            DMA('sp', posT[:], Dr['posT'][l], (), ['posT'])
            for g in range(2):
                S.barrier()
                AR.reset()
                qz = AR.get([4, 2048], BF16)
                KsT = AR.get([2048], BF16)
                KwT = AR.get([2048], BF16)
                Vaug = AR.get([16, 2, 65], BF16)
                gates = AR.get([16, 12], F32)
                mixC = AR.get([16, 256], BF16)
                kcmpT = AR.get([127], BF16)
                vcaug = AR.get([97], BF16)
                hidT = AR.get([2, 127], BF16)
                off_r = AR.off
                kvcT = AR.get([2048], BF16)
                kcp = AR.get([32, 127], BF16)
                wv, wk = wload(kchunks(Dr['w_inCf'][l][:, g * 640:g * 640 + 512]), [8, 512])
                wv2, wk2 = wload(kchunks(Dr['w_inCf'][l][:, g * 640 + 512:g * 640 + 640]), [8, 128])
                S.op('dve', lambda e: e.memset(qz, 0.0), (), ['qT'])
                dsts = [(None, 'qT', 0.125), (None, 'qT', 0.125), (KsT, 'KsT', 1.0), (KwT, 'KwT', 1.0),
                        (kvcT, 'kvcT', 1.0)]
                n = 0
                for bi_, (dst, dk, scl) in enumerate(dsts):
                    for tb in range(4):
                        b = n % 4
                        n += 1
                        for k in range(8):
                            lw = wv[:, k, bi_ * 128:(bi_ + 1) * 128] if bi_ < 4 else wv2[:, k, :]
                            MM(bank(b), lw, hT[:, k, tb * 512:(tb + 1) * 512], k == 0, k == 7,
                               [wk if bi_ < 4 else wk2, hk(k, tb)], [BK[b]])
                        if dst is None:
                            ACT(qz[0:64, 2 * bi_, tb * 512:(tb + 1) * 512], bank(b)[0:64, :], AF.Identity, [BK[b]], [dk], scale=scl)
                            TS(qz[64:128, 2 * bi_ + 1, tb * 512:(tb + 1) * 512], bank(b)[64:128, :], scl, None, ALU.mult, None, [BK[b]], [dk])
                        elif n % 2:
                            ACT(dst[:, tb * 512:(tb + 1) * 512], bank(b), AF.Identity, [BK[b]], [dk], scale=scl)
                        else:
                            TS(dst[:, tb * 512:(tb + 1) * 512], bank(b), scl, None, ALU.mult, None, [BK[b]], [dk])
                wv3, wk3 = wload(kchunks(Dr['w_inCt'][l][:, g * 140:(g + 1) * 140]), [8, 140])
                S.op('dve', lambda e: e.memset(Vaug[:, :, :, 64:65], 1.0), (), ['Vaug'])
                for tt in range(16):
                    b = 4 + tt % 4
                    for k in range(8):
                        MM(bank(b)[:, 0:140], hT[:, k, tt * 128:(tt + 1) * 128], wv3[:, k, :], k == 0, k == 7,
                           [hk(k, tt // 4), wk3], [BK[b]])
                    CP(Vaug[:, tt, :, 0:64], bank(b)[:, 0:128].rearrange("p (s d) -> p s d", s=2), [BK[b]], ['Vaug'], eng='dve')
                    ACT(gates[:, tt, :], bank(b)[:, 128:140], AF.Sigmoid, [BK[b]], ['gates'])
                kv_t = kvcT.tensor
                win = bass.AP(kv_t, kvcT.offset, [list(kvcT.ap[0]), [1, 32], [16, 127]])
                TT(kcp, win, posT[:, 0:32].unsqueeze(2).to_broadcast([128, 32, 127]), ALU.add, ['kvcT', 'posT'], ['kcp'])
                for kv in range(2):
                    ps_ = slice(kv * 64, (kv + 1) * 64)
                    for ll in range(32):
                        MM(bank(kv)[0:64, 0:127], w1s[ps_, ll * 64:(ll + 1) * 64], kcp[ps_, ll, :],
                           ll == 0, ll == 31, ['w1s', 'kcp'], [BK[kv]])
                    ACT(hidT[0:64, kv, :], bank(kv)[0:64, 0:127], AF.Gelu_apprx_tanh, [BK[kv]], ['hidT'])
                MM(bank(2)[:, 0:127], w2k[:], hidT[0:64, 0, :], True, True, ['w2k', 'hidT'], ['B2'])
                CP(kcmpT, bank(2)[:, 0:127], ['B2'], ['kcmpT'], eng='dve')
                MM(bank(3)[0:127, 0:64], hidT[0:64, 1, :], w2v[:], True, True, ['w2v', 'hidT'], ['B3'])
                S.op('dve', lambda e: e.memset(vcaug[:, 64:65], 1.0), (), ['vcaug'])
                CP(vcaug[0:127, 0:64], bank(3)[0:127, 0:64], ['B3'], ['vcaug'], eng='dve')
                CP(vcaug[:, 65:97], ovl[:, 0:32], ['ovl'], ['vcaug'], eng='dve')
                S.barrier()
                AR.reset(off_r)
                PT = [AR.get([512], BF16) for _ in range(4)]
                MbT = [AR.get([128], BF16) for _ in range(2)]
                for j_ in range(2):
                    S.op('dve', (lambda t_: (lambda e: e.memset(t_, 0.0)))(MbT[j_]), (), ['MbT%d' % j_])
                Mb = [AR.get([32], BF16) for _ in range(2)]
                ocg = [AR.get([4, 64], F32) for _ in range(2)]
                t1 = [AR.get([4, 64], F32) for _ in range(2)]
                t2 = [AR.get([4, 64], F32) for _ in range(2)]
                sm = [AR.get([96], F32) for _ in range(2)]
                impt = [AR.get([4, 32], F32) for _ in range(2)]
                pn = [0]

                def qslice(r, qt):
                    return qz[:, r, qt * 128:(qt + 1) * 128]

                def attend(qt, kts, KT, vsel, bS, bO, extra_fn):
                    def scores(ki):
                        kt = kts[ki]
                        bs_ = bS[ki % len(bS)]
                        extras = extra_fn(kt)
                        v4 = bank(bs_).rearrange("p (r t) -> p r t", r=4)
                        MM(v4, KT[:, kt * 128:(kt + 1) * 128], qz[:, :, qt * 128:(qt + 1) * 128],
                           True, not extras, ['KsT', 'KwT', 'qT'], [BK[bs_]])
                        for ei, (lh, rh, ks) in enumerate(extras):
                            MM(v4, lh, rh, False, ei == len(extras) - 1, ks, [BK[bs_]])

                    def rest(ki):
                        kt = kts[ki]
                        bs_ = bS[ki % len(bS)]
                        i = pn[0] % 4
                        pn[0] += 1
                        ACT(PT[i], bank(bs_), AF.Exp, [BK[bs_]], ['PT%d' % i])
                        for r in range(4):
                            MM(bank(bO)[:, r * 65:(r + 1) * 65], PT[i][:, r * 128:(r + 1) * 128], Vaug[:, kt, vsel, :],
                               ki == 0 and r == 0, ki == len(kts) - 1 and r == 3, ['PT%d' % i, 'Vaug'], [BK[bO]])

                    LA = 3
                    for ki in range(min(LA, len(kts))):
                        scores(ki)
                    for ki in range(len(kts)):
                        if ki + LA < len(kts):
                            scores(ki + LA)
                        rest(ki)

                for qt in range(16):
                    j = qt % 2
                    smj = sm[j]
                    sk = 'sm%d' % j
                    qs = slice(qt * 128, (qt + 1) * 128)
                    MM(bank(0)[0:127, :].rearrange("p (r t) -> p r t", r=4), kcmpT[:, :], qz[:, :, qs], True, False,
                       ['kcmpT', 'qT'], ['B0'])
                    MM(bank(0)[0:127, :].rearrange("p (r t) -> p r t", r=4), ident_b[0:127, 0:127],
                       cmpb[0:127, qs].unsqueeze(1).to_broadcast([127, 4, 128]), False, True, ['ident_b', 'cmpb'], ['B0'])
                    i = pn[0] % 4
                    pn[0] += 1
                    ACT(PT[i][0:127, :], bank(0)[0:127, :], AF.Exp, ['B0'], ['PT%d' % i])
                    for r in range(4):
                        MM(bank(1)[:, r * 97:(r + 1) * 97], PT[i][0:127, r * 128:(r + 1) * 128], vcaug[0:127, :],
                           r == 0, r == 3, ['PT%d' % i, 'vcaug'], ['B1'])
                    O = bank(1)[:, 0:388].rearrange("p (r c) -> p r c", r=4)
                    cb4 = causalb[:].unsqueeze(1).to_broadcast([128, 4, 128])
                    wb4 = winb[:].unsqueeze(1).to_broadcast([128, 4, 128])

                    def wmask(kt, qt=qt, cb4=cb4, wb4=wb4):
                        if kt == qt:
                            return [(ident_b[:], cb4, ['ident_b', 'causalb'])]
                        if kt == qt - 4:
                            return [(ident_b[:], wb4, ['ident_b', 'winb'])]
                        return []
                    attend(qt, list(range(max(0, qt - 4), qt + 1)), KwT, 1, [2, 3, 6, 7], 5, wmask)
                    TS(smj[:, 0:4], O[:, :, 64], 1e-30, None, ALU.max, None, ['B1'], [sk])
                    RCP(smj[:, 4:8], smj[:, 0:4], [sk], [sk])
                    TT(impt[j], O[:, :, 65:97], smj[:, 4:8].unsqueeze(2).to_broadcast([128, 4, 32]), ALU.mult, ['B1', sk], ['impt%d' % j])
                    RED(smj[:, 32:64], impt[j].rearrange("p r c -> p c r"), ALU.add, ['impt%d' % j], [sk])
                    TT(smj[:, 32:64], smj[:, 32:64], fbias[:, qt * 32:(qt + 1) * 32], ALU.add, [sk, 'fbias'], [sk])
                    S.op('dve', (lambda o_, i_: (lambda e: e.max(o_, i_)))(smj[:, 64:72], smj[:, 32:64]), [sk], [sk])
                    TS(Mb[j], smj[:, 32:64], smj[:, 71:72], -1.0, ALU.is_ge, ALU.add, [sk], ['Mb%d' % j])
                    pbt = bank(0).bitcast(BF16)
                    TR(pbt[0:32, 0:128], Mb[j], ident_b[:], ['Mb%d' % j, 'ident_b'], ['B0'])
                    CP(MbT[j][0:32, :], pbt[0:32, 0:128], ['B0'], ['MbT%d' % j], eng='dve')
                    gv = gates[:, qt, :].rearrange("p (r c) -> p r c", r=4)
                    TT(smj[:, 8:12], smj[:, 4:8], gv[:, :, 0], ALU.mult, [sk, 'gates'], [sk])
                    TT(ocg[j], O[:, :, 0:64], smj[:, 8:12].unsqueeze(2).to_broadcast([128, 4, 64]), ALU.mult, ['B1', sk], ['ocg%d' % j])

                    mb4 = MbT[j][:, :].unsqueeze(1).to_broadcast([128, 4, 128])

                    def smask(kt, qt=qt, j=j, cb4=cb4, mb4=mb4):
                        ex = [(expand[:, kt * 128:(kt + 1) * 128], mb4, ['expand', 'MbT%d' % j])]
                        if kt == qt:
                            ex.append((ident_b[:], cb4, ['ident_b', 'causalb']))
                        return ex
                    attend(qt, list(range(0, qt + 1)), KsT, 0, [2, 3, 6, 7], 4, smask)
                    Os = bank(4)[:, 0:260].rearrange("p (r c) -> p r c", r=4)
                    Ow = bank(5)[:, 0:260].rearrange("p (r c) -> p r c", r=4)
                    RCP(smj[:, 12:16], Os[:, :, 64], ['B4'], [sk])
                    TT(smj[:, 12:16], smj[:, 12:16], gv[:, :, 1], ALU.mult, [sk, 'gates'], [sk])
                    RCP(smj[:, 16:20], Ow[:, :, 64], ['B5'], [sk])
                    TT(smj[:, 16:20], smj[:, 16:20], gv[:, :, 2], ALU.mult, [sk, 'gates'], [sk])
                    TT(t1[j], Os[:, :, 0:64], smj[:, 12:16].unsqueeze(2).to_broadcast([128, 4, 64]), ALU.mult, ['B4', sk], ['t1_%d' % j])
                    TT(t2[j], Ow[:, :, 0:64], smj[:, 16:20].unsqueeze(2).to_broadcast([128, 4, 64]), ALU.mult, ['B5', sk], ['t2_%d' % j])
                    TT(t1[j], t1[j], ocg[j], ALU.add, ['t1_%d' % j, 'ocg%d' % j], ['t1_%d' % j])
                    TT(mixC[:, qt, :].rearrange("p (r d) -> p r d", r=4), t1[j], t2[j], ALU.add, ['t1_%d' % j, 't2_%d' % j], ['mixC%d' % qt])
                mkeys = ['mixC%d' % tt for tt in range(16)]
                if dbg == 'mixC%d_%d' % (l, g):
                    dump_tm(mixC, 0, 256, mkeys, g * 256)
                S.barrier()
                AR.reset(off_r)
                mixT = AR.get([2, 2048], BF16)
                out_proj(l, mixC, mkeys, 2, 512 + g * 256, mixT)

        for l in range(n_layers):
            hk = lambda k, tb: 'hT%d_%d' % (k, tb)
            if 'A' in mixers:
                S.barrier()
                AR.reset()
                zag = AR.get([16, 512], BF16)
                off_sqv = AR.off
                sqv = AR.get([16, 256], F32)
                st1 = AR.get([64], F32)
                st2 = AR.get([64], F32)
                st3 = AR.get([64], F32)
                mixA = AR.get([16, 256], BF16)
                atmp = [AR.get([256], F32) for _ in range(2)]
                wstage = AR.get([4, 128], F32)
                DMA('sp', wstage, Dr['WsT'][l].rearrange("p (g t) -> p g t", g=4), (), ['wstage'])
                TT(WsT[:].rearrange("p (g t) -> p g t", g=4), wstage,
                   causal01[:].unsqueeze(1).to_broadcast([128, 4, 128]), ALU.mult, ['wstage', 'causal01'], ['WsT'])
                DMA('sp', bsT[:], Dr['bsT'][l], (), ['bsT'])
                DMA('sp', lnbc[:, 0, :], Dr['a_lng'][l], (), ['lnbcA'])
                DMA('sp', lnbc[:, 1, :], Dr['a_lnb'][l], (), ['lnbcA'])
                wv, wk = wload(kchunks(Dr['w_inA'][l]), [8, 512])
                for tt in range(16):
                    b = tt % 4
                    for k in range(8):
                        MM(bank(b), hT[:, k, tt * 128:(tt + 1) * 128], wv[:, k, :], k == 0, k == 7,
                           [hk(k, tt // 4), wk], [BK[b]])
                    ACT(zag[:, tt, :], bank(b), AF.Gelu_apprx_tanh, [BK[b]], ['zag'])
                v4 = zag[:, :, 256:512].rearrange("p t (g d) -> p t g d", g=4)
                TT(sqv, zag[:, :, 256:512], zag[:, :, 256:512], ALU.mult, ['zag'], ['sqv'])
                RED(st1.rearrange("p (t g) -> p t g", t=16), v4, ALU.add, ['zag'], ['st1'])
                RED(st2.rearrange("p (t g) -> p t g", t=16), sqv.rearrange("p t (g d) -> p t g d", g=4), ALU.add, ['sqv'], ['st2'])
                TS(st1, st1, 1.0 / 64, None, ALU.mult, None, ['st1'], ['st1'])
                TT(st3, st1, st1, ALU.mult, ['st1'], ['st3'])
                STT(st2, st2, 1.0 / 64, st3, ALU.mult, ALU.subtract, ['st2', 'st3'], ['st2'])
                TS(st2, st2, 0.0, None, ALU.max, None, ['st2'], ['st2'])
                ACT(st2, st2, AF.Sqrt, ['st2', 'epsA'], ['st2'], bias=epsA[:, 0:1])
                RCP(st2, st2, ['st2'], ['st2'])
                mean_b = st1.rearrange("p (t g) -> p t g", t=16).unsqueeze(3).to_broadcast([128, 16, 4, 64])
                rstd_b = st2.rearrange("p (t g) -> p t g", t=16).unsqueeze(3).to_broadcast([128, 16, 4, 64])
                sq4 = sqv.rearrange("p t (g d) -> p t g d", g=4)
                TT(sq4, v4, mean_b, ALU.subtract, ['zag', 'st1'], ['sqv'])
                TT(sq4, sq4, rstd_b, ALU.mult, ['sqv', 'st2'], ['sqv'])
                gam_b = lnbc[:, 0, :].unsqueeze(1).to_broadcast([128, 16, 256])
                bet_b = lnbc[:, 1, :].unsqueeze(1).to_broadcast([128, 16, 256])
                TT(sqv, sqv, gam_b, ALU.mult, ['sqv', 'lnbcA'], ['sqv'])
                TT(zag[:, :, 256:512], sqv, bet_b, ALU.add, ['sqv', 'lnbcA'], ['zag'])
                bs_b = bsT[:, 0:4].unsqueeze(2).to_broadcast([128, 4, 64])
                for tt in range(16):
                    b = 4 + tt % 4
                    for g in range(4):
                        MM(bank(b)[:, g * 64:(g + 1) * 64], WsT[:, g * 128:(g + 1) * 128],
                           zag[:, tt, 256 + g * 64:256 + (g + 1) * 64], g == 0, g == 3, ['WsT', 'zag'], [BK[b]])
                    at = atmp[tt % 2]
                    ak = 'atmp%d' % (tt % 2)
                    TT(at.rearrange("p (g d) -> p g d", g=4), bank(b)[:, 0:256].rearrange("p (g d) -> p g d", g=4),
                       bs_b, ALU.add, [BK[b], 'bsT'], [ak])
                    TT(mixA[:, tt, :], at, zag[:, tt, 0:256], ALU.mult, [ak, 'zag'], ['mixA%d' % tt])
                if dbg == 'mixA%d' % l:
                    dst = AR.get([256], F32)
                    for tt in range(16):
                        CP(dst, mixA[:, tt, :], ['mixA%d' % tt], ['dst'])
                        DMA('sp', dbg_d[tt * 128:(tt + 1) * 128, 0:256], dst, ['dst'], ['dbg'])
                AR.reset(off_sqv)
                mixT = AR.get([2, 2048], BF16)
                out_proj(l, mixA, ['mixA%d' % tt for tt in range(16)], 2, 0, mixT, ['sqv'])

            if 'B' in mixers:
                mixer_B(l)
            if 'C' in mixers:
                mixer_C(l)

            S.barrier()
            layer_norm(l, 1, last=False)
            if dbg == 'ln1_%d' % l:
                break

            if do_ffn:
                S.barrier()
                AR.reset()
                DMA('sp', cwT[:], Dr['cwT'][l], (), ['cwT'])
                DMA('sp', cbT[:], Dr['cbT'][l], (), ['cbT'])
                gT = AR.get([max(FF_SPLIT), 2048], BF16)
                ctmp = [[AR.get([1024], F32) for _ in range(2)] for _ in range(2)]
                sgt = [AR.get([1024], BF16) for _ in range(2)]
                j0 = 0
                PS4 = [PSA, PSB]
                P4K = [BK[0:4], BK[4:8]]
                for pi_, part_n in enumerate(FF_SPLIT):
                    if l + 1 < n_layers:
                        for blk_ in range(3 * pi_, 3 * pi_ + 3):
                            ada_block_deferred(l + 1, blk_)
                    wgrp = {}
                    for jl in range(part_n):
                        j = j0 + jl
                        if jl % 4 == 0:
                            ng = min(4, part_n - jl)
                            for part in range(2):
                                jj0 = part * 22 + j
                                wgrp[part] = wload(kchunks(Dr['w_up'][l][:, jj0 * 128:(jj0 + ng) * 128]), [8, ng * 128])
                        for part in range(2):
                            jj = part * 22 + j
                            wvf, wk = wgrp[part]
                            wv = wvf[:, :, (jl % 4) * 128:(jl % 4 + 1) * 128]
                            ps = PS4[part]
                            for tb in range(4):
                                for k in range(8):
                                    MM(ps[:, tb * 512:(tb + 1) * 512], wv[:, k, :], hT[:, k, tb * 512:(tb + 1) * 512],
                                       k == 0, k == 7, [wk, hk(k, tb)], [P4K[part][tb]])
                            for hf in range(2):
                                ct = ctmp[part][hf]
                                ck = 'ctmp%d_%d' % (part, hf)
                                o = hf * 1024
                                pk = P4K[part][2 * hf:2 * hf + 2]
                                pkp = P4K[part][max(0, 2 * hf - 1):2 * hf + 2]
                                ACT(ct, ps[:, o:o + 1024], AF.Identity, pk + ['cwT', 'cbT'], [ck],
                                    scale=cwT[:, jj * 3 + 2:jj * 3 + 3], bias=cbT[:, jj:jj + 1])
                                if hf == 0:
                                    STT(ct[:, 1:1024], ps[:, 0:1023], cwT[:, jj * 3 + 1:jj * 3 + 2], ct[:, 1:1024],
                                        ALU.mult, ALU.add, pk + ['cwT', ck], [ck])
                                    STT(ct[:, 2:1024], ps[:, 0:1022], cwT[:, jj * 3:jj * 3 + 1], ct[:, 2:1024],
                                        ALU.mult, ALU.add, pk + ['cwT', ck], [ck])
                                else:
                                    STT(ct, ps[:, o - 1:o + 1023], cwT[:, jj * 3 + 1:jj * 3 + 2], ct,
                                        ALU.mult, ALU.add, pkp + ['cwT', ck], [ck])
                                    STT(ct, ps[:, o - 2:o + 1022], cwT[:, jj * 3:jj * 3 + 1], ct,
                                        ALU.mult, ALU.add, pkp + ['cwT', ck], [ck])
                                if part == 0:
                                    ACT(sgt[hf], ct, AF.Silu, [ck], ['sgt%d' % hf])
                                else:
                                    TT(gT[:, jl, o:o + 1024], ct, sgt[hf], ALU.mult, [ck, 'sgt%d' % hf], ['gT%d' % jl])
                    nsl = (part_n + 3) // 4
                    wvs = []
                    for s in range(nsl):
                        r0 = (j0 + 4 * s) * 128
                        nr = min(4, part_n - 4 * s)
                        wvs.append(wload(kchunks(Dr['w_down'][l][r0:r0 + nr * 128, :]), [nr, 1024]))
                    bi2 = 0
                    for fb in range(8):
                        for tb in range(4):
                            b = bi2 % 8
                            bi2 += 1
                            for jl in range(part_n):
                                wv, wk = wvs[jl // 4]
                                MM(bank(b), wv[:, jl % 4, fb * 128:(fb + 1) * 128], gT[:, jl, tb * 512:(tb + 1) * 512],
                                   jl == 0, jl == part_n - 1, [wk, 'gT%d' % jl], [BK[b]])
                            xs = xT[:, fb, tb * 512:(tb + 1) * 512]
                            STT(xs, bank(b), drv[l][:, 24 + fb:25 + fb], xs, ALU.mult, ALU.add,
                                [BK[b], 'drv%d' % l, 'xT%d_%d' % (fb, tb)], ['xT%d_%d' % (fb, tb)])
                    j0 += part_n
                S.barrier()
            layer_norm(l, 2, last=(l == n_layers - 1))

        S.barrier()
        AR.reset()
        ost = [AR.get([1024], F32) for _ in range(4)]
        bi = 0
        for tt in range(16):
            o = ost[tt % 4]
            ok = 'ost%d' % (tt % 4)
            for cg in range(2):
                b = bi % 8
                bi += 1
                for q in range(4):
                    c = cg * 4 + q
                    TR(bank(b)[:, q * 128:(q + 1) * 128], xT[:, c, tt * 128:(tt + 1) * 128], ident_f[:],
                       ['xT%d_%d' % (c, tt // 4), 'ident_f'], [BK[b]])
                CP(o[:, cg * 512:(cg + 1) * 512], bank(b), [BK[b]], [ok], eng=EV())
            DMA('sp', out_d[tt * 128:(tt + 1) * 128, :], o, [ok], ['out'])
        S.finish()
        S.replay()
    return nc


_CACHE = {}


def kernel(**inputs):
    inp = {k: np.asarray(v) for k, v in inputs.items()}
    if 'nc' not in _CACHE:
        _CACHE['nc'] = build()
    nc = _CACHE['nc']
    w = _prep_weights(inp)
    tb = _tables()
    in_maps = []
    for b in range(8):
        m = dict(w)
        m.update(tb)
        m['x'] = np.ascontiguousarray(inp['x'][b], dtype=np.float32)
        m['cT'] = _pad64(inp['c'][b].reshape(8, 128).T)
        in_maps.append(m)
    res = run_bass_kernel_spmd(nc, in_maps, core_ids=list(range(8)))
    out = np.stack([np.asarray(r['out'], dtype=np.float32) for r in res.results], 0)
    return out
```
